# Optimizing a Trainium2 kernel written in Bass

```python
import math
import jax, jax.numpy as jnp
from jax import lax
import numpy as np

D_MODEL = 1024
BATCH = 4
SEQ = 8192
DEPTH = 2

GLA_HEADS = D_MODEL // 256
GLA_DK = 32
GLA_DV = 64
GLA_GATE_RANK = 16
GLA_TAU = 16.0
GLA_CHUNK = 16
NSA_HEADS = D_MODEL // 256
NSA_KV_HEADS = NSA_HEADS // 2
NSA_DH = 64
NSA_CMP_LEN = 32
NSA_CMP_STRIDE = 16
NSA_CMP_HIDDEN = 128
NSA_SEL_BLOCK = 64
NSA_SEL_TOPK = 16
NSA_SEL_LOCAL = 2
NSA_WINDOW = 512
NSA_QBLOCK = 128
SSD_HEADS = D_MODEL // 128
SSD_HEAD_DIM = 64
SSD_GROUPS = 2
SSD_STATE = 128
SSD_CONV = 4
SSD_CHUNK = 128
SSD_D_INNER = SSD_HEADS * SSD_HEAD_DIM
SSD_CONV_DIM = SSD_D_INNER + 2 * SSD_GROUPS * SSD_STATE

D_MIX = GLA_HEADS * GLA_DV + NSA_HEADS * NSA_DH + SSD_D_INNER
D_FF = 4 * D_MODEL
ROPE_THETA = 10000.0
NORM_EPS = 1e-6
NEG = -1e30
BIG = 1e30

IN_SPLITS = (
    GLA_HEADS * GLA_DK,
    GLA_HEADS * GLA_DK,
    GLA_HEADS * GLA_DV,
    GLA_GATE_RANK,
    GLA_HEADS * GLA_DV,
    NSA_HEADS * NSA_DH,
    6 * NSA_KV_HEADS * NSA_DH,
    3 * NSA_HEADS,
    SSD_D_INNER,
    SSD_CONV_DIM,
    SSD_HEADS,
)
D_IN = sum(IN_SPLITS)

kernel_name = "hybrid_gla_nsa_ssd_block"


def rms_norm(x, w):
    xf = x.astype(jnp.float32)
    y = xf * lax.rsqrt(jnp.mean(xf * xf, axis=-1, keepdims=True) + NORM_EPS)
    return (y * w.astype(jnp.float32)).astype(x.dtype)


def rope(x, pos):
    half = x.shape[-1] // 2
    inv = ROPE_THETA ** (-jnp.arange(half, dtype=jnp.float32) / half)
    ang = pos.astype(jnp.float32)[:, None] * inv[None, :]
    cos = jnp.cos(ang)[None, :, None, :]
    sin = jnp.sin(ang)[None, :, None, :]
    x1 = x[..., :half].astype(jnp.float32)
    x2 = x[..., half:].astype(jnp.float32)
    return jnp.concatenate([x1 * cos - x2 * sin, x2 * cos + x1 * sin], axis=-1).astype(x.dtype)


def gla_mixer(q, k, v, g_lr, r, w_gate2, b_gate, norm_w):
    f32 = jnp.float32
    Bsz, S, _ = q.shape
    H, dk, dv, C = GLA_HEADS, GLA_DK, GLA_DV, GLA_CHUNK
    nc = S // C
    log_a = jax.nn.log_sigmoid((g_lr @ w_gate2 + b_gate).astype(f32)) / GLA_TAU

    def chunks(t, d):
        return t.astype(f32).reshape(Bsz, nc, C, H, d).transpose(0, 3, 1, 2, 4)

    qc = chunks(q, dk) * (dk ** -0.5)
    kc = chunks(k, dk)
    vc = chunks(v, dv)
    bc = jnp.cumsum(chunks(log_a, dk), axis=3)
    causal = jnp.tril(jnp.ones((C, C), dtype=bool))
    diff = bc[..., :, None, :] - bc[..., None, :, :]
    decay = jnp.exp(jnp.where(causal[..., None], diff, -jnp.inf))
    attn = jnp.einsum('bhnid,bhnjd,bhnijd->bhnij', qc, kc, decay)
    o_intra = jnp.einsum('bhnij,bhnjd->bhnid', attn, vc)
    g_tot = bc[..., -1, :]
    u = jnp.einsum('bhnck,bhncv->bhnkv', kc * jnp.exp(g_tot[..., None, :] - bc), vc)

    def step(state, inp):
        g, uc = inp
        return jnp.exp(g)[..., None] * state + uc, state

    init = jnp.zeros((Bsz, H, dk, dv), f32)
    _, s_enter = lax.scan(step, init, (jnp.moveaxis(g_tot, 2, 0), jnp.moveaxis(u, 2, 0)))
    s_enter = jnp.moveaxis(s_enter, 0, 2)
    o_inter = jnp.einsum('bhnck,bhnkv->bhncv', qc * jnp.exp(bc), s_enter)
    o = (o_intra + o_inter).transpose(0, 2, 3, 1, 4).reshape(Bsz, S, H, dv)
    o = rms_norm(o, norm_w.reshape(H, dv)).reshape(Bsz, S, H * dv)
    return (o * jax.nn.silu(r.astype(f32))).astype(q.dtype)


def nsa_compress(t, pos_emb, w1, w2):
    Bsz, S, G, dh = t.shape
    nsub = NSA_CMP_LEN // NSA_CMP_STRIDE
    sub = t.reshape(Bsz, S // NSA_CMP_STRIDE, NSA_CMP_STRIDE, G, dh)
    n_cmp = S // NSA_CMP_STRIDE - nsub + 1
    blocks = jnp.concatenate([sub[:, i:i + n_cmp] for i in range(nsub)], axis=2)
    blocks = blocks + pos_emb[None, None, :, None, :]
    flat = blocks.transpose(0, 3, 1, 2, 4).reshape(Bsz, G, n_cmp, NSA_CMP_LEN * dh)
    return jax.nn.gelu(flat @ w1) @ w2


def nsa_mixer(q, kv, gates, pos, cmp_pos_k, cmp_w1_k, cmp_w2_k,
              cmp_pos_v, cmp_w1_v, cmp_w2_v, norm_w):
    f32 = jnp.float32
    Bsz, S, _ = q.shape
    H, G, dh = NSA_HEADS, NSA_KV_HEADS, NSA_DH
    R = H // G
    QB = NSA_QBLOCK
    out_dtype = q.dtype
    t_idx = jnp.arange(S)

    q = rope(q.reshape(Bsz, S, H, dh), pos)
    k_cmp, v_cmp, k_slc, v_slc, k_win, v_win = [
        t.reshape(Bsz, S, G, dh) for t in jnp.split(kv, 6, axis=-1)]
    k_cmp, k_slc, k_win = rope(k_cmp, pos), rope(k_slc, pos), rope(k_win, pos)
    qg = q.reshape(Bsz, S, G, R, dh).transpose(0, 2, 3, 1, 4).astype(f32) * (dh ** -0.5)

    kc = nsa_compress(k_cmp, cmp_pos_k, cmp_w1_k, cmp_w2_k).astype(f32)
    vc = nsa_compress(v_cmp, cmp_pos_v, cmp_w1_v, cmp_w2_v).astype(f32)
    n_cmp = kc.shape[2]
    cmp_start = jnp.arange(n_cmp) * NSA_CMP_STRIDE
    cmp_mask = (cmp_start + NSA_CMP_LEN - 1)[None, :] <= t_idx[:, None]
    s_cmp = jnp.einsum('bgrsd,bgnd->bgrsn', qg, kc)
    p_cmp = jax.nn.softmax(jnp.where(cmp_mask, s_cmp, NEG), axis=-1)
    p_cmp = jnp.where(cmp_mask, p_cmp, 0.0)
    o_cmp = jnp.einsum('bgrsn,bgnd->bgrsd', p_cmp, vc)

    n_slc = S // NSA_SEL_BLOCK
    slc_start = jnp.arange(n_slc) * NSA_SEL_BLOCK
    overlap = ((cmp_start[:, None] < slc_start[None, :] + NSA_SEL_BLOCK)
               & (cmp_start[:, None] + NSA_CMP_LEN > slc_start[None, :])).astype(f32)
    imp = jnp.einsum('bgrsn,nj->bgsj', p_cmp, overlap)
    q_blk = t_idx // NSA_SEL_BLOCK
    j = jnp.arange(n_slc)
    forced = (j[None, :] == 0) | ((j[None, :] <= q_blk[:, None])
                                  & (j[None, :] > q_blk[:, None] - NSA_SEL_LOCAL))
    future = j[None, :] > q_blk[:, None]
    imp = jnp.where(forced, BIG, jnp.where(future, NEG, imp))
    n_top = min(NSA_SEL_TOPK, n_slc)
    top_val, top_idx = lax.top_k(imp, n_top)
    top_ok = top_val > 0.5 * NEG

    kb = k_slc.astype(f32).transpose(0, 2, 1, 3).reshape(Bsz, G, n_slc, NSA_SEL_BLOCK, dh)
    vb = v_slc.astype(f32).transpose(0, 2, 1, 3).reshape(Bsz, G, n_slc, NSA_SEL_BLOCK, dh)
    nq = S // QB
    qs = jnp.moveaxis(qg.reshape(Bsz, G, R, nq, QB, dh), 3, 0)
    idx_s = jnp.moveaxis(top_idx.reshape(Bsz, G, nq, QB, n_top), 2, 0)
    ok_s = jnp.moveaxis(top_ok.reshape(Bsz, G, nq, QB, n_top), 2, 0)
    tq = t_idx.reshape(nq, QB)
    bi = jnp.arange(Bsz)[:, None, None, None]
    gi = jnp.arange(G)[None, :, None, None]
    offs = jnp.arange(NSA_SEL_BLOCK)

    def sel_block(args):
        qb, ib, okb, tb = args
        kg = kb[bi, gi, ib]
        vg = vb[bi, gi, ib]
        kpos = ib[..., None] * NSA_SEL_BLOCK + offs
        m = okb[..., None] & (kpos <= tb[None, None, :, None, None])
        s = jnp.einsum('bgrqd,bgqnkd->bgrqnk', qb, kg)
        s = jnp.where(m[:, :, None], s, NEG)
        p = jax.nn.softmax(s.reshape(Bsz, G, R, QB, -1), axis=-1).reshape(s.shape)
        return jnp.einsum('bgrqnk,bgqnkd->bgrqd', p, vg)

    o_slc = lax.map(sel_block, (qs, idx_s, ok_s, tq))
    o_slc = jnp.moveaxis(o_slc, 0, 3).reshape(Bsz, G, R, S, dh)

    pad = NSA_WINDOW // QB

    def band(t):
        tb_ = t.astype(f32).transpose(0, 2, 1, 3).reshape(Bsz, G, nq, QB, dh)
        tb_ = jnp.pad(tb_, ((0, 0), (0, 0), (pad, 0), (0, 0), (0, 0)))
        return jnp.concatenate([tb_[:, :, i:i + nq] for i in range(pad + 1)], axis=3)

    kw, vw = band(k_win), band(v_win)
    qw = qg.reshape(Bsz, G, R, nq, QB, dh)
    kpos_w = (jnp.arange(nq)[:, None] - pad) * QB + jnp.arange((pad + 1) * QB)[None, :]
    dist = tq[:, :, None] - kpos_w[:, None, :]
    wmask = (kpos_w[:, None, :] >= 0) & (dist >= 0) & (dist < NSA_WINDOW)
    s_w = jnp.einsum('bgrwqd,bgwkd->bgrwqk', qw, kw)
    p_w = jax.nn.softmax(jnp.where(wmask, s_w, NEG), axis=-1)
    o_win = jnp.einsum('bgrwqk,bgwkd->bgrwqd', p_w, vw).reshape(Bsz, G, R, S, dh)

    gt = jax.nn.sigmoid(gates.astype(f32)).reshape(Bsz, S, G, R, 3).transpose(0, 2, 3, 1, 4)
    o = gt[..., 0:1] * o_cmp + gt[..., 1:2] * o_slc + gt[..., 2:3] * o_win
    o = o.transpose(0, 3, 1, 2, 4).reshape(Bsz, S, H, dh)
    o = rms_norm(o, norm_w.reshape(H, dh))
    return o.reshape(Bsz, S, H * dh).astype(out_dtype)


def ssd_mixer(z, xbc, dt, conv_w, conv_b, dt_bias, a_log, d_skip, norm_w):
    f32 = jnp.float32
    Bsz, S, _ = z.shape
    H, P, G, N, L = SSD_HEADS, SSD_HEAD_DIM, SSD_GROUPS, SSD_STATE, SSD_CHUNK
    nc = S // L
    xbc = lax.conv_general_dilated(
        xbc, conv_w[:, None, :].astype(xbc.dtype), window_strides=(1,),
        padding=[(SSD_CONV - 1, 0)], dimension_numbers=('NWC', 'WIO', 'NWC'),
        feature_group_count=SSD_CONV_DIM) + conv_b
    xbc = jax.nn.silu(xbc.astype(f32))
    xs, Bm, Cm = jnp.split(xbc, [SSD_D_INNER, SSD_D_INNER + G * N], axis=-1)
    xs = xs.reshape(Bsz, S, H, P)
    hpg = H // G
    Bm = jnp.repeat(Bm.reshape(Bsz, S, G, N), hpg, axis=2)
    Cm = jnp.repeat(Cm.reshape(Bsz, S, G, N), hpg, axis=2)
    dt = jax.nn.softplus(dt.astype(f32) + dt_bias.astype(f32))
    A = -jnp.exp(a_log.astype(f32))
    xdt = (xs * dt[..., None]).reshape(Bsz, nc, L, H, P)
    Bc = Bm.reshape(Bsz, nc, L, H, N)
    Cc = Cm.reshape(Bsz, nc, L, H, N)
    acs = jnp.cumsum((dt * A).reshape(Bsz, nc, L, H).transpose(0, 3, 1, 2), axis=-1)
    causal = jnp.tril(jnp.ones((L, L), dtype=bool))
    seg = jnp.exp(jnp.where(causal, acs[..., :, None] - acs[..., None, :], -jnp.inf))
    cb = jnp.einsum('bclhn,bcshn->bhcls', Cc, Bc)
    y_diag = jnp.einsum('bhcls,bcshp->bclhp', cb * seg, xdt)
    decay_states = jnp.exp(acs[..., -1:] - acs)
    states = jnp.einsum('bclhn,bhcl,bclhp->bchpn', Bc, decay_states, xdt)
    chunk_decay = jnp.exp(acs[..., -1])

    def step(h, inp):
        dec, st = inp
        return dec[..., None, None] * h + st, h

    init = jnp.zeros((Bsz, H, P, N), f32)
    _, h_enter = lax.scan(step, init, (jnp.moveaxis(chunk_decay, 2, 0), jnp.moveaxis(states, 1, 0)))
    h_enter = jnp.moveaxis(h_enter, 0, 1)
    y_off = jnp.einsum('bclhn,bchpn,bhcl->bclhp', Cc, h_enter, jnp.exp(acs))
    y = (y_diag + y_off).reshape(Bsz, S, H, P) + d_skip.astype(f32)[:, None] * xs
    y = y.reshape(Bsz, S, H * P) * jax.nn.silu(z.astype(f32))
    y = rms_norm(y.reshape(Bsz, S, G, H * P // G), norm_w.reshape(G, -1)).reshape(Bsz, S, H * P)
    return y.astype(z.dtype)


def setup_inputs(seed: int = 0) -> dict:
    key = jax.random.key(seed)
    ks = iter(jax.random.split(key, 32))
    f32 = jnp.float32
    Ld = DEPTH

    def nrm(shape, scale):
        return jax.random.normal(next(ks), shape, f32) * scale

    def gain(shape):
        return 1.0 + nrm(shape, 0.02)

    cmp_in = NSA_CMP_LEN * NSA_DH
    x = jax.random.normal(next(ks), (BATCH, SEQ, D_MODEL), f32)
    dt0 = jnp.exp(jax.random.uniform(next(ks), (Ld, SSD_HEADS), f32,
                                     math.log(1e-3), math.log(1e-1)))
    dt_bias = dt0 + jnp.log(-jnp.expm1(-dt0))
    a_log = jnp.log(jax.random.uniform(next(ks), (Ld, SSD_HEADS), f32, 1.0, 16.0))
    return {
        "x": x,
        "norm1_w": gain((Ld, D_MODEL)),
        "w_in": nrm((Ld, D_MODEL, D_IN), D_MODEL ** -0.5),
        "gla_gate_w2": nrm((Ld, GLA_GATE_RANK, GLA_HEADS * GLA_DK), GLA_GATE_RANK ** -0.5),
        "gla_gate_b": nrm((Ld, GLA_HEADS * GLA_DK), 0.1),
        "gla_norm_w": gain((Ld, GLA_HEADS * GLA_DV)),
        "nsa_cmp_pos_k": nrm((Ld, NSA_CMP_LEN, NSA_DH), 0.1),
        "nsa_cmp_w1_k": nrm((Ld, cmp_in, NSA_CMP_HIDDEN), cmp_in ** -0.5),
        "nsa_cmp_w2_k": nrm((Ld, NSA_CMP_HIDDEN, NSA_DH), NSA_CMP_HIDDEN ** -0.5),
        "nsa_cmp_pos_v": nrm((Ld, NSA_CMP_LEN, NSA_DH), 0.1),
        "nsa_cmp_w1_v": nrm((Ld, cmp_in, NSA_CMP_HIDDEN), cmp_in ** -0.5),
        "nsa_cmp_w2_v": nrm((Ld, NSA_CMP_HIDDEN, NSA_DH), NSA_CMP_HIDDEN ** -0.5),
        "nsa_norm_w": gain((Ld, NSA_HEADS * NSA_DH)),
        "ssd_conv_w": nrm((Ld, SSD_CONV, SSD_CONV_DIM), SSD_CONV ** -0.5),
        "ssd_conv_b": nrm((Ld, SSD_CONV_DIM), 0.02),
        "ssd_dt_bias": dt_bias,
        "ssd_a_log": a_log,
        "ssd_d": gain((Ld, SSD_HEADS)),
        "ssd_norm_w": gain((Ld, SSD_D_INNER)),
        "w_out": nrm((Ld, D_MIX, D_MODEL), D_MIX ** -0.5),
        "norm2_w": gain((Ld, D_MODEL)),
        "w_up": nrm((Ld, D_MODEL, D_FF), D_MODEL ** -0.5),
        "w_down": nrm((Ld, D_FF, D_MODEL), 0.5 * D_FF ** -0.5),
        "final_norm_w": gain((D_MODEL,)),
    }


def reference(x, norm1_w, w_in, gla_gate_w2, gla_gate_b, gla_norm_w,
              nsa_cmp_pos_k, nsa_cmp_w1_k, nsa_cmp_w2_k,
              nsa_cmp_pos_v, nsa_cmp_w1_v, nsa_cmp_w2_v, nsa_norm_w,
              ssd_conv_w, ssd_conv_b, ssd_dt_bias, ssd_a_log, ssd_d, ssd_norm_w,
              w_out, norm2_w, w_up, w_down, final_norm_w):
    S = x.shape[1]
    pos = jnp.arange(S)
    split_at = np.cumsum(IN_SPLITS)[:-1].tolist()
    h = x
    for l in range(DEPTH):
        u = rms_norm(h, norm1_w[l])
        proj = u @ w_in[l]
        (gq, gk, gv, glr, gr, nq_, nkv, ngate, sz, sxbc, sdt) = jnp.split(proj, split_at, axis=-1)
        y_gla = gla_mixer(gq, gk, gv, glr, gr, gla_gate_w2[l], gla_gate_b[l], gla_norm_w[l])
        y_nsa = nsa_mixer(nq_, nkv, ngate, pos,
                          nsa_cmp_pos_k[l], nsa_cmp_w1_k[l], nsa_cmp_w2_k[l],
                          nsa_cmp_pos_v[l], nsa_cmp_w1_v[l], nsa_cmp_w2_v[l], nsa_norm_w[l])
        y_ssd = ssd_mixer(sz, sxbc, sdt, ssd_conv_w[l], ssd_conv_b[l], ssd_dt_bias[l],
                          ssd_a_log[l], ssd_d[l], ssd_norm_w[l])
        mix = jnp.concatenate([y_gla, y_nsa, y_ssd], axis=-1)
        h = h + mix @ w_out[l]
        u = rms_norm(h, norm2_w[l])
        h = h + jnp.square(jax.nn.relu(u @ w_up[l])) @ w_down[l]
    return rms_norm(h, final_norm_w)
```

```python
import numpy as np
import ml_dtypes
import concourse.bass as bass
import concourse.mybir as mybir
from concourse.bass_utils import run_bass_kernel_spmd
from contextlib import ExitStack
import threading

F32 = mybir.dt.float32
BF16 = mybir.dt.bfloat16
AF = mybir.ActivationFunctionType
ALU = mybir.AluOpType
AX = mybir.AxisListType

D = 1024
DIN = 3364
DFF = 4096
NPROJ = 2340
EPS = 1e-6

C_GQ, C_GK, C_GV, C_GLR, C_GR = 0, 128, 256, 512, 528
C_NQ, C_KCMP, C_KSLC, C_KWIN, C_VCMP, C_VSLC, C_VWIN, C_NG = 784, 1040, 1168, 1296, 1424, 1552, 1680, 1808
C_Z, C_DT = 1820, 2332
WMAP = [(0, 784, 0), (784, 64, 784), (912, 64, 848), (848, 64, 912), (976, 64, 976), (1040, 128, 1040), (1296, 128, 1168), (1552, 128, 1296), (1168, 128, 1424),
        (1424, 128, 1552), (1680, 652, 1680), (3356, 8, 2332), (2332, 1024, -1)]


class KB:
    EPOCH = 30000

    def __init__(self, nc, same_engine_sync=True):
        self.nc = nc
        self.es = ExitStack()
        self.eng = {"pe": nc.tensor, "act": nc.scalar, "dve": nc.vector,
                    "pool": nc.gpsimd, "sp": nc.sync}
        self.esem = {}
        self.ecnt = {}
        self.nsem = 0
        self.dsem = {}
        self.waited = {e: {} for e in self.eng}
        self.last_w = {}
        self.readers = {}
        self.same = same_engine_sync
        self.sem_owner = {}
        self.ninst = 0
        self.limit = None
        self.hook = None
        for e in self.eng:
            self._new_esem(e)

    def _sem(self, name):
        self.nsem += 1
        return self.es.enter_context(self.nc.semaphore(f"s{self.nsem}_{name}"))

    def _new_esem(self, e):
        s = self._sem(e)
        self.esem[e] = s
        self.ecnt[e] = 0
        self.sem_owner[id(s)] = e

    def _wait(self, e, deps):
        best = {}
        for item in deps:
            if item is None:
                continue
            if len(item) == 3:
                s, v, raw = item
            else:
                (s, v), raw = item, True
            owner = self.sem_owner.get(id(s))
            if owner == e and (e == "pe" or not self.same or not raw):
                continue
            if best.get(id(s), (None, 0))[1] < v:
                best[id(s)] = (s, v)
        for sid, (s, v) in best.items():
            if self.waited[e].get(sid, 0) < v:
                self.eng[e].wait_ge(s, v)
                self.waited[e][sid] = v

    def _deps(self, reads, writes):
        deps = []
        for k in reads:
            ev = self.last_w.get(k)
            if ev is not None:
                deps.append((ev[0], ev[1], True))
        for k in writes:
            ev = self.last_w.get(k)
            if ev is not None:
                deps.append((ev[0], ev[1], False))
            for ev in self.readers.get(k, []):
                deps.append((ev[0], ev[1], False))
        return deps

    def _commit(self, ev, reads, writes):
        for k in writes:
            self.last_w[k] = ev
            self.readers[k] = []
        for k in reads:
            if k in writes:
                continue
            self.readers.setdefault(k, []).append(ev)

    def op(self, e, fn, reads=(), writes=()):
        if self.hook is not None:
            self.hook()
        if self.limit is not None and self.ninst >= self.limit:
            return None
        self._wait(e, self._deps(reads, writes))
        inst = fn()
        if self.ecnt[e] >= self.EPOCH:
            self._new_esem(e)
        self.ecnt[e] += 1
        s = self.esem[e]
        inst.then_inc(s, 1)
        self._commit((s, self.ecnt[e]), reads, writes)
        self.ninst += 1
        return inst

    def dma(self, q, key, out, in_, reads=(), writes=(), **kw):
        if self.hook is not None:
            self.hook()
        if self.limit is not None and self.ninst >= self.limit:
            return None
        self._wait(q, self._deps(reads, writes))
        if key not in self.dsem:
            self.dsem[key] = [self._sem("d"), 0]
        ent = self.dsem[key]
        inst = self.eng[q].dma_start(out=out, in_=in_, **kw)
        ent[1] += 16
        inst.then_inc(ent[0], 16)
        self._commit((ent[0], ent[1]), reads, writes)
        self.ninst += 1
        return inst

    def barrier(self):
        deps = list(self.last_w.values())
        for r in self.readers.values():
            deps.extend(r)
        for e in self.eng:
            self._wait(e, deps)
        self.readers = {k: [] for k in self.readers}

    def drain(self, e="sp"):
        deps = list(self.last_w.values())
        for r in self.readers.values():
            deps.extend(r)
        self._wait(e, deps)


def interleave(kb, fa, fb):
    sem = {"a": threading.Semaphore(0), "b": threading.Semaphore(0)}
    done = {"a": False, "b": False}
    err = []
    loc = threading.local()

    def hook():
        me = loc.me
        other = "b" if me == "a" else "a"
        if done[other]:
            return
        sem[other].release()
        sem[me].acquire()

    def run(me, f):
        loc.me = me
        other = "b" if me == "a" else "a"
        sem[me].acquire()
        try:
            f()
        except BaseException as ex:
            err.append(ex)
        done[me] = True
        sem[other].release()

    kb.hook = hook
    ta = threading.Thread(target=run, args=("a", fa))
    tb = threading.Thread(target=run, args=("b", fb))
    ta.start()
    tb.start()
    sem["a"].release()
    ta.join()
    tb.join()
    kb.hook = None
    if err:
        raise err[0]


class Prog:
    def __init__(self, S, L=2, dbg=()):
        self.S = S
        self.NT = S // 128
        self.L = L
        self.dbg = dbg
        nc = self.nc = bass.Bass("TRN2", target_bir_lowering=False)
        self.kb = KB(nc)
        Ld = L

        def inp(name, shape, dt=F32):
            return nc.dram_tensor(name, list(shape), dt, kind="ExternalInput").ap()

        def scr(name, shape, dt=F32):
            return nc.dram_tensor(name, list(shape), dt, kind="Internal").ap()

        self.x = inp("x", [S, D])
        self.norm1_w = inp("norm1_w", [Ld, D])
        self.w_in = inp("w_in", [Ld, D, DIN])
        self.gla_gate_w2 = inp("gla_gate_w2", [Ld, 16, 128])
        self.gla_gate_b = inp("gla_gate_b", [Ld, 128])
        self.gla_norm_w = inp("gla_norm_w", [Ld, 256])
        self.nsa_cmp_pos_k = inp("nsa_cmp_pos_k", [Ld, 32, 64])
        self.nsa_cmp_w1_k = inp("nsa_cmp_w1_k", [Ld, 2048, 128])
        self.nsa_cmp_w2_k = inp("nsa_cmp_w2_k", [Ld, 128, 64])
        self.nsa_cmp_pos_v = inp("nsa_cmp_pos_v", [Ld, 32, 64])
        self.nsa_cmp_w1_v = inp("nsa_cmp_w1_v", [Ld, 2048, 128])
        self.nsa_cmp_w2_v = inp("nsa_cmp_w2_v", [Ld, 128, 64])
        self.nsa_norm_w = inp("nsa_norm_w", [Ld, 256])
        self.ssd_conv_w = inp("ssd_conv_w", [Ld, 4, 1024])
        self.ssd_conv_b = inp("ssd_conv_b", [Ld, 1024])
        self.ssd_dt_bias = inp("ssd_dt_bias", [Ld, 8])
        self.ssd_a_log = inp("ssd_a_log", [Ld, 8])
        self.ssd_d = inp("ssd_d", [Ld, 8])
        self.ssd_norm_w = inp("ssd_norm_w", [Ld, 512])
        self.w_out = inp("w_out", [Ld, D, D])
        self.norm2_w = inp("norm2_w", [Ld, D])
        self.w_up = inp("w_up", [Ld, D, DFF])
        self.w_down = inp("w_down", [Ld, DFF, D])
        self.final_norm_w = inp("final_norm_w", [D])
        self.ident_f = inp("ident_f", [128, 128])
        self.ident_b = inp("ident_b", [128, 128], BF16)
        self.c_tri = inp("c_tri", [128, 128])
        self.c_slt = inp("c_slt", [128, 128])
        self.c_ones = inp("c_ones", [128, 128])
        self.c_hm = inp("c_hm", [128, 4])
        self.c_bd = inp("c_bd", [128, 256])
        self.mix = scr("mix", [S, D], BF16)
        NS = S // 64
        self.NS = NS
        self.NCC = max(1, (S // 16 - 1 + 127) // 128)
        self.rope_cos = inp("rope_cos", [S, 32])
        self.rope_sin = inp("rope_sin", [S, 32])
        self.c_Ew = inp("c_Ew", [128, S], BF16)
        self.c_cmask = inp("c_cmask", [128, 17, 128], BF16)
        self.c_ovl = inp("c_ovl", [128, self.NCC, NS], BF16)
        self.c_Fw = inp("c_Fw", [128, 2 * NS - 2])
        self.c_Kw = inp("c_Kw", [128, 2 * NS - 2])
        self.c_trib = inp("c_trib", [128, 128], BF16)
        self.c_sltb = inp("c_sltb", [128, 128], BF16)
        self.kcT_d = scr("kcT_d", [128, S + 128], BF16)
        self.vcT_d = scr("vcT_d", [128, S + 128], BF16)
        self.proj = scr("proj", [S, NPROJ])
        self.xbcT = scr("xbcT", [D, S])
        self.hbuf = scr("hbuf", [S, D])
        self.outs = {}
        for name, shape in dbg:
            self.outs[name] = nc.dram_tensor(name, list(shape), F32, kind="ExternalOutput").ap()

    def sb(self, es, name, shape, dt=F32):
        self._uid = getattr(self, "_uid", 0) + 1
        return es.enter_context(self.nc.sbuf_tensor(f"{name}_u{self._uid}", list(shape), dt))

    def ps(self, es, name, shape, dt=F32):
        full = 512 if dt == F32 else 1024
        self._uid = getattr(self, "_uid", 0) + 1
        t = es.enter_context(self.nc.psum_tensor(f"{name}_u{self._uid}", [128, full], dt))
        n = 1
        for d in shape[1:]:
            n *= d
        assert n <= full
        v = t[0:shape[0], 0:n]
        if len(shape) == 3:
            v = v.rearrange("p (a b) -> p a b", a=shape[1])
        return v

    def stage_d1(self, l, hsrc):
        nc, kb, NT = self.nc, self.kb, self.NT
        with ExitStack() as es:
            Wtm = self.sb(es, "d1_Wtm", [128, 8, NPROJ], BF16)
            Wx = self.sb(es, "d1_Wx", [128, 8, 1024], BF16)
            stg = [self.sb(es, f"d1_stg{i}", [128, DIN]) for i in range(2)]
            nw = self.sb(es, "d1_nw", [128, 8])
            idb = self.sb(es, "d1_idb", [128, 128], BF16)
            ht = [self.sb(es, f"d1_ht{i}", [128, D]) for i in range(2)]
            junk = self.sb(es, "d1_junk", [128, D], BF16)
            ss = [self.sb(es, f"d1_ss{i}", [128, 1]) for i in range(2)]
            rs = [self.sb(es, f"d1_rs{i}", [128, 1]) for i in range(2)]
            u = [self.sb(es, f"d1_u{i}", [128, D], BF16) for i in range(2)]
            uT = [self.sb(es, f"d1_uT{i}", [128, 8, 128], BF16) for i in range(2)]
            ot = [self.sb(es, f"d1_ot{i}", [128, NPROJ]) for i in range(2)]
            xo = [self.sb(es, f"d1_xo{i}", [128, 8, 128]) for i in range(2)]
            pT = self.ps(es, "d1_pT", [128, 8, 128], BF16)
            pm = [self.ps(es, f"d1_pm{i}", [128, 512]) for i in range(3)]
            px = [self.ps(es, f"d1_px{i}", [128, 4, 128]) for i in range(2)]

            kb.dma("sp", "d1_idb", idb[:], self.ident_b[:, :], writes=["d1_idb"])
            for c in range(8):
                kb.dma("sp", "d1_nw", nw[:, c:c + 1],
                       self.norm1_w[l, c * 128:(c + 1) * 128].rearrange("(p o) -> p o", o=1),
                       writes=["d1_nw"])
            for c in range(8):
                s = c % 2
                kb.dma("sp", f"d1_stg{s}", stg[s][:], self.w_in[l, c * 128:(c + 1) * 128, :],
                       writes=[f"d1_stg{s}"])
                for j, (so, n, do) in enumerate(WMAP):
                    dst = Wx[:, c, 0:n] if do < 0 else Wtm[:, c, do:do + n]
                    wk = f"d1_W{c}"
                    if j % 2 == 0:
                        kb.op("dve", lambda: nc.vector.tensor_scalar(
                            out=dst, in0=stg[s][:, so:so + n], scalar1=nw[:, c:c + 1], scalar2=None,
                            op0=ALU.mult), reads=[f"d1_stg{s}", "d1_nw"], writes=[wk + f"_{j}"])
                    else:
                        kb.op("act", lambda: nc.scalar.activation(
                            out=dst, in_=stg[s][:, so:so + n], func=AF.Copy, scale=nw[:, c:c + 1]),
                            reads=[f"d1_stg{s}", "d1_nw"], writes=[wk + f"_{j}"])
            Wkeys = [f"d1_W{c}_{j}" for c in range(8) for j in range(len(WMAP))]

            def A(i):
                s = i % 2
                kb.dma("sp", f"d1_ht{s}", ht[s][:], hsrc[i * 128:(i + 1) * 128, :],
                       reads=[("h", i)], writes=[f"d1_ht{s}"])
                kb.op("act", lambda: nc.scalar.activation(out=junk[:], in_=ht[s][:], func=AF.Square,
                                                          accum_out=ss[s][:]),
                      reads=[f"d1_ht{s}"], writes=["d1_junk", f"d1_ss{s}"])
                kb.op("dve", lambda: nc.vector.tensor_scalar(out=rs[s][:], in0=ss[s][:], scalar1=1.0 / D,
                                                             scalar2=EPS, op0=ALU.mult, op1=ALU.add),
                      reads=[f"d1_ss{s}"], writes=[f"d1_rs{s}"])
                kb.op("act", lambda: nc.scalar.activation(out=rs[s][:], in_=rs[s][:], func=AF.Sqrt),
                      reads=[f"d1_rs{s}"], writes=[f"d1_rs{s}"])
                kb.op("dve", lambda: nc.vector.reciprocal(out=rs[s][:], in_=rs[s][:]),
                      reads=[f"d1_rs{s}"], writes=[f"d1_rs{s}"])
                kb.op("dve", lambda: nc.vector.tensor_scalar(out=u[s][:], in0=ht[s][:], scalar1=rs[s][:],
                                                             scalar2=None, op0=ALU.mult),
                      reads=[f"d1_ht{s}", f"d1_rs{s}"], writes=[f"d1_u{s}"])
                for c in range(8):
                    kb.op("pe", lambda: nc.tensor.transpose(out=pT[:, c, :], in_=u[s][:, c * 128:(c + 1) * 128],
                                                            identity=idb[:]),
                          reads=[f"d1_u{s}", "d1_idb"], writes=["d1_pT"])
                kb.op("act", lambda: nc.scalar.copy(out=uT[s][:], in_=pT[:]),
                      reads=["d1_pT"], writes=[f"d1_uT{s}"])

            def B(i):
                s = i % 2
                off = 0
                k = 0
                while off < NPROJ:
                    n = min(512, NPROJ - off)
                    p = pm[k % 3]
                    pk = f"d1_pm{k % 3}"
                    for c in range(8):
                        kb.op("pe", lambda: nc.tensor.matmul(out=p[:, 0:n], lhsT=uT[s][:, c, :],
                                                             rhs=Wtm[:, c, off:off + n],
                                                             start=(c == 0), stop=(c == 7)),
                              reads=[f"d1_uT{s}"] + (Wkeys if i == 0 and c == 0 and k == 0 else []),
                              writes=[pk])
                    if k % 2 == 0:
                        kb.op("dve", lambda: nc.vector.tensor_copy(out=ot[s][:, off:off + n], in_=p[:, 0:n]),
                              reads=[pk], writes=[f"d1_ot{s}_{k}"])
                    else:
                        kb.op("act", lambda: nc.scalar.copy(out=ot[s][:, off:off + n], in_=p[:, 0:n]),
                              reads=[pk], writes=[f"d1_ot{s}_{k}"])
                    off += n
                    k += 1
                kb.dma("sp", f"d1_ot{s}", self.proj[i * 128:(i + 1) * 128, :], ot[s][:],
                       reads=[f"d1_ot{s}_{j}" for j in range(k)], writes=[("proj", i)])
                for half in range(2):
                    p = px[half]
                    pk = f"d1_px{half}"
                    for j in range(4):
                        cc = half * 4 + j
                        for c in range(8):
                            kb.op("pe", lambda: nc.tensor.matmul(out=p[:, j, :], lhsT=Wx[:, c, cc * 128:(cc + 1) * 128],
                                                                 rhs=uT[s][:, c, :],
                                                                 start=(c == 0), stop=(c == 7)),
                                  reads=[f"d1_uT{s}"], writes=[pk])
                    if half == 0:
                        kb.op("dve", lambda: nc.vector.tensor_copy(out=xo[s][:, 0:4, :], in_=p[:]),
                              reads=[pk], writes=[f"d1_xo{s}_0"])
                    else:
                        kb.op("act", lambda: nc.scalar.copy(out=xo[s][:, 4:8, :], in_=p[:]),
                              reads=[pk], writes=[f"d1_xo{s}_1"])
                kb.dma("sp", f"d1_xo{s}",
                       self.xbcT.rearrange("(cc p) t -> p cc t", p=128)[:, :, i * 128:(i + 1) * 128],
                       xo[s][:], reads=[f"d1_xo{s}_0", f"d1_xo{s}_1"], writes=[("xbcT", i)])

            A(0)
            for i in range(NT):
                if i + 1 < NT:
                    interleave(kb, (lambda i=i: B(i)), (lambda i=i: A(i + 1)))
                else:
                    B(i)

    def dump(self, name, src_ap):
        kb = self.kb
        kb.drain("sp")
        kb.dma("sp", "dump_" + name, self.outs[name], src_ap, writes=[("dump", name)])

    def finish(self):
        self.kb.drain("sp")
        self.kb.es.close()


def build_test_d1(S):
    p = Prog(S, L=1, dbg=[("o_proj", [S, NPROJ]), ("o_xbcT", [D, S])])
    with p.kb.es:
        p.stage_d1(0, p.x)
        p.dump("o_proj", p.proj[:, :])
        p.dump("o_xbcT", p.xbcT[:, :])
        p.kb.drain("sp")
    return p


def consts(S):
    r = np.arange(128)
    NS = S // 64
    NCC = max(1, (S // 16 - 1 + 127) // 128)
    pos = np.arange(S, dtype=np.float32)
    inv = (np.float32(10000.0) ** (-np.arange(32, dtype=np.float32) / np.float32(32))).astype(np.float32)
    ang = (pos[:, None] * inv[None, :]).astype(np.float32)
    Ew = np.zeros((128, S), np.float32)
    cc = np.arange(S)
    Ew[cc // 64 % 128, cc] = 1.0
    cm = np.zeros((128, 17, 128), np.float32)
    for d_ in range(17):
        cm[:, d_, :] = (128 * d_ + r[None, :] - 16 * r[:, None] >= 31)
    n_all = np.arange(NCC * 128)
    j_all = np.arange(NS)
    ov = ((16 * n_all[:, None] < 64 * j_all[None, :] + 64) & (16 * n_all[:, None] + 32 > 64 * j_all[None, :])).astype(np.float32)
    ov = ov.reshape(NCC, 128, NS).transpose(1, 0, 2)
    cw = np.arange(2 * NS - 2)
    rel = cw[None, :] - (NS - 2) - (r[:, None] >= 64)
    Fw = np.where(rel > 0, -1e30, np.where(rel >= -1, 1e30, 0.0)).astype(np.float32)
    Kw = ((rel < -1)).astype(np.float32)
    bf = ml_dtypes.bfloat16
    hm = np.zeros((128, 4), np.float32)
    bd = np.zeros((128, 256), np.float32)
    for h in range(4):
        hm[32 * h:32 * h + 32, h] = 1.0
        bd[32 * h:32 * h + 32, 64 * h:64 * h + 64] = 1.0
    return {
        "ident_f": np.eye(128, dtype=np.float32),
        "ident_b": np.eye(128, dtype=np.float32).astype(ml_dtypes.bfloat16),
        "c_tri": (r[:, None] <= r[None, :]).astype(np.float32),
        "c_slt": (r[:, None] > r[None, :]).astype(np.float32),
        "c_ones": np.ones((128, 128), np.float32),
        "c_hm": hm,
        "c_bd": bd,
        "rope_cos": np.cos(ang).astype(np.float32),
        "rope_sin": np.sin(ang).astype(np.float32),
        "c_Ew": Ew.astype(bf),
        "c_cmask": cm.astype(bf),
        "c_ovl": np.ascontiguousarray(ov).astype(bf),
        "c_Fw": Fw,
        "c_Kw": Kw,
        "c_trib": (r[:, None] <= r[None, :]).astype(np.float32).astype(bf),
        "c_sltb": (r[:, None] > r[None, :]).astype(np.float32).astype(bf),
    }


class Tile:
    def __init__(self, t, k):
        self.t = t
        self.k = k


def _stage_ssd(self, l):
    nc, kb, NT = self.nc, self.kb, self.NT
    with ExitStack() as es:
        def T(name, shape, dt=F32):
            return Tile(self.sb(es, "ssd_" + name, shape, dt), "ssd_" + name)

        def T2(name, shape, dt=F32):
            return [T(f"{name}{j}", shape, dt) for j in range(2)]

        def PT(name, shape, dt=F32):
            return Tile(self.ps(es, "ssd_" + name, shape, dt), "ssd_" + name)

        def V(fn, r, w):
            return kb.op("dve", fn, [t.k for t in r], [t.k for t in w])

        def AC(fn, r, w):
            return kb.op("act", fn, [t.k for t in r], [t.k for t in w])

        def PE(fn, r, w):
            return kb.op("pe", fn, [t.k for t in r], [t.k for t in w])

        def GP(fn, r, w):
            return kb.op("pool", fn, [t.k for t in r], [t.k for t in w])

        def LD(t, dst, src, reads=(), **kw):
            return kb.dma("sp", t.k, dst, src, reads=list(reads), writes=[t.k], **kw)

        cw = T("cw", [128, 4, 8]); cb = T("cb", [128, 8]); dtb = T("dtb", [128, 8])
        Aneg = T("Aneg", [128, 8]); dsk = T("dsk", [128, 8]); nwb = T("nwb", [128, 512])
        tri = T("tri", [128, 128]); slt = T("slt", [128, 128]); ones = T("ones", [128, 128])
        idf = T("idf", [128, 128]); idb = T("idb", [128, 128], BF16)
        hT = T("hT", [128, 512]); hTb = T("hTb", [128, 512], BF16)
        xh = T2("xh", [128, 8, 131]); zt = T2("zt", [128, 512]); dtt = T2("dtt", [128, 8])
        acc = T2("acc", [128, 8, 128])
        xsT = T2("xsT", [128, 4, 128])
        BT = T2("BT", [128, 2, 128], BF16); CT = T2("CT", [128, 2, 128], BF16)
        xtm = T2("xtm", [128, 512]); Btm = T2("Btm", [128, 2, 128], BF16)
        sp_a = T2("sp_a", [128, 8]); dts = T2("dts", [128, 8]); dA = T2("dA", [128, 8])
        acs = T2("acs", [128, 16]); eacs = T2("eacs", [128, 8]); dsd = T2("dsd", [128, 8]); cd = T2("cd", [128, 8])
        xdt = T2("xdt", [128, 512], BF16); xdd = T2("xdd", [128, 512], BF16)
        cbm = T2("cbm", [128, 2, 128])
        dAt = T("dAt", [128, 8, 128]); seg = T2("seg", [128, 4, 128]); MT = T2("MT", [128, 4, 128], BF16)
        yd = T2("yd", [128, 512]); y = T2("y", [128, 512]); sz = T2("sz", [128, 512])
        junk = T("junk", [128, 256]); ss = T2("ss", [128, 2]); mo = T2("mo", [128, 512], BF16)
        pxT = PT("pxT", [128, 512]); pBt = PT("pBt", [128, 2, 128], BF16); pacs = PT("pacs", [128, 16])
        pcb = PT("pcb", [128, 2, 128]); pD = PT("pD", [128, 4, 128]); py = PT("py", [128, 512])
        pyo = PT("pyo", [128, 512]); pU = PT("pU", [128, 512])

        for k in range(4):
            LD(cw, cw.t[:, k, :], self.ssd_conv_w[l, k, :].rearrange("(cc p) -> p cc", p=128),
               allow_slow_non_contiguous=True)
        LD(cb, cb.t[:], self.ssd_conv_b[l, :].rearrange("(cc p) -> p cc", p=128), allow_slow_non_contiguous=True)
        LD(dtb, dtb.t[:], self.ssd_dt_bias[l, :].partition_broadcast(128))
        LD(Aneg, Aneg.t[:], self.ssd_a_log[l, :].partition_broadcast(128))
        LD(dsk, dsk.t[:], self.ssd_d[l, :].partition_broadcast(128))
        LD(nwb, nwb.t[:], self.ssd_norm_w[l, :].partition_broadcast(128))
        LD(tri, tri.t[:], self.c_tri[:, :]); LD(slt, slt.t[:], self.c_slt[:, :]); LD(ones, ones.t[:], self.c_ones[:, :])
        LD(idf, idf.t[:], self.ident_f[:, :]); LD(idb, idb.t[:], self.ident_b[:, :])
        AC(lambda: nc.scalar.activation(out=Aneg.t[:], in_=Aneg.t[:], func=AF.Exp), [Aneg], [Aneg])
        V(lambda: nc.vector.tensor_scalar(out=Aneg.t[:], in0=Aneg.t[:], scalar1=-1.0, scalar2=None, op0=ALU.mult), [Aneg], [Aneg])
        V(lambda: nc.vector.memset(hT.t[:], 0.0), [], [hT])
        V(lambda: nc.vector.memset(hTb.t[:], 0.0), [], [hTb])
        for j in range(2):
            V(lambda: nc.vector.memset(xh[j].t[:], 0.0), [], [xh[j]])
        xv = self.xbcT.rearrange("(cc p) t -> p cc t", p=128)

        def front(i):
            s = i % 2
            t0 = i * 128
            if i == 0:
                kb.dma("sp", xh[s].k, xh[s].t[:, :, 3:131], xv[:, :, 0:128], reads=[("xbcT", 0)], writes=[xh[s].k])
            else:
                kb.dma("sp", xh[s].k, xh[s].t[:, :, 0:131], xv[:, :, t0 - 3:t0 + 128],
                       reads=[("xbcT", i), ("xbcT", i - 1)], writes=[xh[s].k])
            kb.dma("sp", zt[s].k, zt[s].t[:], self.proj[t0:t0 + 128, C_Z:C_Z + 512], reads=[("proj", i)], writes=[zt[s].k])
            kb.dma("sp", dtt[s].k, dtt[s].t[:], self.proj[t0:t0 + 128, C_DT:C_DT + 8], reads=[("proj", i)], writes=[dtt[s].k])
            for cc in range(8):
                eng = V
                ne = nc.vector
                GP(lambda: nc.gpsimd.tensor_scalar(out=acc[s].t[:, cc, :], in0=xh[s].t[:, cc, 0:128], scalar1=cw.t[:, 0, cc:cc + 1],
                                                   scalar2=cb.t[:, cc:cc + 1], op0=ALU.mult, op1=ALU.add),
                   [xh[s], cw, cb], [Tile(None, acc[s].k + f"_{cc}")])
                for k in range(1, 4):
                    eng(lambda: ne.scalar_tensor_tensor(out=acc[s].t[:, cc, :], in0=xh[s].t[:, cc, k:k + 128],
                                                        scalar=cw.t[:, k, cc:cc + 1], in1=acc[s].t[:, cc, :],
                                                        op0=ALU.mult, op1=ALU.add),
                        [xh[s], cw, Tile(None, acc[s].k + f"_{cc}")], [Tile(None, acc[s].k + f"_{cc}")])
            AC(lambda: nc.scalar.activation(out=xsT[s].t[:], in_=acc[s].t[:, 0:4, :], func=AF.Silu), [Tile(None, acc[s].k + f"_{c_}") for c_ in range(0, 4)], [xsT[s]])
            AC(lambda: nc.scalar.activation(out=BT[s].t[:], in_=acc[s].t[:, 4:6, :], func=AF.Silu), [Tile(None, acc[s].k + f"_{c_}") for c_ in range(4, 6)], [BT[s]])
            AC(lambda: nc.scalar.activation(out=CT[s].t[:], in_=acc[s].t[:, 6:8, :], func=AF.Silu), [Tile(None, acc[s].k + f"_{c_}") for c_ in range(6, 8)], [CT[s]])
            for cc in range(4):
                PE(lambda: nc.tensor.transpose(out=pxT.t[:, cc * 128:(cc + 1) * 128], in_=xsT[s].t[:, cc, :], identity=idf.t[:]),
                   [xsT[s], idf], [pxT])
            V(lambda: nc.vector.tensor_copy(out=xtm[s].t[:], in_=pxT.t[:]), [pxT], [xtm[s]])
            for g in range(2):
                PE(lambda: nc.tensor.transpose(out=pBt.t[:, g, :], in_=BT[s].t[:, g, :], identity=idb.t[:]), [BT[s], idb], [pBt])
            AC(lambda: nc.scalar.copy(out=Btm[s].t[:], in_=pBt.t[:]), [pBt], [Btm[s]])
            V(lambda: nc.vector.tensor_tensor(out=dts[s].t[:], in0=dtt[s].t[:], in1=dtb.t[:], op=ALU.add), [dtt[s], dtb], [dts[s]])
            V(lambda: nc.vector.scalar_tensor_tensor(out=sp_a[s].t[:], in0=dts[s].t[:], scalar=-1.0, in1=dts[s].t[:],
                                                     op0=ALU.mult, op1=ALU.max), [dts[s]], [sp_a[s]])
            AC(lambda: nc.scalar.activation(out=sp_a[s].t[:], in_=sp_a[s].t[:], func=AF.Exp, scale=-1.0), [sp_a[s]], [sp_a[s]])
            AC(lambda: nc.scalar.activation(out=sp_a[s].t[:], in_=sp_a[s].t[:], func=AF.Ln, bias=1.0), [sp_a[s]], [sp_a[s]])
            V(lambda: nc.vector.scalar_tensor_tensor(out=dts[s].t[:], in0=dts[s].t[:], scalar=0.0, in1=sp_a[s].t[:],
                                                     op0=ALU.max, op1=ALU.add), [dts[s], sp_a[s]], [dts[s]])
            V(lambda: nc.vector.tensor_tensor(out=dA[s].t[:], in0=dts[s].t[:], in1=Aneg.t[:], op=ALU.mult), [dts[s], Aneg], [dA[s]])
            PE(lambda: nc.tensor.matmul(out=pacs.t[:, 0:8], lhsT=tri.t[:], rhs=dA[s].t[:], start=True, stop=True), [tri, dA[s]], [pacs])
            PE(lambda: nc.tensor.matmul(out=pacs.t[:, 8:16], lhsT=ones.t[:], rhs=dA[s].t[:], start=True, stop=True), [ones, dA[s]], [pacs])
            V(lambda: nc.vector.tensor_copy(out=acs[s].t[:], in_=pacs.t[:]), [pacs], [acs[s]])
            AC(lambda: nc.scalar.activation(out=eacs[s].t[:], in_=acs[s].t[:, 0:8], func=AF.Exp), [acs[s]], [eacs[s]])
            AC(lambda: nc.scalar.activation(out=cd[s].t[:], in_=acs[s].t[:, 8:16], func=AF.Exp), [acs[s]], [cd[s]])
            V(lambda: nc.vector.tensor_tensor(out=dsd[s].t[:], in0=acs[s].t[:, 8:16], in1=acs[s].t[:, 0:8], op=ALU.subtract), [acs[s]], [dsd[s]])
            AC(lambda: nc.scalar.activation(out=dsd[s].t[:], in_=dsd[s].t[:], func=AF.Exp), [dsd[s]], [dsd[s]])
            V(lambda: nc.vector.tensor_tensor(out=dsd[s].t[:], in0=dsd[s].t[:], in1=dts[s].t[:], op=ALU.mult), [dsd[s], dts[s]], [dsd[s]])
            x3 = xtm[s].t[:].rearrange("p (h d) -> p h d", h=8)
            V(lambda: nc.vector.tensor_tensor(out=xdt[s].t[:].rearrange("p (h d) -> p h d", h=8), in0=x3,
                                              in1=dts[s].t[:].unsqueeze(2).to_broadcast([128, 8, 64]), op=ALU.mult),
              [xtm[s], dts[s]], [xdt[s]])
            GP(lambda: nc.gpsimd.tensor_tensor(out=xdd[s].t[:].rearrange("p (h d) -> p h d", h=8), in0=x3,
                                               in1=dsd[s].t[:].unsqueeze(2).to_broadcast([128, 8, 64]), op=ALU.mult),
               [xtm[s], dsd[s]], [xdd[s]])
            for g in range(2):
                PE(lambda: nc.tensor.matmul(out=pcb.t[:, g, :], lhsT=BT[s].t[:, g, :], rhs=CT[s].t[:, g, :], start=True, stop=True),
                   [BT[s], CT[s]], [pcb])
            V(lambda: nc.vector.tensor_tensor(out=cbm[s].t[:], in0=pcb.t[:], in1=tri.t[:].unsqueeze(1).to_broadcast([128, 2, 128]),
                                              op=ALU.mult), [pcb, tri], [cbm[s]])
        def tail(i):
            s = i % 2
            t0 = i * 128
            x3 = xtm[s].t[:].rearrange("p (h d) -> p h d", h=8)
            GP(lambda: nc.gpsimd.tensor_tensor(out=dAt.t[:], in0=tri.t[:].unsqueeze(1).to_broadcast([128, 8, 128]),
                                               in1=dA[s].t[:].unsqueeze(2).to_broadcast([128, 8, 128]), op=ALU.mult), [tri, dA[s]], [dAt])
            for g in range(2):
                for j in range(4):
                    PE(lambda: nc.tensor.matmul(out=pD.t[:, j, :], lhsT=slt.t[:], rhs=dAt.t[:, 4 * g + j, :], start=True, stop=True),
                       [slt, dAt], [pD])
                AC(lambda: nc.scalar.activation(out=seg[g].t[:], in_=pD.t[:], func=AF.Exp), [pD], [seg[g]])
                V(lambda: nc.vector.tensor_tensor(out=MT[g].t[:], in0=seg[g].t[:], in1=cbm[s].t[:, g, :].unsqueeze(1).to_broadcast([128, 4, 128]),
                                                  op=ALU.mult), [seg[g], cbm[s]], [MT[g]])
                for j in range(4):
                    h = 4 * g + j
                    PE(lambda: nc.tensor.matmul(out=py.t[:, h * 64:(h + 1) * 64], lhsT=MT[g].t[:, j, :], rhs=xdt[s].t[:, h * 64:(h + 1) * 64],
                                                start=True, stop=True), [MT[g], xdt[s]], [py])
            for g in range(2):
                PE(lambda: nc.tensor.matmul(out=pyo.t[:, g * 256:(g + 1) * 256], lhsT=CT[s].t[:, g, :], rhs=hTb.t[:, g * 256:(g + 1) * 256],
                                            start=True, stop=True), [CT[s], hTb], [pyo])
            for g in range(2):
                PE(lambda: nc.tensor.matmul(out=pU.t[:, g * 256:(g + 1) * 256], lhsT=Btm[s].t[:, g, :], rhs=xdd[s].t[:, g * 256:(g + 1) * 256],
                                            start=True, stop=True), [Btm[s], xdd[s]], [pU])
            AC(lambda: nc.scalar.copy(out=yd[s].t[:], in_=py.t[:]), [py], [yd[s]])
            V(lambda: nc.vector.tensor_tensor(out=y[s].t[:].rearrange("p (h d) -> p h d", h=8),
                                              in0=pyo.t[:].rearrange("p (h d) -> p h d", h=8),
                                              in1=eacs[s].t[:].unsqueeze(2).to_broadcast([128, 8, 64]), op=ALU.mult),
              [pyo, eacs[s]], [y[s]])
            V(lambda: nc.vector.tensor_tensor(out=hT.t[:].rearrange("p (h d) -> p h d", h=8),
                                              in0=hT.t[:].rearrange("p (h d) -> p h d", h=8),
                                              in1=cd[s].t[:].unsqueeze(2).to_broadcast([128, 8, 64]), op=ALU.mult),
              [hT, cd[s]], [hT])
            V(lambda: nc.vector.tensor_tensor(out=hT.t[:], in0=pU.t[:], in1=hT.t[:], op=ALU.add), [pU, hT], [hT])
            AC(lambda: nc.scalar.copy(out=hTb.t[:], in_=hT.t[:]), [hT], [hTb])
            GP(lambda: nc.gpsimd.tensor_tensor(out=y[s].t[:], in0=y[s].t[:], in1=yd[s].t[:], op=ALU.add), [y[s], yd[s]], [y[s]])
            GP(lambda: nc.gpsimd.tensor_tensor(out=yd[s].t[:].rearrange("p (h d) -> p h d", h=8), in0=x3,
                                               in1=dsk.t[:].unsqueeze(2).to_broadcast([128, 8, 64]), op=ALU.mult),
               [xtm[s], dsk], [yd[s]])
            GP(lambda: nc.gpsimd.tensor_tensor(out=y[s].t[:], in0=y[s].t[:], in1=yd[s].t[:], op=ALU.add), [y[s], yd[s]], [y[s]])
            AC(lambda: nc.scalar.activation(out=sz[s].t[:], in_=zt[s].t[:], func=AF.Silu), [zt[s]], [sz[s]])
            V(lambda: nc.vector.tensor_tensor(out=y[s].t[:], in0=y[s].t[:], in1=sz[s].t[:], op=ALU.mult), [y[s], sz[s]], [y[s]])
            for g in range(2):
                AC(lambda: nc.scalar.activation(out=junk.t[:], in_=y[s].t[:, g * 256:(g + 1) * 256], func=AF.Square,
                                                accum_out=ss[s].t[:, g:g + 1]), [y[s]], [junk, ss[s]])
            ssk = [ss[s]]
            V(lambda: nc.vector.tensor_scalar(out=ss[s].t[:], in0=ss[s].t[:], scalar1=1.0 / 256, scalar2=EPS, op0=ALU.mult, op1=ALU.add),
              ssk, [ss[s]])
            AC(lambda: nc.scalar.activation(out=ss[s].t[:], in_=ss[s].t[:], func=AF.Sqrt), [ss[s]], [ss[s]])
            V(lambda: nc.vector.reciprocal(out=ss[s].t[:], in_=ss[s].t[:]), [ss[s]], [ss[s]])
            for g in range(2):
                V(lambda: nc.vector.scalar_tensor_tensor(out=mo[s].t[:, g * 256:(g + 1) * 256], in0=y[s].t[:, g * 256:(g + 1) * 256],
                                                         scalar=ss[s].t[:, g:g + 1], in1=nwb.t[:, g * 256:(g + 1) * 256],
                                                         op0=ALU.mult, op1=ALU.mult), [y[s], ss[s], nwb], [Tile(None, f"ssd_mo{s}_{g}")])
            kb.dma("sp", mo[s].k, self.mix[t0:t0 + 128, 512:1024], mo[s].t[:],
                   reads=[f"ssd_mo{s}_0", f"ssd_mo{s}_1"], writes=[("mix_ssd", i)])

        front(0)
        for i in range(NT):
            if i + 1 < NT:
                interleave(kb, (lambda i=i: tail(i)), (lambda i=i: front(i + 1)))
            else:
                tail(i)


Prog.stage_ssd = _stage_ssd


def _stage_gla(self, l):
    nc, kb, NT = self.nc, self.kb, self.NT
    with ExitStack() as es:
        def T(name, shape, dt=F32):
            return Tile(self.sb(es, "gla_" + name, shape, dt), "gla_" + name)

        def T2(name, shape, dt=F32):
            return [T(f"{name}{j}", shape, dt) for j in range(2)]

        def PT(name, shape, dt=F32):
            return Tile(self.ps(es, "gla_" + name, shape, dt), "gla_" + name)

        def V(fn, r, w):
            return kb.op("dve", fn, [t.k for t in r], [t.k for t in w])

        def AC(fn, r, w):
            return kb.op("act", fn, [t.k for t in r], [t.k for t in w])

        def PE(fn, r, w):
            return kb.op("pe", fn, [t.k for t in r], [t.k for t in w])

        def GP(fn, r, w):
            return kb.op("pool", fn, [t.k for t in r], [t.k for t in w])

        def LD(t, dst, src, reads=(), **kw):
            return kb.dma("sp", t.k, dst, src, reads=list(reads), writes=[t.k], **kw)

        w2 = T("w2", [16, 128]); bb = T("bb", [128, 128]); nwb = T("nwb", [128, 256])
        tri = T("tri", [128, 128]); tri16 = T("tri16", [128, 128]); o16 = T("o16", [128, 2])
        idf = T("idf", [128, 128]); idb = T("idb", [128, 128], BF16)
        hm = T("hm", [128, 4]); bd = T("bd", [128, 256])
        Sbd = T("Sbd", [128, 256]); Sbb = T("Sbb", [128, 256], BF16)
        gin = T2("gin", [128, 784])
        glrT = T2("glrT", [16, 128]); zv = T2("zv", [128, 128]); az = T2("az", [128, 128]); la = T2("la", [128, 128])
        eb = T2("eb", [128, 128]); enb = T2("enb", [128, 128])
        qt = T2("qtok", [128, 128], BF16); kt = T2("ktok", [128, 128], BF16); vb = T2("vb", [128, 256], BF16)
        qT = T2("qT", [128, 128], BF16); kTm = T2("kTm", [128, 4, 128], BF16)
        egt = T2("egt", [128, 2]); ATm = T2("ATm", [128, 4, 128], BF16)
        sq = T2("sq", [128, 256]); ss = T2("ss", [128, 4]); on = T2("on", [128, 256]); sr = T2("sr", [128, 256])
        mo = T2("mo", [128, 256], BF16); um = T2("um", [128, 256])
        pgT = PT("pgT", [16, 128]); pz = PT("pz", [128, 128]); pbc = PT("pbc", [128, 128])
        pqk = PT("pqk", [128, 2, 128], BF16); pgt = PT("pgt", [128, 2]); pA = PT("pA", [128, 4, 128])
        po = PT("po", [128, 256]); pU = PT("pU", [128, 256])

        LD(w2, w2.t[:], self.gla_gate_w2[l, :, :])
        LD(bb, bb.t[:], self.gla_gate_b[l, :].partition_broadcast(128))
        LD(nwb, nwb.t[:], self.gla_norm_w[l, :].partition_broadcast(128))
        LD(tri, tri.t[:], self.c_tri[:, :]); LD(idf, idf.t[:], self.ident_f[:, :]); LD(idb, idb.t[:], self.ident_b[:, :])
        LD(hm, hm.t[:], self.c_hm[:, :]); LD(bd, bd.t[:], self.c_bd[:, :])
        V(lambda: nc.vector.tensor_scalar(out=tri16.t[:], in0=tri.t[:], scalar1=1.0 / 16, scalar2=None, op0=ALU.mult), [tri], [tri16])
        V(lambda: nc.vector.memset(o16.t[:], 1.0 / 16), [], [o16])
        V(lambda: nc.vector.memset(Sbd.t[:], 0.0), [], [Sbd])
        V(lambda: nc.vector.memset(Sbb.t[:], 0.0), [], [Sbb])

        def front(i):
            s = i % 2
            t0 = i * 128
            kb.dma("sp", gin[s].k, gin[s].t[:], self.proj[t0:t0 + 128, 0:784], reads=[("proj", i)], writes=[gin[s].k])
            q_ = gin[s].t[:, C_GQ:C_GQ + 128]; k_ = gin[s].t[:, C_GK:C_GK + 128]; v_ = gin[s].t[:, C_GV:C_GV + 256]
            glr_ = gin[s].t[:, C_GLR:C_GLR + 16]; r_ = gin[s].t[:, C_GR:C_GR + 256]
            PE(lambda: nc.tensor.transpose(out=pgT.t[:], in_=glr_, identity=idf.t[:]), [gin[s], idf], [pgT])
            V(lambda: nc.vector.tensor_copy(out=glrT[s].t[:], in_=pgT.t[:]), [pgT], [glrT[s]])
            PE(lambda: nc.tensor.matmul(out=pz.t[:], lhsT=glrT[s].t[:], rhs=w2.t[:], start=True, stop=True), [glrT[s], w2], [pz])
            V(lambda: nc.vector.tensor_tensor(out=zv[s].t[:], in0=pz.t[:], in1=bb.t[:], op=ALU.add), [pz, bb], [zv[s]])
            V(lambda: nc.vector.scalar_tensor_tensor(out=az[s].t[:], in0=zv[s].t[:], scalar=-1.0, in1=zv[s].t[:], op0=ALU.mult, op1=ALU.max),
              [zv[s]], [az[s]])
            AC(lambda: nc.scalar.activation(out=az[s].t[:], in_=az[s].t[:], func=AF.Exp, scale=-1.0), [az[s]], [az[s]])
            AC(lambda: nc.scalar.activation(out=az[s].t[:], in_=az[s].t[:], func=AF.Ln, bias=1.0), [az[s]], [az[s]])
            V(lambda: nc.vector.scalar_tensor_tensor(out=la[s].t[:], in0=zv[s].t[:], scalar=0.0, in1=az[s].t[:], op0=ALU.min, op1=ALU.subtract),
              [zv[s], az[s]], [la[s]])
            PE(lambda: nc.tensor.matmul(out=pbc.t[:], lhsT=tri16.t[:], rhs=la[s].t[:], start=True, stop=True), [tri16, la[s]], [pbc])
            PE(lambda: nc.tensor.matmul(out=pgt.t[:], lhsT=la[s].t[:], rhs=o16.t[:], start=True, stop=True), [la[s], o16], [pgt])
            AC(lambda: nc.scalar.activation(out=eb[s].t[:], in_=pbc.t[:], func=AF.Exp), [pbc], [eb[s]])
            AC(lambda: nc.scalar.activation(out=enb[s].t[:], in_=pbc.t[:], func=AF.Exp, scale=-1.0), [pbc], [enb[s]])
            AC(lambda: nc.scalar.activation(out=egt[s].t[:], in_=pgt.t[:], func=AF.Exp), [pgt], [egt[s]])
            V(lambda: nc.vector.scalar_tensor_tensor(out=qt[s].t[:], in0=q_, scalar=32.0 ** -0.5, in1=eb[s].t[:], op0=ALU.mult, op1=ALU.mult),
              [gin[s], eb[s]], [qt[s]])
            V(lambda: nc.vector.tensor_tensor(out=kt[s].t[:], in0=k_, in1=enb[s].t[:], op=ALU.mult), [gin[s], enb[s]], [kt[s]])
            GP(lambda: nc.gpsimd.tensor_copy(out=vb[s].t[:], in_=v_), [gin[s]], [vb[s]])
            PE(lambda: nc.tensor.transpose(out=pqk.t[:, 0, :], in_=qt[s].t[:], identity=idb.t[:]), [qt[s], idb], [pqk])
            PE(lambda: nc.tensor.transpose(out=pqk.t[:, 1, :], in_=kt[s].t[:], identity=idb.t[:]), [kt[s], idb], [pqk])
            AC(lambda: nc.scalar.copy(out=qT[s].t[:], in_=pqk.t[:, 0, :]), [pqk], [qT[s]])
            for h in range(4):
                AC(lambda: nc.scalar.activation(out=kTm[s].t[:, h, :], in_=pqk.t[:, 1, :], func=AF.Copy, scale=hm.t[:, h:h + 1]),
                   [pqk, hm], [kTm[s]])
            for h in range(4):
                PE(lambda: nc.tensor.matmul(out=pA.t[:, h, :], lhsT=kTm[s].t[:, h, :], rhs=qT[s].t[:], start=True, stop=True),
                   [kTm[s], qT[s]], [pA])
            V(lambda: nc.vector.tensor_tensor(out=ATm[s].t[:], in0=pA.t[:], in1=tri.t[:].unsqueeze(1).to_broadcast([128, 4, 128]), op=ALU.mult),
              [pA, tri], [ATm[s]])
        def tail(i):
            s = i % 2
            t0 = i * 128
            r_ = gin[s].t[:, C_GR:C_GR + 256]
            PE(lambda: nc.tensor.matmul(out=po.t[:], lhsT=qT[s].t[:], rhs=Sbb.t[:], start=True, stop=False), [qT[s], Sbb], [po])
            for h in range(4):
                PE(lambda: nc.tensor.matmul(out=po.t[:, h * 64:(h + 1) * 64], lhsT=ATm[s].t[:, h, :], rhs=vb[s].t[:, h * 64:(h + 1) * 64],
                                            start=False, stop=(h == 3)), [ATm[s], vb[s]], [po])
            PE(lambda: nc.tensor.matmul(out=pU.t[:], lhsT=kt[s].t[:], rhs=vb[s].t[:], start=True, stop=True), [kt[s], vb[s]], [pU])
            V(lambda: nc.vector.tensor_tensor(out=um[s].t[:], in0=pU.t[:], in1=bd.t[:], op=ALU.mult), [pU, bd], [um[s]])
            V(lambda: nc.vector.tensor_tensor(out=Sbd.t[:], in0=Sbd.t[:], in1=um[s].t[:], op=ALU.add), [Sbd, um[s]], [Sbd])
            V(lambda: nc.vector.tensor_scalar(out=Sbd.t[:], in0=Sbd.t[:], scalar1=egt[s].t[:, 0:1], scalar2=None, op0=ALU.mult), [Sbd, egt[s]], [Sbd])
            AC(lambda: nc.scalar.copy(out=Sbb.t[:], in_=Sbd.t[:]), [Sbd], [Sbb])
            AC(lambda: nc.scalar.activation(out=sq[s].t[:], in_=po.t[:], func=AF.Square), [po], [sq[s]])
            V(lambda: nc.vector.tensor_reduce(out=ss[s].t[:], in_=sq[s].t[:].rearrange("p (h d) -> p h d", h=4), axis=AX.X, op=ALU.add),
              [sq[s]], [ss[s]])
            V(lambda: nc.vector.tensor_scalar(out=ss[s].t[:], in0=ss[s].t[:], scalar1=1.0 / 64, scalar2=EPS, op0=ALU.mult, op1=ALU.add), [ss[s]], [ss[s]])
            AC(lambda: nc.scalar.activation(out=ss[s].t[:], in_=ss[s].t[:], func=AF.Sqrt), [ss[s]], [ss[s]])
            V(lambda: nc.vector.reciprocal(out=ss[s].t[:], in_=ss[s].t[:]), [ss[s]], [ss[s]])
            V(lambda: nc.vector.tensor_tensor(out=on[s].t[:].rearrange("p (h d) -> p h d", h=4), in0=po.t[:].rearrange("p (h d) -> p h d", h=4),
                                              in1=ss[s].t[:].unsqueeze(2).to_broadcast([128, 4, 64]), op=ALU.mult), [po, ss[s]], [on[s]])
            AC(lambda: nc.scalar.activation(out=sr[s].t[:], in_=r_, func=AF.Silu), [gin[s]], [sr[s]])
            GP(lambda: nc.gpsimd.tensor_tensor(out=sr[s].t[:], in0=sr[s].t[:], in1=nwb.t[:], op=ALU.mult), [sr[s], nwb], [sr[s]])
            V(lambda: nc.vector.tensor_tensor(out=mo[s].t[:], in0=on[s].t[:], in1=sr[s].t[:], op=ALU.mult), [on[s], sr[s]], [mo[s]])
            kb.dma("sp", mo[s].k, self.mix[t0:t0 + 128, 0:256], mo[s].t[:], reads=[mo[s].k], writes=[("mix_gla", i)])

        front(0)
        for i in range(NT):
            if i + 1 < NT:
                interleave(kb, (lambda i=i: tail(i)), (lambda i=i: front(i + 1)))
            else:
                tail(i)


Prog.stage_gla = _stage_gla


def build_test_mix(S, which, limit=None):
    p = Prog(S, L=1, dbg=[("o_mix", [S, D])])
    with p.kb.es as es:
        p.stage_d1(0, p.x)
        p.kb.barrier()
        if limit is not None:
            p.kb.limit = p.kb.ninst + limit
        if "ssd" in which:
            p.stage_ssd(0)
        if "gla" in which:
            p.stage_gla(0)
        if "nsa" in which:
            p.stage_nsa(0)
        kb, nc = p.kb, p.nc
        kb.limit = None
        kb.barrier()
        mb = p.sb(es, "dump_mb", [128, D], BF16)
        mf = p.sb(es, "dump_mf", [128, D])
        for i in range(p.NT):
            kb.dma("sp", "dump_mb", mb[:], p.mix[i * 128:(i + 1) * 128, :], writes=["dump_mb"])
            kb.op("dve", lambda: nc.vector.tensor_copy(out=mf[:], in_=mb[:]), reads=["dump_mb"], writes=["dump_mf"])
            kb.dma("sp", "dump_mf", p.outs["o_mix"][i * 128:(i + 1) * 128, :], mf[:], reads=["dump_mf"], writes=[("o_mix", i)])
        kb.drain("sp")
    return p


def _stage_d2(self, l, hsrc, hdst, final):
    nc, kb, NT = self.nc, self.kb, self.NT
    with ExitStack() as es:
        def T(name, shape, dt=F32):
            return Tile(self.sb(es, "d2_" + name, shape, dt), "d2_" + name)

        def T2(name, shape, dt=F32):
            return [T(f"{name}{j}", shape, dt) for j in range(2)]

        def PT(name, shape, dt=F32):
            return Tile(self.ps(es, "d2_" + name, shape, dt), "d2_" + name)

        def V(fn, r, w):
            return kb.op("dve", fn, [t.k for t in r], [t.k for t in w])

        def AC(fn, r, w):
            return kb.op("act", fn, [t.k for t in r], [t.k for t in w])

        def PE(fn, r, w):
            return kb.op("pe", fn, [t.k for t in r], [t.k for t in w])

        def GP(fn, r, w):
            return kb.op("pool", fn, [t.k for t in r], [t.k for t in w])

        Wo = T("Wo", [128, 8, 1024], BF16); Wu = T("Wu", [128, 8, 4096], BF16); Wd = T("Wd", [128, 32, 1024], BF16)
        stg = T2("stg", [128, 1024]); nw2 = T("nw2", [128, 8]); idb = T("idb", [128, 128], BF16)
        ht = T2("ht", [128, 1024]); mt = T2("mt", [128, 1024], BF16)
        mT = T("mT", [128, 8, 128], BF16); h1 = T2("h1", [128, 1024]); junk = T("junk", [128, 1024], BF16)
        ss = T("ss", [128, 1]); u2 = T("u2", [128, 1024], BF16); u2T = T2("u2T", [128, 8, 128], BF16)
        tmp = T2("tmp", [128, 512]); hidT = T("hidT", [128, 32, 128], BF16); ho = T2("ho", [128, 1024])
        pT = PT("pT", [128, 8, 128], BF16); po = [PT(f"po{j}", [128, 512]) for j in range(2)]
        pu = [PT(f"pu{j}", [128, 4, 128]) for j in range(2)]
        pd = [PT(f"pd{j}", [128, 512]) for j in range(2)]
        if final:
            fnw = T("fnw", [128, 1024]); ss2 = T("ss2", [128, 1])
            kb.dma("sp", fnw.k, fnw.t[:], self.final_norm_w.partition_broadcast(128), writes=[fnw.k])

        kb.dma("sp", idb.k, idb.t[:], self.ident_b[:, :], writes=[idb.k])
        for c in range(8):
            kb.dma("sp", nw2.k, nw2.t[:, c:c + 1], self.norm2_w[l, c * 128:(c + 1) * 128].rearrange("(p o) -> p o", o=1), writes=[nw2.k])
        n = 0
        jobs = [(Wo, c, 0, self.w_out[l, c * 128:(c + 1) * 128, :], False) for c in range(8)]
        jobs += [(Wu, c, q * 1024, self.w_up[l, c * 128:(c + 1) * 128, q * 1024:(q + 1) * 1024], True) for c in range(8) for q in range(4)]
        jobs += [(Wd, f, 0, self.w_down[l, f * 128:(f + 1) * 128, :], False) for f in range(32)]
        for (W, c, off, src, scaled) in jobs:
            sg = stg[n % 2]
            kb.dma("sp", sg.k, sg.t[:], src, writes=[sg.k])
            dst = W.t[:, c, off:off + 1024]
            if scaled:
                if n % 2 == 0:
                    V(lambda: nc.vector.tensor_scalar(out=dst, in0=sg.t[:], scalar1=nw2.t[:, c:c + 1], scalar2=None, op0=ALU.mult), [sg, nw2], [W])
                else:
                    AC(lambda: nc.scalar.activation(out=dst, in_=sg.t[:], func=AF.Copy, scale=nw2.t[:, c:c + 1]), [sg, nw2], [W])
            else:
                if n % 2 == 0:
                    V(lambda: nc.vector.tensor_copy(out=dst, in_=sg.t[:]), [sg], [W])
                else:
                    AC(lambda: nc.scalar.copy(out=dst, in_=sg.t[:]), [sg], [W])
            n += 1

        def front(i):
            s = i % 2
            t0 = i * 128
            kb.dma("sp", ht[s].k, ht[s].t[:], hsrc[t0:t0 + 128, :], reads=[("h", i)], writes=[ht[s].k])
            kb.dma("sp", mt[s].k, mt[s].t[:], self.mix[t0:t0 + 128, :], reads=[("mix_gla", i), ("mix_nsa", i), ("mix_ssd", i)], writes=[mt[s].k])
            for c in range(8):
                PE(lambda: nc.tensor.transpose(out=pT.t[:, c, :], in_=mt[s].t[:, c * 128:(c + 1) * 128], identity=idb.t[:]), [mt[s], idb], [pT])
            AC(lambda: nc.scalar.copy(out=mT.t[:], in_=pT.t[:]), [pT], [mT])
            for hf in range(2):
                for c in range(8):
                    PE(lambda: nc.tensor.matmul(out=po[hf].t[:], lhsT=mT.t[:, c, :], rhs=Wo.t[:, c, hf * 512:(hf + 1) * 512],
                                                start=(c == 0), stop=(c == 7)), [mT, Wo], [po[hf]])
                V(lambda: nc.vector.tensor_tensor(out=h1[s].t[:, hf * 512:(hf + 1) * 512], in0=po[hf].t[:], in1=ht[s].t[:, hf * 512:(hf + 1) * 512], op=ALU.add),
                  [po[hf], ht[s]], [h1[s]])
            AC(lambda: nc.scalar.activation(out=junk.t[:], in_=h1[s].t[:], func=AF.Square, accum_out=ss.t[:]), [h1[s]], [junk, ss])
            V(lambda: nc.vector.tensor_scalar(out=ss.t[:], in0=ss.t[:], scalar1=1.0 / D, scalar2=EPS, op0=ALU.mult, op1=ALU.add), [ss], [ss])
            AC(lambda: nc.scalar.activation(out=ss.t[:], in_=ss.t[:], func=AF.Sqrt), [ss], [ss])
            V(lambda: nc.vector.reciprocal(out=ss.t[:], in_=ss.t[:]), [ss], [ss])
            V(lambda: nc.vector.tensor_scalar(out=u2.t[:], in0=h1[s].t[:], scalar1=ss.t[:], scalar2=None, op0=ALU.mult), [h1[s], ss], [u2])
            for c in range(8):
                PE(lambda: nc.tensor.transpose(out=pT.t[:, c, :], in_=u2.t[:, c * 128:(c + 1) * 128], identity=idb.t[:]), [u2, idb], [pT])
            AC(lambda: nc.scalar.copy(out=u2T[s].t[:], in_=pT.t[:]), [pT], [u2T[s]])
        def tail(i):
            s = i % 2
            t0 = i * 128
            for fg in range(8):
                p = pu[fg % 2]
                for j in range(4):
                    f = fg * 4 + j
                    for c in range(8):
                        PE(lambda: nc.tensor.matmul(out=p.t[:, j, :], lhsT=Wu.t[:, c, f * 128:(f + 1) * 128], rhs=u2T[s].t[:, c, :],
                                                    start=(c == 0), stop=(c == 7)), [Wu, u2T[s]], [p])
                tm = tmp[fg % 2]
                AC(lambda: nc.scalar.activation(out=tm.t[:], in_=p.t[:].rearrange("p a b -> p (a b)"), func=AF.Relu), [p], [tm])
                GP(lambda: nc.gpsimd.tensor_tensor(out=hidT.t[:, fg * 4:(fg + 1) * 4, :].rearrange("p a b -> p (a b)"), in0=tm.t[:], in1=tm.t[:], op=ALU.mult),
                   [tm], [Tile(None, f"d2_hid{fg}")])
            hk = [Tile(None, f"d2_hid{fg}") for fg in range(8)]
            for f in range(32):
                for hf in range(2):
                    PE(lambda: nc.tensor.matmul(out=pd[hf].t[:], lhsT=hidT.t[:, f, :], rhs=Wd.t[:, f, hf * 512:(hf + 1) * 512],
                                                start=(f == 0), stop=(f == 31)), [hk[f // 4], Wd], [pd[hf]])
            for hf in range(2):
                V(lambda: nc.vector.tensor_tensor(out=ho[s].t[:, hf * 512:(hf + 1) * 512], in0=pd[hf].t[:], in1=h1[s].t[:, hf * 512:(hf + 1) * 512], op=ALU.add),
                  [pd[hf], h1[s]], [ho[s]])
            if final:
                AC(lambda: nc.scalar.activation(out=junk.t[:], in_=ho[s].t[:], func=AF.Square, accum_out=ss2.t[:]), [ho[s]], [junk, ss2])
                V(lambda: nc.vector.tensor_scalar(out=ss2.t[:], in0=ss2.t[:], scalar1=1.0 / D, scalar2=EPS, op0=ALU.mult, op1=ALU.add), [ss2], [ss2])
                AC(lambda: nc.scalar.activation(out=ss2.t[:], in_=ss2.t[:], func=AF.Sqrt), [ss2], [ss2])
                V(lambda: nc.vector.reciprocal(out=ss2.t[:], in_=ss2.t[:]), [ss2], [ss2])
                V(lambda: nc.vector.scalar_tensor_tensor(out=ho[s].t[:], in0=ho[s].t[:], scalar=ss2.t[:], in1=fnw.t[:], op0=ALU.mult, op1=ALU.mult),
                  [ho[s], ss2, fnw], [ho[s]])
            kb.dma("sp", ho[s].k, hdst[t0:t0 + 128, :], ho[s].t[:], reads=[ho[s].k], writes=[("hout", l, i)] if final else [("h", i)])

        front(0)
        for i in range(NT):
            if i + 1 < NT:
                interleave(kb, (lambda i=i: tail(i)), (lambda i=i: front(i + 1)))
            else:
                tail(i)


Prog.stage_d2 = _stage_d2


def _stage_nsa(self, l):
    nc, kb, NT, S, NS, NCC = self.nc, self.kb, self.NT, self.S, self.NS, self.NCC
    NCB = S // 16 - 1
    with ExitStack() as es:
        def Tn(es_, name, shape, dt=F32):
            return Tile(self.sb(es_, "nsa_" + name, shape, dt), "nsa_" + name)

        def T(name, shape, dt=F32):
            return Tn(es, name, shape, dt)

        def V(fn, r, w):
            return kb.op("dve", fn, [t.k for t in r], [t.k for t in w])

        def AC(fn, r, w):
            return kb.op("act", fn, [t.k for t in r], [t.k for t in w])

        def PE(fn, r, w):
            return kb.op("pe", fn, [t.k for t in r], [t.k for t in w])

        def GP(fn, r, w):
            return kb.op("pool", fn, [t.k for t in r], [t.k for t in w])

        def LD(t, dst, src, reads=(), **kw):
            return kb.dma("sp", t.k, dst, src, reads=list(reads), writes=[t.k], **kw)

        QT = T("QT", [128, NT, 2, 128], BF16); KsT = T("KsT", [128, S], BF16); KwT = T("KwT", [128, S], BF16)
        KCT = T("KCT", [128, NCC * 128], BF16)
        VsA = T("VsA", [128, NT, 2, 65], BF16); VwA = T("VwA", [128, NT, 2, 65], BF16)
        RC = T("RC", [128, NCC, 2, 65 + NS], BF16); GA = T("GA", [128, NT, 12])
        idb = T("idb", [128, 128], BF16); nwb = T("nwb", [128, 256])
        LD(idb, idb.t[:], self.ident_b[:, :])
        LD(nwb, nwb.t[:], self.nsa_norm_w[l, :].partition_broadcast(128))
        V(lambda: nc.vector.memset(KCT.t[:], 0.0), [], [KCT])
        V(lambda: nc.vector.memset(RC.t[:], 0.0), [], [RC])
        V(lambda: nc.vector.memset(VsA.t[:], 1.0), [], [VsA])
        V(lambda: nc.vector.memset(VwA.t[:], 1.0), [], [VwA])

        with ExitStack() as e1:
            nin = [Tn(e1, f"nin{j}", [128, 1036]) for j in range(2)]
            cs = [Tn(e1, f"cs{j}", [128, 2, 32]) for j in range(2)]
            ta2 = [Tn(e1, f"ta{j}", [128, 10, 32]) for j in range(2)]; tb2 = [Tn(e1, f"tb{j}", [128, 10, 32]) for j in range(2)]
            rq = [Tn(e1, f"rq{j}", [128, 768], BF16) for j in range(2)]
            kv = [Tn(e1, f"kv{j}", [128, 2, 128], BF16) for j in range(2)]
            pT2 = [Tile(self.ps(e1, f"nsa_pT1{j}", [128, 6, 128], BF16), f"nsa_pT1{j}") for j in range(2)]

            def n1(i):
                s = i % 2
                t0 = i * 128
                ta, tb, pT = ta2[s], tb2[s], pT2[s]
                kb.dma("sp", nin[s].k, nin[s].t[:], self.proj[t0:t0 + 128, C_NQ:C_NQ + 1036], reads=[("proj", i)], writes=[nin[s].k])
                kb.dma("sp", cs[s].k + "c", cs[s].t[:, 0, :], self.rope_cos[t0:t0 + 128, :], writes=[cs[s].k + "c"])
                kb.dma("sp", cs[s].k + "s", cs[s].t[:, 1, :], self.rope_sin[t0:t0 + 128, :], writes=[cs[s].k + "s"])
                csk = [Tile(None, cs[s].k + "c"), Tile(None, cs[s].k + "s")]
                x3 = nin[s].t[:, 0:640].rearrange("p (h d) -> p h d", h=10)
                o3 = rq[s].t[:, 0:640].rearrange("p (h d) -> p h d", h=10)
                cb_ = cs[s].t[:, 0, :].unsqueeze(1).to_broadcast([128, 10, 32])
                sb_ = cs[s].t[:, 1, :].unsqueeze(1).to_broadcast([128, 10, 32])
                V(lambda: nc.vector.tensor_tensor(out=ta.t[:], in0=x3[:, :, 0:32], in1=cb_, op=ALU.mult), [nin[s]] + csk, [ta])
                V(lambda: nc.vector.tensor_tensor(out=tb.t[:], in0=x3[:, :, 32:64], in1=sb_, op=ALU.mult), [nin[s]] + csk, [tb])
                V(lambda: nc.vector.tensor_tensor(out=o3[:, :, 0:32], in0=ta.t[:], in1=tb.t[:], op=ALU.subtract), [ta, tb], [Tile(None, rq[s].k + "a")])
                V(lambda: nc.vector.tensor_tensor(out=ta.t[:], in0=x3[:, :, 32:64], in1=cb_, op=ALU.mult), [nin[s]] + csk, [ta])
                V(lambda: nc.vector.tensor_tensor(out=tb.t[:], in0=x3[:, :, 0:32], in1=sb_, op=ALU.mult), [nin[s]] + csk, [tb])
                V(lambda: nc.vector.tensor_tensor(out=o3[:, :, 32:64], in0=ta.t[:], in1=tb.t[:], op=ALU.add), [ta, tb], [Tile(None, rq[s].k + "b")])
                AC(lambda: nc.scalar.copy(out=rq[s].t[:, 640:768], in_=nin[s].t[:, 640:768]), [nin[s]], [Tile(None, rq[s].k + "c")])
                rqk = [Tile(None, rq[s].k + x) for x in "abc"]
                for b in range(6):
                    PE(lambda: nc.tensor.transpose(out=pT.t[:, b, :], in_=rq[s].t[:, b * 128:(b + 1) * 128], identity=idb.t[:]), rqk + [idb], [pT])
                AC(lambda: nc.scalar.copy(out=QT.t[:, i, :, :], in_=pT.t[:, 0:2, :]), [pT], [Tile(None, f"nsa_QT{i}")])
                AC(lambda: nc.scalar.copy(out=KsT.t[:, t0:t0 + 128], in_=pT.t[:, 3, :]), [pT], [Tile(None, f"nsa_KsT{i}")])
                AC(lambda: nc.scalar.copy(out=KwT.t[:, t0:t0 + 128], in_=pT.t[:, 4, :]), [pT], [Tile(None, f"nsa_KwT{i}")])
                AC(lambda: nc.scalar.copy(out=kv[s].t[:, 0, :], in_=pT.t[:, 2, :]), [pT], [kv[s]])
                AC(lambda: nc.scalar.copy(out=kv[s].t[:, 1, :], in_=pT.t[:, 5, :]), [pT], [kv[s]])
                kb.dma("sp", kv[s].k + "k", self.kcT_d[:, t0:t0 + 128], kv[s].t[:, 0, :], reads=[kv[s].k], writes=[("kcT_d", i)])
                kb.dma("sp", kv[s].k + "v", self.vcT_d[:, t0:t0 + 128], kv[s].t[:, 1, :], reads=[kv[s].k], writes=[("vcT_d", i)])
                GP(lambda: nc.gpsimd.tensor_copy(out=VsA.t[:, i, :, 0:64], in_=nin[s].t[:, 768:896].rearrange("p (g d) -> p g d", g=2)),
                   [nin[s], VsA], [Tile(None, f"nsa_VsA{i}")])
                GP(lambda: nc.gpsimd.tensor_copy(out=VwA.t[:, i, :, 0:64], in_=nin[s].t[:, 896:1024].rearrange("p (g d) -> p g d", g=2)),
                   [nin[s], VwA], [Tile(None, f"nsa_VwA{i}")])
                AC(lambda: nc.scalar.activation(out=GA.t[:, i, :], in_=nin[s].t[:, 1024:1036], func=AF.Sigmoid), [nin[s]], [Tile(None, f"nsa_GA{i}")])

            for i in range(0, NT, 2):
                if i + 1 < NT:
                    interleave(kb, (lambda i=i: n1(i)), (lambda i=i: n1(i + 1)))
                else:
                    n1(i)
            kb.barrier()

        with ExitStack() as e2:
            wst = Tn(e2, "wst", [128, 32, 128]); W1 = [Tn(e2, f"W1{j}", [128, 32, 128], BF16) for j in range(2)]
            w2s = Tn(e2, "w2s", [128, 64]); W2p = Tn(e2, "W2p", [128, 2, 128], BF16); W2v = Tn(e2, "W2v", [128, 64], BF16)
            psT = Tn(e2, "psT", [128, 32]); posb = [Tn(e2, f"posb{j}", [128, 32, 2], BF16) for j in range(2)]
            cbv = [Tn(e2, f"cbv{j}", [128, 2]) for j in range(2)]
            XT = Tn(e2, "XT", [128, 128 * 16 + 16], BF16)
            hb = Tn(e2, "hb", [128, 128]); tt = Tn(e2, "tt", [128, 128]); GT = [Tn(e2, f"GT{g}", [128, 128], BF16) for g in range(2)]
            ph = Tile(self.ps(e2, "nsa_ph", [128, 128]), "nsa_ph"); pc = Tile(self.ps(e2, "nsa_pc", [128, 128]), "nsa_pc")
            pb = Tile(self.ps(e2, "nsa_pb", [128, 2]), "nsa_pb")
            V(lambda: nc.vector.memset(W2p.t[:], 0.0), [], [W2p])
            for wi, (w1d, w2d, posd) in enumerate([(self.nsa_cmp_w1_k, self.nsa_cmp_w2_k, self.nsa_cmp_pos_k),
                                                   (self.nsa_cmp_w1_v, self.nsa_cmp_w2_v, self.nsa_cmp_pos_v)]):
                for half in range(2):
                    LD(wst, wst.t[half * 64:(half + 1) * 64, :, :], w1d[l].rearrange("(i d) j -> d i j", d=64))
                V(lambda: nc.vector.tensor_copy(out=W1[wi].t[:], in_=wst.t[:]), [wst], [W1[wi]])
                LD(w2s, w2s.t[:], w2d[l, :, :])
                if wi == 0:
                    for g in range(2):
                        V(lambda: nc.vector.tensor_copy(out=W2p.t[:, g, g * 64:(g + 1) * 64], in_=w2s.t[:]), [w2s], [W2p])
                else:
                    V(lambda: nc.vector.tensor_copy(out=W2v.t[:], in_=w2s.t[:]), [w2s], [W2v])
                for half in range(2):
                    LD(psT, psT.t[half * 64:(half + 1) * 64, :], posd[l].rearrange("i d -> d i"), allow_slow_non_contiguous=True)
                for j in range(2):
                    V(lambda: nc.vector.tensor_copy(out=posb[wi].t[:, :, j], in_=psT.t[:]), [psT], [posb[wi]])
                for i_ in range(32):
                    PE(lambda: nc.tensor.matmul(out=pb.t[:], lhsT=W1[wi].t[0:64, i_, :], rhs=posb[wi].t[0:64, i_, :], start=(i_ == 0), stop=(i_ == 31)),
                       [W1[wi], posb[wi]], [pb])
                V(lambda: nc.vector.tensor_copy(out=cbv[wi].t[:], in_=pb.t[:]), [pb], [cbv[wi]])
            for c in range(NCC):
                n0 = c * 128
                nn = min(128, NCB - n0)
                ntok = 16 * nn + 16
                for wi, xd, xkey in [(0, self.kcT_d, "kcT_d"), (1, self.vcT_d, "vcT_d")]:
                    LD(XT, XT.t[:, 0:ntok], xd[:, 16 * n0:16 * n0 + ntok],
                       reads=[(xkey, j) for j in range((16 * n0) // 128, min(NT, (16 * n0 + ntok + 127) // 128))])
                    xv = XT.t[:, 0:ntok].rearrange("p (n s) -> p n s", s=16)
                    for g in range(2):
                        for i_ in range(32):
                            a, b = i_ // 16, i_ % 16
                            PE(lambda: nc.tensor.matmul(out=ph.t[:, 0:nn], lhsT=W1[wi].t[g * 64:(g + 1) * 64, i_, :],
                                                        rhs=xv[g * 64:(g + 1) * 64, a:a + nn, b], start=(i_ == 0), stop=(i_ == 31)),
                               [W1[wi], XT], [ph])
                        AC(lambda: nc.scalar.activation(out=hb.t[:, 0:nn], in_=ph.t[:, 0:nn], func=AF.Identity, bias=cbv[wi].t[:, 0:1]), [ph, cbv[wi]], [hb])
                        V(lambda: nc.vector.tensor_tensor(out=tt.t[:, 0:nn], in0=hb.t[:, 0:nn], in1=hb.t[:, 0:nn], op=ALU.mult), [hb], [tt])
                        V(lambda: nc.vector.tensor_scalar(out=tt.t[:, 0:nn], in0=tt.t[:, 0:nn], scalar1=0.044715, scalar2=1.0, op0=ALU.mult, op1=ALU.add), [tt], [tt])
                        V(lambda: nc.vector.tensor_tensor(out=tt.t[:, 0:nn], in0=tt.t[:, 0:nn], in1=hb.t[:, 0:nn], op=ALU.mult), [tt, hb], [tt])
                        AC(lambda: nc.scalar.activation(out=tt.t[:, 0:nn], in_=tt.t[:, 0:nn], func=AF.Tanh, scale=0.7978845608028654), [tt], [tt])
                        V(lambda: nc.vector.tensor_scalar(out=tt.t[:, 0:nn], in0=tt.t[:, 0:nn], scalar1=0.5, scalar2=0.5, op0=ALU.mult, op1=ALU.add), [tt], [tt])
                        if nn < 128:
                            V(lambda: nc.vector.memset(GT[g].t[:], 0.0), [], [GT[g]])
                        V(lambda: nc.vector.tensor_tensor(out=GT[g].t[:, 0:nn], in0=tt.t[:, 0:nn], in1=hb.t[:, 0:nn], op=ALU.mult), [tt, hb], [GT[g]])
                    if wi == 0:
                        for g in range(2):
                            PE(lambda: nc.tensor.matmul(out=pc.t[:, 0:nn], lhsT=W2p.t[:, g, :], rhs=GT[g].t[:, 0:nn], start=(g == 0), stop=(g == 1)),
                               [W2p, GT[g]], [pc])
                        AC(lambda: nc.scalar.copy(out=KCT.t[:, n0:n0 + nn], in_=pc.t[:, 0:nn]), [pc], [KCT])
                    else:
                        for g in range(2):
                            PE(lambda: nc.tensor.matmul(out=pc.t[:, g * 64:(g + 1) * 64], lhsT=GT[g].t[:], rhs=W2v.t[:], start=True, stop=True),
                               [GT[g], W2v], [pc])
                        AC(lambda: nc.scalar.copy(out=RC.t[:, c, :, 0:64], in_=pc.t[:].rearrange("p (g d) -> p g d", g=2)), [pc], [RC])
            ovs = Tn(e2, "ovs", [128, NCC, NS], BF16)
            LD(ovs, ovs.t[:], self.c_ovl[:, :, :])
            for g in range(2):
                V(lambda: nc.vector.tensor_copy(out=RC.t[:, :, g, 65:65 + NS], in_=ovs.t[:]), [ovs, RC], [RC])
                V(lambda: nc.vector.memset(RC.t[:, :, g, 64:65], 1.0), [RC], [RC])
            kb.barrier()

        with ExitStack() as e3:
            def T3(name, shape, dt=F32):
                return Tn(e3, name, shape, dt)
            LA = 2
            Ew = T3("Ew", [128, S], BF16); cmk = T3("cmk", [128, 17, 128], BF16)
            Fw = T3("Fw", [128, 2 * NS - 2]); Kw = T3("Kw", [128, 2 * NS - 2])
            trib = T3("trib", [128, 128], BF16); sltb = T3("sltb", [128, 128], BF16)
            LD(Ew, Ew.t[:], self.c_Ew[:, :]); LD(cmk, cmk.t[:], self.c_cmask[:, :, :])
            LD(Fw, Fw.t[:], self.c_Fw[:, :]); LD(Kw, Kw.t[:], self.c_Kw[:, :])
            LD(trib, trib.t[:], self.c_trib[:, :]); LD(sltb, sltb.t[:], self.c_sltb[:, :])
            qz = [[T3(f"qz{p_}{g}", [128, 2, 128], BF16) for g in range(2)] for p_ in range(2)]
            pe_ = [T3(f"pe{j}", [128, 2, 128], BF16) for j in range(LA + 1)]
            rl = T3("rl", [128, 1]); wgt = T3("wgt", [128, 1])
            IMP = [T3(f"IMP{g}", [128, NS]) for g in range(2)]; impm = [T3(f"impm{g}", [128, NS]) for g in range(2)]
            wk = [T3(f"wk{g}", [128, NS]) for g in range(2)]
            m8a = [T3(f"m8a{g}", [128, 8]) for g in range(2)]; m8b = [T3(f"m8b{g}", [128, 8]) for g in range(2)]
            sneg = [T3(f"sneg{g}", [128, NS], BF16) for g in range(2)]
            SNT = [T3(f"SNT{g}", [128, 128], BF16) for g in range(2)]
            oacc = [T3(f"oacc{j}", [128, 4, 64]) for j in range(2)]
            sq = T3("sq", [128, 256]); ss = T3("ss", [128, 4]); mo = [T3(f"mo{j}", [128, 256], BF16) for j in range(2)]
            psc = [Tile(self.ps(e3, f"nsa_psc{j}", [128, 384]), f"nsa_psc{j}") for j in range(LA + 1)]
            pcv = [Tile(self.ps(e3, f"nsa_pcv{j}", [128, 65 + NS]), f"nsa_pcv{j}") for j in range(2)]
            pos_ = [Tile(self.ps(e3, f"nsa_pos{j}", [128, 65]), f"nsa_pos{j}") for j in range(2)]
            pst = Tile(self.ps(e3, "nsa_pst", [128, 128], BF16), "nsa_pst")
            for g in range(2):
                V(lambda: nc.vector.memset(SNT[g].t[:], 0.0), [], [SNT[g]])
            nsc = [0]
            pending = []

            def emit_score(job):
                j = nsc[0] % (LA + 1)
                nsc[0] += 1
                q_ = job["q"]
                extra = job.get("extra")
                sc3 = psc[j].t[:, 0:256].rearrange("p (r t) -> p r t", r=2)
                PE(lambda: nc.tensor.matmul(out=sc3, lhsT=job["lhsT"], rhs=q_.t[:], start=True, stop=True),
                   job["lt"] + [q_], [psc[j]])
                if extra is not None:
                    sn = job["snt"]
                    PE(lambda: nc.tensor.matmul(out=psc[j].t[:, 256:384], lhsT=extra, rhs=sn.t[:], start=True, stop=True), [Ew, sn], [psc[j]])
                AC(lambda: nc.scalar.activation(out=pe_[j].t[:], in_=sc3, func=AF.Exp, scale=0.125), [psc[j]], [pe_[j]])
                if extra is not None:
                    V(lambda: nc.vector.tensor_tensor(out=pe_[j].t[:], in0=pe_[j].t[:],
                                                      in1=psc[j].t[:, 256:384].unsqueeze(1).to_broadcast([128, 2, 128]), op=ALU.mult),
                      [pe_[j], psc[j]], [pe_[j]])
                if job.get("mask") is not None:
                    mt_, mk = job["mask"]
                    GP(lambda: nc.gpsimd.tensor_tensor(out=pe_[j].t[:], in0=pe_[j].t[:], in1=mk.unsqueeze(1).to_broadcast([128, 2, 128]), op=ALU.mult),
                       [pe_[j], mt_], [pe_[j]])
                job["pe"] = pe_[j]

            def emit_pv(job):
                pt = job["pe"]
                for r in range(2):
                    tgt = job["tgt"][r]
                    PE(lambda: nc.tensor.matmul(out=tgt.t[:], lhsT=pt.t[:, r, :], rhs=job["rhs"], start=job["start"], stop=job["stop"]),
                       [pt] + job["vt"], [tgt])
                if job.get("after") is not None:
                    job["after"]()

            def push(job):
                emit_score(job)
                pending.append(job)
                while len(pending) > LA:
                    emit_pv(pending.pop(0))

            def finish(pt, oa, h, b, i, first):
                V(lambda: nc.vector.tensor_scalar(out=rl.t[:], in0=pt.t[:, 64:65], scalar1=1e-30, scalar2=None, op0=ALU.max), [pt], [rl])
                V(lambda: nc.vector.reciprocal(out=rl.t[:], in_=rl.t[:]), [rl], [rl])
                V(lambda: nc.vector.tensor_tensor(out=wgt.t[:], in0=rl.t[:], in1=GA.t[:, i, 3 * h + b:3 * h + b + 1], op=ALU.mult),
                  [rl, Tile(None, f"nsa_GA{i}")], [wgt])
                if first:
                    V(lambda: nc.vector.tensor_scalar(out=oa.t[:, h, :], in0=pt.t[:, 0:64], scalar1=wgt.t[:], scalar2=None, op0=ALU.mult), [pt, wgt], [oa])
                else:
                    V(lambda: nc.vector.scalar_tensor_tensor(out=oa.t[:, h, :], in0=pt.t[:, 0:64], scalar=wgt.t[:], in1=oa.t[:, h, :],
                                                             op0=ALU.mult, op1=ALU.add), [pt, wgt, oa], [oa])

            def after_cmp(i, g, oa):
                def f():
                    for r in range(2):
                        finish(pcv[r], oa, 2 * g + r, 0, i, True)
                        if r == 0:
                            V(lambda: nc.vector.tensor_scalar(out=IMP[g].t[:], in0=pcv[r].t[:, 65:65 + NS], scalar1=rl.t[:], scalar2=None, op0=ALU.mult),
                              [pcv[r], rl], [IMP[g]])
                        else:
                            V(lambda: nc.vector.scalar_tensor_tensor(out=IMP[g].t[:], in0=pcv[r].t[:, 65:65 + NS], scalar=rl.t[:], in1=IMP[g].t[:],
                                                                     op0=ALU.mult, op1=ALU.add), [pcv[r], rl, IMP[g]], [IMP[g]])
                    fo = NS - 2 - 2 * i
                    V(lambda: nc.vector.tensor_tensor(out=impm[g].t[:], in0=IMP[g].t[:], in1=Kw.t[:, fo:fo + NS], op=ALU.mult), [IMP[g], Kw], [impm[g]])
                    V(lambda: nc.vector.tensor_tensor(out=impm[g].t[:], in0=impm[g].t[:], in1=Fw.t[:, fo:fo + NS], op=ALU.add), [impm[g], Fw], [impm[g]])
                    V(lambda: nc.vector.memset(impm[g].t[:, 0:1], 1e30), [impm[g]], [impm[g]])
                    V(lambda: nc.vector.max(out=m8a[g].t[:], in_=impm[g].t[:]), [impm[g]], [m8a[g]])
                    V(lambda: nc.vector.match_replace(out=wk[g].t[:], in_to_replace=m8a[g].t[:], in_values=impm[g].t[:], imm_value=-3e38),
                      [m8a[g], impm[g]], [wk[g]])
                    V(lambda: nc.vector.max(out=m8b[g].t[:], in_=wk[g].t[:]), [wk[g]], [m8b[g]])
                    V(lambda: nc.vector.tensor_scalar(out=sneg[g].t[:], in0=impm[g].t[:], scalar1=m8b[g].t[:, 7:8], scalar2=None, op0=ALU.is_ge),
                      [impm[g], m8b[g]], [sneg[g]])
                return f

            def after_fin(tgt, i, g, b, oa):
                def f():
                    for r in range(2):
                        finish(tgt[r], oa, 2 * g + r, b, i, False)
                return f

            def tail(i, oa):
                def f():
                    s = i % 2
                    t0 = i * 128
                    o2 = oa.t[:].rearrange("p h d -> p (h d)")
                    AC(lambda: nc.scalar.activation(out=sq.t[:], in_=o2, func=AF.Square), [oa], [sq])
                    V(lambda: nc.vector.tensor_reduce(out=ss.t[:], in_=sq.t[:].rearrange("p (h d) -> p h d", h=4), axis=AX.X, op=ALU.add), [sq], [ss])
                    V(lambda: nc.vector.tensor_scalar(out=ss.t[:], in0=ss.t[:], scalar1=1.0 / 64, scalar2=EPS, op0=ALU.mult, op1=ALU.add), [ss], [ss])
                    AC(lambda: nc.scalar.activation(out=ss.t[:], in_=ss.t[:], func=AF.Sqrt), [ss], [ss])
                    V(lambda: nc.vector.reciprocal(out=ss.t[:], in_=ss.t[:]), [ss], [ss])
                    V(lambda: nc.vector.tensor_tensor(out=sq.t[:].rearrange("p (h d) -> p h d", h=4), in0=oa.t[:],
                                                      in1=ss.t[:].unsqueeze(2).to_broadcast([128, 4, 64]), op=ALU.mult), [oa, ss, sq], [sq])
                    V(lambda: nc.vector.tensor_tensor(out=mo[s].t[:], in0=sq.t[:], in1=nwb.t[:], op=ALU.mult), [sq, nwb], [mo[s]])
                    kb.dma("sp", mo[s].k, self.mix[t0:t0 + 128, 256:512], mo[s].t[:], reads=[mo[s].k], writes=[("mix_nsa", i)])
                return f

            for i in range(NT):
                qq = qz[i % 2]
                oa = oacc[i % 2]
                for g in range(2):
                    AC(lambda: nc.scalar.copy(out=qq[g].t[:], in_=QT.t[:, i, :, :]), [Tile(None, f"nsa_QT{i}")], [qq[g]])
                    o0 = (1 - g) * 64
                    GP(lambda: nc.gpsimd.memset(qq[g].t[o0:o0 + 64, :, :], 0.0), [qq[g]], [qq[g]])
                ncs = min(NCC, i // 16 + 1)
                k0 = max(0, i - 4)
                for g in range(2):
                    for c in range(ncs):
                        dl = i - 16 * c
                        push(dict(lhsT=KCT.t[:, c * 128:(c + 1) * 128], lt=[KCT], q=qq[g],
                                  mask=(cmk, cmk.t[:, dl, :]) if dl <= 16 else None,
                                  tgt=pcv, rhs=RC.t[:, c, g, :], vt=[RC], start=(c == 0), stop=(c == ncs - 1),
                                  after=after_cmp(i, g, oa) if c == ncs - 1 else None))
                    for kt in range(k0, i + 1):
                        mf = (trib, trib.t[:]) if kt == i else ((sltb, sltb.t[:]) if kt == i - 4 else None)
                        push(dict(lhsT=KwT.t[:, kt * 128:(kt + 1) * 128], lt=[Tile(None, f"nsa_KwT{kt}")], q=qq[g], mask=mf,
                                  tgt=[Tile(pcv[0].t[:, 0:65], pcv[0].k), Tile(pcv[1].t[:, 0:65], pcv[1].k)],
                                  rhs=VwA.t[:, kt, g, :], vt=[Tile(None, f"nsa_VwA{kt}")], start=(kt == k0), stop=(kt == i),
                                  after=after_fin(pcv, i, g, 2, oa) if kt == i else None))
                for g in range(2):
                    PE(lambda: nc.tensor.transpose(out=pst.t[0:NS, :], in_=sneg[g].t[:], identity=idb.t[:]), [sneg[g], idb], [pst])
                    AC(lambda: nc.scalar.copy(out=SNT[g].t[0:NS, :], in_=pst.t[0:NS, :]), [pst], [SNT[g]])
                    for kt in range(i + 1):
                        last = (kt == i)
                        aft = None
                        if last:
                            fin = after_fin(pos_, i, g, 1, oa)
                            if g == 1:
                                tl_ = tail(i, oa)
                                aft = (lambda fin=fin, tl_=tl_: (fin(), tl_()))
                            else:
                                aft = fin
                        push(dict(lhsT=KsT.t[:, kt * 128:(kt + 1) * 128], lt=[Tile(None, f"nsa_KsT{kt}")], q=qq[g],
                                  mask=(trib, trib.t[:]) if last else None, extra=Ew.t[:, kt * 128:(kt + 1) * 128], snt=SNT[g],
                                  tgt=pos_, rhs=VsA.t[:, kt, g, :], vt=[Tile(None, f"nsa_VsA{kt}")], start=(kt == 0), stop=last, after=aft))
            while pending:
                emit_pv(pending.pop(0))


Prog.stage_nsa = _stage_nsa


def build_full(S, L=2):
    p = Prog(S, L=L, dbg=[("out", [S, D])])
    kb = p.kb
    with kb.es:
        h = p.x
        for l in range(L):
            p.stage_d1(l, h)
            kb.barrier()
            p.stage_ssd(l)
            kb.barrier()
            p.stage_gla(l)
            kb.barrier()
            p.stage_nsa(l)
            kb.barrier()
            final = (l == L - 1)
            p.stage_d2(l, h, p.outs["out"] if final else p.hbuf, final)
            kb.barrier()
            h = p.hbuf
        kb.drain("sp")
    return p


_WNAMES = ["norm1_w", "w_in", "gla_gate_w2", "gla_gate_b", "gla_norm_w", "nsa_cmp_pos_k", "nsa_cmp_w1_k", "nsa_cmp_w2_k",
           "nsa_cmp_pos_v", "nsa_cmp_w1_v", "nsa_cmp_w2_v", "nsa_norm_w", "ssd_conv_w", "ssd_conv_b", "ssd_dt_bias",
           "ssd_a_log", "ssd_d", "ssd_norm_w", "w_out", "norm2_w", "w_up", "w_down", "final_norm_w"]


def kernel(**inputs):
    x = np.asarray(inputs["x"], dtype=np.float32)
    B, S, _ = x.shape
    L = int(np.asarray(inputs["w_in"]).shape[0])
    p = build_full(S, L)
    base = {k: np.ascontiguousarray(np.asarray(inputs[k], dtype=np.float32)) for k in _WNAMES}
    base.update(consts(S))
    in_maps = []
    for b in range(B):
        m = dict(base)
        m["x"] = np.ascontiguousarray(x[b])
        in_maps.append(m)
    res = run_bass_kernel_spmd(p.nc, in_maps, core_ids=list(range(B)))
    return np.stack([np.asarray(res.results[b]["out"], dtype=np.float32) for b in range(B)], axis=0)
```

```python
import numpy as np
import ml_dtypes
import concourse.bass as bass
import concourse.mybir as mybir
from concourse.bass_utils import run_bass_kernel_spmd
from contextlib import ExitStack
import threading

F32 = mybir.dt.float32
BF16 = mybir.dt.bfloat16
AF = mybir.ActivationFunctionType
ALU = mybir.AluOpType
AX = mybir.AxisListType

D = 1024
DIN = 3364
DFF = 4096
NPROJ = 2340
EPS = 1e-6

C_GQ, C_GK, C_GV, C_GLR, C_GR = 0, 128, 256, 512, 528
C_NQ, C_KCMP, C_KSLC, C_KWIN, C_VCMP, C_VSLC, C_VWIN, C_NG = 784, 1040, 1168, 1296, 1424, 1552, 1680, 1808
C_Z, C_DT = 1820, 2332
WMAP = [(0, 784, 0), (784, 64, 784), (912, 64, 848), (848, 64, 912), (976, 64, 976), (1040, 128, 1040), (1296, 128, 1168), (1552, 128, 1296), (1168, 128, 1424),
        (1424, 128, 1552), (1680, 652, 1680), (3356, 8, 2332), (2332, 1024, -1)]


class KB:
    EPOCH = 30000

    def __init__(self, nc, same_engine_sync=True):
        self.nc = nc
        self.es = ExitStack()
        self.eng = {"pe": nc.tensor, "act": nc.scalar, "dve": nc.vector,
                    "pool": nc.gpsimd, "sp": nc.sync}
        self.esem = {}
        self.ecnt = {}
        self.nsem = 0
        self.dsem = {}
        self.waited = {e: {} for e in self.eng}
        self.last_w = {}
        self.readers = {}
        self.same = same_engine_sync
        self.sem_owner = {}
        self.ninst = 0
        self.limit = None
        self.hook = None
        for e in self.eng:
            self._new_esem(e)

    def _sem(self, name):
        self.nsem += 1
        return self.es.enter_context(self.nc.semaphore(f"s{self.nsem}_{name}"))

    def _new_esem(self, e):
        s = self._sem(e)
        self.esem[e] = s
        self.ecnt[e] = 0
        self.sem_owner[id(s)] = e

    def _wait(self, e, deps):
        best = {}
        for item in deps:
            if item is None:
                continue
            if len(item) == 3:
                s, v, raw = item
            else:
                (s, v), raw = item, True
            owner = self.sem_owner.get(id(s))
            if owner == e and (e == "pe" or not self.same):
                continue
            if best.get(id(s), (None, 0))[1] < v:
                best[id(s)] = (s, v)
        for sid, (s, v) in best.items():
            if self.waited[e].get(sid, 0) < v:
                self.eng[e].wait_ge(s, v)
                self.waited[e][sid] = v

    def _deps(self, reads, writes):
        deps = []
        for k in reads:
            ev = self.last_w.get(k)
            if ev is not None:
                deps.append((ev[0], ev[1], True))
        for k in writes:
            ev = self.last_w.get(k)
            if ev is not None:
                deps.append((ev[0], ev[1], False))
            for ev in self.readers.get(k, []):
                deps.append((ev[0], ev[1], False))
        return deps

    def _commit(self, ev, reads, writes):
        for k in writes:
            self.last_w[k] = ev
            self.readers[k] = []
        for k in reads:
            if k in writes:
                continue
            self.readers.setdefault(k, []).append(ev)

    def op(self, e, fn, reads=(), writes=()):
        if self.hook is not None:
            self.hook()
        if self.limit is not None and self.ninst >= self.limit:
            return None
        self._wait(e, self._deps(reads, writes))
        inst = fn()
        if self.ecnt[e] >= self.EPOCH:
            self._new_esem(e)
        self.ecnt[e] += 1
        s = self.esem[e]
        inst.then_inc(s, 1)
        self._commit((s, self.ecnt[e]), reads, writes)
        self.ninst += 1
        return inst

    def dma(self, q, key, out, in_, reads=(), writes=(), **kw):
        if self.hook is not None:
            self.hook()
        if self.limit is not None and self.ninst >= self.limit:
            return None
        self._wait(q, self._deps(reads, writes))
        if key not in self.dsem:
            self.dsem[key] = [self._sem("d"), 0]
        ent = self.dsem[key]
        inst = self.eng[q].dma_start(out=out, in_=in_, **kw)
        ent[1] += 16
        inst.then_inc(ent[0], 16)
        self._commit((ent[0], ent[1]), reads, writes)
        self.ninst += 1
        return inst

    def barrier(self):
        deps = list(self.last_w.values())
        for r in self.readers.values():
            deps.extend(r)
        for e in self.eng:
            self._wait(e, deps)
        self.readers = {k: [] for k in self.readers}

    def drain(self, e="sp"):
        deps = list(self.last_w.values())
        for r in self.readers.values():
            deps.extend(r)
        self._wait(e, deps)


def interleave(kb, fa, fb):
    sem = {"a": threading.Semaphore(0), "b": threading.Semaphore(0)}
    done = {"a": False, "b": False}
    err = []
    loc = threading.local()

    def hook():
        me = loc.me
        other = "b" if me == "a" else "a"
        if done[other]:
            return
        sem[other].release()
        sem[me].acquire()

    def run(me, f):
        loc.me = me
        other = "b" if me == "a" else "a"
        sem[me].acquire()
        try:
            f()
        except BaseException as ex:
            err.append(ex)
        done[me] = True
        sem[other].release()

    kb.hook = hook
    ta = threading.Thread(target=run, args=("a", fa))
    tb = threading.Thread(target=run, args=("b", fb))
    ta.start()
    tb.start()
    sem["a"].release()
    ta.join()
    tb.join()
    kb.hook = None
    if err:
        raise err[0]


class Prog:
    def __init__(self, S, L=2, dbg=()):
        self.S = S
        self.NT = S // 128
        self.L = L
        self.dbg = dbg
        nc = self.nc = bass.Bass("TRN2", target_bir_lowering=False)
        self.kb = KB(nc)
        Ld = L

        def inp(name, shape, dt=F32):
            return nc.dram_tensor(name, list(shape), dt, kind="ExternalInput").ap()

        def scr(name, shape, dt=F32):
            return nc.dram_tensor(name, list(shape), dt, kind="Internal").ap()

        self.x = inp("x", [S, D])
        self.norm1_w = inp("norm1_w", [Ld, D])
        self.w_in = inp("w_in", [Ld, D, DIN])
        self.gla_gate_w2 = inp("gla_gate_w2", [Ld, 16, 128])
        self.gla_gate_b = inp("gla_gate_b", [Ld, 128])
        self.gla_norm_w = inp("gla_norm_w", [Ld, 256])
        self.nsa_cmp_pos_k = inp("nsa_cmp_pos_k", [Ld, 32, 64])
        self.nsa_cmp_w1_k = inp("nsa_cmp_w1_k", [Ld, 2048, 128])
        self.nsa_cmp_w2_k = inp("nsa_cmp_w2_k", [Ld, 128, 64])
        self.nsa_cmp_pos_v = inp("nsa_cmp_pos_v", [Ld, 32, 64])
        self.nsa_cmp_w1_v = inp("nsa_cmp_w1_v", [Ld, 2048, 128])
        self.nsa_cmp_w2_v = inp("nsa_cmp_w2_v", [Ld, 128, 64])
        self.nsa_norm_w = inp("nsa_norm_w", [Ld, 256])
        self.ssd_conv_w = inp("ssd_conv_w", [Ld, 4, 1024])
        self.ssd_conv_b = inp("ssd_conv_b", [Ld, 1024])
        self.ssd_dt_bias = inp("ssd_dt_bias", [Ld, 8])
        self.ssd_a_log = inp("ssd_a_log", [Ld, 8])
        self.ssd_d = inp("ssd_d", [Ld, 8])
        self.ssd_norm_w = inp("ssd_norm_w", [Ld, 512])
        self.w_out = inp("w_out", [Ld, D, D])
        self.norm2_w = inp("norm2_w", [Ld, D])
        self.w_up = inp("w_up", [Ld, D, DFF])
        self.w_down = inp("w_down", [Ld, DFF, D])
        self.final_norm_w = inp("final_norm_w", [D])
        self.ident_f = inp("ident_f", [128, 128])
        self.ident_b = inp("ident_b", [128, 128], BF16)
        self.c_tri = inp("c_tri", [128, 128])
        self.c_slt = inp("c_slt", [128, 128])
        self.c_ones = inp("c_ones", [128, 128])
        self.c_hm = inp("c_hm", [128, 4])
        self.c_bd = inp("c_bd", [128, 256])
        self.mix = scr("mix", [S, D], BF16)
        NS = S // 64
        self.NS = NS
        self.NCC = max(1, (S // 16 - 1 + 127) // 128)
        self.rope_cos = inp("rope_cos", [S, 32])
        self.rope_sin = inp("rope_sin", [S, 32])
        self.c_Ew = inp("c_Ew", [128, S], BF16)
        self.c_cmask = inp("c_cmask", [128, 17, 128], BF16)
        self.c_ovl = inp("c_ovl", [128, self.NCC, NS], BF16)
        self.c_Fw = inp("c_Fw", [128, 2 * NS - 2])
        self.c_Kw = inp("c_Kw", [128, 2 * NS - 2])
        self.c_trib = inp("c_trib", [128, 128], BF16)
        self.c_sltb = inp("c_sltb", [128, 128], BF16)
        self.kcT_d = scr("kcT_d", [128, S + 128], BF16)
        self.vcT_d = scr("vcT_d", [128, S + 128], BF16)
        self.proj = scr("proj", [S, NPROJ])
        self.xbcT = scr("xbcT", [D, S])
        self.hbuf = scr("hbuf", [S, D])
        self.outs = {}
        for name, shape in dbg:
            self.outs[name] = nc.dram_tensor(name, list(shape), F32, kind="ExternalOutput").ap()

    def sb(self, es, name, shape, dt=F32):
        self._uid = getattr(self, "_uid", 0) + 1
        return es.enter_context(self.nc.sbuf_tensor(f"{name}_u{self._uid}", list(shape), dt))

    def ps(self, es, name, shape, dt=F32):
        full = 512 if dt == F32 else 1024
        self._uid = getattr(self, "_uid", 0) + 1
        t = es.enter_context(self.nc.psum_tensor(f"{name}_u{self._uid}", [128, full], dt))
        n = 1
        for d in shape[1:]:
            n *= d
        assert n <= full
        v = t[0:shape[0], 0:n]
        if len(shape) == 3:
            v = v.rearrange("p (a b) -> p a b", a=shape[1])
        return v

    def stage_d1(self, l, hsrc):
        nc, kb, NT = self.nc, self.kb, self.NT
        with ExitStack() as es:
            Wtm = self.sb(es, "d1_Wtm", [128, 8, NPROJ], BF16)
            Wx = self.sb(es, "d1_Wx", [128, 8, 1024], BF16)
            stg = [self.sb(es, f"d1_stg{i}", [128, DIN]) for i in range(2)]
            nw = self.sb(es, "d1_nw", [128, 8])
            idb = self.sb(es, "d1_idb", [128, 128], BF16)
            ht = [self.sb(es, f"d1_ht{i}", [128, D]) for i in range(2)]
            junk = self.sb(es, "d1_junk", [128, D], BF16)
            ss = [self.sb(es, f"d1_ss{i}", [128, 1]) for i in range(2)]
            rs = [self.sb(es, f"d1_rs{i}", [128, 1]) for i in range(2)]
            u = [self.sb(es, f"d1_u{i}", [128, D], BF16) for i in range(2)]
            uT = [self.sb(es, f"d1_uT{i}", [128, 8, 128], BF16) for i in range(2)]
            ot = [self.sb(es, f"d1_ot{i}", [128, NPROJ]) for i in range(2)]
            xo = [self.sb(es, f"d1_xo{i}", [128, 8, 128]) for i in range(2)]
            pT = self.ps(es, "d1_pT", [128, 8, 128], BF16)
            pm = [self.ps(es, f"d1_pm{i}", [128, 512]) for i in range(3)]
            px = [self.ps(es, f"d1_px{i}", [128, 4, 128]) for i in range(2)]

            kb.dma("sp", "d1_idb", idb[:], self.ident_b[:, :], writes=["d1_idb"])
            for c in range(8):
                kb.dma("sp", "d1_nw", nw[:, c:c + 1],
                       self.norm1_w[l, c * 128:(c + 1) * 128].rearrange("(p o) -> p o", o=1),
                       writes=["d1_nw"])
            for c in range(8):
                s = c % 2
                kb.dma("sp", f"d1_stg{s}", stg[s][:], self.w_in[l, c * 128:(c + 1) * 128, :],
                       writes=[f"d1_stg{s}"])
                for j, (so, n, do) in enumerate(WMAP):
                    dst = Wx[:, c, 0:n] if do < 0 else Wtm[:, c, do:do + n]
                    wk = f"d1_W{c}"
                    if j % 2 == 0:
                        kb.op("dve", lambda: nc.vector.tensor_scalar(
                            out=dst, in0=stg[s][:, so:so + n], scalar1=nw[:, c:c + 1], scalar2=None,
                            op0=ALU.mult), reads=[f"d1_stg{s}", "d1_nw"], writes=[wk + f"_{j}"])
                    else:
                        kb.op("act", lambda: nc.scalar.activation(
                            out=dst, in_=stg[s][:, so:so + n], func=AF.Copy, scale=nw[:, c:c + 1]),
                            reads=[f"d1_stg{s}", "d1_nw"], writes=[wk + f"_{j}"])
            Wkeys = [f"d1_W{c}_{j}" for c in range(8) for j in range(len(WMAP))]

            def A(i):
                s = i % 2
                kb.dma("sp", f"d1_ht{s}", ht[s][:], hsrc[i * 128:(i + 1) * 128, :],
                       reads=[("h", i)], writes=[f"d1_ht{s}"])
                kb.op("act", lambda: nc.scalar.activation(out=junk[:], in_=ht[s][:], func=AF.Square,
                                                          accum_out=ss[s][:]),
                      reads=[f"d1_ht{s}"], writes=["d1_junk", f"d1_ss{s}"])
                kb.op("dve", lambda: nc.vector.tensor_scalar(out=rs[s][:], in0=ss[s][:], scalar1=1.0 / D,
                                                             scalar2=EPS, op0=ALU.mult, op1=ALU.add),
                      reads=[f"d1_ss{s}"], writes=[f"d1_rs{s}"])
                kb.op("act", lambda: nc.scalar.activation(out=rs[s][:], in_=rs[s][:], func=AF.Sqrt),
                      reads=[f"d1_rs{s}"], writes=[f"d1_rs{s}"])
                kb.op("dve", lambda: nc.vector.reciprocal(out=rs[s][:], in_=rs[s][:]),
                      reads=[f"d1_rs{s}"], writes=[f"d1_rs{s}"])
                kb.op("dve", lambda: nc.vector.tensor_scalar(out=u[s][:], in0=ht[s][:], scalar1=rs[s][:],
                                                             scalar2=None, op0=ALU.mult),
                      reads=[f"d1_ht{s}", f"d1_rs{s}"], writes=[f"d1_u{s}"])
                for c in range(8):
                    kb.op("pe", lambda: nc.tensor.transpose(out=pT[:, c, :], in_=u[s][:, c * 128:(c + 1) * 128],
                                                            identity=idb[:]),
                          reads=[f"d1_u{s}", "d1_idb"], writes=["d1_pT"])
                kb.op("act", lambda: nc.scalar.copy(out=uT[s][:], in_=pT[:]),
                      reads=["d1_pT"], writes=[f"d1_uT{s}"])

            def B(i):
                s = i % 2
                off = 0
                k = 0
                while off < NPROJ:
                    n = min(512, NPROJ - off)
                    p = pm[k % 3]
                    pk = f"d1_pm{k % 3}"
                    for c in range(8):
                        kb.op("pe", lambda: nc.tensor.matmul(out=p[:, 0:n], lhsT=uT[s][:, c, :],
                                                             rhs=Wtm[:, c, off:off + n],
                                                             start=(c == 0), stop=(c == 7)),
                              reads=[f"d1_uT{s}"] + (Wkeys if i == 0 and c == 0 and k == 0 else []),
                              writes=[pk])
                    if k % 2 == 0:
                        kb.op("dve", lambda: nc.vector.tensor_copy(out=ot[s][:, off:off + n], in_=p[:, 0:n]),
                              reads=[pk], writes=[f"d1_ot{s}_{k}"])
                    else:
                        kb.op("act", lambda: nc.scalar.copy(out=ot[s][:, off:off + n], in_=p[:, 0:n]),
                              reads=[pk], writes=[f"d1_ot{s}_{k}"])
                    off += n
                    k += 1
                kb.dma("sp", f"d1_ot{s}", self.proj[i * 128:(i + 1) * 128, :], ot[s][:],
                       reads=[f"d1_ot{s}_{j}" for j in range(k)], writes=[("proj", i)])
                for half in range(2):
                    p = px[half]
                    pk = f"d1_px{half}"
                    for j in range(4):
                        cc = half * 4 + j
                        for c in range(8):
                            kb.op("pe", lambda: nc.tensor.matmul(out=p[:, j, :], lhsT=Wx[:, c, cc * 128:(cc + 1) * 128],
                                                                 rhs=uT[s][:, c, :],
                                                                 start=(c == 0), stop=(c == 7)),
                                  reads=[f"d1_uT{s}"], writes=[pk])
                    if half == 0:
                        kb.op("dve", lambda: nc.vector.tensor_copy(out=xo[s][:, 0:4, :], in_=p[:]),
                              reads=[pk], writes=[f"d1_xo{s}_0"])
                    else:
                        kb.op("act", lambda: nc.scalar.copy(out=xo[s][:, 4:8, :], in_=p[:]),
                              reads=[pk], writes=[f"d1_xo{s}_1"])
                kb.dma("sp", f"d1_xo{s}",
                       self.xbcT.rearrange("(cc p) t -> p cc t", p=128)[:, :, i * 128:(i + 1) * 128],
                       xo[s][:], reads=[f"d1_xo{s}_0", f"d1_xo{s}_1"], writes=[("xbcT", i)])

            A(0)
            for i in range(NT):
                if i + 1 < NT:
                    interleave(kb, (lambda i=i: B(i)), (lambda i=i: A(i + 1)))
                else:
                    B(i)

    def dump(self, name, src_ap):
        kb = self.kb
        kb.drain("sp")
        kb.dma("sp", "dump_" + name, self.outs[name], src_ap, writes=[("dump", name)])

    def finish(self):
        self.kb.drain("sp")
        self.kb.es.close()


def build_test_d1(S):
    p = Prog(S, L=1, dbg=[("o_proj", [S, NPROJ]), ("o_xbcT", [D, S])])
    with p.kb.es:
        p.stage_d1(0, p.x)
        p.dump("o_proj", p.proj[:, :])
        p.dump("o_xbcT", p.xbcT[:, :])
        p.kb.drain("sp")
    return p


def consts(S):
    r = np.arange(128)
    NS = S // 64
    NCC = max(1, (S // 16 - 1 + 127) // 128)
    pos = np.arange(S, dtype=np.float32)
    inv = (np.float32(10000.0) ** (-np.arange(32, dtype=np.float32) / np.float32(32))).astype(np.float32)
    ang = (pos[:, None] * inv[None, :]).astype(np.float32)
    Ew = np.zeros((128, S), np.float32)
    cc = np.arange(S)
    Ew[cc // 64 % 128, cc] = 30000.0
    cm = np.zeros((128, 17, 128), np.float32)
    for d_ in range(17):
        cm[:, d_, :] = (128 * d_ + r[None, :] - 16 * r[:, None] >= 31)
    n_all = np.arange(NCC * 128)
    j_all = np.arange(NS)
    ov = ((16 * n_all[:, None] < 64 * j_all[None, :] + 64) & (16 * n_all[:, None] + 32 > 64 * j_all[None, :])).astype(np.float32)
    ov = ov.reshape(NCC, 128, NS).transpose(1, 0, 2)
    cw = np.arange(2 * NS - 2)
    rel = cw[None, :] - (NS - 2) - (r[:, None] >= 64)
    Fw = np.where(rel > 0, -1e30, np.where(rel >= -1, 1e30, 0.0)).astype(np.float32)
    Kw = ((rel < -1)).astype(np.float32)
    bf = ml_dtypes.bfloat16
    hm = np.zeros((128, 4), np.float32)
    bd = np.zeros((128, 256), np.float32)
    for h in range(4):
        hm[32 * h:32 * h + 32, h] = 1.0
        bd[32 * h:32 * h + 32, 64 * h:64 * h + 64] = 1.0
    return {
        "ident_f": np.eye(128, dtype=np.float32),
        "ident_b": np.eye(128, dtype=np.float32).astype(ml_dtypes.bfloat16),
        "c_tri": (r[:, None] <= r[None, :]).astype(np.float32),
        "c_slt": (r[:, None] > r[None, :]).astype(np.float32),
        "c_ones": np.ones((128, 128), np.float32),
        "c_hm": hm,
        "c_bd": bd,
        "rope_cos": np.cos(ang).astype(np.float32),
        "rope_sin": np.sin(ang).astype(np.float32),
        "c_Ew": Ew.astype(bf),
        "c_cmask": cm.astype(bf),
        "c_ovl": np.ascontiguousarray(ov).astype(bf),
        "c_Fw": Fw,
        "c_Kw": Kw,
        "c_trib": (r[:, None] <= r[None, :]).astype(np.float32).astype(bf),
        "c_sltb": (r[:, None] > r[None, :]).astype(np.float32).astype(bf),
    }


class Tile:
    def __init__(self, t, k):
        self.t = t
        self.k = k


def _stage_ssd(self, l):
    nc, kb, NT = self.nc, self.kb, self.NT
    with ExitStack() as es:
        def T(name, shape, dt=F32):
            return Tile(self.sb(es, "ssd_" + name, shape, dt), "ssd_" + name)

        def T2(name, shape, dt=F32):
            return [T(f"{name}{j}", shape, dt) for j in range(2)]

        def PT(name, shape, dt=F32):
            return Tile(self.ps(es, "ssd_" + name, shape, dt), "ssd_" + name)

        def V(fn, r, w):
            return kb.op("dve", fn, [t.k for t in r], [t.k for t in w])

        def AC(fn, r, w):
            return kb.op("act", fn, [t.k for t in r], [t.k for t in w])

        def PE(fn, r, w):
            return kb.op("pe", fn, [t.k for t in r], [t.k for t in w])

        def GP(fn, r, w):
            return kb.op("pool", fn, [t.k for t in r], [t.k for t in w])

        def LD(t, dst, src, reads=(), **kw):
            return kb.dma("sp", t.k, dst, src, reads=list(reads), writes=[t.k], **kw)

        cw = T("cw", [128, 4, 8]); cb = T("cb", [128, 8]); dtb = T("dtb", [128, 8])
        Aneg = T("Aneg", [128, 8]); dsk = T("dsk", [128, 8]); nwb = T("nwb", [128, 512])
        tri = T("tri", [128, 128]); slt = T("slt", [128, 128]); ones = T("ones", [128, 128])
        idf = T("idf", [128, 128]); idb = T("idb", [128, 128], BF16)
        hT = T("hT", [128, 512]); hTb = T("hTb", [128, 512], BF16)
        xh = T2("xh", [128, 8, 131]); zt = T2("zt", [128, 512]); dtt = T2("dtt", [128, 8])
        acc = T2("acc", [128, 8, 128])
        xsT = T2("xsT", [128, 4, 128])
        BT = T2("BT", [128, 2, 128], BF16); CT = T2("CT", [128, 2, 128], BF16)
        xtm = T2("xtm", [128, 512]); Btm = T2("Btm", [128, 2, 128], BF16)
        sp_a = T2("sp_a", [128, 8]); dts = T2("dts", [128, 8]); dA = T2("dA", [128, 8])
        acs = T2("acs", [128, 16]); eacs = T2("eacs", [128, 8]); dsd = T2("dsd", [128, 8]); cd = T2("cd", [128, 8])
        xdt = T2("xdt", [128, 512], BF16); xdd = T2("xdd", [128, 512], BF16)
        cbm = T2("cbm", [128, 2, 128])
        dAt = T("dAt", [128, 8, 128]); seg = T2("seg", [128, 4, 128]); MT = T2("MT", [128, 4, 128], BF16)
        yd = T2("yd", [128, 512]); y = T2("y", [128, 512]); sz = T2("sz", [128, 512])
        junk = T("junk", [128, 256]); ss = T2("ss", [128, 2]); mo = T2("mo", [128, 512], BF16)
        pxT = PT("pxT", [128, 512]); pBt = PT("pBt", [128, 2, 128], BF16); pacs = PT("pacs", [128, 16])
        pcb = PT("pcb", [128, 2, 128]); pD = PT("pD", [128, 4, 128]); py = PT("py", [128, 512])
        pyo = PT("pyo", [128, 512]); pU = PT("pU", [128, 512])

        for k in range(4):
            LD(cw, cw.t[:, k, :], self.ssd_conv_w[l, k, :].rearrange("(cc p) -> p cc", p=128),
               allow_slow_non_contiguous=True)
        LD(cb, cb.t[:], self.ssd_conv_b[l, :].rearrange("(cc p) -> p cc", p=128), allow_slow_non_contiguous=True)
        LD(dtb, dtb.t[:], self.ssd_dt_bias[l, :].partition_broadcast(128))
        LD(Aneg, Aneg.t[:], self.ssd_a_log[l, :].partition_broadcast(128))
        LD(dsk, dsk.t[:], self.ssd_d[l, :].partition_broadcast(128))
        LD(nwb, nwb.t[:], self.ssd_norm_w[l, :].partition_broadcast(128))
        LD(tri, tri.t[:], self.c_tri[:, :]); LD(slt, slt.t[:], self.c_slt[:, :]); LD(ones, ones.t[:], self.c_ones[:, :])
        LD(idf, idf.t[:], self.ident_f[:, :]); LD(idb, idb.t[:], self.ident_b[:, :])
        AC(lambda: nc.scalar.activation(out=Aneg.t[:], in_=Aneg.t[:], func=AF.Exp), [Aneg], [Aneg])
        V(lambda: nc.vector.tensor_scalar(out=Aneg.t[:], in0=Aneg.t[:], scalar1=-1.0, scalar2=None, op0=ALU.mult), [Aneg], [Aneg])
        V(lambda: nc.vector.memset(hT.t[:], 0.0), [], [hT])
        V(lambda: nc.vector.memset(hTb.t[:], 0.0), [], [hTb])
        for j in range(2):
            V(lambda: nc.vector.memset(xh[j].t[:], 0.0), [], [xh[j]])
        xv = self.xbcT.rearrange("(cc p) t -> p cc t", p=128)

        def front(i):
            s = i % 2
            t0 = i * 128
            if i == 0:
                kb.dma("sp", xh[s].k, xh[s].t[:, :, 3:131], xv[:, :, 0:128], reads=[("xbcT", 0)], writes=[xh[s].k])
            else:
                kb.dma("sp", xh[s].k, xh[s].t[:, :, 0:131], xv[:, :, t0 - 3:t0 + 128],
                       reads=[("xbcT", i), ("xbcT", i - 1)], writes=[xh[s].k])
            kb.dma("sp", zt[s].k, zt[s].t[:], self.proj[t0:t0 + 128, C_Z:C_Z + 512], reads=[("proj", i)], writes=[zt[s].k])
            kb.dma("sp", dtt[s].k, dtt[s].t[:], self.proj[t0:t0 + 128, C_DT:C_DT + 8], reads=[("proj", i)], writes=[dtt[s].k])
            for cc in range(8):
                eng = V
                ne = nc.vector
                GP(lambda: nc.gpsimd.tensor_scalar(out=acc[s].t[:, cc, :], in0=xh[s].t[:, cc, 0:128], scalar1=cw.t[:, 0, cc:cc + 1],
                                                   scalar2=cb.t[:, cc:cc + 1], op0=ALU.mult, op1=ALU.add),
                   [xh[s], cw, cb], [Tile(None, acc[s].k + f"_{cc}")])
                for k in range(1, 4):
                    eng(lambda: ne.scalar_tensor_tensor(out=acc[s].t[:, cc, :], in0=xh[s].t[:, cc, k:k + 128],
                                                        scalar=cw.t[:, k, cc:cc + 1], in1=acc[s].t[:, cc, :],
                                                        op0=ALU.mult, op1=ALU.add),
                        [xh[s], cw, Tile(None, acc[s].k + f"_{cc}")], [Tile(None, acc[s].k + f"_{cc}")])
            AC(lambda: nc.scalar.activation(out=xsT[s].t[:], in_=acc[s].t[:, 0:4, :], func=AF.Silu), [Tile(None, acc[s].k + f"_{c_}") for c_ in range(0, 4)], [xsT[s]])
            AC(lambda: nc.scalar.activation(out=BT[s].t[:], in_=acc[s].t[:, 4:6, :], func=AF.Silu), [Tile(None, acc[s].k + f"_{c_}") for c_ in range(4, 6)], [BT[s]])
            AC(lambda: nc.scalar.activation(out=CT[s].t[:], in_=acc[s].t[:, 6:8, :], func=AF.Silu), [Tile(None, acc[s].k + f"_{c_}") for c_ in range(6, 8)], [CT[s]])
            for cc in range(4):
                PE(lambda: nc.tensor.transpose(out=pxT.t[:, cc * 128:(cc + 1) * 128], in_=xsT[s].t[:, cc, :], identity=idf.t[:]),
                   [xsT[s], idf], [pxT])
            V(lambda: nc.vector.tensor_copy(out=xtm[s].t[:], in_=pxT.t[:]), [pxT], [xtm[s]])
            for g in range(2):
                PE(lambda: nc.tensor.transpose(out=pBt.t[:, g, :], in_=BT[s].t[:, g, :], identity=idb.t[:]), [BT[s], idb], [pBt])
            AC(lambda: nc.scalar.copy(out=Btm[s].t[:], in_=pBt.t[:]), [pBt], [Btm[s]])
            V(lambda: nc.vector.tensor_tensor(out=dts[s].t[:], in0=dtt[s].t[:], in1=dtb.t[:], op=ALU.add), [dtt[s], dtb], [dts[s]])
            V(lambda: nc.vector.scalar_tensor_tensor(out=sp_a[s].t[:], in0=dts[s].t[:], scalar=-1.0, in1=dts[s].t[:],
                                                     op0=ALU.mult, op1=ALU.max), [dts[s]], [sp_a[s]])
            AC(lambda: nc.scalar.activation(out=sp_a[s].t[:], in_=sp_a[s].t[:], func=AF.Exp, scale=-1.0), [sp_a[s]], [sp_a[s]])
            AC(lambda: nc.scalar.activation(out=sp_a[s].t[:], in_=sp_a[s].t[:], func=AF.Ln, bias=1.0), [sp_a[s]], [sp_a[s]])
            V(lambda: nc.vector.scalar_tensor_tensor(out=dts[s].t[:], in0=dts[s].t[:], scalar=0.0, in1=sp_a[s].t[:],
                                                     op0=ALU.max, op1=ALU.add), [dts[s], sp_a[s]], [dts[s]])
            V(lambda: nc.vector.tensor_tensor(out=dA[s].t[:], in0=dts[s].t[:], in1=Aneg.t[:], op=ALU.mult), [dts[s], Aneg], [dA[s]])
            PE(lambda: nc.tensor.matmul(out=pacs.t[:, 0:8], lhsT=tri.t[:], rhs=dA[s].t[:], start=True, stop=True), [tri, dA[s]], [pacs])
            PE(lambda: nc.tensor.matmul(out=pacs.t[:, 8:16], lhsT=ones.t[:], rhs=dA[s].t[:], start=True, stop=True), [ones, dA[s]], [pacs])
            V(lambda: nc.vector.tensor_copy(out=acs[s].t[:], in_=pacs.t[:]), [pacs], [acs[s]])
            AC(lambda: nc.scalar.activation(out=eacs[s].t[:], in_=acs[s].t[:, 0:8], func=AF.Exp), [acs[s]], [eacs[s]])
            AC(lambda: nc.scalar.activation(out=cd[s].t[:], in_=acs[s].t[:, 8:16], func=AF.Exp), [acs[s]], [cd[s]])
            V(lambda: nc.vector.tensor_tensor(out=dsd[s].t[:], in0=acs[s].t[:, 8:16], in1=acs[s].t[:, 0:8], op=ALU.subtract), [acs[s]], [dsd[s]])
            AC(lambda: nc.scalar.activation(out=dsd[s].t[:], in_=dsd[s].t[:], func=AF.Exp), [dsd[s]], [dsd[s]])
            V(lambda: nc.vector.tensor_tensor(out=dsd[s].t[:], in0=dsd[s].t[:], in1=dts[s].t[:], op=ALU.mult), [dsd[s], dts[s]], [dsd[s]])
            x3 = xtm[s].t[:].rearrange("p (h d) -> p h d", h=8)
            V(lambda: nc.vector.tensor_tensor(out=xdt[s].t[:].rearrange("p (h d) -> p h d", h=8), in0=x3,
                                              in1=dts[s].t[:].unsqueeze(2).to_broadcast([128, 8, 64]), op=ALU.mult),
              [xtm[s], dts[s]], [xdt[s]])
            GP(lambda: nc.gpsimd.tensor_tensor(out=xdd[s].t[:].rearrange("p (h d) -> p h d", h=8), in0=x3,
                                               in1=dsd[s].t[:].unsqueeze(2).to_broadcast([128, 8, 64]), op=ALU.mult),
               [xtm[s], dsd[s]], [xdd[s]])
            for g in range(2):
                PE(lambda: nc.tensor.matmul(out=pcb.t[:, g, :], lhsT=BT[s].t[:, g, :], rhs=CT[s].t[:, g, :], start=True, stop=True),
                   [BT[s], CT[s]], [pcb])
            V(lambda: nc.vector.tensor_tensor(out=cbm[s].t[:], in0=pcb.t[:], in1=tri.t[:].unsqueeze(1).to_broadcast([128, 2, 128]),
                                              op=ALU.mult), [pcb, tri], [cbm[s]])
        def tail(i):
            s = i % 2
            t0 = i * 128
            x3 = xtm[s].t[:].rearrange("p (h d) -> p h d", h=8)
            GP(lambda: nc.gpsimd.tensor_tensor(out=dAt.t[:], in0=tri.t[:].unsqueeze(1).to_broadcast([128, 8, 128]),
                                               in1=dA[s].t[:].unsqueeze(2).to_broadcast([128, 8, 128]), op=ALU.mult), [tri, dA[s]], [dAt])
            for g in range(2):
                for j in range(4):
                    PE(lambda: nc.tensor.matmul(out=pD.t[:, j, :], lhsT=slt.t[:], rhs=dAt.t[:, 4 * g + j, :], start=True, stop=True),
                       [slt, dAt], [pD])
                AC(lambda: nc.scalar.activation(out=seg[g].t[:], in_=pD.t[:], func=AF.Exp), [pD], [seg[g]])
                V(lambda: nc.vector.tensor_tensor(out=MT[g].t[:], in0=seg[g].t[:], in1=cbm[s].t[:, g, :].unsqueeze(1).to_broadcast([128, 4, 128]),
                                                  op=ALU.mult), [seg[g], cbm[s]], [MT[g]])
                for j in range(4):
                    h = 4 * g + j
                    PE(lambda: nc.tensor.matmul(out=py.t[:, h * 64:(h + 1) * 64], lhsT=MT[g].t[:, j, :], rhs=xdt[s].t[:, h * 64:(h + 1) * 64],
                                                start=True, stop=True), [MT[g], xdt[s]], [py])
            for g in range(2):
                PE(lambda: nc.tensor.matmul(out=pyo.t[:, g * 256:(g + 1) * 256], lhsT=CT[s].t[:, g, :], rhs=hTb.t[:, g * 256:(g + 1) * 256],
                                            start=True, stop=True), [CT[s], hTb], [pyo])
            for g in range(2):
                PE(lambda: nc.tensor.matmul(out=pU.t[:, g * 256:(g + 1) * 256], lhsT=Btm[s].t[:, g, :], rhs=xdd[s].t[:, g * 256:(g + 1) * 256],
                                            start=True, stop=True), [Btm[s], xdd[s]], [pU])
            AC(lambda: nc.scalar.copy(out=yd[s].t[:], in_=py.t[:]), [py], [yd[s]])
            V(lambda: nc.vector.tensor_tensor(out=y[s].t[:].rearrange("p (h d) -> p h d", h=8),
                                              in0=pyo.t[:].rearrange("p (h d) -> p h d", h=8),
                                              in1=eacs[s].t[:].unsqueeze(2).to_broadcast([128, 8, 64]), op=ALU.mult),
              [pyo, eacs[s]], [y[s]])
            V(lambda: nc.vector.tensor_tensor(out=hT.t[:].rearrange("p (h d) -> p h d", h=8),
                                              in0=hT.t[:].rearrange("p (h d) -> p h d", h=8),
                                              in1=cd[s].t[:].unsqueeze(2).to_broadcast([128, 8, 64]), op=ALU.mult),
              [hT, cd[s]], [hT])
            V(lambda: nc.vector.tensor_tensor(out=hT.t[:], in0=pU.t[:], in1=hT.t[:], op=ALU.add), [pU, hT], [hT])
            AC(lambda: nc.scalar.copy(out=hTb.t[:], in_=hT.t[:]), [hT], [hTb])
            GP(lambda: nc.gpsimd.tensor_tensor(out=y[s].t[:], in0=y[s].t[:], in1=yd[s].t[:], op=ALU.add), [y[s], yd[s]], [y[s]])
            GP(lambda: nc.gpsimd.tensor_tensor(out=yd[s].t[:].rearrange("p (h d) -> p h d", h=8), in0=x3,
                                               in1=dsk.t[:].unsqueeze(2).to_broadcast([128, 8, 64]), op=ALU.mult),
               [xtm[s], dsk], [yd[s]])
            GP(lambda: nc.gpsimd.tensor_tensor(out=y[s].t[:], in0=y[s].t[:], in1=yd[s].t[:], op=ALU.add), [y[s], yd[s]], [y[s]])
            AC(lambda: nc.scalar.activation(out=sz[s].t[:], in_=zt[s].t[:], func=AF.Silu), [zt[s]], [sz[s]])
            V(lambda: nc.vector.tensor_tensor(out=y[s].t[:], in0=y[s].t[:], in1=sz[s].t[:], op=ALU.mult), [y[s], sz[s]], [y[s]])
            for g in range(2):
                AC(lambda: nc.scalar.activation(out=junk.t[:], in_=y[s].t[:, g * 256:(g + 1) * 256], func=AF.Square,
                                                accum_out=ss[s].t[:, g:g + 1]), [y[s]], [junk, ss[s]])
            ssk = [ss[s]]
            V(lambda: nc.vector.tensor_scalar(out=ss[s].t[:], in0=ss[s].t[:], scalar1=1.0 / 256, scalar2=EPS, op0=ALU.mult, op1=ALU.add),
              ssk, [ss[s]])
            AC(lambda: nc.scalar.activation(out=ss[s].t[:], in_=ss[s].t[:], func=AF.Sqrt), [ss[s]], [ss[s]])
            V(lambda: nc.vector.reciprocal(out=ss[s].t[:], in_=ss[s].t[:]), [ss[s]], [ss[s]])
            for g in range(2):
                V(lambda: nc.vector.scalar_tensor_tensor(out=mo[s].t[:, g * 256:(g + 1) * 256], in0=y[s].t[:, g * 256:(g + 1) * 256],
                                                         scalar=ss[s].t[:, g:g + 1], in1=nwb.t[:, g * 256:(g + 1) * 256],
                                                         op0=ALU.mult, op1=ALU.mult), [y[s], ss[s], nwb], [Tile(None, f"ssd_mo{s}_{g}")])
            kb.dma("sp", mo[s].k, self.mix[t0:t0 + 128, 512:1024], mo[s].t[:],
                   reads=[f"ssd_mo{s}_0", f"ssd_mo{s}_1"], writes=[("mix_ssd", i)])

        front(0)
        for i in range(NT):
            if i + 1 < NT:
                interleave(kb, (lambda i=i: tail(i)), (lambda i=i: front(i + 1)))
            else:
                tail(i)


Prog.stage_ssd = _stage_ssd


def _stage_gla(self, l):
    nc, kb, NT = self.nc, self.kb, self.NT
    with ExitStack() as es:
        def T(name, shape, dt=F32):
            return Tile(self.sb(es, "gla_" + name, shape, dt), "gla_" + name)

        def T2(name, shape, dt=F32):
            return [T(f"{name}{j}", shape, dt) for j in range(2)]

        def PT(name, shape, dt=F32):
            return Tile(self.ps(es, "gla_" + name, shape, dt), "gla_" + name)

        def V(fn, r, w):
            return kb.op("dve", fn, [t.k for t in r], [t.k for t in w])

        def AC(fn, r, w):
            return kb.op("act", fn, [t.k for t in r], [t.k for t in w])

        def PE(fn, r, w):
            return kb.op("pe", fn, [t.k for t in r], [t.k for t in w])

        def GP(fn, r, w):
            return kb.op("pool", fn, [t.k for t in r], [t.k for t in w])

        def LD(t, dst, src, reads=(), **kw):
            return kb.dma("sp", t.k, dst, src, reads=list(reads), writes=[t.k], **kw)

        w2 = T("w2", [16, 128]); bb = T("bb", [128, 128]); nwb = T("nwb", [128, 256])
        tri = T("tri", [128, 128]); tri16 = T("tri16", [128, 128]); o16 = T("o16", [128, 2])
        idf = T("idf", [128, 128]); idb = T("idb", [128, 128], BF16)
        hm = T("hm", [128, 4]); bd = T("bd", [128, 256])
        Sbd = T("Sbd", [128, 256]); Sbb = T("Sbb", [128, 256], BF16)
        gin = T2("gin", [128, 784])
        glrT = T2("glrT", [16, 128]); zv = T2("zv", [128, 128]); az = T2("az", [128, 128]); la = T2("la", [128, 128])
        eb = T2("eb", [128, 128]); enb = T2("enb", [128, 128])
        qt = T2("qtok", [128, 128], BF16); kt = T2("ktok", [128, 128], BF16); vb = T2("vb", [128, 256], BF16)
        qT = T2("qT", [128, 128], BF16); kTm = T2("kTm", [128, 4, 128], BF16)
        egt = T2("egt", [128, 2]); ATm = T2("ATm", [128, 4, 128], BF16)
        sq = T2("sq", [128, 256]); ss = T2("ss", [128, 4]); on = T2("on", [128, 256]); sr = T2("sr", [128, 256])
        mo = T2("mo", [128, 256], BF16); um = T2("um", [128, 256])
        pgT = PT("pgT", [16, 128]); pz = PT("pz", [128, 128]); pbc = PT("pbc", [128, 128])
        pqk = PT("pqk", [128, 2, 128], BF16); pgt = PT("pgt", [128, 2]); pA = PT("pA", [128, 4, 128])
        po = PT("po", [128, 256]); pU = PT("pU", [128, 256])

        LD(w2, w2.t[:], self.gla_gate_w2[l, :, :])
        LD(bb, bb.t[:], self.gla_gate_b[l, :].partition_broadcast(128))
        LD(nwb, nwb.t[:], self.gla_norm_w[l, :].partition_broadcast(128))
        LD(tri, tri.t[:], self.c_tri[:, :]); LD(idf, idf.t[:], self.ident_f[:, :]); LD(idb, idb.t[:], self.ident_b[:, :])
        LD(hm, hm.t[:], self.c_hm[:, :]); LD(bd, bd.t[:], self.c_bd[:, :])
        V(lambda: nc.vector.tensor_scalar(out=tri16.t[:], in0=tri.t[:], scalar1=1.0 / 16, scalar2=None, op0=ALU.mult), [tri], [tri16])
        V(lambda: nc.vector.memset(o16.t[:], 1.0 / 16), [], [o16])
        V(lambda: nc.vector.memset(Sbd.t[:], 0.0), [], [Sbd])
        V(lambda: nc.vector.memset(Sbb.t[:], 0.0), [], [Sbb])

        def front(i):
            s = i % 2
            t0 = i * 128
            kb.dma("sp", gin[s].k, gin[s].t[:], self.proj[t0:t0 + 128, 0:784], reads=[("proj", i)], writes=[gin[s].k])
            q_ = gin[s].t[:, C_GQ:C_GQ + 128]; k_ = gin[s].t[:, C_GK:C_GK + 128]; v_ = gin[s].t[:, C_GV:C_GV + 256]
            glr_ = gin[s].t[:, C_GLR:C_GLR + 16]; r_ = gin[s].t[:, C_GR:C_GR + 256]
            PE(lambda: nc.tensor.transpose(out=pgT.t[:], in_=glr_, identity=idf.t[:]), [gin[s], idf], [pgT])
            V(lambda: nc.vector.tensor_copy(out=glrT[s].t[:], in_=pgT.t[:]), [pgT], [glrT[s]])
            PE(lambda: nc.tensor.matmul(out=pz.t[:], lhsT=glrT[s].t[:], rhs=w2.t[:], start=True, stop=True), [glrT[s], w2], [pz])
            V(lambda: nc.vector.tensor_tensor(out=zv[s].t[:], in0=pz.t[:], in1=bb.t[:], op=ALU.add), [pz, bb], [zv[s]])
            V(lambda: nc.vector.scalar_tensor_tensor(out=az[s].t[:], in0=zv[s].t[:], scalar=-1.0, in1=zv[s].t[:], op0=ALU.mult, op1=ALU.max),
              [zv[s]], [az[s]])
            AC(lambda: nc.scalar.activation(out=az[s].t[:], in_=az[s].t[:], func=AF.Exp, scale=-1.0), [az[s]], [az[s]])
            AC(lambda: nc.scalar.activation(out=az[s].t[:], in_=az[s].t[:], func=AF.Ln, bias=1.0), [az[s]], [az[s]])
            V(lambda: nc.vector.scalar_tensor_tensor(out=la[s].t[:], in0=zv[s].t[:], scalar=0.0, in1=az[s].t[:], op0=ALU.min, op1=ALU.subtract),
              [zv[s], az[s]], [la[s]])
            PE(lambda: nc.tensor.matmul(out=pbc.t[:], lhsT=tri16.t[:], rhs=la[s].t[:], start=True, stop=True), [tri16, la[s]], [pbc])
            PE(lambda: nc.tensor.matmul(out=pgt.t[:], lhsT=la[s].t[:], rhs=o16.t[:], start=True, stop=True), [la[s], o16], [pgt])
            AC(lambda: nc.scalar.activation(out=eb[s].t[:], in_=pbc.t[:], func=AF.Exp), [pbc], [eb[s]])
            AC(lambda: nc.scalar.activation(out=enb[s].t[:], in_=pbc.t[:], func=AF.Exp, scale=-1.0), [pbc], [enb[s]])
            AC(lambda: nc.scalar.activation(out=egt[s].t[:], in_=pgt.t[:], func=AF.Exp), [pgt], [egt[s]])
            V(lambda: nc.vector.scalar_tensor_tensor(out=qt[s].t[:], in0=q_, scalar=32.0 ** -0.5, in1=eb[s].t[:], op0=ALU.mult, op1=ALU.mult),
              [gin[s], eb[s]], [qt[s]])
            V(lambda: nc.vector.tensor_tensor(out=kt[s].t[:], in0=k_, in1=enb[s].t[:], op=ALU.mult), [gin[s], enb[s]], [kt[s]])
            GP(lambda: nc.gpsimd.tensor_copy(out=vb[s].t[:], in_=v_), [gin[s]], [vb[s]])
            PE(lambda: nc.tensor.transpose(out=pqk.t[:, 0, :], in_=qt[s].t[:], identity=idb.t[:]), [qt[s], idb], [pqk])
            PE(lambda: nc.tensor.transpose(out=pqk.t[:, 1, :], in_=kt[s].t[:], identity=idb.t[:]), [kt[s], idb], [pqk])
            AC(lambda: nc.scalar.copy(out=qT[s].t[:], in_=pqk.t[:, 0, :]), [pqk], [qT[s]])
            for h in range(4):
                AC(lambda: nc.scalar.activation(out=kTm[s].t[:, h, :], in_=pqk.t[:, 1, :], func=AF.Copy, scale=hm.t[:, h:h + 1]),
                   [pqk, hm], [kTm[s]])
            for h in range(4):
                PE(lambda: nc.tensor.matmul(out=pA.t[:, h, :], lhsT=kTm[s].t[:, h, :], rhs=qT[s].t[:], start=True, stop=True),
                   [kTm[s], qT[s]], [pA])
            V(lambda: nc.vector.tensor_tensor(out=ATm[s].t[:], in0=pA.t[:], in1=tri.t[:].unsqueeze(1).to_broadcast([128, 4, 128]), op=ALU.mult),
              [pA, tri], [ATm[s]])
        def tail(i):
            s = i % 2
            t0 = i * 128
            r_ = gin[s].t[:, C_GR:C_GR + 256]
            PE(lambda: nc.tensor.matmul(out=po.t[:], lhsT=qT[s].t[:], rhs=Sbb.t[:], start=True, stop=False), [qT[s], Sbb], [po])
            for h in range(4):
                PE(lambda: nc.tensor.matmul(out=po.t[:, h * 64:(h + 1) * 64], lhsT=ATm[s].t[:, h, :], rhs=vb[s].t[:, h * 64:(h + 1) * 64],
                                            start=False, stop=(h == 3)), [ATm[s], vb[s]], [po])
            PE(lambda: nc.tensor.matmul(out=pU.t[:], lhsT=kt[s].t[:], rhs=vb[s].t[:], start=True, stop=True), [kt[s], vb[s]], [pU])
            V(lambda: nc.vector.tensor_tensor(out=um[s].t[:], in0=pU.t[:], in1=bd.t[:], op=ALU.mult), [pU, bd], [um[s]])
            V(lambda: nc.vector.tensor_tensor(out=Sbd.t[:], in0=Sbd.t[:], in1=um[s].t[:], op=ALU.add), [Sbd, um[s]], [Sbd])
            V(lambda: nc.vector.tensor_scalar(out=Sbd.t[:], in0=Sbd.t[:], scalar1=egt[s].t[:, 0:1], scalar2=None, op0=ALU.mult), [Sbd, egt[s]], [Sbd])
            AC(lambda: nc.scalar.copy(out=Sbb.t[:], in_=Sbd.t[:]), [Sbd], [Sbb])
            AC(lambda: nc.scalar.activation(out=sq[s].t[:], in_=po.t[:], func=AF.Square), [po], [sq[s]])
            V(lambda: nc.vector.tensor_reduce(out=ss[s].t[:], in_=sq[s].t[:].rearrange("p (h d) -> p h d", h=4), axis=AX.X, op=ALU.add),
              [sq[s]], [ss[s]])
            V(lambda: nc.vector.tensor_scalar(out=ss[s].t[:], in0=ss[s].t[:], scalar1=1.0 / 64, scalar2=EPS, op0=ALU.mult, op1=ALU.add), [ss[s]], [ss[s]])
            AC(lambda: nc.scalar.activation(out=ss[s].t[:], in_=ss[s].t[:], func=AF.Sqrt), [ss[s]], [ss[s]])
            V(lambda: nc.vector.reciprocal(out=ss[s].t[:], in_=ss[s].t[:]), [ss[s]], [ss[s]])
            V(lambda: nc.vector.tensor_tensor(out=on[s].t[:].rearrange("p (h d) -> p h d", h=4), in0=po.t[:].rearrange("p (h d) -> p h d", h=4),
                                              in1=ss[s].t[:].unsqueeze(2).to_broadcast([128, 4, 64]), op=ALU.mult), [po, ss[s]], [on[s]])
            AC(lambda: nc.scalar.activation(out=sr[s].t[:], in_=r_, func=AF.Silu), [gin[s]], [sr[s]])
            GP(lambda: nc.gpsimd.tensor_tensor(out=sr[s].t[:], in0=sr[s].t[:], in1=nwb.t[:], op=ALU.mult), [sr[s], nwb], [sr[s]])
            V(lambda: nc.vector.tensor_tensor(out=mo[s].t[:], in0=on[s].t[:], in1=sr[s].t[:], op=ALU.mult), [on[s], sr[s]], [mo[s]])
            kb.dma("sp", mo[s].k, self.mix[t0:t0 + 128, 0:256], mo[s].t[:], reads=[mo[s].k], writes=[("mix_gla", i)])

        front(0)
        for i in range(NT):
            if i + 1 < NT:
                interleave(kb, (lambda i=i: tail(i)), (lambda i=i: front(i + 1)))
            else:
                tail(i)


Prog.stage_gla = _stage_gla


def build_test_mix(S, which, limit=None):
    p = Prog(S, L=1, dbg=[("o_mix", [S, D])])
    with p.kb.es as es:
        p.stage_d1(0, p.x)
        p.kb.barrier()
        if limit is not None:
            p.kb.limit = p.kb.ninst + limit
        if "ssd" in which:
            p.stage_ssd(0)
        if "gla" in which:
            p.stage_gla(0)
        if "nsa" in which:
            p.stage_nsa(0)
        kb, nc = p.kb, p.nc
        kb.limit = None
        kb.barrier()
        mb = p.sb(es, "dump_mb", [128, D], BF16)
        mf = p.sb(es, "dump_mf", [128, D])
        for i in range(p.NT):
            kb.dma("sp", "dump_mb", mb[:], p.mix[i * 128:(i + 1) * 128, :], writes=["dump_mb"])
            kb.op("dve", lambda: nc.vector.tensor_copy(out=mf[:], in_=mb[:]), reads=["dump_mb"], writes=["dump_mf"])
            kb.dma("sp", "dump_mf", p.outs["o_mix"][i * 128:(i + 1) * 128, :], mf[:], reads=["dump_mf"], writes=[("o_mix", i)])
        kb.drain("sp")
    return p


def _stage_d2(self, l, hsrc, hdst, final):
    nc, kb, NT = self.nc, self.kb, self.NT
    with ExitStack() as es:
        def T(name, shape, dt=F32):
            return Tile(self.sb(es, "d2_" + name, shape, dt), "d2_" + name)

        def T2(name, shape, dt=F32):
            return [T(f"{name}{j}", shape, dt) for j in range(2)]

        def PT(name, shape, dt=F32):
            return Tile(self.ps(es, "d2_" + name, shape, dt), "d2_" + name)

        def V(fn, r, w):
            return kb.op("dve", fn, [t.k for t in r], [t.k for t in w])

        def AC(fn, r, w):
            return kb.op("act", fn, [t.k for t in r], [t.k for t in w])

        def PE(fn, r, w):
            return kb.op("pe", fn, [t.k for t in r], [t.k for t in w])

        def GP(fn, r, w):
            return kb.op("pool", fn, [t.k for t in r], [t.k for t in w])

        Wo = T("Wo", [128, 8, 1024], BF16); Wu = T("Wu", [128, 8, 4096], BF16); Wd = T("Wd", [128, 32, 1024], BF16)
        stg = T2("stg", [128, 1024]); nw2 = T("nw2", [128, 8]); idb = T("idb", [128, 128], BF16)
        ht = T2("ht", [128, 1024]); mt = T2("mt", [128, 1024], BF16)
        mT = T("mT", [128, 8, 128], BF16); h1 = T2("h1", [128, 1024]); junk = T("junk", [128, 1024], BF16)
        ss = T("ss", [128, 1]); u2 = T("u2", [128, 1024], BF16); u2T = T2("u2T", [128, 8, 128], BF16)
        tmp = T2("tmp", [128, 512]); hidT = T("hidT", [128, 32, 128], BF16); ho = T2("ho", [128, 1024])
        pT = PT("pT", [128, 8, 128], BF16); po = [PT(f"po{j}", [128, 512]) for j in range(2)]
        pu = [PT(f"pu{j}", [128, 4, 128]) for j in range(2)]
        pd = [PT(f"pd{j}", [128, 512]) for j in range(2)]
        if final:
            fnw = T("fnw", [128, 1024]); ss2 = T("ss2", [128, 1])
            kb.dma("sp", fnw.k, fnw.t[:], self.final_norm_w.partition_broadcast(128), writes=[fnw.k])

        kb.dma("sp", idb.k, idb.t[:], self.ident_b[:, :], writes=[idb.k])
        for c in range(8):
            kb.dma("sp", nw2.k, nw2.t[:, c:c + 1], self.norm2_w[l, c * 128:(c + 1) * 128].rearrange("(p o) -> p o", o=1), writes=[nw2.k])
        n = 0
        jobs = [(Wo, c, 0, self.w_out[l, c * 128:(c + 1) * 128, :], False) for c in range(8)]
        jobs += [(Wu, c, q * 1024, self.w_up[l, c * 128:(c + 1) * 128, q * 1024:(q + 1) * 1024], True) for c in range(8) for q in range(4)]
        jobs += [(Wd, f, 0, self.w_down[l, f * 128:(f + 1) * 128, :], False) for f in range(32)]
        for (W, c, off, src, scaled) in jobs:
            sg = stg[n % 2]
            kb.dma("sp", sg.k, sg.t[:], src, writes=[sg.k])
            dst = W.t[:, c, off:off + 1024]
            if scaled:
                if n % 2 == 0:
                    V(lambda: nc.vector.tensor_scalar(out=dst, in0=sg.t[:], scalar1=nw2.t[:, c:c + 1], scalar2=None, op0=ALU.mult), [sg, nw2], [W])
                else:
                    AC(lambda: nc.scalar.activation(out=dst, in_=sg.t[:], func=AF.Copy, scale=nw2.t[:, c:c + 1]), [sg, nw2], [W])
            else:
                if n % 2 == 0:
                    V(lambda: nc.vector.tensor_copy(out=dst, in_=sg.t[:]), [sg], [W])
                else:
                    AC(lambda: nc.scalar.copy(out=dst, in_=sg.t[:]), [sg], [W])
            n += 1

        def front(i):
            s = i % 2
            t0 = i * 128
            kb.dma("sp", ht[s].k, ht[s].t[:], hsrc[t0:t0 + 128, :], reads=[("h", i)], writes=[ht[s].k])
            kb.dma("sp", mt[s].k, mt[s].t[:], self.mix[t0:t0 + 128, :], reads=[("mix_gla", i), ("mix_nsa", i), ("mix_ssd", i)], writes=[mt[s].k])
            for c in range(8):
                PE(lambda: nc.tensor.transpose(out=pT.t[:, c, :], in_=mt[s].t[:, c * 128:(c + 1) * 128], identity=idb.t[:]), [mt[s], idb], [pT])
            AC(lambda: nc.scalar.copy(out=mT.t[:], in_=pT.t[:]), [pT], [mT])
            for hf in range(2):
                for c in range(8):
                    PE(lambda: nc.tensor.matmul(out=po[hf].t[:], lhsT=mT.t[:, c, :], rhs=Wo.t[:, c, hf * 512:(hf + 1) * 512],
                                                start=(c == 0), stop=(c == 7)), [mT, Wo], [po[hf]])
                V(lambda: nc.vector.tensor_tensor(out=h1[s].t[:, hf * 512:(hf + 1) * 512], in0=po[hf].t[:], in1=ht[s].t[:, hf * 512:(hf + 1) * 512], op=ALU.add),
                  [po[hf], ht[s]], [h1[s]])
            AC(lambda: nc.scalar.activation(out=junk.t[:], in_=h1[s].t[:], func=AF.Square, accum_out=ss.t[:]), [h1[s]], [junk, ss])
            V(lambda: nc.vector.tensor_scalar(out=ss.t[:], in0=ss.t[:], scalar1=1.0 / D, scalar2=EPS, op0=ALU.mult, op1=ALU.add), [ss], [ss])
            AC(lambda: nc.scalar.activation(out=ss.t[:], in_=ss.t[:], func=AF.Sqrt), [ss], [ss])
            V(lambda: nc.vector.reciprocal(out=ss.t[:], in_=ss.t[:]), [ss], [ss])
            V(lambda: nc.vector.tensor_scalar(out=u2.t[:], in0=h1[s].t[:], scalar1=ss.t[:], scalar2=None, op0=ALU.mult), [h1[s], ss], [u2])
            for c in range(8):
                PE(lambda: nc.tensor.transpose(out=pT.t[:, c, :], in_=u2.t[:, c * 128:(c + 1) * 128], identity=idb.t[:]), [u2, idb], [pT])
            AC(lambda: nc.scalar.copy(out=u2T[s].t[:], in_=pT.t[:]), [pT], [u2T[s]])
        def tail(i):
            s = i % 2
            t0 = i * 128
            for fg in range(8):
                p = pu[fg % 2]
                for j in range(4):
                    f = fg * 4 + j
                    for c in range(8):
                        PE(lambda: nc.tensor.matmul(out=p.t[:, j, :], lhsT=Wu.t[:, c, f * 128:(f + 1) * 128], rhs=u2T[s].t[:, c, :],
                                                    start=(c == 0), stop=(c == 7)), [Wu, u2T[s]], [p])
                tm = tmp[fg % 2]
                AC(lambda: nc.scalar.activation(out=tm.t[:], in_=p.t[:].rearrange("p a b -> p (a b)"), func=AF.Relu), [p], [tm])
                GP(lambda: nc.gpsimd.tensor_tensor(out=hidT.t[:, fg * 4:(fg + 1) * 4, :].rearrange("p a b -> p (a b)"), in0=tm.t[:], in1=tm.t[:], op=ALU.mult),
                   [tm], [Tile(None, f"d2_hid{fg}")])
            hk = [Tile(None, f"d2_hid{fg}") for fg in range(8)]
            for f in range(32):
                for hf in range(2):
                    PE(lambda: nc.tensor.matmul(out=pd[hf].t[:], lhsT=hidT.t[:, f, :], rhs=Wd.t[:, f, hf * 512:(hf + 1) * 512],
                                                start=(f == 0), stop=(f == 31)), [hk[f // 4], Wd], [pd[hf]])
            for hf in range(2):
                V(lambda: nc.vector.tensor_tensor(out=ho[s].t[:, hf * 512:(hf + 1) * 512], in0=pd[hf].t[:], in1=h1[s].t[:, hf * 512:(hf + 1) * 512], op=ALU.add),
                  [pd[hf], h1[s]], [ho[s]])
            if final:
                AC(lambda: nc.scalar.activation(out=junk.t[:], in_=ho[s].t[:], func=AF.Square, accum_out=ss2.t[:]), [ho[s]], [junk, ss2])
                V(lambda: nc.vector.tensor_scalar(out=ss2.t[:], in0=ss2.t[:], scalar1=1.0 / D, scalar2=EPS, op0=ALU.mult, op1=ALU.add), [ss2], [ss2])
                AC(lambda: nc.scalar.activation(out=ss2.t[:], in_=ss2.t[:], func=AF.Sqrt), [ss2], [ss2])
                V(lambda: nc.vector.reciprocal(out=ss2.t[:], in_=ss2.t[:]), [ss2], [ss2])
                V(lambda: nc.vector.scalar_tensor_tensor(out=ho[s].t[:], in0=ho[s].t[:], scalar=ss2.t[:], in1=fnw.t[:], op0=ALU.mult, op1=ALU.mult),
                  [ho[s], ss2, fnw], [ho[s]])
            kb.dma("sp", ho[s].k, hdst[t0:t0 + 128, :], ho[s].t[:], reads=[ho[s].k], writes=[("hout", l, i)] if final else [("h", i)])

        front(0)
        for i in range(NT):
            if i + 1 < NT:
                interleave(kb, (lambda i=i: tail(i)), (lambda i=i: front(i + 1)))
            else:
                tail(i)


Prog.stage_d2 = _stage_d2


def _stage_nsa(self, l):
    nc, kb, NT, S, NS, NCC = self.nc, self.kb, self.NT, self.S, self.NS, self.NCC
    NCB = S // 16 - 1
    with ExitStack() as es:
        def Tn(es_, name, shape, dt=F32):
            return Tile(self.sb(es_, "nsa_" + name, shape, dt), "nsa_" + name)

        def T(name, shape, dt=F32):
            return Tn(es, name, shape, dt)

        def V(fn, r, w):
            return kb.op("dve", fn, [t.k for t in r], [t.k for t in w])

        def AC(fn, r, w):
            return kb.op("act", fn, [t.k for t in r], [t.k for t in w])

        def PE(fn, r, w):
            return kb.op("pe", fn, [t.k for t in r], [t.k for t in w])

        def GP(fn, r, w):
            return kb.op("pool", fn, [t.k for t in r], [t.k for t in w])

        def LD(t, dst, src, reads=(), **kw):
            return kb.dma("sp", t.k, dst, src, reads=list(reads), writes=[t.k], **kw)

        QT = T("QT", [128, NT, 2, 128], BF16); KsT = T("KsT", [128, S], BF16); KwT = T("KwT", [128, S], BF16)
        KCT = T("KCT", [128, NCC * 128], BF16)
        VsA = T("VsA", [128, NT, 2, 65], BF16); VwA = T("VwA", [128, NT, 2, 65], BF16)
        RC = T("RC", [128, NCC, 2, 65 + NS], BF16); GA = T("GA", [128, NT, 12])
        idb = T("idb", [128, 128], BF16); nwb = T("nwb", [128, 256])
        LD(idb, idb.t[:], self.ident_b[:, :])
        LD(nwb, nwb.t[:], self.nsa_norm_w[l, :].partition_broadcast(128))
        V(lambda: nc.vector.memset(KCT.t[:], 0.0), [], [KCT])
        V(lambda: nc.vector.memset(RC.t[:], 0.0), [], [RC])
        V(lambda: nc.vector.memset(VsA.t[:], 1.0), [], [VsA])
        V(lambda: nc.vector.memset(VwA.t[:], 1.0), [], [VwA])

        with ExitStack() as e1:
            nin = [Tn(e1, f"nin{j}", [128, 1036]) for j in range(2)]
            cs = [Tn(e1, f"cs{j}", [128, 2, 32]) for j in range(2)]
            ta2 = [Tn(e1, f"ta{j}", [128, 10, 32]) for j in range(2)]; tb2 = [Tn(e1, f"tb{j}", [128, 10, 32]) for j in range(2)]
            rq = [Tn(e1, f"rq{j}", [128, 768], BF16) for j in range(2)]
            kv = [Tn(e1, f"kv{j}", [128, 2, 128], BF16) for j in range(2)]
            pT2 = [Tile(self.ps(e1, f"nsa_pT1{j}", [128, 6, 128], BF16), f"nsa_pT1{j}") for j in range(2)]

            def n1(i):
                s = i % 2
                t0 = i * 128
                ta, tb, pT = ta2[s], tb2[s], pT2[s]
                kb.dma("sp", nin[s].k, nin[s].t[:], self.proj[t0:t0 + 128, C_NQ:C_NQ + 1036], reads=[("proj", i)], writes=[nin[s].k])
                kb.dma("sp", cs[s].k + "c", cs[s].t[:, 0, :], self.rope_cos[t0:t0 + 128, :], writes=[cs[s].k + "c"])
                kb.dma("sp", cs[s].k + "s", cs[s].t[:, 1, :], self.rope_sin[t0:t0 + 128, :], writes=[cs[s].k + "s"])
                csk = [Tile(None, cs[s].k + "c"), Tile(None, cs[s].k + "s")]
                x3 = nin[s].t[:, 0:640].rearrange("p (h d) -> p h d", h=10)
                o3 = rq[s].t[:, 0:640].rearrange("p (h d) -> p h d", h=10)
                cb_ = cs[s].t[:, 0, :].unsqueeze(1).to_broadcast([128, 10, 32])
                sb_ = cs[s].t[:, 1, :].unsqueeze(1).to_broadcast([128, 10, 32])
                V(lambda: nc.vector.tensor_tensor(out=ta.t[:], in0=x3[:, :, 0:32], in1=cb_, op=ALU.mult), [nin[s]] + csk, [ta])
                V(lambda: nc.vector.tensor_tensor(out=tb.t[:], in0=x3[:, :, 32:64], in1=sb_, op=ALU.mult), [nin[s]] + csk, [tb])
                V(lambda: nc.vector.tensor_tensor(out=o3[:, :, 0:32], in0=ta.t[:], in1=tb.t[:], op=ALU.subtract), [ta, tb], [Tile(None, rq[s].k + "a")])
                V(lambda: nc.vector.tensor_tensor(out=ta.t[:], in0=x3[:, :, 32:64], in1=cb_, op=ALU.mult), [nin[s]] + csk, [ta])
                V(lambda: nc.vector.tensor_tensor(out=tb.t[:], in0=x3[:, :, 0:32], in1=sb_, op=ALU.mult), [nin[s]] + csk, [tb])
                V(lambda: nc.vector.tensor_tensor(out=o3[:, :, 32:64], in0=ta.t[:], in1=tb.t[:], op=ALU.add), [ta, tb], [Tile(None, rq[s].k + "b")])
                AC(lambda: nc.scalar.copy(out=rq[s].t[:, 640:768], in_=nin[s].t[:, 640:768]), [nin[s]], [Tile(None, rq[s].k + "c")])
                rqk = [Tile(None, rq[s].k + x) for x in "abc"]
                for b in range(6):
                    PE(lambda: nc.tensor.transpose(out=pT.t[:, b, :], in_=rq[s].t[:, b * 128:(b + 1) * 128], identity=idb.t[:]), rqk + [idb], [pT])
                AC(lambda: nc.scalar.copy(out=QT.t[:, i, :, :], in_=pT.t[:, 0:2, :]), [pT], [Tile(None, f"nsa_QT{i}")])
                AC(lambda: nc.scalar.copy(out=KsT.t[:, t0:t0 + 128], in_=pT.t[:, 3, :]), [pT], [Tile(None, f"nsa_KsT{i}")])
                AC(lambda: nc.scalar.copy(out=KwT.t[:, t0:t0 + 128], in_=pT.t[:, 4, :]), [pT], [Tile(None, f"nsa_KwT{i}")])
                AC(lambda: nc.scalar.copy(out=kv[s].t[:, 0, :], in_=pT.t[:, 2, :]), [pT], [kv[s]])
                AC(lambda: nc.scalar.copy(out=kv[s].t[:, 1, :], in_=pT.t[:, 5, :]), [pT], [kv[s]])
                kb.dma("sp", kv[s].k + "k", self.kcT_d[:, t0:t0 + 128], kv[s].t[:, 0, :], reads=[kv[s].k], writes=[("kcT_d", i)])
                kb.dma("sp", kv[s].k + "v", self.vcT_d[:, t0:t0 + 128], kv[s].t[:, 1, :], reads=[kv[s].k], writes=[("vcT_d", i)])
                GP(lambda: nc.gpsimd.tensor_copy(out=VsA.t[:, i, :, 0:64], in_=nin[s].t[:, 768:896].rearrange("p (g d) -> p g d", g=2)),
                   [nin[s], VsA], [Tile(None, f"nsa_VsA{i}")])
                GP(lambda: nc.gpsimd.tensor_copy(out=VwA.t[:, i, :, 0:64], in_=nin[s].t[:, 896:1024].rearrange("p (g d) -> p g d", g=2)),
                   [nin[s], VwA], [Tile(None, f"nsa_VwA{i}")])
                AC(lambda: nc.scalar.activation(out=GA.t[:, i, :], in_=nin[s].t[:, 1024:1036], func=AF.Sigmoid), [nin[s]], [Tile(None, f"nsa_GA{i}")])

            for i in range(0, NT, 2):
                if i + 1 < NT:
                    interleave(kb, (lambda i=i: n1(i)), (lambda i=i: n1(i + 1)))
                else:
                    n1(i)
            kb.barrier()

        with ExitStack() as e2:
            wst = Tn(e2, "wst", [128, 32, 128]); W1 = [Tn(e2, f"W1{j}", [128, 32, 128], BF16) for j in range(2)]
            w2s = Tn(e2, "w2s", [128, 64]); W2p = Tn(e2, "W2p", [128, 2, 128], BF16); W2v = Tn(e2, "W2v", [128, 64], BF16)
            psT = Tn(e2, "psT", [128, 32]); posb = [Tn(e2, f"posb{j}", [128, 32, 2], BF16) for j in range(2)]
            cbv = [Tn(e2, f"cbv{j}", [128, 2]) for j in range(2)]
            XT = Tn(e2, "XT", [128, 128 * 16 + 16], BF16)
            hb = Tn(e2, "hb", [128, 128]); tt = Tn(e2, "tt", [128, 128]); GT = [Tn(e2, f"GT{g}", [128, 128], BF16) for g in range(2)]
            ph = Tile(self.ps(e2, "nsa_ph", [128, 128]), "nsa_ph"); pc = Tile(self.ps(e2, "nsa_pc", [128, 128]), "nsa_pc")
            pb = Tile(self.ps(e2, "nsa_pb", [128, 2]), "nsa_pb")
            V(lambda: nc.vector.memset(W2p.t[:], 0.0), [], [W2p])
            for wi, (w1d, w2d, posd) in enumerate([(self.nsa_cmp_w1_k, self.nsa_cmp_w2_k, self.nsa_cmp_pos_k),
                                                   (self.nsa_cmp_w1_v, self.nsa_cmp_w2_v, self.nsa_cmp_pos_v)]):
                for half in range(2):
                    LD(wst, wst.t[half * 64:(half + 1) * 64, :, :], w1d[l].rearrange("(i d) j -> d i j", d=64))
                V(lambda: nc.vector.tensor_copy(out=W1[wi].t[:], in_=wst.t[:]), [wst], [W1[wi]])
                LD(w2s, w2s.t[:], w2d[l, :, :])
                if wi == 0:
                    for g in range(2):
                        V(lambda: nc.vector.tensor_copy(out=W2p.t[:, g, g * 64:(g + 1) * 64], in_=w2s.t[:]), [w2s], [W2p])
                else:
                    V(lambda: nc.vector.tensor_copy(out=W2v.t[:], in_=w2s.t[:]), [w2s], [W2v])
                for half in range(2):
                    LD(psT, psT.t[half * 64:(half + 1) * 64, :], posd[l].rearrange("i d -> d i"), allow_slow_non_contiguous=True)
                for j in range(2):
                    V(lambda: nc.vector.tensor_copy(out=posb[wi].t[:, :, j], in_=psT.t[:]), [psT], [posb[wi]])
                for i_ in range(32):
                    PE(lambda: nc.tensor.matmul(out=pb.t[:], lhsT=W1[wi].t[0:64, i_, :], rhs=posb[wi].t[0:64, i_, :], start=(i_ == 0), stop=(i_ == 31)),
                       [W1[wi], posb[wi]], [pb])
                V(lambda: nc.vector.tensor_copy(out=cbv[wi].t[:], in_=pb.t[:]), [pb], [cbv[wi]])
            for c in range(NCC):
                n0 = c * 128
                nn = min(128, NCB - n0)
                ntok = 16 * nn + 16
                for wi, xd, xkey in [(0, self.kcT_d, "kcT_d"), (1, self.vcT_d, "vcT_d")]:
                    LD(XT, XT.t[:, 0:ntok], xd[:, 16 * n0:16 * n0 + ntok],
                       reads=[(xkey, j) for j in range((16 * n0) // 128, min(NT, (16 * n0 + ntok + 127) // 128))])
                    xv = XT.t[:, 0:ntok].rearrange("p (n s) -> p n s", s=16)
                    for g in range(2):
                        for i_ in range(32):
                            a, b = i_ // 16, i_ % 16
                            PE(lambda: nc.tensor.matmul(out=ph.t[:, 0:nn], lhsT=W1[wi].t[g * 64:(g + 1) * 64, i_, :],
                                                        rhs=xv[g * 64:(g + 1) * 64, a:a + nn, b], start=(i_ == 0), stop=(i_ == 31)),
                               [W1[wi], XT], [ph])
                        AC(lambda: nc.scalar.activation(out=hb.t[:, 0:nn], in_=ph.t[:, 0:nn], func=AF.Identity, bias=cbv[wi].t[:, 0:1]), [ph, cbv[wi]], [hb])
                        V(lambda: nc.vector.tensor_tensor(out=tt.t[:, 0:nn], in0=hb.t[:, 0:nn], in1=hb.t[:, 0:nn], op=ALU.mult), [hb], [tt])
                        V(lambda: nc.vector.tensor_scalar(out=tt.t[:, 0:nn], in0=tt.t[:, 0:nn], scalar1=0.044715, scalar2=1.0, op0=ALU.mult, op1=ALU.add), [tt], [tt])
                        V(lambda: nc.vector.tensor_tensor(out=tt.t[:, 0:nn], in0=tt.t[:, 0:nn], in1=hb.t[:, 0:nn], op=ALU.mult), [tt, hb], [tt])
                        AC(lambda: nc.scalar.activation(out=tt.t[:, 0:nn], in_=tt.t[:, 0:nn], func=AF.Tanh, scale=0.7978845608028654), [tt], [tt])
                        V(lambda: nc.vector.tensor_scalar(out=tt.t[:, 0:nn], in0=tt.t[:, 0:nn], scalar1=0.5, scalar2=0.5, op0=ALU.mult, op1=ALU.add), [tt], [tt])
                        if nn < 128:
                            V(lambda: nc.vector.memset(GT[g].t[:], 0.0), [], [GT[g]])
                        V(lambda: nc.vector.tensor_tensor(out=GT[g].t[:, 0:nn], in0=tt.t[:, 0:nn], in1=hb.t[:, 0:nn], op=ALU.mult), [tt, hb], [GT[g]])
                    if wi == 0:
                        for g in range(2):
                            PE(lambda: nc.tensor.matmul(out=pc.t[:, 0:nn], lhsT=W2p.t[:, g, :], rhs=GT[g].t[:, 0:nn], start=(g == 0), stop=(g == 1)),
                               [W2p, GT[g]], [pc])
                        AC(lambda: nc.scalar.copy(out=KCT.t[:, n0:n0 + nn], in_=pc.t[:, 0:nn]), [pc], [KCT])
                    else:
                        for g in range(2):
                            PE(lambda: nc.tensor.matmul(out=pc.t[:, g * 64:(g + 1) * 64], lhsT=GT[g].t[:], rhs=W2v.t[:], start=True, stop=True),
                               [GT[g], W2v], [pc])
                        AC(lambda: nc.scalar.copy(out=RC.t[:, c, :, 0:64], in_=pc.t[:].rearrange("p (g d) -> p g d", g=2)), [pc], [RC])
            ovs = Tn(e2, "ovs", [128, NCC, NS], BF16)
            LD(ovs, ovs.t[:], self.c_ovl[:, :, :])
            for g in range(2):
                V(lambda: nc.vector.tensor_copy(out=RC.t[:, :, g, 65:65 + NS], in_=ovs.t[:]), [ovs, RC], [RC])
                V(lambda: nc.vector.memset(RC.t[:, :, g, 64:65], 1.0), [RC], [RC])
            kb.barrier()

        with ExitStack() as e3:
            def T3(name, shape, dt=F32):
                return Tn(e3, name, shape, dt)
            LA = 2
            Ew = T3("Ew", [128, S], BF16); cmk = T3("cmk", [128, 17, 128], BF16)
            Fw = T3("Fw", [128, 2 * NS - 2]); Kw = T3("Kw", [128, 2 * NS - 2])
            trib = T3("trib", [128, 128], BF16); sltb = T3("sltb", [128, 128], BF16)
            LD(Ew, Ew.t[:], self.c_Ew[:, :]); LD(cmk, cmk.t[:], self.c_cmask[:, :, :])
            LD(Fw, Fw.t[:], self.c_Fw[:, :]); LD(Kw, Kw.t[:], self.c_Kw[:, :])
            LD(trib, trib.t[:], self.c_trib[:, :]); LD(sltb, sltb.t[:], self.c_sltb[:, :])
            qz = [[T3(f"qz{p_}{g}", [128, 2, 128], BF16) for g in range(2)] for p_ in range(2)]
            pe_ = [T3(f"pe{j}", [128, 2, 128], BF16) for j in range(LA + 1)]
            rl = T3("rl", [128, 1]); wgt = T3("wgt", [128, 1])
            IMP = [T3(f"IMP{g}", [128, NS]) for g in range(2)]; impm = [T3(f"impm{g}", [128, NS]) for g in range(2)]
            wk = [T3(f"wk{g}", [128, NS]) for g in range(2)]
            m8a = [T3(f"m8a{g}", [128, 8]) for g in range(2)]; m8b = [T3(f"m8b{g}", [128, 8]) for g in range(2)]
            sneg = [T3(f"sneg{g}", [128, NS], BF16) for g in range(2)]
            SNT = [T3(f"SNT{g}", [128, 2, 128], BF16) for g in range(2)]
            oacc = [T3(f"oacc{j}", [128, 4, 64]) for j in range(2)]
            sq = T3("sq", [128, 256]); ss = T3("ss", [128, 4]); mo = [T3(f"mo{j}", [128, 256], BF16) for j in range(2)]
            psc = [Tile(self.ps(e3, f"nsa_psc{j}", [128, 2, 128]), f"nsa_psc{j}") for j in range(LA + 1)]
            pcv = [Tile(self.ps(e3, f"nsa_pcv{j}", [128, 65 + NS]), f"nsa_pcv{j}") for j in range(2)]
            pos_ = [Tile(self.ps(e3, f"nsa_pos{j}", [128, 65]), f"nsa_pos{j}") for j in range(2)]
            pst = Tile(self.ps(e3, "nsa_pst", [128, 128], BF16), "nsa_pst")
            for g in range(2):
                V(lambda: nc.vector.memset(SNT[g].t[:], 0.0), [], [SNT[g]])
            nsc = [0]
            pending = []

            def emit_score(job):
                j = nsc[0] % (LA + 1)
                nsc[0] += 1
                q_ = job["q"]
                extra = job.get("extra")
                PE(lambda: nc.tensor.matmul(out=psc[j].t[:], lhsT=job["lhsT"], rhs=q_.t[:], start=True, stop=(extra is None)),
                   job["lt"] + [q_], [psc[j]])
                if extra is not None:
                    sn = job["snt"]
                    PE(lambda: nc.tensor.matmul(out=psc[j].t[:], lhsT=extra, rhs=sn.t[:], start=False, stop=True), [Ew, sn], [psc[j]])
                AC(lambda: nc.scalar.activation(out=pe_[j].t[:], in_=psc[j].t[:], func=AF.Exp, scale=0.125), [psc[j]], [pe_[j]])
                if job.get("mask") is not None:
                    mt_, mk = job["mask"]
                    GP(lambda: nc.gpsimd.tensor_tensor(out=pe_[j].t[:], in0=pe_[j].t[:], in1=mk.unsqueeze(1).to_broadcast([128, 2, 128]), op=ALU.mult),
                       [pe_[j], mt_], [pe_[j]])
                job["pe"] = pe_[j]

            def emit_pv(job):
                pt = job["pe"]
                for r in range(2):
                    tgt = job["tgt"][r]
                    PE(lambda: nc.tensor.matmul(out=tgt.t[:], lhsT=pt.t[:, r, :], rhs=job["rhs"], start=job["start"], stop=job["stop"]),
                       [pt] + job["vt"], [tgt])
                if job.get("after") is not None:
                    job["after"]()

            def push(job):
                emit_score(job)
                pending.append(job)
                while len(pending) > LA:
                    emit_pv(pending.pop(0))

            def finish(pt, oa, h, b, i, first):
                V(lambda: nc.vector.tensor_scalar(out=rl.t[:], in0=pt.t[:, 64:65], scalar1=1e-30, scalar2=None, op0=ALU.max), [pt], [rl])
                V(lambda: nc.vector.reciprocal(out=rl.t[:], in_=rl.t[:]), [rl], [rl])
                V(lambda: nc.vector.tensor_tensor(out=wgt.t[:], in0=rl.t[:], in1=GA.t[:, i, 3 * h + b:3 * h + b + 1], op=ALU.mult),
                  [rl, Tile(None, f"nsa_GA{i}")], [wgt])
                if first:
                    V(lambda: nc.vector.tensor_scalar(out=oa.t[:, h, :], in0=pt.t[:, 0:64], scalar1=wgt.t[:], scalar2=None, op0=ALU.mult), [pt, wgt], [oa])
                else:
                    V(lambda: nc.vector.scalar_tensor_tensor(out=oa.t[:, h, :], in0=pt.t[:, 0:64], scalar=wgt.t[:], in1=oa.t[:, h, :],
                                                             op0=ALU.mult, op1=ALU.add), [pt, wgt, oa], [oa])

            def after_cmp(i, g, oa):
                def f():
                    for r in range(2):
                        finish(pcv[r], oa, 2 * g + r, 0, i, True)
                        if r == 0:
                            V(lambda: nc.vector.tensor_scalar(out=IMP[g].t[:], in0=pcv[r].t[:, 65:65 + NS], scalar1=rl.t[:], scalar2=None, op0=ALU.mult),
                              [pcv[r], rl], [IMP[g]])
                        else:
                            V(lambda: nc.vector.scalar_tensor_tensor(out=IMP[g].t[:], in0=pcv[r].t[:, 65:65 + NS], scalar=rl.t[:], in1=IMP[g].t[:],
                                                                     op0=ALU.mult, op1=ALU.add), [pcv[r], rl, IMP[g]], [IMP[g]])
                    fo = NS - 2 - 2 * i
                    V(lambda: nc.vector.tensor_tensor(out=impm[g].t[:], in0=IMP[g].t[:], in1=Kw.t[:, fo:fo + NS], op=ALU.mult), [IMP[g], Kw], [impm[g]])
                    V(lambda: nc.vector.tensor_tensor(out=impm[g].t[:], in0=impm[g].t[:], in1=Fw.t[:, fo:fo + NS], op=ALU.add), [impm[g], Fw], [impm[g]])
                    V(lambda: nc.vector.memset(impm[g].t[:, 0:1], 1e30), [impm[g]], [impm[g]])
                    V(lambda: nc.vector.max(out=m8a[g].t[:], in_=impm[g].t[:]), [impm[g]], [m8a[g]])
                    V(lambda: nc.vector.match_replace(out=wk[g].t[:], in_to_replace=m8a[g].t[:], in_values=impm[g].t[:], imm_value=-3e38),
                      [m8a[g], impm[g]], [wk[g]])
                    V(lambda: nc.vector.max(out=m8b[g].t[:], in_=wk[g].t[:]), [wk[g]], [m8b[g]])
                    V(lambda: nc.vector.tensor_scalar(out=sneg[g].t[:], in0=impm[g].t[:], scalar1=m8b[g].t[:, 7:8], scalar2=1.0, op0=ALU.is_ge, op1=ALU.subtract),
                      [impm[g], m8b[g]], [sneg[g]])
                return f

            def after_fin(tgt, i, g, b, oa):
                def f():
                    for r in range(2):
                        finish(tgt[r], oa, 2 * g + r, b, i, False)
                return f

            def tail(i, oa):
                def f():
                    s = i % 2
                    t0 = i * 128
                    o2 = oa.t[:].rearrange("p h d -> p (h d)")
                    AC(lambda: nc.scalar.activation(out=sq.t[:], in_=o2, func=AF.Square), [oa], [sq])
                    V(lambda: nc.vector.tensor_reduce(out=ss.t[:], in_=sq.t[:].rearrange("p (h d) -> p h d", h=4), axis=AX.X, op=ALU.add), [sq], [ss])
                    V(lambda: nc.vector.tensor_scalar(out=ss.t[:], in0=ss.t[:], scalar1=1.0 / 64, scalar2=EPS, op0=ALU.mult, op1=ALU.add), [ss], [ss])
                    AC(lambda: nc.scalar.activation(out=ss.t[:], in_=ss.t[:], func=AF.Sqrt), [ss], [ss])
                    V(lambda: nc.vector.reciprocal(out=ss.t[:], in_=ss.t[:]), [ss], [ss])
                    V(lambda: nc.vector.tensor_tensor(out=sq.t[:].rearrange("p (h d) -> p h d", h=4), in0=oa.t[:],
                                                      in1=ss.t[:].unsqueeze(2).to_broadcast([128, 4, 64]), op=ALU.mult), [oa, ss, sq], [sq])
                    V(lambda: nc.vector.tensor_tensor(out=mo[s].t[:], in0=sq.t[:], in1=nwb.t[:], op=ALU.mult), [sq, nwb], [mo[s]])
                    kb.dma("sp", mo[s].k, self.mix[t0:t0 + 128, 256:512], mo[s].t[:], reads=[mo[s].k], writes=[("mix_nsa", i)])
                return f

            for i in range(NT):
                qq = qz[i % 2]
                oa = oacc[i % 2]
                for g in range(2):
                    AC(lambda: nc.scalar.copy(out=qq[g].t[:], in_=QT.t[:, i, :, :]), [Tile(None, f"nsa_QT{i}")], [qq[g]])
                    o0 = (1 - g) * 64
                    GP(lambda: nc.gpsimd.memset(qq[g].t[o0:o0 + 64, :, :], 0.0), [qq[g]], [qq[g]])
                ncs = min(NCC, i // 16 + 1)
                k0 = max(0, i - 4)
                for g in range(2):
                    for c in range(ncs):
                        dl = i - 16 * c
                        push(dict(lhsT=KCT.t[:, c * 128:(c + 1) * 128], lt=[KCT], q=qq[g],
                                  mask=(cmk, cmk.t[:, dl, :]) if dl <= 16 else None,
                                  tgt=pcv, rhs=RC.t[:, c, g, :], vt=[RC], start=(c == 0), stop=(c == ncs - 1),
                                  after=after_cmp(i, g, oa) if c == ncs - 1 else None))
                    for kt in range(k0, i + 1):
                        mf = (trib, trib.t[:]) if kt == i else ((sltb, sltb.t[:]) if kt == i - 4 else None)
                        push(dict(lhsT=KwT.t[:, kt * 128:(kt + 1) * 128], lt=[Tile(None, f"nsa_KwT{kt}")], q=qq[g], mask=mf,
                                  tgt=[Tile(pcv[0].t[:, 0:65], pcv[0].k), Tile(pcv[1].t[:, 0:65], pcv[1].k)],
                                  rhs=VwA.t[:, kt, g, :], vt=[Tile(None, f"nsa_VwA{kt}")], start=(kt == k0), stop=(kt == i),
                                  after=after_fin(pcv, i, g, 2, oa) if kt == i else None))
                for g in range(2):
                    PE(lambda: nc.tensor.transpose(out=pst.t[0:NS, :], in_=sneg[g].t[:], identity=idb.t[:]), [sneg[g], idb], [pst])
                    for r in range(2):
                        AC(lambda: nc.scalar.copy(out=SNT[g].t[0:NS, r, :], in_=pst.t[0:NS, :]), [pst], [SNT[g]])
                    for kt in range(i + 1):
                        last = (kt == i)
                        aft = None
                        if last:
                            fin = after_fin(pos_, i, g, 1, oa)
                            if g == 1:
                                tl_ = tail(i, oa)
                                aft = (lambda fin=fin, tl_=tl_: (fin(), tl_()))
                            else:
                                aft = fin
                        push(dict(lhsT=KsT.t[:, kt * 128:(kt + 1) * 128], lt=[Tile(None, f"nsa_KsT{kt}")], q=qq[g],
                                  mask=(trib, trib.t[:]) if last else None, extra=Ew.t[:, kt * 128:(kt + 1) * 128], snt=SNT[g],
                                  tgt=pos_, rhs=VsA.t[:, kt, g, :], vt=[Tile(None, f"nsa_VsA{kt}")], start=(kt == 0), stop=last, after=aft))
            while pending:
                emit_pv(pending.pop(0))


Prog.stage_nsa = _stage_nsa


def build_full(S, L=2):
    p = Prog(S, L=L, dbg=[("out", [S, D])])
    kb = p.kb
    with kb.es:
        h = p.x
        for l in range(L):
            p.stage_d1(l, h)
            kb.barrier()
            p.stage_ssd(l)
            kb.barrier()
            p.stage_gla(l)
            kb.barrier()
            p.stage_nsa(l)
            kb.barrier()
            final = (l == L - 1)
            p.stage_d2(l, h, p.outs["out"] if final else p.hbuf, final)
            kb.barrier()
            h = p.hbuf
        kb.drain("sp")
    return p


_WNAMES = ["norm1_w", "w_in", "gla_gate_w2", "gla_gate_b", "gla_norm_w", "nsa_cmp_pos_k", "nsa_cmp_w1_k", "nsa_cmp_w2_k",
           "nsa_cmp_pos_v", "nsa_cmp_w1_v", "nsa_cmp_w2_v", "nsa_norm_w", "ssd_conv_w", "ssd_conv_b", "ssd_dt_bias",
           "ssd_a_log", "ssd_d", "ssd_norm_w", "w_out", "norm2_w", "w_up", "w_down", "final_norm_w"]


def kernel(**inputs):
    x = np.asarray(inputs["x"], dtype=np.float32)
    B, S, _ = x.shape
    L = int(np.asarray(inputs["w_in"]).shape[0])
    p = build_full(S, L)
    base = {k: np.ascontiguousarray(np.asarray(inputs[k], dtype=np.float32)) for k in _WNAMES}
    base.update(consts(S))
    in_maps = []
    for b in range(B):
        m = dict(base)
        m["x"] = np.ascontiguousarray(x[b])
        in_maps.append(m)
    res = run_bass_kernel_spmd(p.nc, in_maps, core_ids=list(range(B)))
    return np.stack([np.asarray(res.results[b]["out"], dtype=np.float32) for b in range(B)], axis=0)
```

```python
import numpy as np
import ml_dtypes
import concourse.bass as bass
import concourse.mybir as mybir
from concourse.bass_utils import run_bass_kernel_spmd
from contextlib import ExitStack
import threading

F32 = mybir.dt.float32
BF16 = mybir.dt.bfloat16
AF = mybir.ActivationFunctionType
ALU = mybir.AluOpType
AX = mybir.AxisListType

D = 1024
DIN = 3364
DFF = 4096
NPROJ = 2340
EPS = 1e-6

C_GQ, C_GK, C_GV, C_GLR, C_GR = 0, 128, 256, 512, 528
C_NQ, C_KCMP, C_KSLC, C_KWIN, C_VCMP, C_VSLC, C_VWIN, C_NG = 784, 1040, 1168, 1296, 1424, 1552, 1680, 1808
C_Z, C_DT = 1820, 2332
WMAP = [(0, 784, 0), (784, 64, 784), (912, 64, 848), (848, 64, 912), (976, 64, 976), (1040, 128, 1040), (1296, 128, 1168), (1552, 128, 1296), (1168, 128, 1424),
        (1424, 128, 1552), (1680, 652, 1680), (3356, 8, 2332), (2332, 1024, -1)]


class KB:
    EPOCH = 30000

    def __init__(self, nc, same_engine_sync=True):
        self.nc = nc
        self.es = ExitStack()
        self.eng = {"pe": nc.tensor, "act": nc.scalar, "dve": nc.vector,
                    "pool": nc.gpsimd, "sp": nc.sync}
        self.esem = {}
        self.ecnt = {}
        self.nsem = 0
        self.dsem = {}
        self.waited = {e: {} for e in self.eng}
        self.last_w = {}
        self.readers = {}
        self.same = same_engine_sync
        self.sem_owner = {}
        self.ninst = 0
        self.limit = None
        self.hook = None
        for e in self.eng:
            self._new_esem(e)

    def _sem(self, name):
        self.nsem += 1
        return self.es.enter_context(self.nc.semaphore(f"s{self.nsem}_{name}"))

    def _new_esem(self, e):
        s = self._sem(e)
        self.esem[e] = s
        self.ecnt[e] = 0
        self.sem_owner[id(s)] = e

    def _wait(self, e, deps):
        best = {}
        for item in deps:
            if item is None:
                continue
            if len(item) == 3:
                s, v, raw = item
            else:
                (s, v), raw = item, True
            owner = self.sem_owner.get(id(s))
            if owner == e and (e == "pe" or not self.same):
                continue
            if best.get(id(s), (None, 0))[1] < v:
                best[id(s)] = (s, v)
        for sid, (s, v) in best.items():
            if self.waited[e].get(sid, 0) < v:
                self.eng[e].wait_ge(s, v)
                self.waited[e][sid] = v

    def _deps(self, reads, writes):
        deps = []
        for k in reads:
            ev = self.last_w.get(k)
            if ev is not None:
                deps.append((ev[0], ev[1], True))
        for k in writes:
            ev = self.last_w.get(k)
            if ev is not None:
                deps.append((ev[0], ev[1], False))
            for ev in self.readers.get(k, []):
                deps.append((ev[0], ev[1], False))
        return deps

    def _commit(self, ev, reads, writes):
        for k in writes:
            self.last_w[k] = ev
            self.readers[k] = []
        for k in reads:
            if k in writes:
                continue
            self.readers.setdefault(k, []).append(ev)

    def op(self, e, fn, reads=(), writes=()):
        if self.hook is not None:
            self.hook()
        if self.limit is not None and self.ninst >= self.limit:
            return None
        self._wait(e, self._deps(reads, writes))
        inst = fn()
        if self.ecnt[e] >= self.EPOCH:
            self._new_esem(e)
        self.ecnt[e] += 1
        s = self.esem[e]
        inst.then_inc(s, 1)
        self._commit((s, self.ecnt[e]), reads, writes)
        self.ninst += 1
        return inst

    def dma(self, q, key, out, in_, reads=(), writes=(), **kw):
        if self.hook is not None:
            self.hook()
        if self.limit is not None and self.ninst >= self.limit:
            return None
        self._wait(q, self._deps(reads, writes))
        if key not in self.dsem:
            self.dsem[key] = [self._sem("d"), 0]
        ent = self.dsem[key]
        inst = self.eng[q].dma_start(out=out, in_=in_, **kw)
        ent[1] += 16
        inst.then_inc(ent[0], 16)
        self._commit((ent[0], ent[1]), reads, writes)
        self.ninst += 1
        return inst

    def barrier(self):
        deps = list(self.last_w.values())
        for r in self.readers.values():
            deps.extend(r)
        for e in self.eng:
            self._wait(e, deps)
        self.readers = {k: [] for k in self.readers}

    def drain(self, e="sp"):
        deps = list(self.last_w.values())
        for r in self.readers.values():
            deps.extend(r)
        self._wait(e, deps)


def interleave(kb, fa, fb):
    sem = {"a": threading.Semaphore(0), "b": threading.Semaphore(0)}
    done = {"a": False, "b": False}
    err = []
    loc = threading.local()

    def hook():
        me = loc.me
        other = "b" if me == "a" else "a"
        if done[other]:
            return
        sem[other].release()
        sem[me].acquire()

    def run(me, f):
        loc.me = me
        other = "b" if me == "a" else "a"
        sem[me].acquire()
        try:
            f()
        except BaseException as ex:
            err.append(ex)
        done[me] = True
        sem[other].release()

    kb.hook = hook
    ta = threading.Thread(target=run, args=("a", fa))
    tb = threading.Thread(target=run, args=("b", fb))
    ta.start()
    tb.start()
    sem["a"].release()
    ta.join()
    tb.join()
    kb.hook = None
    if err:
        raise err[0]


class Prog:
    def __init__(self, S, L=2, dbg=()):
        self.S = S
        self.NT = S // 128
        self.L = L
        self.dbg = dbg
        nc = self.nc = bass.Bass("TRN2", target_bir_lowering=False)
        self.kb = KB(nc)
        Ld = L

        def inp(name, shape, dt=F32):
            return nc.dram_tensor(name, list(shape), dt, kind="ExternalInput").ap()

        def scr(name, shape, dt=F32):
            return nc.dram_tensor(name, list(shape), dt, kind="Internal").ap()

        self.x = inp("x", [S, D])
        self.norm1_w = inp("norm1_w", [Ld, D])
        self.w_in = inp("w_in", [Ld, D, DIN])
        self.gla_gate_w2 = inp("gla_gate_w2", [Ld, 16, 128])
        self.gla_gate_b = inp("gla_gate_b", [Ld, 128])
        self.gla_norm_w = inp("gla_norm_w", [Ld, 256])
        self.nsa_cmp_pos_k = inp("nsa_cmp_pos_k", [Ld, 32, 64])
        self.nsa_cmp_w1_k = inp("nsa_cmp_w1_k", [Ld, 2048, 128])
        self.nsa_cmp_w2_k = inp("nsa_cmp_w2_k", [Ld, 128, 64])
        self.nsa_cmp_pos_v = inp("nsa_cmp_pos_v", [Ld, 32, 64])
        self.nsa_cmp_w1_v = inp("nsa_cmp_w1_v", [Ld, 2048, 128])
        self.nsa_cmp_w2_v = inp("nsa_cmp_w2_v", [Ld, 128, 64])
        self.nsa_norm_w = inp("nsa_norm_w", [Ld, 256])
        self.ssd_conv_w = inp("ssd_conv_w", [Ld, 4, 1024])
        self.ssd_conv_b = inp("ssd_conv_b", [Ld, 1024])
        self.ssd_dt_bias = inp("ssd_dt_bias", [Ld, 8])
        self.ssd_a_log = inp("ssd_a_log", [Ld, 8])
        self.ssd_d = inp("ssd_d", [Ld, 8])
        self.ssd_norm_w = inp("ssd_norm_w", [Ld, 512])
        self.w_out = inp("w_out", [Ld, D, D])
        self.norm2_w = inp("norm2_w", [Ld, D])
        self.w_up = inp("w_up", [Ld, D, DFF])
        self.w_down = inp("w_down", [Ld, DFF, D])
        self.final_norm_w = inp("final_norm_w", [D])
        self.ident_f = inp("ident_f", [128, 128])
        self.ident_b = inp("ident_b", [128, 128], BF16)
        self.c_tri = inp("c_tri", [128, 128])
        self.c_slt = inp("c_slt", [128, 128])
        self.c_ones = inp("c_ones", [128, 128])
        self.c_hm = inp("c_hm", [128, 4])
        self.c_bd = inp("c_bd", [128, 256])
        self.mix = scr("mix", [S, D], BF16)
        NS = S // 64
        self.NS = NS
        self.NCC = max(1, (S // 16 - 1 + 127) // 128)
        self.rope_cos = inp("rope_cos", [S, 32])
        self.rope_sin = inp("rope_sin", [S, 32])
        self.c_Ew = inp("c_Ew", [128, S], BF16)
        self.c_cmask = inp("c_cmask", [128, 17, 128], BF16)
        self.c_ovl = inp("c_ovl", [128, self.NCC, NS], BF16)
        self.c_Fw = inp("c_Fw", [128, 2 * NS - 2])
        self.c_Kw = inp("c_Kw", [128, 2 * NS - 2])
        self.c_trib = inp("c_trib", [128, 128], BF16)
        self.c_sltb = inp("c_sltb", [128, 128], BF16)
        self.kcT_d = scr("kcT_d", [128, S + 128], BF16)
        self.vcT_d = scr("vcT_d", [128, S + 128], BF16)
        self.proj = scr("proj", [S, NPROJ])
        self.xbcT = scr("xbcT", [D, S])
        self.hbuf = scr("hbuf", [S, D])
        self.outs = {}
        for name, shape in dbg:
            self.outs[name] = nc.dram_tensor(name, list(shape), F32, kind="ExternalOutput").ap()

    def sb(self, es, name, shape, dt=F32):
        self._uid = getattr(self, "_uid", 0) + 1
        return es.enter_context(self.nc.sbuf_tensor(f"{name}_u{self._uid}", list(shape), dt))

    def ps(self, es, name, shape, dt=F32):
        full = 512 if dt == F32 else 1024
        self._uid = getattr(self, "_uid", 0) + 1
        t = es.enter_context(self.nc.psum_tensor(f"{name}_u{self._uid}", [128, full], dt))
        n = 1
        for d in shape[1:]:
            n *= d
        assert n <= full
        v = t[0:shape[0], 0:n]
        if len(shape) == 3:
            v = v.rearrange("p (a b) -> p a b", a=shape[1])
        return v

    def stage_d1(self, l, hsrc):
        nc, kb, NT = self.nc, self.kb, self.NT
        with ExitStack() as es:
            Wtm = self.sb(es, "d1_Wtm", [128, 8, NPROJ], BF16)
            Wx = self.sb(es, "d1_Wx", [128, 8, 1024], BF16)
            stg = [self.sb(es, f"d1_stg{i}", [128, DIN]) for i in range(2)]
            nw = self.sb(es, "d1_nw", [128, 8])
            idb = self.sb(es, "d1_idb", [128, 128], BF16)
            ht = [self.sb(es, f"d1_ht{i}", [128, D]) for i in range(2)]
            junk = self.sb(es, "d1_junk", [128, D], BF16)
            ss = [self.sb(es, f"d1_ss{i}", [128, 1]) for i in range(2)]
            rs = [self.sb(es, f"d1_rs{i}", [128, 1]) for i in range(2)]
            u = [self.sb(es, f"d1_u{i}", [128, D], BF16) for i in range(2)]
            uT = [self.sb(es, f"d1_uT{i}", [128, 8, 128], BF16) for i in range(2)]
            ot = [self.sb(es, f"d1_ot{i}", [128, NPROJ]) for i in range(2)]
            xo = [self.sb(es, f"d1_xo{i}", [128, 8, 128]) for i in range(2)]
            pT = self.ps(es, "d1_pT", [128, 8, 128], BF16)
            pm = [self.ps(es, f"d1_pm{i}", [128, 512]) for i in range(3)]
            px = [self.ps(es, f"d1_px{i}", [128, 4, 128]) for i in range(2)]

            kb.dma("sp", "d1_idb", idb[:], self.ident_b[:, :], writes=["d1_idb"])
            for c in range(8):
                kb.dma("sp", "d1_nw", nw[:, c:c + 1],
                       self.norm1_w[l, c * 128:(c + 1) * 128].rearrange("(p o) -> p o", o=1),
                       writes=["d1_nw"])
            for c in range(8):
                s = c % 2
                kb.dma("sp", f"d1_stg{s}", stg[s][:], self.w_in[l, c * 128:(c + 1) * 128, :],
                       writes=[f"d1_stg{s}"])
                for j, (so, n, do) in enumerate(WMAP):
                    dst = Wx[:, c, 0:n] if do < 0 else Wtm[:, c, do:do + n]
                    wk = f"d1_W{c}"
                    if j % 2 == 0:
                        kb.op("dve", lambda: nc.vector.tensor_scalar(
                            out=dst, in0=stg[s][:, so:so + n], scalar1=nw[:, c:c + 1], scalar2=None,
                            op0=ALU.mult), reads=[f"d1_stg{s}", "d1_nw"], writes=[wk + f"_{j}"])
                    else:
                        kb.op("act", lambda: nc.scalar.activation(
                            out=dst, in_=stg[s][:, so:so + n], func=AF.Copy, scale=nw[:, c:c + 1]),
                            reads=[f"d1_stg{s}", "d1_nw"], writes=[wk + f"_{j}"])
            Wkeys = [f"d1_W{c}_{j}" for c in range(8) for j in range(len(WMAP))]

            def A(i):
                s = i % 2
                kb.dma("sp", f"d1_ht{s}", ht[s][:], hsrc[i * 128:(i + 1) * 128, :],
                       reads=[("h", i)], writes=[f"d1_ht{s}"])
                kb.op("act", lambda: nc.scalar.activation(out=junk[:], in_=ht[s][:], func=AF.Square,
                                                          accum_out=ss[s][:]),
                      reads=[f"d1_ht{s}"], writes=["d1_junk", f"d1_ss{s}"])
                kb.op("dve", lambda: nc.vector.tensor_scalar(out=rs[s][:], in0=ss[s][:], scalar1=1.0 / D,
                                                             scalar2=EPS, op0=ALU.mult, op1=ALU.add),
                      reads=[f"d1_ss{s}"], writes=[f"d1_rs{s}"])
                kb.op("act", lambda: nc.scalar.activation(out=rs[s][:], in_=rs[s][:], func=AF.Sqrt),
                      reads=[f"d1_rs{s}"], writes=[f"d1_rs{s}"])
                kb.op("dve", lambda: nc.vector.reciprocal(out=rs[s][:], in_=rs[s][:]),
                      reads=[f"d1_rs{s}"], writes=[f"d1_rs{s}"])
                kb.op("dve", lambda: nc.vector.tensor_scalar(out=u[s][:], in0=ht[s][:], scalar1=rs[s][:],
                                                             scalar2=None, op0=ALU.mult),
                      reads=[f"d1_ht{s}", f"d1_rs{s}"], writes=[f"d1_u{s}"])
                for c in range(8):
                    kb.op("pe", lambda: nc.tensor.transpose(out=pT[:, c, :], in_=u[s][:, c * 128:(c + 1) * 128],
                                                            identity=idb[:]),
                          reads=[f"d1_u{s}", "d1_idb"], writes=["d1_pT"])
                kb.op("act", lambda: nc.scalar.copy(out=uT[s][:], in_=pT[:]),
                      reads=["d1_pT"], writes=[f"d1_uT{s}"])

            def B(i):
                s = i % 2
                off = 0
                k = 0
                while off < NPROJ:
                    n = min(512, NPROJ - off)
                    p = pm[k % 3]
                    pk = f"d1_pm{k % 3}"
                    for c in range(8):
                        kb.op("pe", lambda: nc.tensor.matmul(out=p[:, 0:n], lhsT=uT[s][:, c, :],
                                                             rhs=Wtm[:, c, off:off + n],
                                                             start=(c == 0), stop=(c == 7)),
                              reads=[f"d1_uT{s}"] + (Wkeys if i == 0 and c == 0 and k == 0 else []),
                              writes=[pk])
                    if k % 2 == 0:
                        kb.op("dve", lambda: nc.vector.tensor_copy(out=ot[s][:, off:off + n], in_=p[:, 0:n]),
                              reads=[pk], writes=[f"d1_ot{s}_{k}"])
                    else:
                        kb.op("act", lambda: nc.scalar.copy(out=ot[s][:, off:off + n], in_=p[:, 0:n]),
                              reads=[pk], writes=[f"d1_ot{s}_{k}"])
                    off += n
                    k += 1
                kb.dma("sp", f"d1_ot{s}", self.proj[i * 128:(i + 1) * 128, :], ot[s][:],
                       reads=[f"d1_ot{s}_{j}" for j in range(k)], writes=[("proj", i)])
                for half in range(2):
                    p = px[half]
                    pk = f"d1_px{half}"
                    for j in range(4):
                        cc = half * 4 + j
                        for c in range(8):
                            kb.op("pe", lambda: nc.tensor.matmul(out=p[:, j, :], lhsT=Wx[:, c, cc * 128:(cc + 1) * 128],
                                                                 rhs=uT[s][:, c, :],
                                                                 start=(c == 0), stop=(c == 7)),
                                  reads=[f"d1_uT{s}"], writes=[pk])
                    if half == 0:
                        kb.op("dve", lambda: nc.vector.tensor_copy(out=xo[s][:, 0:4, :], in_=p[:]),
                              reads=[pk], writes=[f"d1_xo{s}_0"])
                    else:
                        kb.op("act", lambda: nc.scalar.copy(out=xo[s][:, 4:8, :], in_=p[:]),
                              reads=[pk], writes=[f"d1_xo{s}_1"])
                kb.dma("sp", f"d1_xo{s}",
                       self.xbcT.rearrange("(cc p) t -> p cc t", p=128)[:, :, i * 128:(i + 1) * 128],
                       xo[s][:], reads=[f"d1_xo{s}_0", f"d1_xo{s}_1"], writes=[("xbcT", i)])

            A(0)
            for i in range(NT):
                if i + 1 < NT:
                    interleave(kb, (lambda i=i: B(i)), (lambda i=i: A(i + 1)))
                else:
                    B(i)

    def dump(self, name, src_ap):
        kb = self.kb
        kb.drain("sp")
        kb.dma("sp", "dump_" + name, self.outs[name], src_ap, writes=[("dump", name)])

    def finish(self):
        self.kb.drain("sp")
        self.kb.es.close()


def build_test_d1(S):
    p = Prog(S, L=1, dbg=[("o_proj", [S, NPROJ]), ("o_xbcT", [D, S])])
    with p.kb.es:
        p.stage_d1(0, p.x)
        p.dump("o_proj", p.proj[:, :])
        p.dump("o_xbcT", p.xbcT[:, :])
        p.kb.drain("sp")
    return p


def consts(S):
    r = np.arange(128)
    NS = S // 64
    NCC = max(1, (S // 16 - 1 + 127) // 128)
    pos = np.arange(S, dtype=np.float32)
    inv = (np.float32(10000.0) ** (-np.arange(32, dtype=np.float32) / np.float32(32))).astype(np.float32)
    ang = (pos[:, None] * inv[None, :]).astype(np.float32)
    Ew = np.zeros((128, S), np.float32)
    cc = np.arange(S)
    Ew[cc // 64 % 128, cc] = 30000.0
    cm = np.zeros((128, 17, 128), np.float32)
    for d_ in range(17):
        cm[:, d_, :] = (128 * d_ + r[None, :] - 16 * r[:, None] >= 31)
    n_all = np.arange(NCC * 128)
    j_all = np.arange(NS)
    ov = ((16 * n_all[:, None] < 64 * j_all[None, :] + 64) & (16 * n_all[:, None] + 32 > 64 * j_all[None, :])).astype(np.float32)
    ov = ov.reshape(NCC, 128, NS).transpose(1, 0, 2)
    cw = np.arange(2 * NS - 2)
    rel = cw[None, :] - (NS - 2) - (r[:, None] >= 64)
    Fw = np.where(rel > 0, -1e30, np.where(rel >= -1, 1e30, 0.0)).astype(np.float32)
    Kw = ((rel < -1)).astype(np.float32)
    bf = ml_dtypes.bfloat16
    hm = np.zeros((128, 4), np.float32)
    bd = np.zeros((128, 256), np.float32)
    for h in range(4):
        hm[32 * h:32 * h + 32, h] = 1.0
        bd[32 * h:32 * h + 32, 64 * h:64 * h + 64] = 1.0
    return {
        "ident_f": np.eye(128, dtype=np.float32),
        "ident_b": np.eye(128, dtype=np.float32).astype(ml_dtypes.bfloat16),
        "c_tri": (r[:, None] <= r[None, :]).astype(np.float32),
        "c_slt": (r[:, None] > r[None, :]).astype(np.float32),
        "c_ones": np.ones((128, 128), np.float32),
        "c_hm": hm,
        "c_bd": bd,
        "rope_cos": np.cos(ang).astype(np.float32),
        "rope_sin": np.sin(ang).astype(np.float32),
        "c_Ew": Ew.astype(bf),
        "c_cmask": cm.astype(bf),
        "c_ovl": np.ascontiguousarray(ov).astype(bf),
        "c_Fw": Fw,
        "c_Kw": Kw,
        "c_trib": (r[:, None] <= r[None, :]).astype(np.float32).astype(bf),
        "c_sltb": (r[:, None] > r[None, :]).astype(np.float32).astype(bf),
    }


class Tile:
    def __init__(self, t, k):
        self.t = t
        self.k = k


def _stage_ssd(self, l):
    nc, kb, NT = self.nc, self.kb, self.NT
    with ExitStack() as es:
        def T(name, shape, dt=F32):
            return Tile(self.sb(es, "ssd_" + name, shape, dt), "ssd_" + name)

        def T2(name, shape, dt=F32):
            return [T(f"{name}{j}", shape, dt) for j in range(2)]

        def PT(name, shape, dt=F32):
            return Tile(self.ps(es, "ssd_" + name, shape, dt), "ssd_" + name)

        def V(fn, r, w):
            return kb.op("dve", fn, [t.k for t in r], [t.k for t in w])

        def AC(fn, r, w):
            return kb.op("act", fn, [t.k for t in r], [t.k for t in w])

        def PE(fn, r, w):
            return kb.op("pe", fn, [t.k for t in r], [t.k for t in w])

        def GP(fn, r, w):
            return kb.op("pool", fn, [t.k for t in r], [t.k for t in w])

        def LD(t, dst, src, reads=(), **kw):
            return kb.dma("sp", t.k, dst, src, reads=list(reads), writes=[t.k], **kw)

        cw = T("cw", [128, 4, 8]); cb = T("cb", [128, 8]); dtb = T("dtb", [128, 8])
        Aneg = T("Aneg", [128, 8]); dsk = T("dsk", [128, 8]); nwb = T("nwb", [128, 512])
        tri = T("tri", [128, 128]); slt = T("slt", [128, 128]); ones = T("ones", [128, 128])
        idf = T("idf", [128, 128]); idb = T("idb", [128, 128], BF16)
        hT = T("hT", [128, 512]); hTb = T("hTb", [128, 512], BF16)
        xh = T2("xh", [128, 8, 131]); zt = T2("zt", [128, 512]); dtt = T2("dtt", [128, 8])
        acc = T2("acc", [128, 8, 128])
        xsT = T2("xsT", [128, 4, 128])
        BT = T2("BT", [128, 2, 128], BF16); CT = T2("CT", [128, 2, 128], BF16)
        xtm = T2("xtm", [128, 512]); Btm = T2("Btm", [128, 2, 128], BF16)
        sp_a = T2("sp_a", [128, 8]); dts = T2("dts", [128, 8]); dA = T2("dA", [128, 8])
        acs = T2("acs", [128, 16]); eacs = T2("eacs", [128, 8]); dsd = T2("dsd", [128, 8]); cd = T2("cd", [128, 8])
        xdt = T2("xdt", [128, 512], BF16); xdd = T2("xdd", [128, 512], BF16)
        cbm = T2("cbm", [128, 2, 128])
        dAt = T("dAt", [128, 8, 128]); seg = T2("seg", [128, 4, 128]); MT = T2("MT", [128, 4, 128], BF16)
        yd = T2("yd", [128, 512]); y = T2("y", [128, 512]); sz = T2("sz", [128, 512])
        junk = T("junk", [128, 256]); ss = T2("ss", [128, 2]); mo = T2("mo", [128, 512], BF16)
        pxT = PT("pxT", [128, 512]); pBt = PT("pBt", [128, 2, 128], BF16); pacs = PT("pacs", [128, 16])
        pcb = PT("pcb", [128, 2, 128]); pD = PT("pD", [128, 4, 128]); py = PT("py", [128, 512])
        pyo = PT("pyo", [128, 512]); pU = PT("pU", [128, 512])

        for k in range(4):
            LD(cw, cw.t[:, k, :], self.ssd_conv_w[l, k, :].rearrange("(cc p) -> p cc", p=128),
               allow_slow_non_contiguous=True)
        LD(cb, cb.t[:], self.ssd_conv_b[l, :].rearrange("(cc p) -> p cc", p=128), allow_slow_non_contiguous=True)
        LD(dtb, dtb.t[:], self.ssd_dt_bias[l, :].partition_broadcast(128))
        LD(Aneg, Aneg.t[:], self.ssd_a_log[l, :].partition_broadcast(128))
        LD(dsk, dsk.t[:], self.ssd_d[l, :].partition_broadcast(128))
        LD(nwb, nwb.t[:], self.ssd_norm_w[l, :].partition_broadcast(128))
        LD(tri, tri.t[:], self.c_tri[:, :]); LD(slt, slt.t[:], self.c_slt[:, :]); LD(ones, ones.t[:], self.c_ones[:, :])
        LD(idf, idf.t[:], self.ident_f[:, :]); LD(idb, idb.t[:], self.ident_b[:, :])
        AC(lambda: nc.scalar.activation(out=Aneg.t[:], in_=Aneg.t[:], func=AF.Exp), [Aneg], [Aneg])
        V(lambda: nc.vector.tensor_scalar(out=Aneg.t[:], in0=Aneg.t[:], scalar1=-1.0, scalar2=None, op0=ALU.mult), [Aneg], [Aneg])
        V(lambda: nc.vector.memset(hT.t[:], 0.0), [], [hT])
        V(lambda: nc.vector.memset(hTb.t[:], 0.0), [], [hTb])
        for j in range(2):
            V(lambda: nc.vector.memset(xh[j].t[:], 0.0), [], [xh[j]])
        xv = self.xbcT.rearrange("(cc p) t -> p cc t", p=128)

        def front(i):
            s = i % 2
            t0 = i * 128
            if i == 0:
                kb.dma("sp", xh[s].k, xh[s].t[:, :, 3:131], xv[:, :, 0:128], reads=[("xbcT", 0)], writes=[xh[s].k])
            else:
                kb.dma("sp", xh[s].k, xh[s].t[:, :, 0:131], xv[:, :, t0 - 3:t0 + 128],
                       reads=[("xbcT", i), ("xbcT", i - 1)], writes=[xh[s].k])
            kb.dma("sp", zt[s].k, zt[s].t[:], self.proj[t0:t0 + 128, C_Z:C_Z + 512], reads=[("proj", i)], writes=[zt[s].k])
            kb.dma("sp", dtt[s].k, dtt[s].t[:], self.proj[t0:t0 + 128, C_DT:C_DT + 8], reads=[("proj", i)], writes=[dtt[s].k])
            for cc in range(8):
                eng = V
                ne = nc.vector
                GP(lambda: nc.gpsimd.tensor_scalar(out=acc[s].t[:, cc, :], in0=xh[s].t[:, cc, 0:128], scalar1=cw.t[:, 0, cc:cc + 1],
                                                   scalar2=cb.t[:, cc:cc + 1], op0=ALU.mult, op1=ALU.add),
                   [xh[s], cw, cb], [Tile(None, acc[s].k + f"_{cc}")])
                for k in range(1, 4):
                    eng(lambda: ne.scalar_tensor_tensor(out=acc[s].t[:, cc, :], in0=xh[s].t[:, cc, k:k + 128],
                                                        scalar=cw.t[:, k, cc:cc + 1], in1=acc[s].t[:, cc, :],
                                                        op0=ALU.mult, op1=ALU.add),
                        [xh[s], cw, Tile(None, acc[s].k + f"_{cc}")], [Tile(None, acc[s].k + f"_{cc}")])
            AC(lambda: nc.scalar.activation(out=xsT[s].t[:], in_=acc[s].t[:, 0:4, :], func=AF.Silu), [Tile(None, acc[s].k + f"_{c_}") for c_ in range(0, 4)], [xsT[s]])
            AC(lambda: nc.scalar.activation(out=BT[s].t[:], in_=acc[s].t[:, 4:6, :], func=AF.Silu), [Tile(None, acc[s].k + f"_{c_}") for c_ in range(4, 6)], [BT[s]])
            AC(lambda: nc.scalar.activation(out=CT[s].t[:], in_=acc[s].t[:, 6:8, :], func=AF.Silu), [Tile(None, acc[s].k + f"_{c_}") for c_ in range(6, 8)], [CT[s]])
            for cc in range(4):
                PE(lambda: nc.tensor.transpose(out=pxT.t[:, cc * 128:(cc + 1) * 128], in_=xsT[s].t[:, cc, :], identity=idf.t[:]),
                   [xsT[s], idf], [pxT])
            V(lambda: nc.vector.tensor_copy(out=xtm[s].t[:], in_=pxT.t[:]), [pxT], [xtm[s]])
            for g in range(2):
                PE(lambda: nc.tensor.transpose(out=pBt.t[:, g, :], in_=BT[s].t[:, g, :], identity=idb.t[:]), [BT[s], idb], [pBt])
            AC(lambda: nc.scalar.copy(out=Btm[s].t[:], in_=pBt.t[:]), [pBt], [Btm[s]])
            V(lambda: nc.vector.tensor_tensor(out=dts[s].t[:], in0=dtt[s].t[:], in1=dtb.t[:], op=ALU.add), [dtt[s], dtb], [dts[s]])
            V(lambda: nc.vector.scalar_tensor_tensor(out=sp_a[s].t[:], in0=dts[s].t[:], scalar=-1.0, in1=dts[s].t[:],
                                                     op0=ALU.mult, op1=ALU.max), [dts[s]], [sp_a[s]])
            AC(lambda: nc.scalar.activation(out=sp_a[s].t[:], in_=sp_a[s].t[:], func=AF.Exp, scale=-1.0), [sp_a[s]], [sp_a[s]])
            AC(lambda: nc.scalar.activation(out=sp_a[s].t[:], in_=sp_a[s].t[:], func=AF.Ln, bias=1.0), [sp_a[s]], [sp_a[s]])
            V(lambda: nc.vector.scalar_tensor_tensor(out=dts[s].t[:], in0=dts[s].t[:], scalar=0.0, in1=sp_a[s].t[:],
                                                     op0=ALU.max, op1=ALU.add), [dts[s], sp_a[s]], [dts[s]])
            V(lambda: nc.vector.tensor_tensor(out=dA[s].t[:], in0=dts[s].t[:], in1=Aneg.t[:], op=ALU.mult), [dts[s], Aneg], [dA[s]])
            PE(lambda: nc.tensor.matmul(out=pacs.t[:, 0:8], lhsT=tri.t[:], rhs=dA[s].t[:], start=True, stop=True), [tri, dA[s]], [pacs])
            PE(lambda: nc.tensor.matmul(out=pacs.t[:, 8:16], lhsT=ones.t[:], rhs=dA[s].t[:], start=True, stop=True), [ones, dA[s]], [pacs])
            V(lambda: nc.vector.tensor_copy(out=acs[s].t[:], in_=pacs.t[:]), [pacs], [acs[s]])
            AC(lambda: nc.scalar.activation(out=eacs[s].t[:], in_=acs[s].t[:, 0:8], func=AF.Exp), [acs[s]], [eacs[s]])
            AC(lambda: nc.scalar.activation(out=cd[s].t[:], in_=acs[s].t[:, 8:16], func=AF.Exp), [acs[s]], [cd[s]])
            V(lambda: nc.vector.tensor_tensor(out=dsd[s].t[:], in0=acs[s].t[:, 8:16], in1=acs[s].t[:, 0:8], op=ALU.subtract), [acs[s]], [dsd[s]])
            AC(lambda: nc.scalar.activation(out=dsd[s].t[:], in_=dsd[s].t[:], func=AF.Exp), [dsd[s]], [dsd[s]])
            V(lambda: nc.vector.tensor_tensor(out=dsd[s].t[:], in0=dsd[s].t[:], in1=dts[s].t[:], op=ALU.mult), [dsd[s], dts[s]], [dsd[s]])
            x3 = xtm[s].t[:].rearrange("p (h d) -> p h d", h=8)
            V(lambda: nc.vector.tensor_tensor(out=xdt[s].t[:].rearrange("p (h d) -> p h d", h=8), in0=x3,
                                              in1=dts[s].t[:].unsqueeze(2).to_broadcast([128, 8, 64]), op=ALU.mult),
              [xtm[s], dts[s]], [xdt[s]])
            GP(lambda: nc.gpsimd.tensor_tensor(out=xdd[s].t[:].rearrange("p (h d) -> p h d", h=8), in0=x3,
                                               in1=dsd[s].t[:].unsqueeze(2).to_broadcast([128, 8, 64]), op=ALU.mult),
               [xtm[s], dsd[s]], [xdd[s]])
            for g in range(2):
                PE(lambda: nc.tensor.matmul(out=pcb.t[:, g, :], lhsT=BT[s].t[:, g, :], rhs=CT[s].t[:, g, :], start=True, stop=True),
                   [BT[s], CT[s]], [pcb])
            V(lambda: nc.vector.tensor_tensor(out=cbm[s].t[:], in0=pcb.t[:], in1=tri.t[:].unsqueeze(1).to_broadcast([128, 2, 128]),
                                              op=ALU.mult), [pcb, tri], [cbm[s]])
        def tail(i):
            s = i % 2
            t0 = i * 128
            x3 = xtm[s].t[:].rearrange("p (h d) -> p h d", h=8)
            GP(lambda: nc.gpsimd.tensor_tensor(out=dAt.t[:], in0=tri.t[:].unsqueeze(1).to_broadcast([128, 8, 128]),
                                               in1=dA[s].t[:].unsqueeze(2).to_broadcast([128, 8, 128]), op=ALU.mult), [tri, dA[s]], [dAt])
            for g in range(2):
                for j in range(4):
                    PE(lambda: nc.tensor.matmul(out=pD.t[:, j, :], lhsT=slt.t[:], rhs=dAt.t[:, 4 * g + j, :], start=True, stop=True),
                       [slt, dAt], [pD])
                AC(lambda: nc.scalar.activation(out=seg[g].t[:], in_=pD.t[:], func=AF.Exp), [pD], [seg[g]])
                V(lambda: nc.vector.tensor_tensor(out=MT[g].t[:], in0=seg[g].t[:], in1=cbm[s].t[:, g, :].unsqueeze(1).to_broadcast([128, 4, 128]),
                                                  op=ALU.mult), [seg[g], cbm[s]], [MT[g]])
                for j in range(4):
                    h = 4 * g + j
                    PE(lambda: nc.tensor.matmul(out=py.t[:, h * 64:(h + 1) * 64], lhsT=MT[g].t[:, j, :], rhs=xdt[s].t[:, h * 64:(h + 1) * 64],
                                                start=True, stop=True), [MT[g], xdt[s]], [py])
            for g in range(2):
                PE(lambda: nc.tensor.matmul(out=pyo.t[:, g * 256:(g + 1) * 256], lhsT=CT[s].t[:, g, :], rhs=hTb.t[:, g * 256:(g + 1) * 256],
                                            start=True, stop=True), [CT[s], hTb], [pyo])
            for g in range(2):
                PE(lambda: nc.tensor.matmul(out=pU.t[:, g * 256:(g + 1) * 256], lhsT=Btm[s].t[:, g, :], rhs=xdd[s].t[:, g * 256:(g + 1) * 256],
                                            start=True, stop=True), [Btm[s], xdd[s]], [pU])
            AC(lambda: nc.scalar.copy(out=yd[s].t[:], in_=py.t[:]), [py], [yd[s]])
            V(lambda: nc.vector.tensor_tensor(out=y[s].t[:].rearrange("p (h d) -> p h d", h=8),
                                              in0=pyo.t[:].rearrange("p (h d) -> p h d", h=8),
                                              in1=eacs[s].t[:].unsqueeze(2).to_broadcast([128, 8, 64]), op=ALU.mult),
              [pyo, eacs[s]], [y[s]])
            V(lambda: nc.vector.tensor_tensor(out=hT.t[:].rearrange("p (h d) -> p h d", h=8),
                                              in0=hT.t[:].rearrange("p (h d) -> p h d", h=8),
                                              in1=cd[s].t[:].unsqueeze(2).to_broadcast([128, 8, 64]), op=ALU.mult),
              [hT, cd[s]], [hT])
            V(lambda: nc.vector.tensor_tensor(out=hT.t[:], in0=pU.t[:], in1=hT.t[:], op=ALU.add), [pU, hT], [hT])
            AC(lambda: nc.scalar.copy(out=hTb.t[:], in_=hT.t[:]), [hT], [hTb])
            GP(lambda: nc.gpsimd.tensor_tensor(out=y[s].t[:], in0=y[s].t[:], in1=yd[s].t[:], op=ALU.add), [y[s], yd[s]], [y[s]])
            GP(lambda: nc.gpsimd.tensor_tensor(out=yd[s].t[:].rearrange("p (h d) -> p h d", h=8), in0=x3,
                                               in1=dsk.t[:].unsqueeze(2).to_broadcast([128, 8, 64]), op=ALU.mult),
               [xtm[s], dsk], [yd[s]])
            GP(lambda: nc.gpsimd.tensor_tensor(out=y[s].t[:], in0=y[s].t[:], in1=yd[s].t[:], op=ALU.add), [y[s], yd[s]], [y[s]])
            AC(lambda: nc.scalar.activation(out=sz[s].t[:], in_=zt[s].t[:], func=AF.Silu), [zt[s]], [sz[s]])
            V(lambda: nc.vector.tensor_tensor(out=y[s].t[:], in0=y[s].t[:], in1=sz[s].t[:], op=ALU.mult), [y[s], sz[s]], [y[s]])
            for g in range(2):
                AC(lambda: nc.scalar.activation(out=junk.t[:], in_=y[s].t[:, g * 256:(g + 1) * 256], func=AF.Square,
                                                accum_out=ss[s].t[:, g:g + 1]), [y[s]], [junk, ss[s]])
            ssk = [ss[s]]
            V(lambda: nc.vector.tensor_scalar(out=ss[s].t[:], in0=ss[s].t[:], scalar1=1.0 / 256, scalar2=EPS, op0=ALU.mult, op1=ALU.add),
              ssk, [ss[s]])
            AC(lambda: nc.scalar.activation(out=ss[s].t[:], in_=ss[s].t[:], func=AF.Sqrt), [ss[s]], [ss[s]])
            V(lambda: nc.vector.reciprocal(out=ss[s].t[:], in_=ss[s].t[:]), [ss[s]], [ss[s]])
            for g in range(2):
                V(lambda: nc.vector.scalar_tensor_tensor(out=mo[s].t[:, g * 256:(g + 1) * 256], in0=y[s].t[:, g * 256:(g + 1) * 256],
                                                         scalar=ss[s].t[:, g:g + 1], in1=nwb.t[:, g * 256:(g + 1) * 256],
                                                         op0=ALU.mult, op1=ALU.mult), [y[s], ss[s], nwb], [Tile(None, f"ssd_mo{s}_{g}")])
            kb.dma("sp", mo[s].k, self.mix[t0:t0 + 128, 512:1024], mo[s].t[:],
                   reads=[f"ssd_mo{s}_0", f"ssd_mo{s}_1"], writes=[("mix_ssd", i)])

        front(0)
        for i in range(NT):
            if i + 1 < NT:
                interleave(kb, (lambda i=i: tail(i)), (lambda i=i: front(i + 1)))
            else:
                tail(i)


Prog.stage_ssd = _stage_ssd


def _stage_gla(self, l):
    nc, kb, NT = self.nc, self.kb, self.NT
    with ExitStack() as es:
        def T(name, shape, dt=F32):
            return Tile(self.sb(es, "gla_" + name, shape, dt), "gla_" + name)

        def T2(name, shape, dt=F32):
            return [T(f"{name}{j}", shape, dt) for j in range(2)]

        def PT(name, shape, dt=F32):
            return Tile(self.ps(es, "gla_" + name, shape, dt), "gla_" + name)

        def V(fn, r, w):
            return kb.op("dve", fn, [t.k for t in r], [t.k for t in w])

        def AC(fn, r, w):
            return kb.op("act", fn, [t.k for t in r], [t.k for t in w])

        def PE(fn, r, w):
            return kb.op("pe", fn, [t.k for t in r], [t.k for t in w])

        def GP(fn, r, w):
            return kb.op("pool", fn, [t.k for t in r], [t.k for t in w])

        def LD(t, dst, src, reads=(), **kw):
            return kb.dma("sp", t.k, dst, src, reads=list(reads), writes=[t.k], **kw)

        w2 = T("w2", [16, 128]); bb = T("bb", [128, 128]); nwb = T("nwb", [128, 256])
        tri = T("tri", [128, 128]); tri16 = T("tri16", [128, 128]); o16 = T("o16", [128, 2])
        idf = T("idf", [128, 128]); idb = T("idb", [128, 128], BF16)
        hm = T("hm", [128, 4]); bd = T("bd", [128, 256])
        Sbd = T("Sbd", [128, 256]); Sbb = T("Sbb", [128, 256], BF16)
        gin = T2("gin", [128, 784])
        glrT = T2("glrT", [16, 128]); zv = T2("zv", [128, 128]); az = T2("az", [128, 128]); la = T2("la", [128, 128])
        eb = T2("eb", [128, 128]); enb = T2("enb", [128, 128])
        qt = T2("qtok", [128, 128], BF16); kt = T2("ktok", [128, 128], BF16); vb = T2("vb", [128, 256], BF16)
        qT = T2("qT", [128, 128], BF16); kTm = T2("kTm", [128, 4, 128], BF16)
        egt = T2("egt", [128, 2]); ATm = T2("ATm", [128, 4, 128], BF16)
        sq = T2("sq", [128, 256]); ss = T2("ss", [128, 4]); on = T2("on", [128, 256]); sr = T2("sr", [128, 256])
        mo = T2("mo", [128, 256], BF16); um = T2("um", [128, 256])
        pgT = PT("pgT", [16, 128]); pz = PT("pz", [128, 128]); pbc = PT("pbc", [128, 128])
        pqk = PT("pqk", [128, 2, 128], BF16); pgt = PT("pgt", [128, 2]); pA = PT("pA", [128, 4, 128])
        po = PT("po", [128, 256]); pU = PT("pU", [128, 256])

        LD(w2, w2.t[:], self.gla_gate_w2[l, :, :])
        LD(bb, bb.t[:], self.gla_gate_b[l, :].partition_broadcast(128))
        LD(nwb, nwb.t[:], self.gla_norm_w[l, :].partition_broadcast(128))
        LD(tri, tri.t[:], self.c_tri[:, :]); LD(idf, idf.t[:], self.ident_f[:, :]); LD(idb, idb.t[:], self.ident_b[:, :])
        LD(hm, hm.t[:], self.c_hm[:, :]); LD(bd, bd.t[:], self.c_bd[:, :])
        V(lambda: nc.vector.tensor_scalar(out=tri16.t[:], in0=tri.t[:], scalar1=1.0 / 16, scalar2=None, op0=ALU.mult), [tri], [tri16])
        V(lambda: nc.vector.memset(o16.t[:], 1.0 / 16), [], [o16])
        V(lambda: nc.vector.memset(Sbd.t[:], 0.0), [], [Sbd])
        V(lambda: nc.vector.memset(Sbb.t[:], 0.0), [], [Sbb])

        def front(i):
            s = i % 2
            t0 = i * 128
            kb.dma("sp", gin[s].k, gin[s].t[:], self.proj[t0:t0 + 128, 0:784], reads=[("proj", i)], writes=[gin[s].k])
            q_ = gin[s].t[:, C_GQ:C_GQ + 128]; k_ = gin[s].t[:, C_GK:C_GK + 128]; v_ = gin[s].t[:, C_GV:C_GV + 256]
            glr_ = gin[s].t[:, C_GLR:C_GLR + 16]; r_ = gin[s].t[:, C_GR:C_GR + 256]
            PE(lambda: nc.tensor.transpose(out=pgT.t[:], in_=glr_, identity=idf.t[:]), [gin[s], idf], [pgT])
            V(lambda: nc.vector.tensor_copy(out=glrT[s].t[:], in_=pgT.t[:]), [pgT], [glrT[s]])
            PE(lambda: nc.tensor.matmul(out=pz.t[:], lhsT=glrT[s].t[:], rhs=w2.t[:], start=True, stop=True), [glrT[s], w2], [pz])
            V(lambda: nc.vector.tensor_tensor(out=zv[s].t[:], in0=pz.t[:], in1=bb.t[:], op=ALU.add), [pz, bb], [zv[s]])
            V(lambda: nc.vector.scalar_tensor_tensor(out=az[s].t[:], in0=zv[s].t[:], scalar=-1.0, in1=zv[s].t[:], op0=ALU.mult, op1=ALU.max),
              [zv[s]], [az[s]])
            AC(lambda: nc.scalar.activation(out=az[s].t[:], in_=az[s].t[:], func=AF.Exp, scale=-1.0), [az[s]], [az[s]])
            AC(lambda: nc.scalar.activation(out=az[s].t[:], in_=az[s].t[:], func=AF.Ln, bias=1.0), [az[s]], [az[s]])
            V(lambda: nc.vector.scalar_tensor_tensor(out=la[s].t[:], in0=zv[s].t[:], scalar=0.0, in1=az[s].t[:], op0=ALU.min, op1=ALU.subtract),
              [zv[s], az[s]], [la[s]])
            PE(lambda: nc.tensor.matmul(out=pbc.t[:], lhsT=tri16.t[:], rhs=la[s].t[:], start=True, stop=True), [tri16, la[s]], [pbc])
            PE(lambda: nc.tensor.matmul(out=pgt.t[:], lhsT=la[s].t[:], rhs=o16.t[:], start=True, stop=True), [la[s], o16], [pgt])
            AC(lambda: nc.scalar.activation(out=eb[s].t[:], in_=pbc.t[:], func=AF.Exp), [pbc], [eb[s]])
            AC(lambda: nc.scalar.activation(out=enb[s].t[:], in_=pbc.t[:], func=AF.Exp, scale=-1.0), [pbc], [enb[s]])
            AC(lambda: nc.scalar.activation(out=egt[s].t[:], in_=pgt.t[:], func=AF.Exp), [pgt], [egt[s]])
            V(lambda: nc.vector.scalar_tensor_tensor(out=qt[s].t[:], in0=q_, scalar=32.0 ** -0.5, in1=eb[s].t[:], op0=ALU.mult, op1=ALU.mult),
              [gin[s], eb[s]], [qt[s]])
            V(lambda: nc.vector.tensor_tensor(out=kt[s].t[:], in0=k_, in1=enb[s].t[:], op=ALU.mult), [gin[s], enb[s]], [kt[s]])
            GP(lambda: nc.gpsimd.tensor_copy(out=vb[s].t[:], in_=v_), [gin[s]], [vb[s]])
            PE(lambda: nc.tensor.transpose(out=pqk.t[:, 0, :], in_=qt[s].t[:], identity=idb.t[:]), [qt[s], idb], [pqk])
            PE(lambda: nc.tensor.transpose(out=pqk.t[:, 1, :], in_=kt[s].t[:], identity=idb.t[:]), [kt[s], idb], [pqk])
            AC(lambda: nc.scalar.copy(out=qT[s].t[:], in_=pqk.t[:, 0, :]), [pqk], [qT[s]])
            for h in range(4):
                AC(lambda: nc.scalar.activation(out=kTm[s].t[:, h, :], in_=pqk.t[:, 1, :], func=AF.Copy, scale=hm.t[:, h:h + 1]),
                   [pqk, hm], [kTm[s]])
            for h in range(4):
                PE(lambda: nc.tensor.matmul(out=pA.t[:, h, :], lhsT=kTm[s].t[:, h, :], rhs=qT[s].t[:], start=True, stop=True),
                   [kTm[s], qT[s]], [pA])
            V(lambda: nc.vector.tensor_tensor(out=ATm[s].t[:], in0=pA.t[:], in1=tri.t[:].unsqueeze(1).to_broadcast([128, 4, 128]), op=ALU.mult),
              [pA, tri], [ATm[s]])
        def tail(i):
            s = i % 2
            t0 = i * 128
            r_ = gin[s].t[:, C_GR:C_GR + 256]
            PE(lambda: nc.tensor.matmul(out=po.t[:], lhsT=qT[s].t[:], rhs=Sbb.t[:], start=True, stop=False), [qT[s], Sbb], [po])
            for h in range(4):
                PE(lambda: nc.tensor.matmul(out=po.t[:, h * 64:(h + 1) * 64], lhsT=ATm[s].t[:, h, :], rhs=vb[s].t[:, h * 64:(h + 1) * 64],
                                            start=False, stop=(h == 3)), [ATm[s], vb[s]], [po])
            PE(lambda: nc.tensor.matmul(out=pU.t[:], lhsT=kt[s].t[:], rhs=vb[s].t[:], start=True, stop=True), [kt[s], vb[s]], [pU])
            V(lambda: nc.vector.tensor_tensor(out=um[s].t[:], in0=pU.t[:], in1=bd.t[:], op=ALU.mult), [pU, bd], [um[s]])
            V(lambda: nc.vector.tensor_tensor(out=Sbd.t[:], in0=Sbd.t[:], in1=um[s].t[:], op=ALU.add), [Sbd, um[s]], [Sbd])
            V(lambda: nc.vector.tensor_scalar(out=Sbd.t[:], in0=Sbd.t[:], scalar1=egt[s].t[:, 0:1], scalar2=None, op0=ALU.mult), [Sbd, egt[s]], [Sbd])
            AC(lambda: nc.scalar.copy(out=Sbb.t[:], in_=Sbd.t[:]), [Sbd], [Sbb])
            AC(lambda: nc.scalar.activation(out=sq[s].t[:], in_=po.t[:], func=AF.Square), [po], [sq[s]])
            V(lambda: nc.vector.tensor_reduce(out=ss[s].t[:], in_=sq[s].t[:].rearrange("p (h d) -> p h d", h=4), axis=AX.X, op=ALU.add),
              [sq[s]], [ss[s]])
            V(lambda: nc.vector.tensor_scalar(out=ss[s].t[:], in0=ss[s].t[:], scalar1=1.0 / 64, scalar2=EPS, op0=ALU.mult, op1=ALU.add), [ss[s]], [ss[s]])
            AC(lambda: nc.scalar.activation(out=ss[s].t[:], in_=ss[s].t[:], func=AF.Sqrt), [ss[s]], [ss[s]])
            V(lambda: nc.vector.reciprocal(out=ss[s].t[:], in_=ss[s].t[:]), [ss[s]], [ss[s]])
            V(lambda: nc.vector.tensor_tensor(out=on[s].t[:].rearrange("p (h d) -> p h d", h=4), in0=po.t[:].rearrange("p (h d) -> p h d", h=4),
                                              in1=ss[s].t[:].unsqueeze(2).to_broadcast([128, 4, 64]), op=ALU.mult), [po, ss[s]], [on[s]])
            AC(lambda: nc.scalar.activation(out=sr[s].t[:], in_=r_, func=AF.Silu), [gin[s]], [sr[s]])
            GP(lambda: nc.gpsimd.tensor_tensor(out=sr[s].t[:], in0=sr[s].t[:], in1=nwb.t[:], op=ALU.mult), [sr[s], nwb], [sr[s]])
            V(lambda: nc.vector.tensor_tensor(out=mo[s].t[:], in0=on[s].t[:], in1=sr[s].t[:], op=ALU.mult), [on[s], sr[s]], [mo[s]])
            kb.dma("sp", mo[s].k, self.mix[t0:t0 + 128, 0:256], mo[s].t[:], reads=[mo[s].k], writes=[("mix_gla", i)])

        front(0)
        for i in range(NT):
            if i + 1 < NT:
                interleave(kb, (lambda i=i: tail(i)), (lambda i=i: front(i + 1)))
            else:
                tail(i)


Prog.stage_gla = _stage_gla


def build_test_mix(S, which, limit=None):
    p = Prog(S, L=1, dbg=[("o_mix", [S, D])])
    with p.kb.es as es:
        p.stage_d1(0, p.x)
        p.kb.barrier()
        if limit is not None:
            p.kb.limit = p.kb.ninst + limit
        if "ssd" in which:
            p.stage_ssd(0)
        if "gla" in which:
            p.stage_gla(0)
        if "nsa" in which:
            p.stage_nsa(0)
        kb, nc = p.kb, p.nc
        kb.limit = None
        kb.barrier()
        mb = p.sb(es, "dump_mb", [128, D], BF16)
        mf = p.sb(es, "dump_mf", [128, D])
        for i in range(p.NT):
            kb.dma("sp", "dump_mb", mb[:], p.mix[i * 128:(i + 1) * 128, :], writes=["dump_mb"])
            kb.op("dve", lambda: nc.vector.tensor_copy(out=mf[:], in_=mb[:]), reads=["dump_mb"], writes=["dump_mf"])
            kb.dma("sp", "dump_mf", p.outs["o_mix"][i * 128:(i + 1) * 128, :], mf[:], reads=["dump_mf"], writes=[("o_mix", i)])
        kb.drain("sp")
    return p


def _stage_d2(self, l, hsrc, hdst, final):
    nc, kb, NT = self.nc, self.kb, self.NT
    with ExitStack() as es:
        def T(name, shape, dt=F32):
            return Tile(self.sb(es, "d2_" + name, shape, dt), "d2_" + name)

        def T2(name, shape, dt=F32):
            return [T(f"{name}{j}", shape, dt) for j in range(2)]

        def PT(name, shape, dt=F32):
            return Tile(self.ps(es, "d2_" + name, shape, dt), "d2_" + name)

        def V(fn, r, w):
            return kb.op("dve", fn, [t.k for t in r], [t.k for t in w])

        def AC(fn, r, w):
            return kb.op("act", fn, [t.k for t in r], [t.k for t in w])

        def PE(fn, r, w):
            return kb.op("pe", fn, [t.k for t in r], [t.k for t in w])

        def GP(fn, r, w):
            return kb.op("pool", fn, [t.k for t in r], [t.k for t in w])

        Wo = T("Wo", [128, 8, 1024], BF16); Wu = T("Wu", [128, 8, 4096], BF16); Wd = T("Wd", [128, 32, 1024], BF16)
        stg = T2("stg", [128, 1024]); nw2 = T("nw2", [128, 8]); idb = T("idb", [128, 128], BF16)
        ht = T2("ht", [128, 1024]); mt = T2("mt", [128, 1024], BF16)
        mT = T("mT", [128, 8, 128], BF16); h1 = T2("h1", [128, 1024]); junk = T("junk", [128, 1024], BF16)
        ss = T("ss", [128, 1]); u2 = T("u2", [128, 1024], BF16); u2T = T2("u2T", [128, 8, 128], BF16)
        tmp = T2("tmp", [128, 512]); hidT = T("hidT", [128, 32, 128], BF16); ho = T2("ho", [128, 1024])
        pT = PT("pT", [128, 8, 128], BF16); po = [PT(f"po{j}", [128, 512]) for j in range(2)]
        pu = [PT(f"pu{j}", [128, 4, 128]) for j in range(2)]
        pd = [PT(f"pd{j}", [128, 512]) for j in range(2)]
        if final:
            fnw = T("fnw", [128, 1024]); ss2 = T("ss2", [128, 1])
            kb.dma("sp", fnw.k, fnw.t[:], self.final_norm_w.partition_broadcast(128), writes=[fnw.k])

        kb.dma("sp", idb.k, idb.t[:], self.ident_b[:, :], writes=[idb.k])
        for c in range(8):
            kb.dma("sp", nw2.k, nw2.t[:, c:c + 1], self.norm2_w[l, c * 128:(c + 1) * 128].rearrange("(p o) -> p o", o=1), writes=[nw2.k])
        n = 0
        jobs = [(Wo, c, 0, self.w_out[l, c * 128:(c + 1) * 128, :], False) for c in range(8)]
        jobs += [(Wu, c, q * 1024, self.w_up[l, c * 128:(c + 1) * 128, q * 1024:(q + 1) * 1024], True) for c in range(8) for q in range(4)]
        jobs += [(Wd, f, 0, self.w_down[l, f * 128:(f + 1) * 128, :], False) for f in range(32)]
        for (W, c, off, src, scaled) in jobs:
            sg = stg[n % 2]
            kb.dma("sp", sg.k, sg.t[:], src, writes=[sg.k])
            dst = W.t[:, c, off:off + 1024]
            if scaled:
                if n % 2 == 0:
                    V(lambda: nc.vector.tensor_scalar(out=dst, in0=sg.t[:], scalar1=nw2.t[:, c:c + 1], scalar2=None, op0=ALU.mult), [sg, nw2], [W])
                else:
                    AC(lambda: nc.scalar.activation(out=dst, in_=sg.t[:], func=AF.Copy, scale=nw2.t[:, c:c + 1]), [sg, nw2], [W])
            else:
                if n % 2 == 0:
                    V(lambda: nc.vector.tensor_copy(out=dst, in_=sg.t[:]), [sg], [W])
                else:
                    AC(lambda: nc.scalar.copy(out=dst, in_=sg.t[:]), [sg], [W])
            n += 1

        def front(i):
            s = i % 2
            t0 = i * 128
            kb.dma("sp", ht[s].k, ht[s].t[:], hsrc[t0:t0 + 128, :], reads=[("h", i)], writes=[ht[s].k])
            kb.dma("sp", mt[s].k, mt[s].t[:], self.mix[t0:t0 + 128, :], reads=[("mix_gla", i), ("mix_nsa", i), ("mix_ssd", i)], writes=[mt[s].k])
            for c in range(8):
                PE(lambda: nc.tensor.transpose(out=pT.t[:, c, :], in_=mt[s].t[:, c * 128:(c + 1) * 128], identity=idb.t[:]), [mt[s], idb], [pT])
            AC(lambda: nc.scalar.copy(out=mT.t[:], in_=pT.t[:]), [pT], [mT])
            for hf in range(2):
                for c in range(8):
                    PE(lambda: nc.tensor.matmul(out=po[hf].t[:], lhsT=mT.t[:, c, :], rhs=Wo.t[:, c, hf * 512:(hf + 1) * 512],
                                                start=(c == 0), stop=(c == 7)), [mT, Wo], [po[hf]])
                V(lambda: nc.vector.tensor_tensor(out=h1[s].t[:, hf * 512:(hf + 1) * 512], in0=po[hf].t[:], in1=ht[s].t[:, hf * 512:(hf + 1) * 512], op=ALU.add),
                  [po[hf], ht[s]], [h1[s]])
            AC(lambda: nc.scalar.activation(out=junk.t[:], in_=h1[s].t[:], func=AF.Square, accum_out=ss.t[:]), [h1[s]], [junk, ss])
            V(lambda: nc.vector.tensor_scalar(out=ss.t[:], in0=ss.t[:], scalar1=1.0 / D, scalar2=EPS, op0=ALU.mult, op1=ALU.add), [ss], [ss])
            AC(lambda: nc.scalar.activation(out=ss.t[:], in_=ss.t[:], func=AF.Sqrt), [ss], [ss])
            V(lambda: nc.vector.reciprocal(out=ss.t[:], in_=ss.t[:]), [ss], [ss])
            V(lambda: nc.vector.tensor_scalar(out=u2.t[:], in0=h1[s].t[:], scalar1=ss.t[:], scalar2=None, op0=ALU.mult), [h1[s], ss], [u2])
            for c in range(8):
                PE(lambda: nc.tensor.transpose(out=pT.t[:, c, :], in_=u2.t[:, c * 128:(c + 1) * 128], identity=idb.t[:]), [u2, idb], [pT])
            AC(lambda: nc.scalar.copy(out=u2T[s].t[:], in_=pT.t[:]), [pT], [u2T[s]])
        def tail(i):
            s = i % 2
            t0 = i * 128
            for fg in range(8):
                p = pu[fg % 2]
                for j in range(4):
                    f = fg * 4 + j
                    for c in range(8):
                        PE(lambda: nc.tensor.matmul(out=p.t[:, j, :], lhsT=Wu.t[:, c, f * 128:(f + 1) * 128], rhs=u2T[s].t[:, c, :],
                                                    start=(c == 0), stop=(c == 7)), [Wu, u2T[s]], [p])
                tm = tmp[fg % 2]
                AC(lambda: nc.scalar.activation(out=tm.t[:], in_=p.t[:].rearrange("p a b -> p (a b)"), func=AF.Relu), [p], [tm])
                GP(lambda: nc.gpsimd.tensor_tensor(out=hidT.t[:, fg * 4:(fg + 1) * 4, :].rearrange("p a b -> p (a b)"), in0=tm.t[:], in1=tm.t[:], op=ALU.mult),
                   [tm], [Tile(None, f"d2_hid{fg}")])
            hk = [Tile(None, f"d2_hid{fg}") for fg in range(8)]
            for f in range(32):
                for hf in range(2):
                    PE(lambda: nc.tensor.matmul(out=pd[hf].t[:], lhsT=hidT.t[:, f, :], rhs=Wd.t[:, f, hf * 512:(hf + 1) * 512],
                                                start=(f == 0), stop=(f == 31)), [hk[f // 4], Wd], [pd[hf]])
            for hf in range(2):
                V(lambda: nc.vector.tensor_tensor(out=ho[s].t[:, hf * 512:(hf + 1) * 512], in0=pd[hf].t[:], in1=h1[s].t[:, hf * 512:(hf + 1) * 512], op=ALU.add),
                  [pd[hf], h1[s]], [ho[s]])
            if final:
                AC(lambda: nc.scalar.activation(out=junk.t[:], in_=ho[s].t[:], func=AF.Square, accum_out=ss2.t[:]), [ho[s]], [junk, ss2])
                V(lambda: nc.vector.tensor_scalar(out=ss2.t[:], in0=ss2.t[:], scalar1=1.0 / D, scalar2=EPS, op0=ALU.mult, op1=ALU.add), [ss2], [ss2])
                AC(lambda: nc.scalar.activation(out=ss2.t[:], in_=ss2.t[:], func=AF.Sqrt), [ss2], [ss2])
                V(lambda: nc.vector.reciprocal(out=ss2.t[:], in_=ss2.t[:]), [ss2], [ss2])
                V(lambda: nc.vector.scalar_tensor_tensor(out=ho[s].t[:], in0=ho[s].t[:], scalar=ss2.t[:], in1=fnw.t[:], op0=ALU.mult, op1=ALU.mult),
                  [ho[s], ss2, fnw], [ho[s]])
            kb.dma("sp", ho[s].k, hdst[t0:t0 + 128, :], ho[s].t[:], reads=[ho[s].k], writes=[("hout", l, i)] if final else [("h", i)])

        front(0)
        for i in range(NT):
            if i + 1 < NT:
                interleave(kb, (lambda i=i: tail(i)), (lambda i=i: front(i + 1)))
            else:
                tail(i)


Prog.stage_d2 = _stage_d2


def _stage_nsa(self, l):
    nc, kb, NT, S, NS, NCC = self.nc, self.kb, self.NT, self.S, self.NS, self.NCC
    NCB = S // 16 - 1
    with ExitStack() as es:
        def Tn(es_, name, shape, dt=F32):
            return Tile(self.sb(es_, "nsa_" + name, shape, dt), "nsa_" + name)

        def T(name, shape, dt=F32):
            return Tn(es, name, shape, dt)

        def V(fn, r, w):
            return kb.op("dve", fn, [t.k for t in r], [t.k for t in w])

        def AC(fn, r, w):
            return kb.op("act", fn, [t.k for t in r], [t.k for t in w])

        def PE(fn, r, w):
            return kb.op("pe", fn, [t.k for t in r], [t.k for t in w])

        def GP(fn, r, w):
            return kb.op("pool", fn, [t.k for t in r], [t.k for t in w])

        def LD(t, dst, src, reads=(), **kw):
            return kb.dma("sp", t.k, dst, src, reads=list(reads), writes=[t.k], **kw)

        QT = T("QT", [128, NT, 2, 128], BF16); KsT = T("KsT", [128, S], BF16); KwT = T("KwT", [128, S], BF16)
        KCT = T("KCT", [128, NCC * 128], BF16)
        VsA = T("VsA", [128, NT, 2, 65], BF16); VwA = T("VwA", [128, NT, 2, 65], BF16)
        RC = T("RC", [128, NCC, 2, 65 + NS], BF16); GA = T("GA", [128, NT, 12])
        idb = T("idb", [128, 128], BF16); nwb = T("nwb", [128, 256])
        LD(idb, idb.t[:], self.ident_b[:, :])
        LD(nwb, nwb.t[:], self.nsa_norm_w[l, :].partition_broadcast(128))
        V(lambda: nc.vector.memset(KCT.t[:], 0.0), [], [KCT])
        V(lambda: nc.vector.memset(RC.t[:], 0.0), [], [RC])
        V(lambda: nc.vector.memset(VsA.t[:], 1.0), [], [VsA])
        V(lambda: nc.vector.memset(VwA.t[:], 1.0), [], [VwA])

        with ExitStack() as e1:
            nin = [Tn(e1, f"nin{j}", [128, 1036]) for j in range(2)]
            cs = [Tn(e1, f"cs{j}", [128, 2, 32]) for j in range(2)]
            ta2 = [Tn(e1, f"ta{j}", [128, 10, 32]) for j in range(2)]; tb2 = [Tn(e1, f"tb{j}", [128, 10, 32]) for j in range(2)]
            rq = [Tn(e1, f"rq{j}", [128, 768], BF16) for j in range(2)]
            kv = [Tn(e1, f"kv{j}", [128, 2, 128], BF16) for j in range(2)]
            pT2 = [Tile(self.ps(e1, f"nsa_pT1{j}", [128, 6, 128], BF16), f"nsa_pT1{j}") for j in range(2)]

            def n1(i):
                s = i % 2
                t0 = i * 128
                ta, tb, pT = ta2[s], tb2[s], pT2[s]
                kb.dma("sp", nin[s].k, nin[s].t[:], self.proj[t0:t0 + 128, C_NQ:C_NQ + 1036], reads=[("proj", i)], writes=[nin[s].k])
                kb.dma("sp", cs[s].k + "c", cs[s].t[:, 0, :], self.rope_cos[t0:t0 + 128, :], writes=[cs[s].k + "c"])
                kb.dma("sp", cs[s].k + "s", cs[s].t[:, 1, :], self.rope_sin[t0:t0 + 128, :], writes=[cs[s].k + "s"])
                csk = [Tile(None, cs[s].k + "c"), Tile(None, cs[s].k + "s")]
                x3 = nin[s].t[:, 0:640].rearrange("p (h d) -> p h d", h=10)
                o3 = rq[s].t[:, 0:640].rearrange("p (h d) -> p h d", h=10)
                cb_ = cs[s].t[:, 0, :].unsqueeze(1).to_broadcast([128, 10, 32])
                sb_ = cs[s].t[:, 1, :].unsqueeze(1).to_broadcast([128, 10, 32])
                V(lambda: nc.vector.tensor_tensor(out=ta.t[:], in0=x3[:, :, 0:32], in1=cb_, op=ALU.mult), [nin[s]] + csk, [ta])
                V(lambda: nc.vector.tensor_tensor(out=tb.t[:], in0=x3[:, :, 32:64], in1=sb_, op=ALU.mult), [nin[s]] + csk, [tb])
                V(lambda: nc.vector.tensor_tensor(out=o3[:, :, 0:32], in0=ta.t[:], in1=tb.t[:], op=ALU.subtract), [ta, tb], [Tile(None, rq[s].k + "a")])
                V(lambda: nc.vector.tensor_tensor(out=ta.t[:], in0=x3[:, :, 32:64], in1=cb_, op=ALU.mult), [nin[s]] + csk, [ta])
                V(lambda: nc.vector.tensor_tensor(out=tb.t[:], in0=x3[:, :, 0:32], in1=sb_, op=ALU.mult), [nin[s]] + csk, [tb])
                V(lambda: nc.vector.tensor_tensor(out=o3[:, :, 32:64], in0=ta.t[:], in1=tb.t[:], op=ALU.add), [ta, tb], [Tile(None, rq[s].k + "b")])
                AC(lambda: nc.scalar.copy(out=rq[s].t[:, 640:768], in_=nin[s].t[:, 640:768]), [nin[s]], [Tile(None, rq[s].k + "c")])
                rqk = [Tile(None, rq[s].k + x) for x in "abc"]
                for b in range(6):
                    PE(lambda: nc.tensor.transpose(out=pT.t[:, b, :], in_=rq[s].t[:, b * 128:(b + 1) * 128], identity=idb.t[:]), rqk + [idb], [pT])
                AC(lambda: nc.scalar.copy(out=QT.t[:, i, :, :], in_=pT.t[:, 0:2, :]), [pT], [Tile(None, f"nsa_QT{i}")])
                AC(lambda: nc.scalar.copy(out=KsT.t[:, t0:t0 + 128], in_=pT.t[:, 3, :]), [pT], [Tile(None, f"nsa_KsT{i}")])
                AC(lambda: nc.scalar.copy(out=KwT.t[:, t0:t0 + 128], in_=pT.t[:, 4, :]), [pT], [Tile(None, f"nsa_KwT{i}")])
                AC(lambda: nc.scalar.copy(out=kv[s].t[:, 0, :], in_=pT.t[:, 2, :]), [pT], [kv[s]])
                AC(lambda: nc.scalar.copy(out=kv[s].t[:, 1, :], in_=pT.t[:, 5, :]), [pT], [kv[s]])
                kb.dma("sp", kv[s].k + "k", self.kcT_d[:, t0:t0 + 128], kv[s].t[:, 0, :], reads=[kv[s].k], writes=[("kcT_d", i)])
                kb.dma("sp", kv[s].k + "v", self.vcT_d[:, t0:t0 + 128], kv[s].t[:, 1, :], reads=[kv[s].k], writes=[("vcT_d", i)])
                GP(lambda: nc.gpsimd.tensor_copy(out=VsA.t[:, i, :, 0:64], in_=nin[s].t[:, 768:896].rearrange("p (g d) -> p g d", g=2)),
                   [nin[s], VsA], [Tile(None, f"nsa_VsA{i}")])
                GP(lambda: nc.gpsimd.tensor_copy(out=VwA.t[:, i, :, 0:64], in_=nin[s].t[:, 896:1024].rearrange("p (g d) -> p g d", g=2)),
                   [nin[s], VwA], [Tile(None, f"nsa_VwA{i}")])
                AC(lambda: nc.scalar.activation(out=GA.t[:, i, :], in_=nin[s].t[:, 1024:1036], func=AF.Sigmoid), [nin[s]], [Tile(None, f"nsa_GA{i}")])

            for i in range(0, NT, 2):
                if i + 1 < NT:
                    interleave(kb, (lambda i=i: n1(i)), (lambda i=i: n1(i + 1)))
                else:
                    n1(i)
            kb.barrier()

        with ExitStack() as e2:
            wst = Tn(e2, "wst", [128, 32, 128]); W1 = [Tn(e2, f"W1{j}", [128, 32, 128], BF16) for j in range(2)]
            w2s = Tn(e2, "w2s", [128, 64]); W2p = Tn(e2, "W2p", [128, 2, 128], BF16); W2v = Tn(e2, "W2v", [128, 64], BF16)
            psT = Tn(e2, "psT", [128, 32]); posb = [Tn(e2, f"posb{j}", [128, 32, 2], BF16) for j in range(2)]
            cbv = [Tn(e2, f"cbv{j}", [128, 2]) for j in range(2)]
            XT = Tn(e2, "XT", [128, 128 * 16 + 16], BF16)
            hb = Tn(e2, "hb", [128, 128]); tt = Tn(e2, "tt", [128, 128]); GT = [Tn(e2, f"GT{g}", [128, 128], BF16) for g in range(2)]
            ph = Tile(self.ps(e2, "nsa_ph", [128, 128]), "nsa_ph"); pc = Tile(self.ps(e2, "nsa_pc", [128, 128]), "nsa_pc")
            pb = Tile(self.ps(e2, "nsa_pb", [128, 2]), "nsa_pb")
            V(lambda: nc.vector.memset(W2p.t[:], 0.0), [], [W2p])
            for wi, (w1d, w2d, posd) in enumerate([(self.nsa_cmp_w1_k, self.nsa_cmp_w2_k, self.nsa_cmp_pos_k),
                                                   (self.nsa_cmp_w1_v, self.nsa_cmp_w2_v, self.nsa_cmp_pos_v)]):
                for half in range(2):
                    LD(wst, wst.t[half * 64:(half + 1) * 64, :, :], w1d[l].rearrange("(i d) j -> d i j", d=64))
                V(lambda: nc.vector.tensor_copy(out=W1[wi].t[:], in_=wst.t[:]), [wst], [W1[wi]])
                LD(w2s, w2s.t[:], w2d[l, :, :])
                if wi == 0:
                    for g in range(2):
                        V(lambda: nc.vector.tensor_copy(out=W2p.t[:, g, g * 64:(g + 1) * 64], in_=w2s.t[:]), [w2s], [W2p])
                else:
                    V(lambda: nc.vector.tensor_copy(out=W2v.t[:], in_=w2s.t[:]), [w2s], [W2v])
                for half in range(2):
                    LD(psT, psT.t[half * 64:(half + 1) * 64, :], posd[l].rearrange("i d -> d i"), allow_slow_non_contiguous=True)
                for j in range(2):
                    V(lambda: nc.vector.tensor_copy(out=posb[wi].t[:, :, j], in_=psT.t[:]), [psT], [posb[wi]])
                for i_ in range(32):
                    PE(lambda: nc.tensor.matmul(out=pb.t[:], lhsT=W1[wi].t[0:64, i_, :], rhs=posb[wi].t[0:64, i_, :], start=(i_ == 0), stop=(i_ == 31)),
                       [W1[wi], posb[wi]], [pb])
                V(lambda: nc.vector.tensor_copy(out=cbv[wi].t[:], in_=pb.t[:]), [pb], [cbv[wi]])
            for c in range(NCC):
                n0 = c * 128
                nn = min(128, NCB - n0)
                ntok = 16 * nn + 16
                for wi, xd, xkey in [(0, self.kcT_d, "kcT_d"), (1, self.vcT_d, "vcT_d")]:
                    LD(XT, XT.t[:, 0:ntok], xd[:, 16 * n0:16 * n0 + ntok],
                       reads=[(xkey, j) for j in range((16 * n0) // 128, min(NT, (16 * n0 + ntok + 127) // 128))])
                    xv = XT.t[:, 0:ntok].rearrange("p (n s) -> p n s", s=16)
                    for g in range(2):
                        for i_ in range(32):
                            a, b = i_ // 16, i_ % 16
                            PE(lambda: nc.tensor.matmul(out=ph.t[:, 0:nn], lhsT=W1[wi].t[g * 64:(g + 1) * 64, i_, :],
                                                        rhs=xv[g * 64:(g + 1) * 64, a:a + nn, b], start=(i_ == 0), stop=(i_ == 31)),
                               [W1[wi], XT], [ph])
                        AC(lambda: nc.scalar.activation(out=hb.t[:, 0:nn], in_=ph.t[:, 0:nn], func=AF.Identity, bias=cbv[wi].t[:, 0:1]), [ph, cbv[wi]], [hb])
                        V(lambda: nc.vector.tensor_tensor(out=tt.t[:, 0:nn], in0=hb.t[:, 0:nn], in1=hb.t[:, 0:nn], op=ALU.mult), [hb], [tt])
                        V(lambda: nc.vector.tensor_scalar(out=tt.t[:, 0:nn], in0=tt.t[:, 0:nn], scalar1=0.044715, scalar2=1.0, op0=ALU.mult, op1=ALU.add), [tt], [tt])
                        V(lambda: nc.vector.tensor_tensor(out=tt.t[:, 0:nn], in0=tt.t[:, 0:nn], in1=hb.t[:, 0:nn], op=ALU.mult), [tt, hb], [tt])
                        AC(lambda: nc.scalar.activation(out=tt.t[:, 0:nn], in_=tt.t[:, 0:nn], func=AF.Tanh, scale=0.7978845608028654), [tt], [tt])
                        V(lambda: nc.vector.tensor_scalar(out=tt.t[:, 0:nn], in0=tt.t[:, 0:nn], scalar1=0.5, scalar2=0.5, op0=ALU.mult, op1=ALU.add), [tt], [tt])
                        if nn < 128:
                            V(lambda: nc.vector.memset(GT[g].t[:], 0.0), [], [GT[g]])
                        V(lambda: nc.vector.tensor_tensor(out=GT[g].t[:, 0:nn], in0=tt.t[:, 0:nn], in1=hb.t[:, 0:nn], op=ALU.mult), [tt, hb], [GT[g]])
                    if wi == 0:
                        for g in range(2):
                            PE(lambda: nc.tensor.matmul(out=pc.t[:, 0:nn], lhsT=W2p.t[:, g, :], rhs=GT[g].t[:, 0:nn], start=(g == 0), stop=(g == 1)),
                               [W2p, GT[g]], [pc])
                        AC(lambda: nc.scalar.copy(out=KCT.t[:, n0:n0 + nn], in_=pc.t[:, 0:nn]), [pc], [KCT])
                    else:
                        for g in range(2):
                            PE(lambda: nc.tensor.matmul(out=pc.t[:, g * 64:(g + 1) * 64], lhsT=GT[g].t[:], rhs=W2v.t[:], start=True, stop=True),
                               [GT[g], W2v], [pc])
                        AC(lambda: nc.scalar.copy(out=RC.t[:, c, :, 0:64], in_=pc.t[:].rearrange("p (g d) -> p g d", g=2)), [pc], [RC])
            ovs = Tn(e2, "ovs", [128, NCC, NS], BF16)
            LD(ovs, ovs.t[:], self.c_ovl[:, :, :])
            for g in range(2):
                V(lambda: nc.vector.tensor_copy(out=RC.t[:, :, g, 65:65 + NS], in_=ovs.t[:]), [ovs, RC], [RC])
                V(lambda: nc.vector.memset(RC.t[:, :, g, 64:65], 1.0), [RC], [RC])
            kb.barrier()

        with ExitStack() as e3:
            def T3(name, shape, dt=F32):
                return Tn(e3, name, shape, dt)
            LA = 4
            Ew = T3("Ew", [128, S], BF16); cmk = T3("cmk", [128, 17, 128], BF16)
            Fw = T3("Fw", [128, 2 * NS - 2]); Kw = T3("Kw", [128, 2 * NS - 2])
            trib = T3("trib", [128, 128], BF16); sltb = T3("sltb", [128, 128], BF16)
            LD(Ew, Ew.t[:], self.c_Ew[:, :]); LD(cmk, cmk.t[:], self.c_cmask[:, :, :])
            LD(Fw, Fw.t[:], self.c_Fw[:, :]); LD(Kw, Kw.t[:], self.c_Kw[:, :])
            LD(trib, trib.t[:], self.c_trib[:, :]); LD(sltb, sltb.t[:], self.c_sltb[:, :])
            qz = [[T3(f"qz{p_}{g}", [128, 2, 128], BF16) for g in range(2)] for p_ in range(2)]
            pe_ = [T3(f"pe{j}", [128, 2, 128], BF16) for j in range(LA + 1)]
            rl = T3("rl", [128, 1]); wgt = T3("wgt", [128, 1])
            IMP = [T3(f"IMP{g}", [128, NS]) for g in range(2)]; impm = [T3(f"impm{g}", [128, NS]) for g in range(2)]
            wk = [T3(f"wk{g}", [128, NS]) for g in range(2)]
            m8a = [T3(f"m8a{g}", [128, 8]) for g in range(2)]; m8b = [T3(f"m8b{g}", [128, 8]) for g in range(2)]
            sneg = [T3(f"sneg{g}", [128, NS], BF16) for g in range(2)]
            SNT = [T3(f"SNT{g}", [128, 2, 128], BF16) for g in range(2)]
            oacc = [T3(f"oacc{j}", [128, 4, 64]) for j in range(2)]
            sq = T3("sq", [128, 256]); ss = T3("ss", [128, 4]); mo = [T3(f"mo{j}", [128, 256], BF16) for j in range(2)]
            psc = [Tile(self.ps(e3, f"nsa_psc{j}", [128, 2, 128]), f"nsa_psc{j}") for j in range(LA + 1)]
            pcvb = Tile(self.ps(e3, "nsa_pcv", [128, 2, 65 + NS]), "nsa_pcv")
            posb = Tile(self.ps(e3, "nsa_pos", [128, 2, 65]), "nsa_pos")
            pcv = [Tile(pcvb.t[:, r, :], pcvb.k) for r in range(2)]
            pos_ = [Tile(posb.t[:, r, :], posb.k) for r in range(2)]
            pst = Tile(self.ps(e3, "nsa_pst", [128, 128], BF16), "nsa_pst")
            for g in range(2):
                V(lambda: nc.vector.memset(SNT[g].t[:], 0.0), [], [SNT[g]])
            nsc = [0]
            pending = []

            def emit_score(job):
                j = nsc[0] % (LA + 1)
                nsc[0] += 1
                q_ = job["q"]
                extra = job.get("extra")
                PE(lambda: nc.tensor.matmul(out=psc[j].t[:], lhsT=job["lhsT"], rhs=q_.t[:], start=True, stop=(extra is None)),
                   job["lt"] + [q_], [psc[j]])
                if extra is not None:
                    sn = job["snt"]
                    PE(lambda: nc.tensor.matmul(out=psc[j].t[:], lhsT=extra, rhs=sn.t[:], start=False, stop=True), [Ew, sn], [psc[j]])
                AC(lambda: nc.scalar.activation(out=pe_[j].t[:], in_=psc[j].t[:], func=AF.Exp, scale=0.125), [psc[j]], [pe_[j]])
                if job.get("mask") is not None:
                    mt_, mk = job["mask"]
                    GP(lambda: nc.gpsimd.tensor_tensor(out=pe_[j].t[:], in0=pe_[j].t[:], in1=mk.unsqueeze(1).to_broadcast([128, 2, 128]), op=ALU.mult),
                       [pe_[j], mt_], [pe_[j]])
                job["pe"] = pe_[j]

            def emit_pv(job):
                pt = job["pe"]
                for r in range(2):
                    tgt = job["tgt"][r]
                    PE(lambda: nc.tensor.matmul(out=tgt.t[:], lhsT=pt.t[:, r, :], rhs=job["rhs"], start=(job["start"] and r == 0),
                                                stop=(job["stop"] and r == 1)),
                       [pt] + job["vt"], [tgt])
                if job.get("after") is not None:
                    job["after"]()

            def push(job):
                emit_score(job)
                pending.append(job)
                while len(pending) > LA:
                    emit_pv(pending.pop(0))

            def finish(pt, oa, h, b, i, first):
                V(lambda: nc.vector.tensor_scalar(out=rl.t[:], in0=pt.t[:, 64:65], scalar1=1e-30, scalar2=None, op0=ALU.max), [pt], [rl])
                V(lambda: nc.vector.reciprocal(out=rl.t[:], in_=rl.t[:]), [rl], [rl])
                V(lambda: nc.vector.tensor_tensor(out=wgt.t[:], in0=rl.t[:], in1=GA.t[:, i, 3 * h + b:3 * h + b + 1], op=ALU.mult),
                  [rl, Tile(None, f"nsa_GA{i}")], [wgt])
                if first:
                    V(lambda: nc.vector.tensor_scalar(out=oa.t[:, h, :], in0=pt.t[:, 0:64], scalar1=wgt.t[:], scalar2=None, op0=ALU.mult), [pt, wgt], [oa])
                else:
                    V(lambda: nc.vector.scalar_tensor_tensor(out=oa.t[:, h, :], in0=pt.t[:, 0:64], scalar=wgt.t[:], in1=oa.t[:, h, :],
                                                             op0=ALU.mult, op1=ALU.add), [pt, wgt, oa], [oa])

            def after_cmp(i, g, oa):
                def f():
                    for r in range(2):
                        finish(pcv[r], oa, 2 * g + r, 0, i, True)
                        if r == 0:
                            V(lambda: nc.vector.tensor_scalar(out=IMP[g].t[:], in0=pcv[r].t[:, 65:65 + NS], scalar1=rl.t[:], scalar2=None, op0=ALU.mult),
                              [pcv[r], rl], [IMP[g]])
                        else:
                            V(lambda: nc.vector.scalar_tensor_tensor(out=IMP[g].t[:], in0=pcv[r].t[:, 65:65 + NS], scalar=rl.t[:], in1=IMP[g].t[:],
                                                                     op0=ALU.mult, op1=ALU.add), [pcv[r], rl, IMP[g]], [IMP[g]])
                    fo = NS - 2 - 2 * i
                    V(lambda: nc.vector.tensor_tensor(out=impm[g].t[:], in0=IMP[g].t[:], in1=Kw.t[:, fo:fo + NS], op=ALU.mult), [IMP[g], Kw], [impm[g]])
                    V(lambda: nc.vector.tensor_tensor(out=impm[g].t[:], in0=impm[g].t[:], in1=Fw.t[:, fo:fo + NS], op=ALU.add), [impm[g], Fw], [impm[g]])
                    V(lambda: nc.vector.memset(impm[g].t[:, 0:1], 1e30), [impm[g]], [impm[g]])
                    V(lambda: nc.vector.max(out=m8a[g].t[:], in_=impm[g].t[:]), [impm[g]], [m8a[g]])
                    V(lambda: nc.vector.match_replace(out=wk[g].t[:], in_to_replace=m8a[g].t[:], in_values=impm[g].t[:], imm_value=-3e38),
                      [m8a[g], impm[g]], [wk[g]])
                    V(lambda: nc.vector.max(out=m8b[g].t[:], in_=wk[g].t[:]), [wk[g]], [m8b[g]])
                    V(lambda: nc.vector.tensor_scalar(out=sneg[g].t[:], in0=impm[g].t[:], scalar1=m8b[g].t[:, 7:8], scalar2=1.0, op0=ALU.is_ge, op1=ALU.subtract),
                      [impm[g], m8b[g]], [sneg[g]])
                return f

            def after_fin(tgt, i, g, b, oa):
                def f():
                    for r in range(2):
                        finish(tgt[r], oa, 2 * g + r, b, i, False)
                return f

            def tail(i, oa):
                def f():
                    s = i % 2
                    t0 = i * 128
                    o2 = oa.t[:].rearrange("p h d -> p (h d)")
                    AC(lambda: nc.scalar.activation(out=sq.t[:], in_=o2, func=AF.Square), [oa], [sq])
                    V(lambda: nc.vector.tensor_reduce(out=ss.t[:], in_=sq.t[:].rearrange("p (h d) -> p h d", h=4), axis=AX.X, op=ALU.add), [sq], [ss])
                    V(lambda: nc.vector.tensor_scalar(out=ss.t[:], in0=ss.t[:], scalar1=1.0 / 64, scalar2=EPS, op0=ALU.mult, op1=ALU.add), [ss], [ss])
                    AC(lambda: nc.scalar.activation(out=ss.t[:], in_=ss.t[:], func=AF.Sqrt), [ss], [ss])
                    V(lambda: nc.vector.reciprocal(out=ss.t[:], in_=ss.t[:]), [ss], [ss])
                    V(lambda: nc.vector.tensor_tensor(out=sq.t[:].rearrange("p (h d) -> p h d", h=4), in0=oa.t[:],
                                                      in1=ss.t[:].unsqueeze(2).to_broadcast([128, 4, 64]), op=ALU.mult), [oa, ss, sq], [sq])
                    V(lambda: nc.vector.tensor_tensor(out=mo[s].t[:], in0=sq.t[:], in1=nwb.t[:], op=ALU.mult), [sq, nwb], [mo[s]])
                    kb.dma("sp", mo[s].k, self.mix[t0:t0 + 128, 256:512], mo[s].t[:], reads=[mo[s].k], writes=[("mix_nsa", i)])
                return f

            for i in range(NT):
                qq = qz[i % 2]
                oa = oacc[i % 2]
                for g in range(2):
                    AC(lambda: nc.scalar.copy(out=qq[g].t[:], in_=QT.t[:, i, :, :]), [Tile(None, f"nsa_QT{i}")], [qq[g]])
                    o0 = (1 - g) * 64
                    GP(lambda: nc.gpsimd.memset(qq[g].t[o0:o0 + 64, :, :], 0.0), [qq[g]], [qq[g]])
                ncs = min(NCC, i // 16 + 1)
                k0 = max(0, i - 4)
                for g in range(2):
                    for c in range(ncs):
                        dl = i - 16 * c
                        push(dict(lhsT=KCT.t[:, c * 128:(c + 1) * 128], lt=[KCT], q=qq[g],
                                  mask=(cmk, cmk.t[:, dl, :]) if dl <= 16 else None,
                                  tgt=pcv, rhs=RC.t[:, c, g, :], vt=[RC], start=(c == 0), stop=(c == ncs - 1),
                                  after=after_cmp(i, g, oa) if c == ncs - 1 else None, cmpg=g))
                    for kt in range(k0, i + 1):
                        mf = (trib, trib.t[:]) if kt == i else ((sltb, sltb.t[:]) if kt == i - 4 else None)
                        push(dict(lhsT=KwT.t[:, kt * 128:(kt + 1) * 128], lt=[Tile(None, f"nsa_KwT{kt}")], q=qq[g], mask=mf,
                                  tgt=[Tile(pcv[0].t[:, 0:65], pcv[0].k), Tile(pcv[1].t[:, 0:65], pcv[1].k)],
                                  rhs=VwA.t[:, kt, g, :], vt=[Tile(None, f"nsa_VwA{kt}")], start=(kt == k0), stop=(kt == i),
                                  after=after_fin(pcv, i, g, 2, oa) if kt == i else None))
                while any(j_.get("cmpg") is not None for j_ in pending):
                    emit_pv(pending.pop(0))
                for g in range(2):
                    PE(lambda: nc.tensor.transpose(out=pst.t[0:NS, :], in_=sneg[g].t[:], identity=idb.t[:]), [sneg[g], idb], [pst])
                    for r in range(2):
                        AC(lambda: nc.scalar.copy(out=SNT[g].t[0:NS, r, :], in_=pst.t[0:NS, :]), [pst], [SNT[g]])
                    for kt in range(i + 1):
                        last = (kt == i)
                        aft = None
                        if last:
                            fin = after_fin(pos_, i, g, 1, oa)
                            if g == 1:
                                tl_ = tail(i, oa)
                                aft = (lambda fin=fin, tl_=tl_: (fin(), tl_()))
                            else:
                                aft = fin
                        push(dict(lhsT=KsT.t[:, kt * 128:(kt + 1) * 128], lt=[Tile(None, f"nsa_KsT{kt}")], q=qq[g],
                                  mask=(trib, trib.t[:]) if last else None, extra=Ew.t[:, kt * 128:(kt + 1) * 128], snt=SNT[g],
                                  tgt=pos_, rhs=VsA.t[:, kt, g, :], vt=[Tile(None, f"nsa_VsA{kt}")], start=(kt == 0), stop=last, after=aft))
            while pending:
                emit_pv(pending.pop(0))


Prog.stage_nsa = _stage_nsa


def build_full(S, L=2):
    p = Prog(S, L=L, dbg=[("out", [S, D])])
    kb = p.kb
    with kb.es:
        h = p.x
        for l in range(L):
            p.stage_d1(l, h)
            kb.barrier()
            p.stage_ssd(l)
            kb.barrier()
            p.stage_gla(l)
            kb.barrier()
            p.stage_nsa(l)
            kb.barrier()
            final = (l == L - 1)
            p.stage_d2(l, h, p.outs["out"] if final else p.hbuf, final)
            kb.barrier()
            h = p.hbuf
        kb.drain("sp")
    return p


_WNAMES = ["norm1_w", "w_in", "gla_gate_w2", "gla_gate_b", "gla_norm_w", "nsa_cmp_pos_k", "nsa_cmp_w1_k", "nsa_cmp_w2_k",
           "nsa_cmp_pos_v", "nsa_cmp_w1_v", "nsa_cmp_w2_v", "nsa_norm_w", "ssd_conv_w", "ssd_conv_b", "ssd_dt_bias",
           "ssd_a_log", "ssd_d", "ssd_norm_w", "w_out", "norm2_w", "w_up", "w_down", "final_norm_w"]


def kernel(**inputs):
    x = np.asarray(inputs["x"], dtype=np.float32)
    B, S, _ = x.shape
    L = int(np.asarray(inputs["w_in"]).shape[0])
    p = build_full(S, L)
    base = {k: np.ascontiguousarray(np.asarray(inputs[k], dtype=np.float32)) for k in _WNAMES}
    base.update(consts(S))
    in_maps = []
    for b in range(B):
        m = dict(base)
        m["x"] = np.ascontiguousarray(x[b])
        in_maps.append(m)
    res = run_bass_kernel_spmd(p.nc, in_maps, core_ids=list(range(B)))
    return np.stack([np.asarray(res.results[b]["out"], dtype=np.float32) for b in range(B)], axis=0)
```

```python
import numpy as np
import ml_dtypes
import concourse.bass as bass
import concourse.mybir as mybir
from concourse.bass_utils import run_bass_kernel_spmd
from contextlib import ExitStack
import threading

F32 = mybir.dt.float32
BF16 = mybir.dt.bfloat16
AF = mybir.ActivationFunctionType
ALU = mybir.AluOpType
AX = mybir.AxisListType

D = 1024
DIN = 3364
DFF = 4096
NPROJ = 2340
EPS = 1e-6

C_GQ, C_GK, C_GV, C_GLR, C_GR = 0, 128, 256, 512, 528
C_NQ, C_KCMP, C_KSLC, C_KWIN, C_VCMP, C_VSLC, C_VWIN, C_NG = 784, 1040, 1168, 1296, 1424, 1552, 1680, 1808
C_Z, C_DT = 1820, 2332
WMAP = [(0, 784, 0), (784, 64, 784), (912, 64, 848), (848, 64, 912), (976, 64, 976), (1040, 128, 1040), (1296, 128, 1168), (1552, 128, 1296), (1168, 128, 1424),
        (1424, 128, 1552), (1680, 652, 1680), (3356, 8, 2332), (2332, 1024, -1)]


class KB:
    EPOCH = 30000

    def __init__(self, nc, same_engine_sync=True):
        self.nc = nc
        self.es = ExitStack()
        self.eng = {"pe": nc.tensor, "act": nc.scalar, "dve": nc.vector,
                    "pool": nc.gpsimd, "sp": nc.sync}
        self.esem = {}
        self.ecnt = {}
        self.nsem = 0
        self.dsem = {}
        self.waited = {e: {} for e in self.eng}
        self.last_w = {}
        self.readers = {}
        self.same = same_engine_sync
        self.sem_owner = {}
        self.ninst = 0
        self.limit = None
        self.hook = None
        for e in self.eng:
            self._new_esem(e)

    def _sem(self, name):
        self.nsem += 1
        return self.es.enter_context(self.nc.semaphore(f"s{self.nsem}_{name}"))

    def _new_esem(self, e):
        s = self._sem(e)
        self.esem[e] = s
        self.ecnt[e] = 0
        self.sem_owner[id(s)] = e

    def _wait(self, e, deps):
        best = {}
        for item in deps:
            if item is None:
                continue
            if len(item) == 3:
                s, v, raw = item
            else:
                (s, v), raw = item, True
            owner = self.sem_owner.get(id(s))
            if owner == e and (e == "pe" or not self.same):
                continue
            if best.get(id(s), (None, 0))[1] < v:
                best[id(s)] = (s, v)
        for sid, (s, v) in best.items():
            if self.waited[e].get(sid, 0) < v:
                self.eng[e].wait_ge(s, v)
                self.waited[e][sid] = v

    def _deps(self, reads, writes):
        deps = []
        for k in reads:
            ev = self.last_w.get(k)
            if ev is not None:
                deps.append((ev[0], ev[1], True))
        for k in writes:
            ev = self.last_w.get(k)
            if ev is not None:
                deps.append((ev[0], ev[1], False))
            for ev in self.readers.get(k, []):
                deps.append((ev[0], ev[1], False))
        return deps

    def _commit(self, ev, reads, writes):
        for k in writes:
            self.last_w[k] = ev
            self.readers[k] = []
        for k in reads:
            if k in writes:
                continue
            self.readers.setdefault(k, []).append(ev)

    def op(self, e, fn, reads=(), writes=()):
        if self.hook is not None:
            self.hook()
        if self.limit is not None and self.ninst >= self.limit:
            return None
        self._wait(e, self._deps(reads, writes))
        inst = fn()
        if self.ecnt[e] >= self.EPOCH:
            self._new_esem(e)
        self.ecnt[e] += 1
        s = self.esem[e]
        inst.then_inc(s, 1)
        self._commit((s, self.ecnt[e]), reads, writes)
        self.ninst += 1
        return inst

    def dma(self, q, key, out, in_, reads=(), writes=(), **kw):
        if self.hook is not None:
            self.hook()
        if self.limit is not None and self.ninst >= self.limit:
            return None
        self._wait(q, self._deps(reads, writes))
        if key not in self.dsem:
            self.dsem[key] = [self._sem("d"), 0]
        ent = self.dsem[key]
        inst = self.eng[q].dma_start(out=out, in_=in_, **kw)
        ent[1] += 16
        inst.then_inc(ent[0], 16)
        self._commit((ent[0], ent[1]), reads, writes)
        self.ninst += 1
        return inst

    def barrier(self):
        deps = list(self.last_w.values())
        for r in self.readers.values():
            deps.extend(r)
        for e in self.eng:
            self._wait(e, deps)
        self.readers = {k: [] for k in self.readers}

    def drain(self, e="sp"):
        deps = list(self.last_w.values())
        for r in self.readers.values():
            deps.extend(r)
        self._wait(e, deps)


def interleave(kb, fa, fb, ra=1, rb=1):
    sem = {"a": threading.Semaphore(0), "b": threading.Semaphore(0)}
    done = {"a": False, "b": False}
    err = []
    loc = threading.local()

    quota = {"a": ra, "b": rb}
    cnt = {"a": 0, "b": 0}

    def hook():
        me = loc.me
        other = "b" if me == "a" else "a"
        if done[other]:
            return
        cnt[me] += 1
        if cnt[me] < quota[me]:
            return
        cnt[me] = 0
        sem[other].release()
        sem[me].acquire()

    def run(me, f):
        loc.me = me
        other = "b" if me == "a" else "a"
        sem[me].acquire()
        try:
            f()
        except BaseException as ex:
            err.append(ex)
        done[me] = True
        sem[other].release()

    kb.hook = hook
    ta = threading.Thread(target=run, args=("a", fa))
    tb = threading.Thread(target=run, args=("b", fb))
    ta.start()
    tb.start()
    sem["a"].release()
    ta.join()
    tb.join()
    kb.hook = None
    if err:
        raise err[0]


class Prog:
    def __init__(self, S, L=2, dbg=()):
        self.S = S
        self.NT = S // 128
        self.L = L
        self.dbg = dbg
        nc = self.nc = bass.Bass("TRN2", target_bir_lowering=False)
        self.kb = KB(nc)
        Ld = L

        def inp(name, shape, dt=F32):
            return nc.dram_tensor(name, list(shape), dt, kind="ExternalInput").ap()

        def scr(name, shape, dt=F32):
            return nc.dram_tensor(name, list(shape), dt, kind="Internal").ap()

        self.x = inp("x", [S, D])
        self.norm1_w = inp("norm1_w", [Ld, D])
        self.w_in = inp("w_in", [Ld, D, DIN])
        self.gla_gate_w2 = inp("gla_gate_w2", [Ld, 16, 128])
        self.gla_gate_b = inp("gla_gate_b", [Ld, 128])
        self.gla_norm_w = inp("gla_norm_w", [Ld, 256])
        self.nsa_cmp_pos_k = inp("nsa_cmp_pos_k", [Ld, 32, 64])
        self.nsa_cmp_w1_k = inp("nsa_cmp_w1_k", [Ld, 2048, 128])
        self.nsa_cmp_w2_k = inp("nsa_cmp_w2_k", [Ld, 128, 64])
        self.nsa_cmp_pos_v = inp("nsa_cmp_pos_v", [Ld, 32, 64])
        self.nsa_cmp_w1_v = inp("nsa_cmp_w1_v", [Ld, 2048, 128])
        self.nsa_cmp_w2_v = inp("nsa_cmp_w2_v", [Ld, 128, 64])
        self.nsa_norm_w = inp("nsa_norm_w", [Ld, 256])
        self.ssd_conv_w = inp("ssd_conv_w", [Ld, 4, 1024])
        self.ssd_conv_b = inp("ssd_conv_b", [Ld, 1024])
        self.ssd_dt_bias = inp("ssd_dt_bias", [Ld, 8])
        self.ssd_a_log = inp("ssd_a_log", [Ld, 8])
        self.ssd_d = inp("ssd_d", [Ld, 8])
        self.ssd_norm_w = inp("ssd_norm_w", [Ld, 512])
        self.w_out = inp("w_out", [Ld, D, D])
        self.norm2_w = inp("norm2_w", [Ld, D])
        self.w_up = inp("w_up", [Ld, D, DFF])
        self.w_down = inp("w_down", [Ld, DFF, D])
        self.final_norm_w = inp("final_norm_w", [D])
        self.ident_f = inp("ident_f", [128, 128])
        self.ident_b = inp("ident_b", [128, 128], BF16)
        self.c_tri = inp("c_tri", [128, 128])
        self.c_slt = inp("c_slt", [128, 128])
        self.c_ones = inp("c_ones", [128, 128])
        self.c_hm = inp("c_hm", [128, 4])
        self.c_bd = inp("c_bd", [128, 256])
        self.mix = scr("mix", [S, D], BF16)
        NS = S // 64
        self.NS = NS
        self.NCC = max(1, (S // 16 - 1 + 127) // 128)
        self.rope_cos = inp("rope_cos", [S, 32])
        self.rope_sin = inp("rope_sin", [S, 32])
        self.c_Ew = inp("c_Ew", [128, S], BF16)
        self.c_cmask = inp("c_cmask", [128, 17, 128], BF16)
        self.c_ovl = inp("c_ovl", [128, self.NCC, NS], BF16)
        self.c_Fw = inp("c_Fw", [128, 2 * NS - 2])
        self.c_Kw = inp("c_Kw", [128, 2 * NS - 2])
        self.c_trib = inp("c_trib", [128, 128], BF16)
        self.c_sltb = inp("c_sltb", [128, 128], BF16)
        self.kcT_d = scr("kcT_d", [128, S + 128], BF16)
        self.vcT_d = scr("vcT_d", [128, S + 128], BF16)
        self.proj = scr("proj", [S, NPROJ])
        self.xbcT = scr("xbcT", [D, S])
        self.hbuf = scr("hbuf", [S, D])
        self.outs = {}
        for name, shape in dbg:
            self.outs[name] = nc.dram_tensor(name, list(shape), F32, kind="ExternalOutput").ap()

    def sb(self, es, name, shape, dt=F32):
        self._uid = getattr(self, "_uid", 0) + 1
        return es.enter_context(self.nc.sbuf_tensor(f"{name}_u{self._uid}", list(shape), dt))

    def ps(self, es, name, shape, dt=F32):
        full = 512 if dt == F32 else 1024
        self._uid = getattr(self, "_uid", 0) + 1
        t = es.enter_context(self.nc.psum_tensor(f"{name}_u{self._uid}", [128, full], dt))
        n = 1
        for d in shape[1:]:
            n *= d
        assert n <= full
        v = t[0:shape[0], 0:n]
        if len(shape) == 3:
            v = v.rearrange("p (a b) -> p a b", a=shape[1])
        return v

    def stage_d1(self, l, hsrc):
        nc, kb, NT = self.nc, self.kb, self.NT
        with ExitStack() as es:
            Wtm = self.sb(es, "d1_Wtm", [128, 8, NPROJ], BF16)
            Wx = self.sb(es, "d1_Wx", [128, 8, 1024], BF16)
            stg = [self.sb(es, f"d1_stg{i}", [128, DIN]) for i in range(2)]
            nw = self.sb(es, "d1_nw", [128, 8])
            idb = self.sb(es, "d1_idb", [128, 128], BF16)
            ht = [self.sb(es, f"d1_ht{i}", [128, D]) for i in range(2)]
            junk = self.sb(es, "d1_junk", [128, D], BF16)
            ss = [self.sb(es, f"d1_ss{i}", [128, 1]) for i in range(2)]
            rs = [self.sb(es, f"d1_rs{i}", [128, 1]) for i in range(2)]
            u = [self.sb(es, f"d1_u{i}", [128, D], BF16) for i in range(2)]
            uT = [self.sb(es, f"d1_uT{i}", [128, 8, 128], BF16) for i in range(2)]
            ot = [self.sb(es, f"d1_ot{i}", [128, NPROJ]) for i in range(2)]
            xo = [self.sb(es, f"d1_xo{i}", [128, 8, 128]) for i in range(2)]
            pT = self.ps(es, "d1_pT", [128, 8, 128], BF16)
            pm = [self.ps(es, f"d1_pm{i}", [128, 512]) for i in range(3)]
            px = [self.ps(es, f"d1_px{i}", [128, 4, 128]) for i in range(2)]

            kb.dma("sp", "d1_idb", idb[:], self.ident_b[:, :], writes=["d1_idb"])
            for c in range(8):
                kb.dma("sp", "d1_nw", nw[:, c:c + 1],
                       self.norm1_w[l, c * 128:(c + 1) * 128].rearrange("(p o) -> p o", o=1),
                       writes=["d1_nw"])
            for c in range(8):
                s = c % 2
                kb.dma("sp", f"d1_stg{s}", stg[s][:], self.w_in[l, c * 128:(c + 1) * 128, :],
                       writes=[f"d1_stg{s}"])
                for j, (so, n, do) in enumerate(WMAP):
                    dst = Wx[:, c, 0:n] if do < 0 else Wtm[:, c, do:do + n]
                    wk = f"d1_W{c}"
                    if j % 2 == 0:
                        kb.op("dve", lambda: nc.vector.tensor_scalar(
                            out=dst, in0=stg[s][:, so:so + n], scalar1=nw[:, c:c + 1], scalar2=None,
                            op0=ALU.mult), reads=[f"d1_stg{s}", "d1_nw"], writes=[wk + f"_{j}"])
                    else:
                        kb.op("act", lambda: nc.scalar.activation(
                            out=dst, in_=stg[s][:, so:so + n], func=AF.Copy, scale=nw[:, c:c + 1]),
                            reads=[f"d1_stg{s}", "d1_nw"], writes=[wk + f"_{j}"])
            Wkeys = [f"d1_W{c}_{j}" for c in range(8) for j in range(len(WMAP))]

            def LDA(i):
                s = i % 2
                kb.dma("sp", f"d1_ht{s}", ht[s][:], hsrc[i * 128:(i + 1) * 128, :],
                       reads=[("h", i)], writes=[f"d1_ht{s}"])

            def A(i):
                s = i % 2
                kb.op("act", lambda: nc.scalar.activation(out=junk[:], in_=ht[s][:], func=AF.Square,
                                                          accum_out=ss[s][:]),
                      reads=[f"d1_ht{s}"], writes=["d1_junk", f"d1_ss{s}"])
                kb.op("dve", lambda: nc.vector.tensor_scalar(out=rs[s][:], in0=ss[s][:], scalar1=1.0 / D,
                                                             scalar2=EPS, op0=ALU.mult, op1=ALU.add),
                      reads=[f"d1_ss{s}"], writes=[f"d1_rs{s}"])
                kb.op("act", lambda: nc.scalar.activation(out=rs[s][:], in_=rs[s][:], func=AF.Sqrt),
                      reads=[f"d1_rs{s}"], writes=[f"d1_rs{s}"])
                kb.op("dve", lambda: nc.vector.reciprocal(out=rs[s][:], in_=rs[s][:]),
                      reads=[f"d1_rs{s}"], writes=[f"d1_rs{s}"])
                kb.op("dve", lambda: nc.vector.tensor_scalar(out=u[s][:], in0=ht[s][:], scalar1=rs[s][:],
                                                             scalar2=None, op0=ALU.mult),
                      reads=[f"d1_ht{s}", f"d1_rs{s}"], writes=[f"d1_u{s}"])
                for c in range(8):
                    kb.op("pe", lambda: nc.tensor.transpose(out=pT[:, c, :], in_=u[s][:, c * 128:(c + 1) * 128],
                                                            identity=idb[:]),
                          reads=[f"d1_u{s}", "d1_idb"], writes=["d1_pT"])
                kb.op("act", lambda: nc.scalar.copy(out=uT[s][:], in_=pT[:]),
                      reads=["d1_pT"], writes=[f"d1_uT{s}"])

            def B(i):
                s = i % 2
                off = 0
                k = 0
                while off < NPROJ:
                    n = min(512, NPROJ - off)
                    p = pm[k % 3]
                    pk = f"d1_pm{k % 3}"
                    for c in range(8):
                        kb.op("pe", lambda: nc.tensor.matmul(out=p[:, 0:n], lhsT=uT[s][:, c, :],
                                                             rhs=Wtm[:, c, off:off + n],
                                                             start=(c == 0), stop=(c == 7)),
                              reads=[f"d1_uT{s}"] + (Wkeys if i == 0 and c == 0 and k == 0 else []),
                              writes=[pk])
                    if k % 2 == 0:
                        kb.op("dve", lambda: nc.vector.tensor_copy(out=ot[s][:, off:off + n], in_=p[:, 0:n]),
                              reads=[pk], writes=[f"d1_ot{s}_{k}"])
                    else:
                        kb.op("act", lambda: nc.scalar.copy(out=ot[s][:, off:off + n], in_=p[:, 0:n]),
                              reads=[pk], writes=[f"d1_ot{s}_{k}"])
                    off += n
                    k += 1
                kb.dma("sp", f"d1_ot{s}", self.proj[i * 128:(i + 1) * 128, :], ot[s][:],
                       reads=[f"d1_ot{s}_{j}" for j in range(k)], writes=[("proj", i)])
                for half in range(2):
                    p = px[half]
                    pk = f"d1_px{half}"
                    for j in range(4):
                        cc = half * 4 + j
                        for c in range(8):
                            kb.op("pe", lambda: nc.tensor.matmul(out=p[:, j, :], lhsT=Wx[:, c, cc * 128:(cc + 1) * 128],
                                                                 rhs=uT[s][:, c, :],
                                                                 start=(c == 0), stop=(c == 7)),
                                  reads=[f"d1_uT{s}"], writes=[pk])
                    if half == 0:
                        kb.op("dve", lambda: nc.vector.tensor_copy(out=xo[s][:, 0:4, :], in_=p[:]),
                              reads=[pk], writes=[f"d1_xo{s}_0"])
                    else:
                        kb.op("act", lambda: nc.scalar.copy(out=xo[s][:, 4:8, :], in_=p[:]),
                              reads=[pk], writes=[f"d1_xo{s}_1"])
                kb.dma("sp", f"d1_xo{s}",
                       self.xbcT.rearrange("(cc p) t -> p cc t", p=128)[:, :, i * 128:(i + 1) * 128],
                       xo[s][:], reads=[f"d1_xo{s}_0", f"d1_xo{s}_1"], writes=[("xbcT", i)])

            LDA(0)
            if NT > 1:
                LDA(1)
            A(0)
            for i in range(NT):
                if i + 2 < NT:
                    LDA(i + 2)
                if i + 1 < NT:
                    interleave(kb, (lambda i=i: B(i)), (lambda i=i: A(i + 1)), ra=6, rb=1)
                else:
                    B(i)

    def dump(self, name, src_ap):
        kb = self.kb
        kb.drain("sp")
        kb.dma("sp", "dump_" + name, self.outs[name], src_ap, writes=[("dump", name)])

    def finish(self):
        self.kb.drain("sp")
        self.kb.es.close()


def build_test_d1(S):
    p = Prog(S, L=1, dbg=[("o_proj", [S, NPROJ]), ("o_xbcT", [D, S])])
    with p.kb.es:
        p.stage_d1(0, p.x)
        p.dump("o_proj", p.proj[:, :])
        p.dump("o_xbcT", p.xbcT[:, :])
        p.kb.drain("sp")
    return p


def consts(S):
    r = np.arange(128)
    NS = S // 64
    NCC = max(1, (S // 16 - 1 + 127) // 128)
    pos = np.arange(S, dtype=np.float32)
    inv = (np.float32(10000.0) ** (-np.arange(32, dtype=np.float32) / np.float32(32))).astype(np.float32)
    ang = (pos[:, None] * inv[None, :]).astype(np.float32)
    Ew = np.zeros((128, S), np.float32)
    cc = np.arange(S)
    Ew[cc // 64 % 128, cc] = 30000.0
    cm = np.zeros((128, 17, 128), np.float32)
    for d_ in range(17):
        cm[:, d_, :] = (128 * d_ + r[None, :] - 16 * r[:, None] >= 31)
    n_all = np.arange(NCC * 128)
    j_all = np.arange(NS)
    ov = ((16 * n_all[:, None] < 64 * j_all[None, :] + 64) & (16 * n_all[:, None] + 32 > 64 * j_all[None, :])).astype(np.float32)
    ov = ov.reshape(NCC, 128, NS).transpose(1, 0, 2)
    cw = np.arange(2 * NS - 2)
    rel = cw[None, :] - (NS - 2) - (r[:, None] >= 64)
    Fw = np.where(rel > 0, -1e30, np.where(rel >= -1, 1e30, 0.0)).astype(np.float32)
    Kw = ((rel < -1)).astype(np.float32)
    bf = ml_dtypes.bfloat16
    hm = np.zeros((128, 4), np.float32)
    bd = np.zeros((128, 256), np.float32)
    for h in range(4):
        hm[32 * h:32 * h + 32, h] = 1.0
        bd[32 * h:32 * h + 32, 64 * h:64 * h + 64] = 1.0
    return {
        "ident_f": np.eye(128, dtype=np.float32),
        "ident_b": np.eye(128, dtype=np.float32).astype(ml_dtypes.bfloat16),
        "c_tri": (r[:, None] <= r[None, :]).astype(np.float32),
        "c_slt": (r[:, None] > r[None, :]).astype(np.float32),
        "c_ones": np.ones((128, 128), np.float32),
        "c_hm": hm,
        "c_bd": bd,
        "rope_cos": np.cos(ang).astype(np.float32),
        "rope_sin": np.sin(ang).astype(np.float32),
        "c_Ew": Ew.astype(bf),
        "c_cmask": cm.astype(bf),
        "c_ovl": np.ascontiguousarray(ov).astype(bf),
        "c_Fw": Fw,
        "c_Kw": Kw,
        "c_trib": (r[:, None] <= r[None, :]).astype(np.float32).astype(bf),
        "c_sltb": (r[:, None] > r[None, :]).astype(np.float32).astype(bf),
    }


class Tile:
    def __init__(self, t, k):
        self.t = t
        self.k = k


def _stage_ssd(self, l):
    nc, kb, NT = self.nc, self.kb, self.NT
    with ExitStack() as es:
        def T(name, shape, dt=F32):
            return Tile(self.sb(es, "ssd_" + name, shape, dt), "ssd_" + name)

        def T2(name, shape, dt=F32):
            return [T(f"{name}{j}", shape, dt) for j in range(2)]

        def PT(name, shape, dt=F32):
            return Tile(self.ps(es, "ssd_" + name, shape, dt), "ssd_" + name)

        def V(fn, r, w):
            return kb.op("dve", fn, [t.k for t in r], [t.k for t in w])

        def AC(fn, r, w):
            return kb.op("act", fn, [t.k for t in r], [t.k for t in w])

        def PE(fn, r, w):
            return kb.op("pe", fn, [t.k for t in r], [t.k for t in w])

        def GP(fn, r, w):
            return kb.op("pool", fn, [t.k for t in r], [t.k for t in w])

        def LD(t, dst, src, reads=(), **kw):
            return kb.dma("sp", t.k, dst, src, reads=list(reads), writes=[t.k], **kw)

        cw = T("cw", [128, 4, 8]); cb = T("cb", [128, 8]); dtb = T("dtb", [128, 8])
        Aneg = T("Aneg", [128, 8]); dsk = T("dsk", [128, 8]); nwb = T("nwb", [128, 512])
        tri = T("tri", [128, 128]); slt = T("slt", [128, 128]); ones = T("ones", [128, 128])
        idf = T("idf", [128, 128]); idb = T("idb", [128, 128], BF16)
        hT = T("hT", [128, 512]); hTb = T("hTb", [128, 512], BF16)
        xh = T2("xh", [128, 8, 131]); zt = T2("zt", [128, 512]); dtt = T2("dtt", [128, 8])
        acc = T2("acc", [128, 8, 128])
        xsT = T2("xsT", [128, 4, 128])
        BT = T2("BT", [128, 2, 128], BF16); CT = T2("CT", [128, 2, 128], BF16)
        xtm = T2("xtm", [128, 512]); Btm = T2("Btm", [128, 2, 128], BF16)
        sp_a = T2("sp_a", [128, 8]); dts = T2("dts", [128, 8]); dA = T2("dA", [128, 8])
        acs = T2("acs", [128, 16]); eacs = T2("eacs", [128, 8]); dsd = T2("dsd", [128, 8]); cd = T2("cd", [128, 8])
        xdt = T2("xdt", [128, 512], BF16); xdd = T2("xdd", [128, 512], BF16)
        cbm = T2("cbm", [128, 2, 128])
        dAt = T("dAt", [128, 8, 128]); seg = T2("seg", [128, 4, 128]); MT = T2("MT", [128, 4, 128], BF16)
        yd = T2("yd", [128, 512]); y = T2("y", [128, 512]); sz = T2("sz", [128, 512])
        junk = T("junk", [128, 256]); ss = T2("ss", [128, 2]); mo = T2("mo", [128, 512], BF16)
        pxT = PT("pxT", [128, 512]); pBt = PT("pBt", [128, 2, 128], BF16); pacs = PT("pacs", [128, 16])
        pcb = PT("pcb", [128, 2, 128]); pD = PT("pD", [128, 4, 128]); py = PT("py", [128, 512])
        pyo = PT("pyo", [128, 512]); pU = PT("pU", [128, 512])

        for k in range(4):
            LD(cw, cw.t[:, k, :], self.ssd_conv_w[l, k, :].rearrange("(cc p) -> p cc", p=128),
               allow_slow_non_contiguous=True)
        LD(cb, cb.t[:], self.ssd_conv_b[l, :].rearrange("(cc p) -> p cc", p=128), allow_slow_non_contiguous=True)
        LD(dtb, dtb.t[:], self.ssd_dt_bias[l, :].partition_broadcast(128))
        LD(Aneg, Aneg.t[:], self.ssd_a_log[l, :].partition_broadcast(128))
        LD(dsk, dsk.t[:], self.ssd_d[l, :].partition_broadcast(128))
        LD(nwb, nwb.t[:], self.ssd_norm_w[l, :].partition_broadcast(128))
        LD(tri, tri.t[:], self.c_tri[:, :]); LD(slt, slt.t[:], self.c_slt[:, :]); LD(ones, ones.t[:], self.c_ones[:, :])
        LD(idf, idf.t[:], self.ident_f[:, :]); LD(idb, idb.t[:], self.ident_b[:, :])
        AC(lambda: nc.scalar.activation(out=Aneg.t[:], in_=Aneg.t[:], func=AF.Exp), [Aneg], [Aneg])
        V(lambda: nc.vector.tensor_scalar(out=Aneg.t[:], in0=Aneg.t[:], scalar1=-1.0, scalar2=None, op0=ALU.mult), [Aneg], [Aneg])
        V(lambda: nc.vector.memset(hT.t[:], 0.0), [], [hT])
        V(lambda: nc.vector.memset(hTb.t[:], 0.0), [], [hTb])
        for j in range(2):
            V(lambda: nc.vector.memset(xh[j].t[:], 0.0), [], [xh[j]])
        xv = self.xbcT.rearrange("(cc p) t -> p cc t", p=128)

        def front(i):
            s = i % 2
            t0 = i * 128
            if i == 0:
                kb.dma("sp", xh[s].k, xh[s].t[:, :, 3:131], xv[:, :, 0:128], reads=[("xbcT", 0)], writes=[xh[s].k])
            else:
                kb.dma("sp", xh[s].k, xh[s].t[:, :, 0:131], xv[:, :, t0 - 3:t0 + 128],
                       reads=[("xbcT", i), ("xbcT", i - 1)], writes=[xh[s].k])
            kb.dma("sp", zt[s].k, zt[s].t[:], self.proj[t0:t0 + 128, C_Z:C_Z + 512], reads=[("proj", i)], writes=[zt[s].k])
            kb.dma("sp", dtt[s].k, dtt[s].t[:], self.proj[t0:t0 + 128, C_DT:C_DT + 8], reads=[("proj", i)], writes=[dtt[s].k])
            for cc in range(8):
                eng = V
                ne = nc.vector
                GP(lambda: nc.gpsimd.tensor_scalar(out=acc[s].t[:, cc, :], in0=xh[s].t[:, cc, 0:128], scalar1=cw.t[:, 0, cc:cc + 1],
                                                   scalar2=cb.t[:, cc:cc + 1], op0=ALU.mult, op1=ALU.add),
                   [xh[s], cw, cb], [Tile(None, acc[s].k + f"_{cc}")])
                for k in range(1, 4):
                    eng(lambda: ne.scalar_tensor_tensor(out=acc[s].t[:, cc, :], in0=xh[s].t[:, cc, k:k + 128],
                                                        scalar=cw.t[:, k, cc:cc + 1], in1=acc[s].t[:, cc, :],
                                                        op0=ALU.mult, op1=ALU.add),
                        [xh[s], cw, Tile(None, acc[s].k + f"_{cc}")], [Tile(None, acc[s].k + f"_{cc}")])
            AC(lambda: nc.scalar.activation(out=xsT[s].t[:], in_=acc[s].t[:, 0:4, :], func=AF.Silu), [Tile(None, acc[s].k + f"_{c_}") for c_ in range(0, 4)], [xsT[s]])
            AC(lambda: nc.scalar.activation(out=BT[s].t[:], in_=acc[s].t[:, 4:6, :], func=AF.Silu), [Tile(None, acc[s].k + f"_{c_}") for c_ in range(4, 6)], [BT[s]])
            AC(lambda: nc.scalar.activation(out=CT[s].t[:], in_=acc[s].t[:, 6:8, :], func=AF.Silu), [Tile(None, acc[s].k + f"_{c_}") for c_ in range(6, 8)], [CT[s]])
            for cc in range(4):
                PE(lambda: nc.tensor.transpose(out=pxT.t[:, cc * 128:(cc + 1) * 128], in_=xsT[s].t[:, cc, :], identity=idf.t[:]),
                   [xsT[s], idf], [pxT])
            V(lambda: nc.vector.tensor_copy(out=xtm[s].t[:], in_=pxT.t[:]), [pxT], [xtm[s]])
            for g in range(2):
                PE(lambda: nc.tensor.transpose(out=pBt.t[:, g, :], in_=BT[s].t[:, g, :], identity=idb.t[:]), [BT[s], idb], [pBt])
            AC(lambda: nc.scalar.copy(out=Btm[s].t[:], in_=pBt.t[:]), [pBt], [Btm[s]])
            V(lambda: nc.vector.tensor_tensor(out=dts[s].t[:], in0=dtt[s].t[:], in1=dtb.t[:], op=ALU.add), [dtt[s], dtb], [dts[s]])
            V(lambda: nc.vector.scalar_tensor_tensor(out=sp_a[s].t[:], in0=dts[s].t[:], scalar=-1.0, in1=dts[s].t[:],
                                                     op0=ALU.mult, op1=ALU.max), [dts[s]], [sp_a[s]])
            AC(lambda: nc.scalar.activation(out=sp_a[s].t[:], in_=sp_a[s].t[:], func=AF.Exp, scale=-1.0), [sp_a[s]], [sp_a[s]])
            AC(lambda: nc.scalar.activation(out=sp_a[s].t[:], in_=sp_a[s].t[:], func=AF.Ln, bias=1.0), [sp_a[s]], [sp_a[s]])
            V(lambda: nc.vector.scalar_tensor_tensor(out=dts[s].t[:], in0=dts[s].t[:], scalar=0.0, in1=sp_a[s].t[:],
                                                     op0=ALU.max, op1=ALU.add), [dts[s], sp_a[s]], [dts[s]])
            V(lambda: nc.vector.tensor_tensor(out=dA[s].t[:], in0=dts[s].t[:], in1=Aneg.t[:], op=ALU.mult), [dts[s], Aneg], [dA[s]])
            PE(lambda: nc.tensor.matmul(out=pacs.t[:, 0:8], lhsT=tri.t[:], rhs=dA[s].t[:], start=True, stop=True), [tri, dA[s]], [pacs])
            PE(lambda: nc.tensor.matmul(out=pacs.t[:, 8:16], lhsT=ones.t[:], rhs=dA[s].t[:], start=True, stop=True), [ones, dA[s]], [pacs])
            V(lambda: nc.vector.tensor_copy(out=acs[s].t[:], in_=pacs.t[:]), [pacs], [acs[s]])
            AC(lambda: nc.scalar.activation(out=eacs[s].t[:], in_=acs[s].t[:, 0:8], func=AF.Exp), [acs[s]], [eacs[s]])
            AC(lambda: nc.scalar.activation(out=cd[s].t[:], in_=acs[s].t[:, 8:16], func=AF.Exp), [acs[s]], [cd[s]])
            V(lambda: nc.vector.tensor_tensor(out=dsd[s].t[:], in0=acs[s].t[:, 8:16], in1=acs[s].t[:, 0:8], op=ALU.subtract), [acs[s]], [dsd[s]])
            AC(lambda: nc.scalar.activation(out=dsd[s].t[:], in_=dsd[s].t[:], func=AF.Exp), [dsd[s]], [dsd[s]])
            V(lambda: nc.vector.tensor_tensor(out=dsd[s].t[:], in0=dsd[s].t[:], in1=dts[s].t[:], op=ALU.mult), [dsd[s], dts[s]], [dsd[s]])
            x3 = xtm[s].t[:].rearrange("p (h d) -> p h d", h=8)
            V(lambda: nc.vector.tensor_tensor(out=xdt[s].t[:].rearrange("p (h d) -> p h d", h=8), in0=x3,
                                              in1=dts[s].t[:].unsqueeze(2).to_broadcast([128, 8, 64]), op=ALU.mult),
              [xtm[s], dts[s]], [xdt[s]])
            GP(lambda: nc.gpsimd.tensor_tensor(out=xdd[s].t[:].rearrange("p (h d) -> p h d", h=8), in0=x3,
                                               in1=dsd[s].t[:].unsqueeze(2).to_broadcast([128, 8, 64]), op=ALU.mult),
               [xtm[s], dsd[s]], [xdd[s]])
            for g in range(2):
                PE(lambda: nc.tensor.matmul(out=pcb.t[:, g, :], lhsT=BT[s].t[:, g, :], rhs=CT[s].t[:, g, :], start=True, stop=True),
                   [BT[s], CT[s]], [pcb])
            V(lambda: nc.vector.tensor_tensor(out=cbm[s].t[:], in0=pcb.t[:], in1=tri.t[:].unsqueeze(1).to_broadcast([128, 2, 128]),
                                              op=ALU.mult), [pcb, tri], [cbm[s]])
        def tail(i):
            s = i % 2
            t0 = i * 128
            x3 = xtm[s].t[:].rearrange("p (h d) -> p h d", h=8)
            GP(lambda: nc.gpsimd.tensor_tensor(out=dAt.t[:], in0=tri.t[:].unsqueeze(1).to_broadcast([128, 8, 128]),
                                               in1=dA[s].t[:].unsqueeze(2).to_broadcast([128, 8, 128]), op=ALU.mult), [tri, dA[s]], [dAt])
            for g in range(2):
                for j in range(4):
                    PE(lambda: nc.tensor.matmul(out=pD.t[:, j, :], lhsT=slt.t[:], rhs=dAt.t[:, 4 * g + j, :], start=True, stop=True),
                       [slt, dAt], [pD])
                AC(lambda: nc.scalar.activation(out=seg[g].t[:], in_=pD.t[:], func=AF.Exp), [pD], [seg[g]])
                V(lambda: nc.vector.tensor_tensor(out=MT[g].t[:], in0=seg[g].t[:], in1=cbm[s].t[:, g, :].unsqueeze(1).to_broadcast([128, 4, 128]),
                                                  op=ALU.mult), [seg[g], cbm[s]], [MT[g]])
                for j in range(4):
                    h = 4 * g + j
                    PE(lambda: nc.tensor.matmul(out=py.t[:, h * 64:(h + 1) * 64], lhsT=MT[g].t[:, j, :], rhs=xdt[s].t[:, h * 64:(h + 1) * 64],
                                                start=True, stop=True), [MT[g], xdt[s]], [py])
            for g in range(2):
                PE(lambda: nc.tensor.matmul(out=pyo.t[:, g * 256:(g + 1) * 256], lhsT=CT[s].t[:, g, :], rhs=hTb.t[:, g * 256:(g + 1) * 256],
                                            start=True, stop=True), [CT[s], hTb], [pyo])
            for g in range(2):
                PE(lambda: nc.tensor.matmul(out=pU.t[:, g * 256:(g + 1) * 256], lhsT=Btm[s].t[:, g, :], rhs=xdd[s].t[:, g * 256:(g + 1) * 256],
                                            start=True, stop=True), [Btm[s], xdd[s]], [pU])
            AC(lambda: nc.scalar.copy(out=yd[s].t[:], in_=py.t[:]), [py], [yd[s]])
            V(lambda: nc.vector.tensor_tensor(out=y[s].t[:].rearrange("p (h d) -> p h d", h=8),
                                              in0=pyo.t[:].rearrange("p (h d) -> p h d", h=8),
                                              in1=eacs[s].t[:].unsqueeze(2).to_broadcast([128, 8, 64]), op=ALU.mult),
              [pyo, eacs[s]], [y[s]])
            V(lambda: nc.vector.tensor_tensor(out=hT.t[:].rearrange("p (h d) -> p h d", h=8),
                                              in0=hT.t[:].rearrange("p (h d) -> p h d", h=8),
                                              in1=cd[s].t[:].unsqueeze(2).to_broadcast([128, 8, 64]), op=ALU.mult),
              [hT, cd[s]], [hT])
            V(lambda: nc.vector.tensor_tensor(out=hT.t[:], in0=pU.t[:], in1=hT.t[:], op=ALU.add), [pU, hT], [hT])
            AC(lambda: nc.scalar.copy(out=hTb.t[:], in_=hT.t[:]), [hT], [hTb])
            GP(lambda: nc.gpsimd.tensor_tensor(out=y[s].t[:], in0=y[s].t[:], in1=yd[s].t[:], op=ALU.add), [y[s], yd[s]], [y[s]])
            GP(lambda: nc.gpsimd.tensor_tensor(out=yd[s].t[:].rearrange("p (h d) -> p h d", h=8), in0=x3,
                                               in1=dsk.t[:].unsqueeze(2).to_broadcast([128, 8, 64]), op=ALU.mult),
               [xtm[s], dsk], [yd[s]])
            GP(lambda: nc.gpsimd.tensor_tensor(out=y[s].t[:], in0=y[s].t[:], in1=yd[s].t[:], op=ALU.add), [y[s], yd[s]], [y[s]])
            AC(lambda: nc.scalar.activation(out=sz[s].t[:], in_=zt[s].t[:], func=AF.Silu), [zt[s]], [sz[s]])
            V(lambda: nc.vector.tensor_tensor(out=y[s].t[:], in0=y[s].t[:], in1=sz[s].t[:], op=ALU.mult), [y[s], sz[s]], [y[s]])
            for g in range(2):
                AC(lambda: nc.scalar.activation(out=junk.t[:], in_=y[s].t[:, g * 256:(g + 1) * 256], func=AF.Square,
                                                accum_out=ss[s].t[:, g:g + 1]), [y[s]], [junk, ss[s]])
            ssk = [ss[s]]
            V(lambda: nc.vector.tensor_scalar(out=ss[s].t[:], in0=ss[s].t[:], scalar1=1.0 / 256, scalar2=EPS, op0=ALU.mult, op1=ALU.add),
              ssk, [ss[s]])
            AC(lambda: nc.scalar.activation(out=ss[s].t[:], in_=ss[s].t[:], func=AF.Sqrt), [ss[s]], [ss[s]])
            V(lambda: nc.vector.reciprocal(out=ss[s].t[:], in_=ss[s].t[:]), [ss[s]], [ss[s]])
            for g in range(2):
                V(lambda: nc.vector.scalar_tensor_tensor(out=mo[s].t[:, g * 256:(g + 1) * 256], in0=y[s].t[:, g * 256:(g + 1) * 256],
                                                         scalar=ss[s].t[:, g:g + 1], in1=nwb.t[:, g * 256:(g + 1) * 256],
                                                         op0=ALU.mult, op1=ALU.mult), [y[s], ss[s], nwb], [Tile(None, f"ssd_mo{s}_{g}")])
            kb.dma("sp", mo[s].k, self.mix[t0:t0 + 128, 512:1024], mo[s].t[:],
                   reads=[f"ssd_mo{s}_0", f"ssd_mo{s}_1"], writes=[("mix_ssd", i)])

        front(0)
        for i in range(NT):
            if i + 1 < NT:
                interleave(kb, (lambda i=i: tail(i)), (lambda i=i: front(i + 1)))
            else:
                tail(i)


Prog.stage_ssd = _stage_ssd


def _stage_gla(self, l):
    nc, kb, NT = self.nc, self.kb, self.NT
    with ExitStack() as es:
        def T(name, shape, dt=F32):
            return Tile(self.sb(es, "gla_" + name, shape, dt), "gla_" + name)

        def T2(name, shape, dt=F32):
            return [T(f"{name}{j}", shape, dt) for j in range(2)]

        def PT(name, shape, dt=F32):
            return Tile(self.ps(es, "gla_" + name, shape, dt), "gla_" + name)

        def V(fn, r, w):
            return kb.op("dve", fn, [t.k for t in r], [t.k for t in w])

        def AC(fn, r, w):
            return kb.op("act", fn, [t.k for t in r], [t.k for t in w])

        def PE(fn, r, w):
            return kb.op("pe", fn, [t.k for t in r], [t.k for t in w])

        def GP(fn, r, w):
            return kb.op("pool", fn, [t.k for t in r], [t.k for t in w])

        def LD(t, dst, src, reads=(), **kw):
            return kb.dma("sp", t.k, dst, src, reads=list(reads), writes=[t.k], **kw)

        w2 = T("w2", [16, 128]); bb = T("bb", [128, 128]); nwb = T("nwb", [128, 256])
        tri = T("tri", [128, 128]); tri16 = T("tri16", [128, 128]); o16 = T("o16", [128, 2])
        idf = T("idf", [128, 128]); idb = T("idb", [128, 128], BF16)
        hm = T("hm", [128, 4]); bd = T("bd", [128, 256])
        Sbd = T("Sbd", [128, 256]); Sbb = T("Sbb", [128, 256], BF16)
        gin = T2("gin", [128, 784])
        glrT = T2("glrT", [16, 128]); zv = T2("zv", [128, 128]); az = T2("az", [128, 128]); la = T2("la", [128, 128])
        eb = T2("eb", [128, 128]); enb = T2("enb", [128, 128])
        qt = T2("qtok", [128, 128], BF16); kt = T2("ktok", [128, 128], BF16); vb = T2("vb", [128, 256], BF16)
        qT = T2("qT", [128, 128], BF16); kTm = T2("kTm", [128, 4, 128], BF16)
        egt = T2("egt", [128, 2]); ATm = T2("ATm", [128, 4, 128], BF16)
        sq = T2("sq", [128, 256]); ss = T2("ss", [128, 4]); on = T2("on", [128, 256]); sr = T2("sr", [128, 256])
        mo = T2("mo", [128, 256], BF16); um = T2("um", [128, 256])
        pgT = PT("pgT", [16, 128]); pz = PT("pz", [128, 128]); pbc = PT("pbc", [128, 128])
        pqk = PT("pqk", [128, 2, 128], BF16); pgt = PT("pgt", [128, 2]); pA = PT("pA", [128, 4, 128])
        po = PT("po", [128, 256]); pU = PT("pU", [128, 256])

        LD(w2, w2.t[:], self.gla_gate_w2[l, :, :])
        LD(bb, bb.t[:], self.gla_gate_b[l, :].partition_broadcast(128))
        LD(nwb, nwb.t[:], self.gla_norm_w[l, :].partition_broadcast(128))
        LD(tri, tri.t[:], self.c_tri[:, :]); LD(idf, idf.t[:], self.ident_f[:, :]); LD(idb, idb.t[:], self.ident_b[:, :])
        LD(hm, hm.t[:], self.c_hm[:, :]); LD(bd, bd.t[:], self.c_bd[:, :])
        V(lambda: nc.vector.tensor_scalar(out=tri16.t[:], in0=tri.t[:], scalar1=1.0 / 16, scalar2=None, op0=ALU.mult), [tri], [tri16])
        V(lambda: nc.vector.memset(o16.t[:], 1.0 / 16), [], [o16])
        V(lambda: nc.vector.memset(Sbd.t[:], 0.0), [], [Sbd])
        V(lambda: nc.vector.memset(Sbb.t[:], 0.0), [], [Sbb])

        def front(i):
            s = i % 2
            t0 = i * 128
            kb.dma("sp", gin[s].k, gin[s].t[:], self.proj[t0:t0 + 128, 0:784], reads=[("proj", i)], writes=[gin[s].k])
            q_ = gin[s].t[:, C_GQ:C_GQ + 128]; k_ = gin[s].t[:, C_GK:C_GK + 128]; v_ = gin[s].t[:, C_GV:C_GV + 256]
            glr_ = gin[s].t[:, C_GLR:C_GLR + 16]; r_ = gin[s].t[:, C_GR:C_GR + 256]
            PE(lambda: nc.tensor.transpose(out=pgT.t[:], in_=glr_, identity=idf.t[:]), [gin[s], idf], [pgT])
            V(lambda: nc.vector.tensor_copy(out=glrT[s].t[:], in_=pgT.t[:]), [pgT], [glrT[s]])
            PE(lambda: nc.tensor.matmul(out=pz.t[:], lhsT=glrT[s].t[:], rhs=w2.t[:], start=True, stop=True), [glrT[s], w2], [pz])
            V(lambda: nc.vector.tensor_tensor(out=zv[s].t[:], in0=pz.t[:], in1=bb.t[:], op=ALU.add), [pz, bb], [zv[s]])
            V(lambda: nc.vector.scalar_tensor_tensor(out=az[s].t[:], in0=zv[s].t[:], scalar=-1.0, in1=zv[s].t[:], op0=ALU.mult, op1=ALU.max),
              [zv[s]], [az[s]])
            AC(lambda: nc.scalar.activation(out=az[s].t[:], in_=az[s].t[:], func=AF.Exp, scale=-1.0), [az[s]], [az[s]])
            AC(lambda: nc.scalar.activation(out=az[s].t[:], in_=az[s].t[:], func=AF.Ln, bias=1.0), [az[s]], [az[s]])
            V(lambda: nc.vector.scalar_tensor_tensor(out=la[s].t[:], in0=zv[s].t[:], scalar=0.0, in1=az[s].t[:], op0=ALU.min, op1=ALU.subtract),
              [zv[s], az[s]], [la[s]])
            PE(lambda: nc.tensor.matmul(out=pbc.t[:], lhsT=tri16.t[:], rhs=la[s].t[:], start=True, stop=True), [tri16, la[s]], [pbc])
            PE(lambda: nc.tensor.matmul(out=pgt.t[:], lhsT=la[s].t[:], rhs=o16.t[:], start=True, stop=True), [la[s], o16], [pgt])
            AC(lambda: nc.scalar.activation(out=eb[s].t[:], in_=pbc.t[:], func=AF.Exp), [pbc], [eb[s]])
            AC(lambda: nc.scalar.activation(out=enb[s].t[:], in_=pbc.t[:], func=AF.Exp, scale=-1.0), [pbc], [enb[s]])
            AC(lambda: nc.scalar.activation(out=egt[s].t[:], in_=pgt.t[:], func=AF.Exp), [pgt], [egt[s]])
            V(lambda: nc.vector.scalar_tensor_tensor(out=qt[s].t[:], in0=q_, scalar=32.0 ** -0.5, in1=eb[s].t[:], op0=ALU.mult, op1=ALU.mult),
              [gin[s], eb[s]], [qt[s]])
            V(lambda: nc.vector.tensor_tensor(out=kt[s].t[:], in0=k_, in1=enb[s].t[:], op=ALU.mult), [gin[s], enb[s]], [kt[s]])
            GP(lambda: nc.gpsimd.tensor_copy(out=vb[s].t[:], in_=v_), [gin[s]], [vb[s]])
            PE(lambda: nc.tensor.transpose(out=pqk.t[:, 0, :], in_=qt[s].t[:], identity=idb.t[:]), [qt[s], idb], [pqk])
            PE(lambda: nc.tensor.transpose(out=pqk.t[:, 1, :], in_=kt[s].t[:], identity=idb.t[:]), [kt[s], idb], [pqk])
            AC(lambda: nc.scalar.copy(out=qT[s].t[:], in_=pqk.t[:, 0, :]), [pqk], [qT[s]])
            for h in range(4):
                AC(lambda: nc.scalar.activation(out=kTm[s].t[:, h, :], in_=pqk.t[:, 1, :], func=AF.Copy, scale=hm.t[:, h:h + 1]),
                   [pqk, hm], [kTm[s]])
            for h in range(4):
                PE(lambda: nc.tensor.matmul(out=pA.t[:, h, :], lhsT=kTm[s].t[:, h, :], rhs=qT[s].t[:], start=True, stop=True),
                   [kTm[s], qT[s]], [pA])
            V(lambda: nc.vector.tensor_tensor(out=ATm[s].t[:], in0=pA.t[:], in1=tri.t[:].unsqueeze(1).to_broadcast([128, 4, 128]), op=ALU.mult),
              [pA, tri], [ATm[s]])
        def tail(i):
            s = i % 2
            t0 = i * 128
            r_ = gin[s].t[:, C_GR:C_GR + 256]
            PE(lambda: nc.tensor.matmul(out=po.t[:], lhsT=qT[s].t[:], rhs=Sbb.t[:], start=True, stop=False), [qT[s], Sbb], [po])
            for h in range(4):
                PE(lambda: nc.tensor.matmul(out=po.t[:, h * 64:(h + 1) * 64], lhsT=ATm[s].t[:, h, :], rhs=vb[s].t[:, h * 64:(h + 1) * 64],
                                            start=False, stop=(h == 3)), [ATm[s], vb[s]], [po])
            PE(lambda: nc.tensor.matmul(out=pU.t[:], lhsT=kt[s].t[:], rhs=vb[s].t[:], start=True, stop=True), [kt[s], vb[s]], [pU])
            V(lambda: nc.vector.tensor_tensor(out=um[s].t[:], in0=pU.t[:], in1=bd.t[:], op=ALU.mult), [pU, bd], [um[s]])
            V(lambda: nc.vector.tensor_tensor(out=Sbd.t[:], in0=Sbd.t[:], in1=um[s].t[:], op=ALU.add), [Sbd, um[s]], [Sbd])
            V(lambda: nc.vector.tensor_scalar(out=Sbd.t[:], in0=Sbd.t[:], scalar1=egt[s].t[:, 0:1], scalar2=None, op0=ALU.mult), [Sbd, egt[s]], [Sbd])
            AC(lambda: nc.scalar.copy(out=Sbb.t[:], in_=Sbd.t[:]), [Sbd], [Sbb])
            AC(lambda: nc.scalar.activation(out=sq[s].t[:], in_=po.t[:], func=AF.Square), [po], [sq[s]])
            V(lambda: nc.vector.tensor_reduce(out=ss[s].t[:], in_=sq[s].t[:].rearrange("p (h d) -> p h d", h=4), axis=AX.X, op=ALU.add),
              [sq[s]], [ss[s]])
            V(lambda: nc.vector.tensor_scalar(out=ss[s].t[:], in0=ss[s].t[:], scalar1=1.0 / 64, scalar2=EPS, op0=ALU.mult, op1=ALU.add), [ss[s]], [ss[s]])
            AC(lambda: nc.scalar.activation(out=ss[s].t[:], in_=ss[s].t[:], func=AF.Sqrt), [ss[s]], [ss[s]])
            V(lambda: nc.vector.reciprocal(out=ss[s].t[:], in_=ss[s].t[:]), [ss[s]], [ss[s]])
            V(lambda: nc.vector.tensor_tensor(out=on[s].t[:].rearrange("p (h d) -> p h d", h=4), in0=po.t[:].rearrange("p (h d) -> p h d", h=4),
                                              in1=ss[s].t[:].unsqueeze(2).to_broadcast([128, 4, 64]), op=ALU.mult), [po, ss[s]], [on[s]])
            AC(lambda: nc.scalar.activation(out=sr[s].t[:], in_=r_, func=AF.Silu), [gin[s]], [sr[s]])
            GP(lambda: nc.gpsimd.tensor_tensor(out=sr[s].t[:], in0=sr[s].t[:], in1=nwb.t[:], op=ALU.mult), [sr[s], nwb], [sr[s]])
            V(lambda: nc.vector.tensor_tensor(out=mo[s].t[:], in0=on[s].t[:], in1=sr[s].t[:], op=ALU.mult), [on[s], sr[s]], [mo[s]])
            kb.dma("sp", mo[s].k, self.mix[t0:t0 + 128, 0:256], mo[s].t[:], reads=[mo[s].k], writes=[("mix_gla", i)])

        front(0)
        for i in range(NT):
            if i + 1 < NT:
                interleave(kb, (lambda i=i: tail(i)), (lambda i=i: front(i + 1)))
            else:
                tail(i)


Prog.stage_gla = _stage_gla


def build_test_mix(S, which, limit=None):
    p = Prog(S, L=1, dbg=[("o_mix", [S, D])])
    with p.kb.es as es:
        p.stage_d1(0, p.x)
        p.kb.barrier()
        if limit is not None:
            p.kb.limit = p.kb.ninst + limit
        if "ssd" in which:
            p.stage_ssd(0)
        if "gla" in which:
            p.stage_gla(0)
        if "nsa" in which:
            p.stage_nsa(0)
        kb, nc = p.kb, p.nc
        kb.limit = None
        kb.barrier()
        mb = p.sb(es, "dump_mb", [128, D], BF16)
        mf = p.sb(es, "dump_mf", [128, D])
        for i in range(p.NT):
            kb.dma("sp", "dump_mb", mb[:], p.mix[i * 128:(i + 1) * 128, :], writes=["dump_mb"])
            kb.op("dve", lambda: nc.vector.tensor_copy(out=mf[:], in_=mb[:]), reads=["dump_mb"], writes=["dump_mf"])
            kb.dma("sp", "dump_mf", p.outs["o_mix"][i * 128:(i + 1) * 128, :], mf[:], reads=["dump_mf"], writes=[("o_mix", i)])
        kb.drain("sp")
    return p


def _stage_d2(self, l, hsrc, hdst, final):
    nc, kb, NT = self.nc, self.kb, self.NT
    with ExitStack() as es:
        def T(name, shape, dt=F32):
            return Tile(self.sb(es, "d2_" + name, shape, dt), "d2_" + name)

        def T2(name, shape, dt=F32):
            return [T(f"{name}{j}", shape, dt) for j in range(2)]

        def PT(name, shape, dt=F32):
            return Tile(self.ps(es, "d2_" + name, shape, dt), "d2_" + name)

        def V(fn, r, w):
            return kb.op("dve", fn, [t.k for t in r], [t.k for t in w])

        def AC(fn, r, w):
            return kb.op("act", fn, [t.k for t in r], [t.k for t in w])

        def PE(fn, r, w):
            return kb.op("pe", fn, [t.k for t in r], [t.k for t in w])

        def GP(fn, r, w):
            return kb.op("pool", fn, [t.k for t in r], [t.k for t in w])

        Wo = T("Wo", [128, 8, 1024], BF16); Wu = T("Wu", [128, 8, 4096], BF16); Wd = T("Wd", [128, 32, 1024], BF16)
        stg = T2("stg", [128, 1024]); nw2 = T("nw2", [128, 8]); idb = T("idb", [128, 128], BF16)
        ht = T2("ht", [128, 1024]); mt = T2("mt", [128, 1024], BF16)
        mT = T("mT", [128, 8, 128], BF16); h1 = T2("h1", [128, 1024]); junk = T("junk", [128, 1024], BF16)
        ss = T("ss", [128, 1]); u2 = T("u2", [128, 1024], BF16); u2T = T2("u2T", [128, 8, 128], BF16)
        tmp = T2("tmp", [128, 512]); hidT = T("hidT", [128, 32, 128], BF16); ho = T2("ho", [128, 1024])
        pT = PT("pT", [128, 8, 128], BF16); po = [PT(f"po{j}", [128, 512]) for j in range(2)]
        pu = [PT(f"pu{j}", [128, 4, 128]) for j in range(2)]
        pd = [PT(f"pd{j}", [128, 512]) for j in range(2)]
        if final:
            fnw = T("fnw", [128, 1024]); ss2 = T("ss2", [128, 1])
            kb.dma("sp", fnw.k, fnw.t[:], self.final_norm_w.partition_broadcast(128), writes=[fnw.k])

        kb.dma("sp", idb.k, idb.t[:], self.ident_b[:, :], writes=[idb.k])
        for c in range(8):
            kb.dma("sp", nw2.k, nw2.t[:, c:c + 1], self.norm2_w[l, c * 128:(c + 1) * 128].rearrange("(p o) -> p o", o=1), writes=[nw2.k])
        n = 0
        jobs = [(Wo, c, 0, self.w_out[l, c * 128:(c + 1) * 128, :], False) for c in range(8)]
        jobs += [(Wu, c, q * 1024, self.w_up[l, c * 128:(c + 1) * 128, q * 1024:(q + 1) * 1024], True) for c in range(8) for q in range(4)]
        jobs += [(Wd, f, 0, self.w_down[l, f * 128:(f + 1) * 128, :], False) for f in range(32)]
        for (W, c, off, src, scaled) in jobs:
            sg = stg[n % 2]
            kb.dma("sp", sg.k, sg.t[:], src, writes=[sg.k])
            dst = W.t[:, c, off:off + 1024]
            if scaled:
                if n % 2 == 0:
                    V(lambda: nc.vector.tensor_scalar(out=dst, in0=sg.t[:], scalar1=nw2.t[:, c:c + 1], scalar2=None, op0=ALU.mult), [sg, nw2], [W])
                else:
                    AC(lambda: nc.scalar.activation(out=dst, in_=sg.t[:], func=AF.Copy, scale=nw2.t[:, c:c + 1]), [sg, nw2], [W])
            else:
                if n % 2 == 0:
                    V(lambda: nc.vector.tensor_copy(out=dst, in_=sg.t[:]), [sg], [W])
                else:
                    AC(lambda: nc.scalar.copy(out=dst, in_=sg.t[:]), [sg], [W])
            n += 1

        def loads(i):
            s = i % 2
            t0 = i * 128
            kb.dma("sp", ht[s].k, ht[s].t[:], hsrc[t0:t0 + 128, :], reads=[("h", i)], writes=[ht[s].k])
            kb.dma("sp", mt[s].k, mt[s].t[:], self.mix[t0:t0 + 128, :], reads=[("mix_gla", i), ("mix_nsa", i), ("mix_ssd", i)], writes=[mt[s].k])

        def front(i):
            s = i % 2
            t0 = i * 128
            for c in range(8):
                PE(lambda: nc.tensor.transpose(out=pT.t[:, c, :], in_=mt[s].t[:, c * 128:(c + 1) * 128], identity=idb.t[:]), [mt[s], idb], [pT])
            AC(lambda: nc.scalar.copy(out=mT.t[:], in_=pT.t[:]), [pT], [mT])
            for hf in range(2):
                for c in range(8):
                    PE(lambda: nc.tensor.matmul(out=po[hf].t[:], lhsT=mT.t[:, c, :], rhs=Wo.t[:, c, hf * 512:(hf + 1) * 512],
                                                start=(c == 0), stop=(c == 7)), [mT, Wo], [po[hf]])
                V(lambda: nc.vector.tensor_tensor(out=h1[s].t[:, hf * 512:(hf + 1) * 512], in0=po[hf].t[:], in1=ht[s].t[:, hf * 512:(hf + 1) * 512], op=ALU.add),
                  [po[hf], ht[s]], [h1[s]])
            AC(lambda: nc.scalar.activation(out=junk.t[:], in_=h1[s].t[:], func=AF.Square, accum_out=ss.t[:]), [h1[s]], [junk, ss])
            V(lambda: nc.vector.tensor_scalar(out=ss.t[:], in0=ss.t[:], scalar1=1.0 / D, scalar2=EPS, op0=ALU.mult, op1=ALU.add), [ss], [ss])
            AC(lambda: nc.scalar.activation(out=ss.t[:], in_=ss.t[:], func=AF.Sqrt), [ss], [ss])
            V(lambda: nc.vector.reciprocal(out=ss.t[:], in_=ss.t[:]), [ss], [ss])
            V(lambda: nc.vector.tensor_scalar(out=u2.t[:], in0=h1[s].t[:], scalar1=ss.t[:], scalar2=None, op0=ALU.mult), [h1[s], ss], [u2])
            for c in range(8):
                PE(lambda: nc.tensor.transpose(out=pT.t[:, c, :], in_=u2.t[:, c * 128:(c + 1) * 128], identity=idb.t[:]), [u2, idb], [pT])
            AC(lambda: nc.scalar.copy(out=u2T[s].t[:], in_=pT.t[:]), [pT], [u2T[s]])
        def tail(i):
            s = i % 2
            t0 = i * 128
            for fg in range(8):
                p = pu[fg % 2]
                for j in range(4):
                    f = fg * 4 + j
                    for c in range(8):
                        PE(lambda: nc.tensor.matmul(out=p.t[:, j, :], lhsT=Wu.t[:, c, f * 128:(f + 1) * 128], rhs=u2T[s].t[:, c, :],
                                                    start=(c == 0), stop=(c == 7)), [Wu, u2T[s]], [p])
                tm = tmp[fg % 2]
                AC(lambda: nc.scalar.activation(out=tm.t[:], in_=p.t[:].rearrange("p a b -> p (a b)"), func=AF.Relu), [p], [tm])
                GP(lambda: nc.gpsimd.tensor_tensor(out=hidT.t[:, fg * 4:(fg + 1) * 4, :].rearrange("p a b -> p (a b)"), in0=tm.t[:], in1=tm.t[:], op=ALU.mult),
                   [tm], [Tile(None, f"d2_hid{fg}")])
            hk = [Tile(None, f"d2_hid{fg}") for fg in range(8)]
            for f in range(32):
                for hf in range(2):
                    PE(lambda: nc.tensor.matmul(out=pd[hf].t[:], lhsT=hidT.t[:, f, :], rhs=Wd.t[:, f, hf * 512:(hf + 1) * 512],
                                                start=(f == 0), stop=(f == 31)), [hk[f // 4], Wd], [pd[hf]])
            for hf in range(2):
                V(lambda: nc.vector.tensor_tensor(out=ho[s].t[:, hf * 512:(hf + 1) * 512], in0=pd[hf].t[:], in1=h1[s].t[:, hf * 512:(hf + 1) * 512], op=ALU.add),
                  [pd[hf], h1[s]], [ho[s]])
            if final:
                AC(lambda: nc.scalar.activation(out=junk.t[:], in_=ho[s].t[:], func=AF.Square, accum_out=ss2.t[:]), [ho[s]], [junk, ss2])
                V(lambda: nc.vector.tensor_scalar(out=ss2.t[:], in0=ss2.t[:], scalar1=1.0 / D, scalar2=EPS, op0=ALU.mult, op1=ALU.add), [ss2], [ss2])
                AC(lambda: nc.scalar.activation(out=ss2.t[:], in_=ss2.t[:], func=AF.Sqrt), [ss2], [ss2])
                V(lambda: nc.vector.reciprocal(out=ss2.t[:], in_=ss2.t[:]), [ss2], [ss2])
                V(lambda: nc.vector.scalar_tensor_tensor(out=ho[s].t[:], in0=ho[s].t[:], scalar=ss2.t[:], in1=fnw.t[:], op0=ALU.mult, op1=ALU.mult),
                  [ho[s], ss2, fnw], [ho[s]])
            kb.dma("sp", ho[s].k, hdst[t0:t0 + 128, :], ho[s].t[:], reads=[ho[s].k], writes=[("hout", l, i)] if final else [("h", i)])

        loads(0)
        if NT > 1:
            loads(1)
        front(0)
        for i in range(NT):
            if i + 2 < NT:
                loads(i + 2)
            if i + 1 < NT:
                interleave(kb, (lambda i=i: tail(i)), (lambda i=i: front(i + 1)), ra=7, rb=1)
            else:
                tail(i)


Prog.stage_d2 = _stage_d2


def _stage_nsa(self, l):
    nc, kb, NT, S, NS, NCC = self.nc, self.kb, self.NT, self.S, self.NS, self.NCC
    NCB = S // 16 - 1
    with ExitStack() as es:
        def Tn(es_, name, shape, dt=F32):
            return Tile(self.sb(es_, "nsa_" + name, shape, dt), "nsa_" + name)

        def T(name, shape, dt=F32):
            return Tn(es, name, shape, dt)

        def V(fn, r, w):
            return kb.op("dve", fn, [t.k for t in r], [t.k for t in w])

        def AC(fn, r, w):
            return kb.op("act", fn, [t.k for t in r], [t.k for t in w])

        def PE(fn, r, w):
            return kb.op("pe", fn, [t.k for t in r], [t.k for t in w])

        def GP(fn, r, w):
            return kb.op("pool", fn, [t.k for t in r], [t.k for t in w])

        def LD(t, dst, src, reads=(), **kw):
            return kb.dma("sp", t.k, dst, src, reads=list(reads), writes=[t.k], **kw)

        QT = T("QT", [128, NT, 2, 128], BF16); KsT = T("KsT", [128, S], BF16); KwT = T("KwT", [128, S], BF16)
        KCT = T("KCT", [128, NCC * 128], BF16)
        VsA = T("VsA", [128, NT, 2, 65], BF16); VwA = T("VwA", [128, NT, 2, 65], BF16)
        RC = T("RC", [128, NCC, 2, 65 + NS], BF16); GA = T("GA", [128, NT, 12])
        idb = T("idb", [128, 128], BF16); nwb = T("nwb", [128, 256])
        LD(idb, idb.t[:], self.ident_b[:, :])
        LD(nwb, nwb.t[:], self.nsa_norm_w[l, :].partition_broadcast(128))
        V(lambda: nc.vector.memset(KCT.t[:], 0.0), [], [KCT])
        V(lambda: nc.vector.memset(RC.t[:], 0.0), [], [RC])
        V(lambda: nc.vector.memset(VsA.t[:], 1.0), [], [VsA])
        V(lambda: nc.vector.memset(VwA.t[:], 1.0), [], [VwA])

        with ExitStack() as e1:
            nin = [Tn(e1, f"nin{j}", [128, 1036]) for j in range(2)]
            cs = [Tn(e1, f"cs{j}", [128, 2, 32]) for j in range(2)]
            ta2 = [Tn(e1, f"ta{j}", [128, 10, 32]) for j in range(2)]; tb2 = [Tn(e1, f"tb{j}", [128, 10, 32]) for j in range(2)]
            rq = [Tn(e1, f"rq{j}", [128, 768], BF16) for j in range(2)]
            kv = [Tn(e1, f"kv{j}", [128, 2, 128], BF16) for j in range(2)]
            pT2 = [Tile(self.ps(e1, f"nsa_pT1{j}", [128, 6, 128], BF16), f"nsa_pT1{j}") for j in range(2)]

            def n1(i):
                s = i % 2
                t0 = i * 128
                ta, tb, pT = ta2[s], tb2[s], pT2[s]
                kb.dma("sp", nin[s].k, nin[s].t[:], self.proj[t0:t0 + 128, C_NQ:C_NQ + 1036], reads=[("proj", i)], writes=[nin[s].k])
                kb.dma("sp", cs[s].k + "c", cs[s].t[:, 0, :], self.rope_cos[t0:t0 + 128, :], writes=[cs[s].k + "c"])
                kb.dma("sp", cs[s].k + "s", cs[s].t[:, 1, :], self.rope_sin[t0:t0 + 128, :], writes=[cs[s].k + "s"])
                csk = [Tile(None, cs[s].k + "c"), Tile(None, cs[s].k + "s")]
                x3 = nin[s].t[:, 0:640].rearrange("p (h d) -> p h d", h=10)
                o3 = rq[s].t[:, 0:640].rearrange("p (h d) -> p h d", h=10)
                cb_ = cs[s].t[:, 0, :].unsqueeze(1).to_broadcast([128, 10, 32])
                sb_ = cs[s].t[:, 1, :].unsqueeze(1).to_broadcast([128, 10, 32])
                V(lambda: nc.vector.tensor_tensor(out=ta.t[:], in0=x3[:, :, 0:32], in1=cb_, op=ALU.mult), [nin[s]] + csk, [ta])
                V(lambda: nc.vector.tensor_tensor(out=tb.t[:], in0=x3[:, :, 32:64], in1=sb_, op=ALU.mult), [nin[s]] + csk, [tb])
                V(lambda: nc.vector.tensor_tensor(out=o3[:, :, 0:32], in0=ta.t[:], in1=tb.t[:], op=ALU.subtract), [ta, tb], [Tile(None, rq[s].k + "a")])
                V(lambda: nc.vector.tensor_tensor(out=ta.t[:], in0=x3[:, :, 32:64], in1=cb_, op=ALU.mult), [nin[s]] + csk, [ta])
                V(lambda: nc.vector.tensor_tensor(out=tb.t[:], in0=x3[:, :, 0:32], in1=sb_, op=ALU.mult), [nin[s]] + csk, [tb])
                V(lambda: nc.vector.tensor_tensor(out=o3[:, :, 32:64], in0=ta.t[:], in1=tb.t[:], op=ALU.add), [ta, tb], [Tile(None, rq[s].k + "b")])
                AC(lambda: nc.scalar.copy(out=rq[s].t[:, 640:768], in_=nin[s].t[:, 640:768]), [nin[s]], [Tile(None, rq[s].k + "c")])
                rqk = [Tile(None, rq[s].k + x) for x in "abc"]
                for b in range(6):
                    PE(lambda: nc.tensor.transpose(out=pT.t[:, b, :], in_=rq[s].t[:, b * 128:(b + 1) * 128], identity=idb.t[:]), rqk + [idb], [pT])
                AC(lambda: nc.scalar.copy(out=QT.t[:, i, :, :], in_=pT.t[:, 0:2, :]), [pT], [Tile(None, f"nsa_QT{i}")])
                AC(lambda: nc.scalar.copy(out=KsT.t[:, t0:t0 + 128], in_=pT.t[:, 3, :]), [pT], [Tile(None, f"nsa_KsT{i}")])
                AC(lambda: nc.scalar.copy(out=KwT.t[:, t0:t0 + 128], in_=pT.t[:, 4, :]), [pT], [Tile(None, f"nsa_KwT{i}")])
                AC(lambda: nc.scalar.copy(out=kv[s].t[:, 0, :], in_=pT.t[:, 2, :]), [pT], [kv[s]])
                AC(lambda: nc.scalar.copy(out=kv[s].t[:, 1, :], in_=pT.t[:, 5, :]), [pT], [kv[s]])
                kb.dma("sp", kv[s].k + "k", self.kcT_d[:, t0:t0 + 128], kv[s].t[:, 0, :], reads=[kv[s].k], writes=[("kcT_d", i)])
                kb.dma("sp", kv[s].k + "v", self.vcT_d[:, t0:t0 + 128], kv[s].t[:, 1, :], reads=[kv[s].k], writes=[("vcT_d", i)])
                GP(lambda: nc.gpsimd.tensor_copy(out=VsA.t[:, i, :, 0:64], in_=nin[s].t[:, 768:896].rearrange("p (g d) -> p g d", g=2)),
                   [nin[s], VsA], [Tile(None, f"nsa_VsA{i}")])
                GP(lambda: nc.gpsimd.tensor_copy(out=VwA.t[:, i, :, 0:64], in_=nin[s].t[:, 896:1024].rearrange("p (g d) -> p g d", g=2)),
                   [nin[s], VwA], [Tile(None, f"nsa_VwA{i}")])
                AC(lambda: nc.scalar.activation(out=GA.t[:, i, :], in_=nin[s].t[:, 1024:1036], func=AF.Sigmoid), [nin[s]], [Tile(None, f"nsa_GA{i}")])

            for i in range(0, NT, 2):
                if i + 1 < NT:
                    interleave(kb, (lambda i=i: n1(i)), (lambda i=i: n1(i + 1)))
                else:
                    n1(i)
            kb.barrier()

        with ExitStack() as e2:
            wst = Tn(e2, "wst", [128, 32, 128]); W1 = [Tn(e2, f"W1{j}", [128, 32, 128], BF16) for j in range(2)]
            w2s = Tn(e2, "w2s", [128, 64]); W2p = Tn(e2, "W2p", [128, 2, 128], BF16); W2v = Tn(e2, "W2v", [128, 64], BF16)
            psT = Tn(e2, "psT", [128, 32]); posb = [Tn(e2, f"posb{j}", [128, 32, 2], BF16) for j in range(2)]
            cbv = [Tn(e2, f"cbv{j}", [128, 2]) for j in range(2)]
            XT = Tn(e2, "XT", [128, 128 * 16 + 16], BF16)
            hb = Tn(e2, "hb", [128, 128]); tt = Tn(e2, "tt", [128, 128]); GT = [Tn(e2, f"GT{g}", [128, 128], BF16) for g in range(2)]
            ph = Tile(self.ps(e2, "nsa_ph", [128, 128]), "nsa_ph"); pc = Tile(self.ps(e2, "nsa_pc", [128, 128]), "nsa_pc")
            pb = Tile(self.ps(e2, "nsa_pb", [128, 2]), "nsa_pb")
            V(lambda: nc.vector.memset(W2p.t[:], 0.0), [], [W2p])
            for wi, (w1d, w2d, posd) in enumerate([(self.nsa_cmp_w1_k, self.nsa_cmp_w2_k, self.nsa_cmp_pos_k),
                                                   (self.nsa_cmp_w1_v, self.nsa_cmp_w2_v, self.nsa_cmp_pos_v)]):
                for half in range(2):
                    LD(wst, wst.t[half * 64:(half + 1) * 64, :, :], w1d[l].rearrange("(i d) j -> d i j", d=64))
                V(lambda: nc.vector.tensor_copy(out=W1[wi].t[:], in_=wst.t[:]), [wst], [W1[wi]])
                LD(w2s, w2s.t[:], w2d[l, :, :])
                if wi == 0:
                    for g in range(2):
                        V(lambda: nc.vector.tensor_copy(out=W2p.t[:, g, g * 64:(g + 1) * 64], in_=w2s.t[:]), [w2s], [W2p])
                else:
                    V(lambda: nc.vector.tensor_copy(out=W2v.t[:], in_=w2s.t[:]), [w2s], [W2v])
                for half in range(2):
                    LD(psT, psT.t[half * 64:(half + 1) * 64, :], posd[l].rearrange("i d -> d i"), allow_slow_non_contiguous=True)
                for j in range(2):
                    V(lambda: nc.vector.tensor_copy(out=posb[wi].t[:, :, j], in_=psT.t[:]), [psT], [posb[wi]])
                for i_ in range(32):
                    PE(lambda: nc.tensor.matmul(out=pb.t[:], lhsT=W1[wi].t[0:64, i_, :], rhs=posb[wi].t[0:64, i_, :], start=(i_ == 0), stop=(i_ == 31)),
                       [W1[wi], posb[wi]], [pb])
                V(lambda: nc.vector.tensor_copy(out=cbv[wi].t[:], in_=pb.t[:]), [pb], [cbv[wi]])
            for c in range(NCC):
                n0 = c * 128
                nn = min(128, NCB - n0)
                ntok = 16 * nn + 16
                for wi, xd, xkey in [(0, self.kcT_d, "kcT_d"), (1, self.vcT_d, "vcT_d")]:
                    LD(XT, XT.t[:, 0:ntok], xd[:, 16 * n0:16 * n0 + ntok],
                       reads=[(xkey, j) for j in range((16 * n0) // 128, min(NT, (16 * n0 + ntok + 127) // 128))])
                    xv = XT.t[:, 0:ntok].rearrange("p (n s) -> p n s", s=16)
                    for g in range(2):
                        for i_ in range(32):
                            a, b = i_ // 16, i_ % 16
                            PE(lambda: nc.tensor.matmul(out=ph.t[:, 0:nn], lhsT=W1[wi].t[g * 64:(g + 1) * 64, i_, :],
                                                        rhs=xv[g * 64:(g + 1) * 64, a:a + nn, b], start=(i_ == 0), stop=(i_ == 31)),
                               [W1[wi], XT], [ph])
                        AC(lambda: nc.scalar.activation(out=hb.t[:, 0:nn], in_=ph.t[:, 0:nn], func=AF.Identity, bias=cbv[wi].t[:, 0:1]), [ph, cbv[wi]], [hb])
                        V(lambda: nc.vector.tensor_tensor(out=tt.t[:, 0:nn], in0=hb.t[:, 0:nn], in1=hb.t[:, 0:nn], op=ALU.mult), [hb], [tt])
                        V(lambda: nc.vector.tensor_scalar(out=tt.t[:, 0:nn], in0=tt.t[:, 0:nn], scalar1=0.044715, scalar2=1.0, op0=ALU.mult, op1=ALU.add), [tt], [tt])
                        V(lambda: nc.vector.tensor_tensor(out=tt.t[:, 0:nn], in0=tt.t[:, 0:nn], in1=hb.t[:, 0:nn], op=ALU.mult), [tt, hb], [tt])
                        AC(lambda: nc.scalar.activation(out=tt.t[:, 0:nn], in_=tt.t[:, 0:nn], func=AF.Tanh, scale=0.7978845608028654), [tt], [tt])
                        V(lambda: nc.vector.tensor_scalar(out=tt.t[:, 0:nn], in0=tt.t[:, 0:nn], scalar1=0.5, scalar2=0.5, op0=ALU.mult, op1=ALU.add), [tt], [tt])
                        if nn < 128:
                            V(lambda: nc.vector.memset(GT[g].t[:], 0.0), [], [GT[g]])
                        V(lambda: nc.vector.tensor_tensor(out=GT[g].t[:, 0:nn], in0=tt.t[:, 0:nn], in1=hb.t[:, 0:nn], op=ALU.mult), [tt, hb], [GT[g]])
                    if wi == 0:
                        for g in range(2):
                            PE(lambda: nc.tensor.matmul(out=pc.t[:, 0:nn], lhsT=W2p.t[:, g, :], rhs=GT[g].t[:, 0:nn], start=(g == 0), stop=(g == 1)),
                               [W2p, GT[g]], [pc])
                        AC(lambda: nc.scalar.copy(out=KCT.t[:, n0:n0 + nn], in_=pc.t[:, 0:nn]), [pc], [KCT])
                    else:
                        for g in range(2):
                            PE(lambda: nc.tensor.matmul(out=pc.t[:, g * 64:(g + 1) * 64], lhsT=GT[g].t[:], rhs=W2v.t[:], start=True, stop=True),
                               [GT[g], W2v], [pc])
                        AC(lambda: nc.scalar.copy(out=RC.t[:, c, :, 0:64], in_=pc.t[:].rearrange("p (g d) -> p g d", g=2)), [pc], [RC])
            ovs = Tn(e2, "ovs", [128, NCC, NS], BF16)
            LD(ovs, ovs.t[:], self.c_ovl[:, :, :])
            for g in range(2):
                V(lambda: nc.vector.tensor_copy(out=RC.t[:, :, g, 65:65 + NS], in_=ovs.t[:]), [ovs, RC], [RC])
                V(lambda: nc.vector.memset(RC.t[:, :, g, 64:65], 1.0), [RC], [RC])
            kb.barrier()

        with ExitStack() as e3:
            def T3(name, shape, dt=F32):
                return Tn(e3, name, shape, dt)
            LA = 4
            Ew = T3("Ew", [128, S], BF16); cmk = T3("cmk", [128, 17, 128], BF16)
            Fw = T3("Fw", [128, 2 * NS - 2]); Kw = T3("Kw", [128, 2 * NS - 2])
            trib = T3("trib", [128, 128], BF16); sltb = T3("sltb", [128, 128], BF16)
            LD(Ew, Ew.t[:], self.c_Ew[:, :]); LD(cmk, cmk.t[:], self.c_cmask[:, :, :])
            LD(Fw, Fw.t[:], self.c_Fw[:, :]); LD(Kw, Kw.t[:], self.c_Kw[:, :])
            LD(trib, trib.t[:], self.c_trib[:, :]); LD(sltb, sltb.t[:], self.c_sltb[:, :])
            qz = [[T3(f"qz{p_}{g}", [128, 2, 128], BF16) for g in range(2)] for p_ in range(2)]
            pe_ = [T3(f"pe{j}", [128, 2, 128], BF16) for j in range(LA + 1)]
            rl = T3("rl", [128, 1]); wgt = T3("wgt", [128, 1])
            IMP = [T3(f"IMP{g}", [128, NS]) for g in range(2)]; impm = [T3(f"impm{g}", [128, NS]) for g in range(2)]
            wk = [T3(f"wk{g}", [128, NS]) for g in range(2)]
            m8a = [T3(f"m8a{g}", [128, 8]) for g in range(2)]; m8b = [T3(f"m8b{g}", [128, 8]) for g in range(2)]
            sneg = [T3(f"sneg{g}", [128, NS], BF16) for g in range(2)]
            SNT = [T3(f"SNT{g}", [128, 2, 128], BF16) for g in range(2)]
            oacc = [T3(f"oacc{j}", [128, 4, 64]) for j in range(2)]
            sq = T3("sq", [128, 256]); ss = T3("ss", [128, 4]); mo = [T3(f"mo{j}", [128, 256], BF16) for j in range(2)]
            psc = [Tile(self.ps(e3, f"nsa_psc{j}", [128, 2, 128]), f"nsa_psc{j}") for j in range(LA + 1)]
            pcvb = Tile(self.ps(e3, "nsa_pcv", [128, 2, 65 + NS]), "nsa_pcv")
            posb = Tile(self.ps(e3, "nsa_pos", [128, 2, 65]), "nsa_pos")
            pcv = [Tile(pcvb.t[:, r, :], pcvb.k) for r in range(2)]
            pos_ = [Tile(posb.t[:, r, :], posb.k) for r in range(2)]
            pst = Tile(self.ps(e3, "nsa_pst", [128, 128], BF16), "nsa_pst")
            for g in range(2):
                V(lambda: nc.vector.memset(SNT[g].t[:], 0.0), [], [SNT[g]])
            nsc = [0]
            pending = []

            def emit_score(job):
                j = nsc[0] % (LA + 1)
                nsc[0] += 1
                q_ = job["q"]
                extra = job.get("extra")
                PE(lambda: nc.tensor.matmul(out=psc[j].t[:], lhsT=job["lhsT"], rhs=q_.t[:], start=True, stop=(extra is None)),
                   job["lt"] + [q_], [psc[j]])
                if extra is not None:
                    sn = job["snt"]
                    PE(lambda: nc.tensor.matmul(out=psc[j].t[:], lhsT=extra, rhs=sn.t[:], start=False, stop=True), [Ew, sn], [psc[j]])
                AC(lambda: nc.scalar.activation(out=pe_[j].t[:], in_=psc[j].t[:], func=AF.Exp, scale=0.125), [psc[j]], [pe_[j]])
                if job.get("mask") is not None:
                    mt_, mk = job["mask"]
                    GP(lambda: nc.gpsimd.tensor_tensor(out=pe_[j].t[:], in0=pe_[j].t[:], in1=mk.unsqueeze(1).to_broadcast([128, 2, 128]), op=ALU.mult),
                       [pe_[j], mt_], [pe_[j]])
                job["pe"] = pe_[j]

            def emit_pv(job):
                pt = job["pe"]
                for r in range(2):
                    tgt = job["tgt"][r]
                    PE(lambda: nc.tensor.matmul(out=tgt.t[:], lhsT=pt.t[:, r, :], rhs=job["rhs"], start=(job["start"] and r == 0),
                                                stop=(job["stop"] and r == 1)),
                       [pt] + job["vt"], [tgt])
                if job.get("after") is not None:
                    job["after"]()

            def push(job):
                emit_score(job)
                pending.append(job)
                while len(pending) > LA:
                    emit_pv(pending.pop(0))

            def finish(pt, oa, h, b, i, first):
                V(lambda: nc.vector.tensor_scalar(out=rl.t[:], in0=pt.t[:, 64:65], scalar1=1e-30, scalar2=None, op0=ALU.max), [pt], [rl])
                V(lambda: nc.vector.reciprocal(out=rl.t[:], in_=rl.t[:]), [rl], [rl])
                V(lambda: nc.vector.tensor_tensor(out=wgt.t[:], in0=rl.t[:], in1=GA.t[:, i, 3 * h + b:3 * h + b + 1], op=ALU.mult),
                  [rl, Tile(None, f"nsa_GA{i}")], [wgt])
                if first:
                    V(lambda: nc.vector.tensor_scalar(out=oa.t[:, h, :], in0=pt.t[:, 0:64], scalar1=wgt.t[:], scalar2=None, op0=ALU.mult), [pt, wgt], [oa])
                else:
                    V(lambda: nc.vector.scalar_tensor_tensor(out=oa.t[:, h, :], in0=pt.t[:, 0:64], scalar=wgt.t[:], in1=oa.t[:, h, :],
                                                             op0=ALU.mult, op1=ALU.add), [pt, wgt, oa], [oa])

            def after_cmp(i, g, oa):
                def f():
                    for r in range(2):
                        finish(pcv[r], oa, 2 * g + r, 0, i, True)
                        if r == 0:
                            V(lambda: nc.vector.tensor_scalar(out=IMP[g].t[:], in0=pcv[r].t[:, 65:65 + NS], scalar1=rl.t[:], scalar2=None, op0=ALU.mult),
                              [pcv[r], rl], [IMP[g]])
                        else:
                            V(lambda: nc.vector.scalar_tensor_tensor(out=IMP[g].t[:], in0=pcv[r].t[:, 65:65 + NS], scalar=rl.t[:], in1=IMP[g].t[:],
                                                                     op0=ALU.mult, op1=ALU.add), [pcv[r], rl, IMP[g]], [IMP[g]])
                    fo = NS - 2 - 2 * i
                    V(lambda: nc.vector.tensor_tensor(out=impm[g].t[:], in0=IMP[g].t[:], in1=Kw.t[:, fo:fo + NS], op=ALU.mult), [IMP[g], Kw], [impm[g]])
                    V(lambda: nc.vector.tensor_tensor(out=impm[g].t[:], in0=impm[g].t[:], in1=Fw.t[:, fo:fo + NS], op=ALU.add), [impm[g], Fw], [impm[g]])
                    V(lambda: nc.vector.memset(impm[g].t[:, 0:1], 1e30), [impm[g]], [impm[g]])
                    V(lambda: nc.vector.max(out=m8a[g].t[:], in_=impm[g].t[:]), [impm[g]], [m8a[g]])
                    V(lambda: nc.vector.match_replace(out=wk[g].t[:], in_to_replace=m8a[g].t[:], in_values=impm[g].t[:], imm_value=-3e38),
                      [m8a[g], impm[g]], [wk[g]])
                    V(lambda: nc.vector.max(out=m8b[g].t[:], in_=wk[g].t[:]), [wk[g]], [m8b[g]])
                    V(lambda: nc.vector.tensor_scalar(out=sneg[g].t[:], in0=impm[g].t[:], scalar1=m8b[g].t[:, 7:8], scalar2=1.0, op0=ALU.is_ge, op1=ALU.subtract),
                      [impm[g], m8b[g]], [sneg[g]])
                return f

            def after_fin(tgt, i, g, b, oa):
                def f():
                    for r in range(2):
                        finish(tgt[r], oa, 2 * g + r, b, i, False)
                return f

            def tail(i, oa):
                def f():
                    s = i % 2
                    t0 = i * 128
                    o2 = oa.t[:].rearrange("p h d -> p (h d)")
                    AC(lambda: nc.scalar.activation(out=sq.t[:], in_=o2, func=AF.Square), [oa], [sq])
                    V(lambda: nc.vector.tensor_reduce(out=ss.t[:], in_=sq.t[:].rearrange("p (h d) -> p h d", h=4), axis=AX.X, op=ALU.add), [sq], [ss])
                    V(lambda: nc.vector.tensor_scalar(out=ss.t[:], in0=ss.t[:], scalar1=1.0 / 64, scalar2=EPS, op0=ALU.mult, op1=ALU.add), [ss], [ss])
                    AC(lambda: nc.scalar.activation(out=ss.t[:], in_=ss.t[:], func=AF.Sqrt), [ss], [ss])
                    V(lambda: nc.vector.reciprocal(out=ss.t[:], in_=ss.t[:]), [ss], [ss])
                    V(lambda: nc.vector.tensor_tensor(out=sq.t[:].rearrange("p (h d) -> p h d", h=4), in0=oa.t[:],
                                                      in1=ss.t[:].unsqueeze(2).to_broadcast([128, 4, 64]), op=ALU.mult), [oa, ss, sq], [sq])
                    V(lambda: nc.vector.tensor_tensor(out=mo[s].t[:], in0=sq.t[:], in1=nwb.t[:], op=ALU.mult), [sq, nwb], [mo[s]])
                    kb.dma("sp", mo[s].k, self.mix[t0:t0 + 128, 256:512], mo[s].t[:], reads=[mo[s].k], writes=[("mix_nsa", i)])
                return f

            for i in range(NT):
                qq = qz[i % 2]
                oa = oacc[i % 2]
                for g in range(2):
                    AC(lambda: nc.scalar.copy(out=qq[g].t[:], in_=QT.t[:, i, :, :]), [Tile(None, f"nsa_QT{i}")], [qq[g]])
                    o0 = (1 - g) * 64
                    GP(lambda: nc.gpsimd.memset(qq[g].t[o0:o0 + 64, :, :], 0.0), [qq[g]], [qq[g]])
                ncs = min(NCC, i // 16 + 1)
                k0 = max(0, i - 4)
                for g in range(2):
                    for c in range(ncs):
                        dl = i - 16 * c
                        push(dict(lhsT=KCT.t[:, c * 128:(c + 1) * 128], lt=[KCT], q=qq[g],
                                  mask=(cmk, cmk.t[:, dl, :]) if dl <= 16 else None,
                                  tgt=pcv, rhs=RC.t[:, c, g, :], vt=[RC], start=(c == 0), stop=(c == ncs - 1),
                                  after=after_cmp(i, g, oa) if c == ncs - 1 else None, cmpg=g))
                    for kt in range(k0, i + 1):
                        mf = (trib, trib.t[:]) if kt == i else ((sltb, sltb.t[:]) if kt == i - 4 else None)
                        push(dict(lhsT=KwT.t[:, kt * 128:(kt + 1) * 128], lt=[Tile(None, f"nsa_KwT{kt}")], q=qq[g], mask=mf,
                                  tgt=[Tile(pcv[0].t[:, 0:65], pcv[0].k), Tile(pcv[1].t[:, 0:65], pcv[1].k)],
                                  rhs=VwA.t[:, kt, g, :], vt=[Tile(None, f"nsa_VwA{kt}")], start=(kt == k0), stop=(kt == i),
                                  after=after_fin(pcv, i, g, 2, oa) if kt == i else None))
                while any(j_.get("cmpg") is not None for j_ in pending):
                    emit_pv(pending.pop(0))
                for g in range(2):
                    PE(lambda: nc.tensor.transpose(out=pst.t[0:NS, :], in_=sneg[g].t[:], identity=idb.t[:]), [sneg[g], idb], [pst])
                    for r in range(2):
                        AC(lambda: nc.scalar.copy(out=SNT[g].t[0:NS, r, :], in_=pst.t[0:NS, :]), [pst], [SNT[g]])
                    for kt in range(i + 1):
                        last = (kt == i)
                        aft = None
                        if last:
                            fin = after_fin(pos_, i, g, 1, oa)
                            if g == 1:
                                tl_ = tail(i, oa)
                                aft = (lambda fin=fin, tl_=tl_: (fin(), tl_()))
                            else:
                                aft = fin
                        push(dict(lhsT=KsT.t[:, kt * 128:(kt + 1) * 128], lt=[Tile(None, f"nsa_KsT{kt}")], q=qq[g],
                                  mask=(trib, trib.t[:]) if last else None, extra=Ew.t[:, kt * 128:(kt + 1) * 128], snt=SNT[g],
                                  tgt=pos_, rhs=VsA.t[:, kt, g, :], vt=[Tile(None, f"nsa_VsA{kt}")], start=(kt == 0), stop=last, after=aft))
            while pending:
                emit_pv(pending.pop(0))


Prog.stage_nsa = _stage_nsa


def build_full(S, L=2):
    p = Prog(S, L=L, dbg=[("out", [S, D])])
    kb = p.kb
    with kb.es:
        h = p.x
        for l in range(L):
            p.stage_d1(l, h)
            kb.barrier()
            p.stage_ssd(l)
            kb.barrier()
            p.stage_gla(l)
            kb.barrier()
            p.stage_nsa(l)
            kb.barrier()
            final = (l == L - 1)
            p.stage_d2(l, h, p.outs["out"] if final else p.hbuf, final)
            kb.barrier()
            h = p.hbuf
        kb.drain("sp")
    return p


_WNAMES = ["norm1_w", "w_in", "gla_gate_w2", "gla_gate_b", "gla_norm_w", "nsa_cmp_pos_k", "nsa_cmp_w1_k", "nsa_cmp_w2_k",
           "nsa_cmp_pos_v", "nsa_cmp_w1_v", "nsa_cmp_w2_v", "nsa_norm_w", "ssd_conv_w", "ssd_conv_b", "ssd_dt_bias",
           "ssd_a_log", "ssd_d", "ssd_norm_w", "w_out", "norm2_w", "w_up", "w_down", "final_norm_w"]


def kernel(**inputs):
    x = np.asarray(inputs["x"], dtype=np.float32)
    B, S, _ = x.shape
    L = int(np.asarray(inputs["w_in"]).shape[0])
    p = build_full(S, L)
    base = {k: np.ascontiguousarray(np.asarray(inputs[k], dtype=np.float32)) for k in _WNAMES}
    base.update(consts(S))
    in_maps = []
    for b in range(B):
        m = dict(base)
        m["x"] = np.ascontiguousarray(x[b])
        in_maps.append(m)
    res = run_bass_kernel_spmd(p.nc, in_maps, core_ids=list(range(B)))
    return np.stack([np.asarray(res.results[b]["out"], dtype=np.float32) for b in range(B)], axis=0)
```

```python
import numpy as np
import ml_dtypes
import concourse.bass as bass
import concourse.mybir as mybir
from concourse.bass_utils import run_bass_kernel_spmd
from contextlib import ExitStack
import threading

F32 = mybir.dt.float32
BF16 = mybir.dt.bfloat16
AF = mybir.ActivationFunctionType
ALU = mybir.AluOpType
AX = mybir.AxisListType

D = 1024
DIN = 3364
DFF = 4096
NPROJ = 2340
EPS = 1e-6

C_GQ, C_GK, C_GV, C_GLR, C_GR = 0, 128, 256, 512, 528
C_NQ, C_KCMP, C_KSLC, C_KWIN, C_VCMP, C_VSLC, C_VWIN, C_NG = 784, 1040, 1168, 1296, 1424, 1552, 1680, 1808
C_Z, C_DT = 1820, 2332
WMAP = [(0, 784, 0), (784, 64, 784), (912, 64, 848), (848, 64, 912), (976, 64, 976), (1040, 128, 1040), (1296, 128, 1168), (1552, 128, 1296), (1168, 128, 1424),
        (1424, 128, 1552), (1680, 652, 1680), (3356, 8, 2332), (2332, 1024, -1)]


class KB:
    EPOCH = 30000

    def __init__(self, nc, same_engine_sync=True):
        self.nc = nc
        self.es = ExitStack()
        self.eng = {"pe": nc.tensor, "act": nc.scalar, "dve": nc.vector,
                    "pool": nc.gpsimd, "sp": nc.sync}
        self.esem = {}
        self.ecnt = {}
        self.nsem = 0
        self.dsem = {}
        self.waited = {e: {} for e in self.eng}
        self.last_w = {}
        self.readers = {}
        self.same = same_engine_sync
        self.sem_owner = {}
        self.ninst = 0
        self.limit = None
        self.hook = None
        for e in self.eng:
            self._new_esem(e)

    def _sem(self, name):
        self.nsem += 1
        return self.es.enter_context(self.nc.semaphore(f"s{self.nsem}_{name}"))

    def _new_esem(self, e):
        s = self._sem(e)
        self.esem[e] = s
        self.ecnt[e] = 0
        self.sem_owner[id(s)] = e

    def _wait(self, e, deps):
        best = {}
        for item in deps:
            if item is None:
                continue
            if len(item) == 3:
                s, v, raw = item
            else:
                (s, v), raw = item, True
            owner = self.sem_owner.get(id(s))
            if owner == e and (e == "pe" or not self.same):
                continue
            if best.get(id(s), (None, 0))[1] < v:
                best[id(s)] = (s, v)
        for sid, (s, v) in best.items():
            if self.waited[e].get(sid, 0) < v:
                self.eng[e].wait_ge(s, v)
                self.waited[e][sid] = v

    def _deps(self, reads, writes):
        deps = []
        for k in reads:
            ev = self.last_w.get(k)
            if ev is not None:
                deps.append((ev[0], ev[1], True))
        for k in writes:
            ev = self.last_w.get(k)
            if ev is not None:
                deps.append((ev[0], ev[1], False))
            for ev in self.readers.get(k, []):
                deps.append((ev[0], ev[1], False))
        return deps

    def _commit(self, ev, reads, writes):
        for k in writes:
            self.last_w[k] = ev
            self.readers[k] = []
        for k in reads:
            if k in writes:
                continue
            self.readers.setdefault(k, []).append(ev)

    def op(self, e, fn, reads=(), writes=()):
        if self.hook is not None:
            self.hook()
        if self.limit is not None and self.ninst >= self.limit:
            return None
        self._wait(e, self._deps(reads, writes))
        inst = fn()
        if self.ecnt[e] >= self.EPOCH:
            self._new_esem(e)
        self.ecnt[e] += 1
        s = self.esem[e]
        inst.then_inc(s, 1)
        self._commit((s, self.ecnt[e]), reads, writes)
        self.ninst += 1
        return inst

    def dma(self, q, key, out, in_, reads=(), writes=(), **kw):
        if self.hook is not None:
            self.hook()
        if self.limit is not None and self.ninst >= self.limit:
            return None
        self._wait(q, self._deps(reads, writes))
        if key not in self.dsem:
            self.dsem[key] = [self._sem("d"), 0]
        ent = self.dsem[key]
        inst = self.eng[q].dma_start(out=out, in_=in_, **kw)
        ent[1] += 16
        inst.then_inc(ent[0], 16)
        self._commit((ent[0], ent[1]), reads, writes)
        self.ninst += 1
        return inst

    def barrier(self):
        deps = list(self.last_w.values())
        for r in self.readers.values():
            deps.extend(r)
        for e in self.eng:
            self._wait(e, deps)
        self.readers = {k: [] for k in self.readers}

    def drain(self, e="sp"):
        deps = list(self.last_w.values())
        for r in self.readers.values():
            deps.extend(r)
        self._wait(e, deps)


def interleave(kb, fa, fb, ra=1, rb=1):
    sem = {"a": threading.Semaphore(0), "b": threading.Semaphore(0)}
    done = {"a": False, "b": False}
    err = []
    loc = threading.local()

    quota = {"a": ra, "b": rb}
    cnt = {"a": 0, "b": 0}

    def hook():
        me = loc.me
        other = "b" if me == "a" else "a"
        if done[other]:
            return
        cnt[me] += 1
        if cnt[me] < quota[me]:
            return
        cnt[me] = 0
        sem[other].release()
        sem[me].acquire()

    def run(me, f):
        loc.me = me
        other = "b" if me == "a" else "a"
        sem[me].acquire()
        try:
            f()
        except BaseException as ex:
            err.append(ex)
        done[me] = True
        sem[other].release()

    kb.hook = hook
    ta = threading.Thread(target=run, args=("a", fa))
    tb = threading.Thread(target=run, args=("b", fb))
    ta.start()
    tb.start()
    sem["a"].release()
    ta.join()
    tb.join()
    kb.hook = None
    if err:
        raise err[0]


class Prog:
    def __init__(self, S, L=2, dbg=()):
        self.S = S
        self.NT = S // 128
        self.L = L
        self.dbg = dbg
        nc = self.nc = bass.Bass("TRN2", target_bir_lowering=False)
        self.kb = KB(nc)
        Ld = L

        def inp(name, shape, dt=F32):
            return nc.dram_tensor(name, list(shape), dt, kind="ExternalInput").ap()

        def scr(name, shape, dt=F32):
            return nc.dram_tensor(name, list(shape), dt, kind="Internal").ap()

        self.x = inp("x", [S, D])
        self.norm1_w = inp("norm1_w", [Ld, D])
        self.w_in = inp("w_in", [Ld, D, DIN])
        self.gla_gate_w2 = inp("gla_gate_w2", [Ld, 16, 128])
        self.gla_gate_b = inp("gla_gate_b", [Ld, 128])
        self.gla_norm_w = inp("gla_norm_w", [Ld, 256])
        self.nsa_cmp_pos_k = inp("nsa_cmp_pos_k", [Ld, 32, 64])
        self.nsa_cmp_w1_k = inp("nsa_cmp_w1_k", [Ld, 2048, 128])
        self.nsa_cmp_w2_k = inp("nsa_cmp_w2_k", [Ld, 128, 64])
        self.nsa_cmp_pos_v = inp("nsa_cmp_pos_v", [Ld, 32, 64])
        self.nsa_cmp_w1_v = inp("nsa_cmp_w1_v", [Ld, 2048, 128])
        self.nsa_cmp_w2_v = inp("nsa_cmp_w2_v", [Ld, 128, 64])
        self.nsa_norm_w = inp("nsa_norm_w", [Ld, 256])
        self.ssd_conv_w = inp("ssd_conv_w", [Ld, 4, 1024])
        self.ssd_conv_b = inp("ssd_conv_b", [Ld, 1024])
        self.ssd_dt_bias = inp("ssd_dt_bias", [Ld, 8])
        self.ssd_a_log = inp("ssd_a_log", [Ld, 8])
        self.ssd_d = inp("ssd_d", [Ld, 8])
        self.ssd_norm_w = inp("ssd_norm_w", [Ld, 512])
        self.w_out = inp("w_out", [Ld, D, D])
        self.norm2_w = inp("norm2_w", [Ld, D])
        self.w_up = inp("w_up", [Ld, D, DFF])
        self.w_down = inp("w_down", [Ld, DFF, D])
        self.final_norm_w = inp("final_norm_w", [D])
        self.ident_f = inp("ident_f", [128, 128])
        self.ident_b = inp("ident_b", [128, 128], BF16)
        self.c_tri = inp("c_tri", [128, 128])
        self.c_slt = inp("c_slt", [128, 128])
        self.c_ones = inp("c_ones", [128, 128])
        self.c_hm = inp("c_hm", [128, 4])
        self.c_bd = inp("c_bd", [128, 256])
        self.mix = scr("mix", [S, D], BF16)
        NS = S // 64
        self.NS = NS
        self.NCC = max(1, (S // 16 - 1 + 127) // 128)
        self.rope_cos = inp("rope_cos", [S, 32])
        self.rope_sin = inp("rope_sin", [S, 32])
        self.c_Ew = inp("c_Ew", [128, S], BF16)
        self.c_cmask = inp("c_cmask", [128, 17, 128], BF16)
        self.c_ovl = inp("c_ovl", [128, self.NCC, NS], BF16)
        self.c_Fw = inp("c_Fw", [128, 2 * NS - 2])
        self.c_Kw = inp("c_Kw", [128, 2 * NS - 2])
        self.c_trib = inp("c_trib", [128, 128], BF16)
        self.c_sltb = inp("c_sltb", [128, 128], BF16)
        self.kcT_d = scr("kcT_d", [128, S + 128], BF16)
        self.vcT_d = scr("vcT_d", [128, S + 128], BF16)
        self.proj = scr("proj", [S, NPROJ])
        self.xbcT = scr("xbcT", [D, S])
        self.hbuf = scr("hbuf", [S, D])
        self.outs = {}
        for name, shape in dbg:
            self.outs[name] = nc.dram_tensor(name, list(shape), F32, kind="ExternalOutput").ap()

    def sb(self, es, name, shape, dt=F32):
        self._uid = getattr(self, "_uid", 0) + 1
        return es.enter_context(self.nc.sbuf_tensor(f"{name}_u{self._uid}", list(shape), dt))

    def ps(self, es, name, shape, dt=F32):
        full = 512 if dt == F32 else 1024
        self._uid = getattr(self, "_uid", 0) + 1
        t = es.enter_context(self.nc.psum_tensor(f"{name}_u{self._uid}", [128, full], dt))
        n = 1
        for d in shape[1:]:
            n *= d
        assert n <= full
        v = t[0:shape[0], 0:n]
        if len(shape) == 3:
            v = v.rearrange("p (a b) -> p a b", a=shape[1])
        return v

    def stage_d1(self, l, hsrc):
        nc, kb, NT = self.nc, self.kb, self.NT
        with ExitStack() as es:
            Wtm = self.sb(es, "d1_Wtm", [128, 8, NPROJ], BF16)
            Wx = self.sb(es, "d1_Wx", [128, 8, 1024], BF16)
            stg = [self.sb(es, f"d1_stg{i}", [128, DIN]) for i in range(2)]
            nw = self.sb(es, "d1_nw", [128, 8])
            idb = self.sb(es, "d1_idb", [128, 128], BF16)
            ht = [self.sb(es, f"d1_ht{i}", [128, D]) for i in range(2)]
            junk = self.sb(es, "d1_junk", [128, D], BF16)
            ss = [self.sb(es, f"d1_ss{i}", [128, 1]) for i in range(2)]
            rs = [self.sb(es, f"d1_rs{i}", [128, 1]) for i in range(2)]
            u = [self.sb(es, f"d1_u{i}", [128, D], BF16) for i in range(2)]
            uT = [self.sb(es, f"d1_uT{i}", [128, 8, 128], BF16) for i in range(2)]
            ot = [self.sb(es, f"d1_ot{i}", [128, NPROJ]) for i in range(2)]
            xo = [self.sb(es, f"d1_xo{i}", [128, 8, 128]) for i in range(2)]
            pT = self.ps(es, "d1_pT", [128, 8, 128], BF16)
            pm = [self.ps(es, f"d1_pm{i}", [128, 512]) for i in range(3)]
            px = [self.ps(es, f"d1_px{i}", [128, 4, 128]) for i in range(2)]

            kb.dma("sp", "d1_idb", idb[:], self.ident_b[:, :], writes=["d1_idb"])
            for c in range(8):
                kb.dma("sp", "d1_nw", nw[:, c:c + 1],
                       self.norm1_w[l, c * 128:(c + 1) * 128].rearrange("(p o) -> p o", o=1),
                       writes=["d1_nw"])
            for c in range(8):
                s = c % 2
                kb.dma("sp", f"d1_stg{s}", stg[s][:], self.w_in[l, c * 128:(c + 1) * 128, :],
                       writes=[f"d1_stg{s}"])
                for j, (so, n, do) in enumerate(WMAP):
                    dst = Wx[:, c, 0:n] if do < 0 else Wtm[:, c, do:do + n]
                    wk = f"d1_W{c}"
                    if j % 2 == 0:
                        kb.op("dve", lambda: nc.vector.tensor_scalar(
                            out=dst, in0=stg[s][:, so:so + n], scalar1=nw[:, c:c + 1], scalar2=None,
                            op0=ALU.mult), reads=[f"d1_stg{s}", "d1_nw"], writes=[wk + f"_{j}"])
                    else:
                        kb.op("act", lambda: nc.scalar.activation(
                            out=dst, in_=stg[s][:, so:so + n], func=AF.Copy, scale=nw[:, c:c + 1]),
                            reads=[f"d1_stg{s}", "d1_nw"], writes=[wk + f"_{j}"])
            Wkeys = [f"d1_W{c}_{j}" for c in range(8) for j in range(len(WMAP))]

            def LDA(i):
                s = i % 2
                kb.dma("sp", f"d1_ht{s}", ht[s][:], hsrc[i * 128:(i + 1) * 128, :],
                       reads=[("h", i)], writes=[f"d1_ht{s}"])

            def A(i):
                s = i % 2
                kb.op("act", lambda: nc.scalar.activation(out=junk[:], in_=ht[s][:], func=AF.Square,
                                                          accum_out=ss[s][:]),
                      reads=[f"d1_ht{s}"], writes=["d1_junk", f"d1_ss{s}"])
                kb.op("dve", lambda: nc.vector.tensor_scalar(out=rs[s][:], in0=ss[s][:], scalar1=1.0 / D,
                                                             scalar2=EPS, op0=ALU.mult, op1=ALU.add),
                      reads=[f"d1_ss{s}"], writes=[f"d1_rs{s}"])
                kb.op("act", lambda: nc.scalar.activation(out=rs[s][:], in_=rs[s][:], func=AF.Sqrt),
                      reads=[f"d1_rs{s}"], writes=[f"d1_rs{s}"])
                kb.op("dve", lambda: nc.vector.reciprocal(out=rs[s][:], in_=rs[s][:]),
                      reads=[f"d1_rs{s}"], writes=[f"d1_rs{s}"])
                kb.op("dve", lambda: nc.vector.tensor_scalar(out=u[s][:], in0=ht[s][:], scalar1=rs[s][:],
                                                             scalar2=None, op0=ALU.mult),
                      reads=[f"d1_ht{s}", f"d1_rs{s}"], writes=[f"d1_u{s}"])
                for c in range(8):
                    kb.op("pe", lambda: nc.tensor.transpose(out=pT[:, c, :], in_=u[s][:, c * 128:(c + 1) * 128],
                                                            identity=idb[:]),
                          reads=[f"d1_u{s}", "d1_idb"], writes=["d1_pT"])
                kb.op("act", lambda: nc.scalar.copy(out=uT[s][:], in_=pT[:]),
                      reads=["d1_pT"], writes=[f"d1_uT{s}"])

            def B(i):
                s = i % 2
                off = 0
                k = 0
                while off < NPROJ:
                    n = min(512, NPROJ - off)
                    p = pm[k % 3]
                    pk = f"d1_pm{k % 3}"
                    for c in range(8):
                        kb.op("pe", lambda: nc.tensor.matmul(out=p[:, 0:n], lhsT=uT[s][:, c, :],
                                                             rhs=Wtm[:, c, off:off + n],
                                                             start=(c == 0), stop=(c == 7)),
                              reads=[f"d1_uT{s}"] + (Wkeys if i == 0 and c == 0 and k == 0 else []),
                              writes=[pk])
                    if k % 2 == 0:
                        kb.op("dve", lambda: nc.vector.tensor_copy(out=ot[s][:, off:off + n], in_=p[:, 0:n]),
                              reads=[pk], writes=[f"d1_ot{s}_{k}"])
                    else:
                        kb.op("act", lambda: nc.scalar.copy(out=ot[s][:, off:off + n], in_=p[:, 0:n]),
                              reads=[pk], writes=[f"d1_ot{s}_{k}"])
                    off += n
                    k += 1
                kb.dma("sp", f"d1_ot{s}", self.proj[i * 128:(i + 1) * 128, :], ot[s][:],
                       reads=[f"d1_ot{s}_{j}" for j in range(k)], writes=[("proj", i)])
                for half in range(2):
                    p = px[half]
                    pk = f"d1_px{half}"
                    for j in range(4):
                        cc = half * 4 + j
                        for c in range(8):
                            kb.op("pe", lambda: nc.tensor.matmul(out=p[:, j, :], lhsT=Wx[:, c, cc * 128:(cc + 1) * 128],
                                                                 rhs=uT[s][:, c, :],
                                                                 start=(c == 0), stop=(c == 7)),
                                  reads=[f"d1_uT{s}"], writes=[pk])
                    if half == 0:
                        kb.op("dve", lambda: nc.vector.tensor_copy(out=xo[s][:, 0:4, :], in_=p[:]),
                              reads=[pk], writes=[f"d1_xo{s}_0"])
                    else:
                        kb.op("act", lambda: nc.scalar.copy(out=xo[s][:, 4:8, :], in_=p[:]),
                              reads=[pk], writes=[f"d1_xo{s}_1"])
                kb.dma("sp", f"d1_xo{s}",
                       self.xbcT.rearrange("(cc p) t -> p cc t", p=128)[:, :, i * 128:(i + 1) * 128],
                       xo[s][:], reads=[f"d1_xo{s}_0", f"d1_xo{s}_1"], writes=[("xbcT", i)])

            LDA(0)
            if NT > 1:
                LDA(1)
            A(0)
            for i in range(NT):
                if i + 2 < NT:
                    LDA(i + 2)
                if i + 1 < NT:
                    interleave(kb, (lambda i=i: B(i)), (lambda i=i: A(i + 1)), ra=6, rb=1)
                else:
                    B(i)

    def dump(self, name, src_ap):
        kb = self.kb
        kb.drain("sp")
        kb.dma("sp", "dump_" + name, self.outs[name], src_ap, writes=[("dump", name)])

    def finish(self):
        self.kb.drain("sp")
        self.kb.es.close()


def build_test_d1(S):
    p = Prog(S, L=1, dbg=[("o_proj", [S, NPROJ]), ("o_xbcT", [D, S])])
    with p.kb.es:
        p.stage_d1(0, p.x)
        p.dump("o_proj", p.proj[:, :])
        p.dump("o_xbcT", p.xbcT[:, :])
        p.kb.drain("sp")
    return p


def consts(S):
    r = np.arange(128)
    NS = S // 64
    NCC = max(1, (S // 16 - 1 + 127) // 128)
    pos = np.arange(S, dtype=np.float32)
    inv = (np.float32(10000.0) ** (-np.arange(32, dtype=np.float32) / np.float32(32))).astype(np.float32)
    ang = (pos[:, None] * inv[None, :]).astype(np.float32)
    Ew = np.zeros((128, S), np.float32)
    cc = np.arange(S)
    Ew[cc // 64 % 128, cc] = 30000.0
    cm = np.zeros((128, 17, 128), np.float32)
    for d_ in range(17):
        cm[:, d_, :] = (128 * d_ + r[None, :] - 16 * r[:, None] >= 31)
    n_all = np.arange(NCC * 128)
    j_all = np.arange(NS)
    ov = ((16 * n_all[:, None] < 64 * j_all[None, :] + 64) & (16 * n_all[:, None] + 32 > 64 * j_all[None, :])).astype(np.float32)
    ov = ov.reshape(NCC, 128, NS).transpose(1, 0, 2)
    cw = np.arange(2 * NS - 2)
    rel = cw[None, :] - (NS - 2) - (r[:, None] >= 64)
    Fw = np.where(rel > 0, -1e30, np.where(rel >= -1, 1e30, 0.0)).astype(np.float32)
    Kw = ((rel < -1)).astype(np.float32)
    bf = ml_dtypes.bfloat16
    hm = np.zeros((128, 4), np.float32)
    bd = np.zeros((128, 256), np.float32)
    for h in range(4):
        hm[32 * h:32 * h + 32, h] = 1.0
        bd[32 * h:32 * h + 32, 64 * h:64 * h + 64] = 1.0
    return {
        "ident_f": np.eye(128, dtype=np.float32),
        "ident_b": np.eye(128, dtype=np.float32).astype(ml_dtypes.bfloat16),
        "c_tri": (r[:, None] <= r[None, :]).astype(np.float32),
        "c_slt": (r[:, None] > r[None, :]).astype(np.float32),
        "c_ones": np.ones((128, 128), np.float32),
        "c_hm": hm,
        "c_bd": bd,
        "rope_cos": np.cos(ang).astype(np.float32),
        "rope_sin": np.sin(ang).astype(np.float32),
        "c_Ew": Ew.astype(bf),
        "c_cmask": cm.astype(bf),
        "c_ovl": np.ascontiguousarray(ov).astype(bf),
        "c_Fw": Fw,
        "c_Kw": Kw,
        "c_trib": (r[:, None] <= r[None, :]).astype(np.float32).astype(bf),
        "c_sltb": (r[:, None] > r[None, :]).astype(np.float32).astype(bf),
    }


class Tile:
    def __init__(self, t, k):
        self.t = t
        self.k = k


def _stage_ssd(self, l):
    nc, kb, NT = self.nc, self.kb, self.NT
    with ExitStack() as es:
        def T(name, shape, dt=F32):
            return Tile(self.sb(es, "ssd_" + name, shape, dt), "ssd_" + name)

        def T2(name, shape, dt=F32):
            return [T(f"{name}{j}", shape, dt) for j in range(2)]

        def PT(name, shape, dt=F32):
            return Tile(self.ps(es, "ssd_" + name, shape, dt), "ssd_" + name)

        def V(fn, r, w):
            return kb.op("dve", fn, [t.k for t in r], [t.k for t in w])

        def AC(fn, r, w):
            return kb.op("act", fn, [t.k for t in r], [t.k for t in w])

        def PE(fn, r, w):
            return kb.op("pe", fn, [t.k for t in r], [t.k for t in w])

        def GP(fn, r, w):
            return kb.op("pool", fn, [t.k for t in r], [t.k for t in w])

        def LD(t, dst, src, reads=(), **kw):
            return kb.dma("sp", t.k, dst, src, reads=list(reads), writes=[t.k], **kw)

        cw = T("cw", [128, 4, 8]); cb = T("cb", [128, 8]); dtb = T("dtb", [128, 8])
        Aneg = T("Aneg", [128, 8]); dsk = T("dsk", [128, 8]); nwb = T("nwb", [128, 512])
        tri = T("tri", [128, 128]); slt = T("slt", [128, 128]); ones = T("ones", [128, 128])
        idf = T("idf", [128, 128]); idb = T("idb", [128, 128], BF16)
        hT = T("hT", [128, 512]); hTb = T("hTb", [128, 512], BF16)
        xh = T2("xh", [128, 8, 131]); zt = T2("zt", [128, 512]); dtt = T2("dtt", [128, 8])
        acc = T2("acc", [128, 8, 128])
        xsT = T2("xsT", [128, 4, 128])
        BT = T2("BT", [128, 2, 128], BF16); CT = T2("CT", [128, 2, 128], BF16)
        xtm = T2("xtm", [128, 512]); Btm = T2("Btm", [128, 2, 128], BF16)
        sp_a = T2("sp_a", [128, 8]); dts = T2("dts", [128, 8]); dA = T2("dA", [128, 8])
        acs = T2("acs", [128, 16]); eacs = T2("eacs", [128, 8]); dsd = T2("dsd", [128, 8]); cd = T2("cd", [128, 8])
        xdt = T2("xdt", [128, 512], BF16); xdd = T2("xdd", [128, 512], BF16)
        cbm = T2("cbm", [128, 2, 128])
        dAt = T("dAt", [128, 8, 128]); seg = T2("seg", [128, 4, 128]); MT = T2("MT", [128, 4, 128], BF16)
        yd = T2("yd", [128, 512]); y = T2("y", [128, 512]); sz = T2("sz", [128, 512])
        junk = T("junk", [128, 256]); ss = T2("ss", [128, 2]); mo = T2("mo", [128, 512], BF16)
        pxT = PT("pxT", [128, 512]); pBt = PT("pBt", [128, 2, 128], BF16); pacs = PT("pacs", [128, 16])
        pcb = PT("pcb", [128, 2, 128]); pD = PT("pD", [128, 4, 128]); py = PT("py", [128, 512])
        pyo = PT("pyo", [128, 512]); pU = PT("pU", [128, 512])

        for k in range(4):
            LD(cw, cw.t[:, k, :], self.ssd_conv_w[l, k, :].rearrange("(cc p) -> p cc", p=128),
               allow_slow_non_contiguous=True)
        LD(cb, cb.t[:], self.ssd_conv_b[l, :].rearrange("(cc p) -> p cc", p=128), allow_slow_non_contiguous=True)
        LD(dtb, dtb.t[:], self.ssd_dt_bias[l, :].partition_broadcast(128))
        LD(Aneg, Aneg.t[:], self.ssd_a_log[l, :].partition_broadcast(128))
        LD(dsk, dsk.t[:], self.ssd_d[l, :].partition_broadcast(128))
        LD(nwb, nwb.t[:], self.ssd_norm_w[l, :].partition_broadcast(128))
        LD(tri, tri.t[:], self.c_tri[:, :]); LD(slt, slt.t[:], self.c_slt[:, :]); LD(ones, ones.t[:], self.c_ones[:, :])
        LD(idf, idf.t[:], self.ident_f[:, :]); LD(idb, idb.t[:], self.ident_b[:, :])
        AC(lambda: nc.scalar.activation(out=Aneg.t[:], in_=Aneg.t[:], func=AF.Exp), [Aneg], [Aneg])
        V(lambda: nc.vector.tensor_scalar(out=Aneg.t[:], in0=Aneg.t[:], scalar1=-1.0, scalar2=None, op0=ALU.mult), [Aneg], [Aneg])
        V(lambda: nc.vector.memset(hT.t[:], 0.0), [], [hT])
        V(lambda: nc.vector.memset(hTb.t[:], 0.0), [], [hTb])
        for j in range(2):
            V(lambda: nc.vector.memset(xh[j].t[:], 0.0), [], [xh[j]])
        xv = self.xbcT.rearrange("(cc p) t -> p cc t", p=128)

        def loads(i):
            s = i % 2
            t0 = i * 128
            if i == 0:
                kb.dma("sp", xh[s].k, xh[s].t[:, :, 3:131], xv[:, :, 0:128], reads=[("xbcT", 0)], writes=[xh[s].k])
            else:
                kb.dma("sp", xh[s].k, xh[s].t[:, :, 0:131], xv[:, :, t0 - 3:t0 + 128],
                       reads=[("xbcT", i), ("xbcT", i - 1)], writes=[xh[s].k])
            kb.dma("sp", zt[s].k, zt[s].t[:], self.proj[t0:t0 + 128, C_Z:C_Z + 512], reads=[("proj", i)], writes=[zt[s].k])
            kb.dma("sp", dtt[s].k, dtt[s].t[:], self.proj[t0:t0 + 128, C_DT:C_DT + 8], reads=[("proj", i)], writes=[dtt[s].k])

        def front(i):
            s = i % 2
            t0 = i * 128
            AC(lambda: nc.scalar.activation(out=sz[s].t[:], in_=zt[s].t[:], func=AF.Silu), [zt[s]], [sz[s]])
            for cc in range(8):
                eng = V
                ne = nc.vector
                GP(lambda: nc.gpsimd.tensor_scalar(out=acc[s].t[:, cc, :], in0=xh[s].t[:, cc, 0:128], scalar1=cw.t[:, 0, cc:cc + 1],
                                                   scalar2=cb.t[:, cc:cc + 1], op0=ALU.mult, op1=ALU.add),
                   [xh[s], cw, cb], [Tile(None, acc[s].k + f"_{cc}")])
                for k in range(1, 4):
                    eng(lambda: ne.scalar_tensor_tensor(out=acc[s].t[:, cc, :], in0=xh[s].t[:, cc, k:k + 128],
                                                        scalar=cw.t[:, k, cc:cc + 1], in1=acc[s].t[:, cc, :],
                                                        op0=ALU.mult, op1=ALU.add),
                        [xh[s], cw, Tile(None, acc[s].k + f"_{cc}")], [Tile(None, acc[s].k + f"_{cc}")])
            AC(lambda: nc.scalar.activation(out=xsT[s].t[:], in_=acc[s].t[:, 0:4, :], func=AF.Silu), [Tile(None, acc[s].k + f"_{c_}") for c_ in range(0, 4)], [xsT[s]])
            AC(lambda: nc.scalar.activation(out=BT[s].t[:], in_=acc[s].t[:, 4:6, :], func=AF.Silu), [Tile(None, acc[s].k + f"_{c_}") for c_ in range(4, 6)], [BT[s]])
            AC(lambda: nc.scalar.activation(out=CT[s].t[:], in_=acc[s].t[:, 6:8, :], func=AF.Silu), [Tile(None, acc[s].k + f"_{c_}") for c_ in range(6, 8)], [CT[s]])
            for cc in range(4):
                PE(lambda: nc.tensor.transpose(out=pxT.t[:, cc * 128:(cc + 1) * 128], in_=xsT[s].t[:, cc, :], identity=idf.t[:]),
                   [xsT[s], idf], [pxT])
            V(lambda: nc.vector.tensor_copy(out=xtm[s].t[:], in_=pxT.t[:]), [pxT], [xtm[s]])
            for g in range(2):
                PE(lambda: nc.tensor.transpose(out=pBt.t[:, g, :], in_=BT[s].t[:, g, :], identity=idb.t[:]), [BT[s], idb], [pBt])
            AC(lambda: nc.scalar.copy(out=Btm[s].t[:], in_=pBt.t[:]), [pBt], [Btm[s]])
            V(lambda: nc.vector.tensor_tensor(out=dts[s].t[:], in0=dtt[s].t[:], in1=dtb.t[:], op=ALU.add), [dtt[s], dtb], [dts[s]])
            V(lambda: nc.vector.scalar_tensor_tensor(out=sp_a[s].t[:], in0=dts[s].t[:], scalar=-1.0, in1=dts[s].t[:],
                                                     op0=ALU.mult, op1=ALU.max), [dts[s]], [sp_a[s]])
            AC(lambda: nc.scalar.activation(out=sp_a[s].t[:], in_=sp_a[s].t[:], func=AF.Exp, scale=-1.0), [sp_a[s]], [sp_a[s]])
            AC(lambda: nc.scalar.activation(out=sp_a[s].t[:], in_=sp_a[s].t[:], func=AF.Ln, bias=1.0), [sp_a[s]], [sp_a[s]])
            V(lambda: nc.vector.scalar_tensor_tensor(out=dts[s].t[:], in0=dts[s].t[:], scalar=0.0, in1=sp_a[s].t[:],
                                                     op0=ALU.max, op1=ALU.add), [dts[s], sp_a[s]], [dts[s]])
            V(lambda: nc.vector.tensor_tensor(out=dA[s].t[:], in0=dts[s].t[:], in1=Aneg.t[:], op=ALU.mult), [dts[s], Aneg], [dA[s]])
            PE(lambda: nc.tensor.matmul(out=pacs.t[:, 0:8], lhsT=tri.t[:], rhs=dA[s].t[:], start=True, stop=True), [tri, dA[s]], [pacs])
            PE(lambda: nc.tensor.matmul(out=pacs.t[:, 8:16], lhsT=ones.t[:], rhs=dA[s].t[:], start=True, stop=True), [ones, dA[s]], [pacs])
            V(lambda: nc.vector.tensor_copy(out=acs[s].t[:], in_=pacs.t[:]), [pacs], [acs[s]])
            AC(lambda: nc.scalar.activation(out=eacs[s].t[:], in_=acs[s].t[:, 0:8], func=AF.Exp), [acs[s]], [eacs[s]])
            AC(lambda: nc.scalar.activation(out=cd[s].t[:], in_=acs[s].t[:, 8:16], func=AF.Exp), [acs[s]], [cd[s]])
            V(lambda: nc.vector.tensor_tensor(out=dsd[s].t[:], in0=acs[s].t[:, 8:16], in1=acs[s].t[:, 0:8], op=ALU.subtract), [acs[s]], [dsd[s]])
            AC(lambda: nc.scalar.activation(out=dsd[s].t[:], in_=dsd[s].t[:], func=AF.Exp), [dsd[s]], [dsd[s]])
            V(lambda: nc.vector.tensor_tensor(out=dsd[s].t[:], in0=dsd[s].t[:], in1=dts[s].t[:], op=ALU.mult), [dsd[s], dts[s]], [dsd[s]])
            x3 = xtm[s].t[:].rearrange("p (h d) -> p h d", h=8)
            V(lambda: nc.vector.tensor_tensor(out=xdt[s].t[:].rearrange("p (h d) -> p h d", h=8), in0=x3,
                                              in1=dts[s].t[:].unsqueeze(2).to_broadcast([128, 8, 64]), op=ALU.mult),
              [xtm[s], dts[s]], [xdt[s]])
            GP(lambda: nc.gpsimd.tensor_tensor(out=xdd[s].t[:].rearrange("p (h d) -> p h d", h=8), in0=x3,
                                               in1=dsd[s].t[:].unsqueeze(2).to_broadcast([128, 8, 64]), op=ALU.mult),
               [xtm[s], dsd[s]], [xdd[s]])
            for g in range(2):
                PE(lambda: nc.tensor.matmul(out=pcb.t[:, g, :], lhsT=BT[s].t[:, g, :], rhs=CT[s].t[:, g, :], start=True, stop=True),
                   [BT[s], CT[s]], [pcb])
            V(lambda: nc.vector.tensor_tensor(out=cbm[s].t[:], in0=pcb.t[:], in1=tri.t[:].unsqueeze(1).to_broadcast([128, 2, 128]),
                                              op=ALU.mult), [pcb, tri], [cbm[s]])
        def tail(i):
            s = i % 2
            t0 = i * 128
            x3 = xtm[s].t[:].rearrange("p (h d) -> p h d", h=8)
            GP(lambda: nc.gpsimd.tensor_tensor(out=dAt.t[:], in0=tri.t[:].unsqueeze(1).to_broadcast([128, 8, 128]),
                                               in1=dA[s].t[:].unsqueeze(2).to_broadcast([128, 8, 128]), op=ALU.mult), [tri, dA[s]], [dAt])
            for g in range(2):
                for j in range(4):
                    PE(lambda: nc.tensor.matmul(out=pD.t[:, j, :], lhsT=slt.t[:], rhs=dAt.t[:, 4 * g + j, :], start=True, stop=True),
                       [slt, dAt], [pD])
                AC(lambda: nc.scalar.activation(out=seg[g].t[:], in_=pD.t[:], func=AF.Exp), [pD], [seg[g]])
                V(lambda: nc.vector.tensor_tensor(out=MT[g].t[:], in0=seg[g].t[:], in1=cbm[s].t[:, g, :].unsqueeze(1).to_broadcast([128, 4, 128]),
                                                  op=ALU.mult), [seg[g], cbm[s]], [MT[g]])
                for j in range(4):
                    h = 4 * g + j
                    PE(lambda: nc.tensor.matmul(out=py.t[:, h * 64:(h + 1) * 64], lhsT=MT[g].t[:, j, :], rhs=xdt[s].t[:, h * 64:(h + 1) * 64],
                                                start=True, stop=True), [MT[g], xdt[s]], [py])
            for g in range(2):
                PE(lambda: nc.tensor.matmul(out=pyo.t[:, g * 256:(g + 1) * 256], lhsT=CT[s].t[:, g, :], rhs=hTb.t[:, g * 256:(g + 1) * 256],
                                            start=True, stop=True), [CT[s], hTb], [pyo])
            for g in range(2):
                PE(lambda: nc.tensor.matmul(out=pU.t[:, g * 256:(g + 1) * 256], lhsT=Btm[s].t[:, g, :], rhs=xdd[s].t[:, g * 256:(g + 1) * 256],
                                            start=True, stop=True), [Btm[s], xdd[s]], [pU])
            AC(lambda: nc.scalar.copy(out=yd[s].t[:], in_=py.t[:]), [py], [yd[s]])
            V(lambda: nc.vector.tensor_tensor(out=y[s].t[:].rearrange("p (h d) -> p h d", h=8),
                                              in0=pyo.t[:].rearrange("p (h d) -> p h d", h=8),
                                              in1=eacs[s].t[:].unsqueeze(2).to_broadcast([128, 8, 64]), op=ALU.mult),
              [pyo, eacs[s]], [y[s]])
            V(lambda: nc.vector.tensor_tensor(out=hT.t[:].rearrange("p (h d) -> p h d", h=8),
                                              in0=hT.t[:].rearrange("p (h d) -> p h d", h=8),
                                              in1=cd[s].t[:].unsqueeze(2).to_broadcast([128, 8, 64]), op=ALU.mult),
              [hT, cd[s]], [hT])
            V(lambda: nc.vector.tensor_tensor(out=hT.t[:], in0=pU.t[:], in1=hT.t[:], op=ALU.add), [pU, hT], [hT])
            AC(lambda: nc.scalar.copy(out=hTb.t[:], in_=hT.t[:]), [hT], [hTb])
            GP(lambda: nc.gpsimd.tensor_tensor(out=y[s].t[:], in0=y[s].t[:], in1=yd[s].t[:], op=ALU.add), [y[s], yd[s]], [y[s]])
            GP(lambda: nc.gpsimd.tensor_tensor(out=yd[s].t[:].rearrange("p (h d) -> p h d", h=8), in0=x3,
                                               in1=dsk.t[:].unsqueeze(2).to_broadcast([128, 8, 64]), op=ALU.mult),
               [xtm[s], dsk], [yd[s]])
            GP(lambda: nc.gpsimd.tensor_tensor(out=y[s].t[:], in0=y[s].t[:], in1=yd[s].t[:], op=ALU.add), [y[s], yd[s]], [y[s]])
            V(lambda: nc.vector.tensor_tensor(out=y[s].t[:], in0=y[s].t[:], in1=sz[s].t[:], op=ALU.mult), [y[s], sz[s]], [y[s]])
            for g in range(2):
                AC(lambda: nc.scalar.activation(out=junk.t[:], in_=y[s].t[:, g * 256:(g + 1) * 256], func=AF.Square,
                                                accum_out=ss[s].t[:, g:g + 1]), [y[s]], [junk, ss[s]])
            ssk = [ss[s]]
            V(lambda: nc.vector.tensor_scalar(out=ss[s].t[:], in0=ss[s].t[:], scalar1=1.0 / 256, scalar2=EPS, op0=ALU.mult, op1=ALU.add),
              ssk, [ss[s]])
            AC(lambda: nc.scalar.activation(out=ss[s].t[:], in_=ss[s].t[:], func=AF.Sqrt), [ss[s]], [ss[s]])
            V(lambda: nc.vector.reciprocal(out=ss[s].t[:], in_=ss[s].t[:]), [ss[s]], [ss[s]])
            for g in range(2):
                V(lambda: nc.vector.scalar_tensor_tensor(out=mo[s].t[:, g * 256:(g + 1) * 256], in0=y[s].t[:, g * 256:(g + 1) * 256],
                                                         scalar=ss[s].t[:, g:g + 1], in1=nwb.t[:, g * 256:(g + 1) * 256],
                                                         op0=ALU.mult, op1=ALU.mult), [y[s], ss[s], nwb], [Tile(None, f"ssd_mo{s}_{g}")])
            kb.dma("sp", mo[s].k, self.mix[t0:t0 + 128, 512:1024], mo[s].t[:],
                   reads=[f"ssd_mo{s}_0", f"ssd_mo{s}_1"], writes=[("mix_ssd", i)])

        loads(0)
        if NT > 1:
            loads(1)
        front(0)
        for i in range(NT):
            if i + 2 < NT:
                loads(i + 2)
            if i + 1 < NT:
                interleave(kb, (lambda i=i: tail(i)), (lambda i=i: front(i + 1)))
            else:
                tail(i)


Prog.stage_ssd = _stage_ssd


def _stage_gla(self, l):
    nc, kb, NT = self.nc, self.kb, self.NT
    with ExitStack() as es:
        def T(name, shape, dt=F32):
            return Tile(self.sb(es, "gla_" + name, shape, dt), "gla_" + name)

        def T2(name, shape, dt=F32):
            return [T(f"{name}{j}", shape, dt) for j in range(2)]

        def PT(name, shape, dt=F32):
            return Tile(self.ps(es, "gla_" + name, shape, dt), "gla_" + name)

        def V(fn, r, w):
            return kb.op("dve", fn, [t.k for t in r], [t.k for t in w])

        def AC(fn, r, w):
            return kb.op("act", fn, [t.k for t in r], [t.k for t in w])

        def PE(fn, r, w):
            return kb.op("pe", fn, [t.k for t in r], [t.k for t in w])

        def GP(fn, r, w):
            return kb.op("pool", fn, [t.k for t in r], [t.k for t in w])

        def LD(t, dst, src, reads=(), **kw):
            return kb.dma("sp", t.k, dst, src, reads=list(reads), writes=[t.k], **kw)

        w2 = T("w2", [16, 128]); bb = T("bb", [128, 128]); nwb = T("nwb", [128, 256])
        tri = T("tri", [128, 128]); tri16 = T("tri16", [128, 128]); o16 = T("o16", [128, 2])
        idf = T("idf", [128, 128]); idb = T("idb", [128, 128], BF16)
        hm = T("hm", [128, 4]); bd = T("bd", [128, 256])
        Sbd = T("Sbd", [128, 256]); Sbb = T("Sbb", [128, 256], BF16)
        gin = T2("gin", [128, 784])
        glrT = T2("glrT", [16, 128]); zv = T2("zv", [128, 128]); az = T2("az", [128, 128]); la = T2("la", [128, 128])
        eb = T2("eb", [128, 128]); enb = T2("enb", [128, 128])
        qt = T2("qtok", [128, 128], BF16); kt = T2("ktok", [128, 128], BF16); vb = T2("vb", [128, 256], BF16)
        qT = T2("qT", [128, 128], BF16); kTm = T2("kTm", [128, 4, 128], BF16)
        egt = T2("egt", [128, 2]); ATm = T2("ATm", [128, 4, 128], BF16)
        sq = T2("sq", [128, 256]); ss = T2("ss", [128, 4]); on = T2("on", [128, 256]); sr = T2("sr", [128, 256])
        mo = T2("mo", [128, 256], BF16); um = T2("um", [128, 256])
        pgT = PT("pgT", [16, 128]); pz = PT("pz", [128, 128]); pbc = PT("pbc", [128, 128])
        pqk = PT("pqk", [128, 2, 128], BF16); pgt = PT("pgt", [128, 2]); pA = PT("pA", [128, 4, 128])
        po = PT("po", [128, 256]); pU = PT("pU", [128, 256])

        LD(w2, w2.t[:], self.gla_gate_w2[l, :, :])
        LD(bb, bb.t[:], self.gla_gate_b[l, :].partition_broadcast(128))
        LD(nwb, nwb.t[:], self.gla_norm_w[l, :].partition_broadcast(128))
        LD(tri, tri.t[:], self.c_tri[:, :]); LD(idf, idf.t[:], self.ident_f[:, :]); LD(idb, idb.t[:], self.ident_b[:, :])
        LD(hm, hm.t[:], self.c_hm[:, :]); LD(bd, bd.t[:], self.c_bd[:, :])
        V(lambda: nc.vector.tensor_scalar(out=tri16.t[:], in0=tri.t[:], scalar1=1.0 / 16, scalar2=None, op0=ALU.mult), [tri], [tri16])
        V(lambda: nc.vector.memset(o16.t[:], 1.0 / 16), [], [o16])
        V(lambda: nc.vector.memset(Sbd.t[:], 0.0), [], [Sbd])
        V(lambda: nc.vector.memset(Sbb.t[:], 0.0), [], [Sbb])

        def loads(i):
            s = i % 2
            t0 = i * 128
            kb.dma("sp", gin[s].k, gin[s].t[:], self.proj[t0:t0 + 128, 0:784], reads=[("proj", i)], writes=[gin[s].k])

        def front(i):
            s = i % 2
            t0 = i * 128
            AC(lambda: nc.scalar.activation(out=sr[s].t[:], in_=gin[s].t[:, C_GR:C_GR + 256], func=AF.Silu), [gin[s]], [sr[s]])
            GP(lambda: nc.gpsimd.tensor_tensor(out=sr[s].t[:], in0=sr[s].t[:], in1=nwb.t[:], op=ALU.mult), [sr[s], nwb], [sr[s]])
            q_ = gin[s].t[:, C_GQ:C_GQ + 128]; k_ = gin[s].t[:, C_GK:C_GK + 128]; v_ = gin[s].t[:, C_GV:C_GV + 256]
            glr_ = gin[s].t[:, C_GLR:C_GLR + 16]; r_ = gin[s].t[:, C_GR:C_GR + 256]
            PE(lambda: nc.tensor.transpose(out=pgT.t[:], in_=glr_, identity=idf.t[:]), [gin[s], idf], [pgT])
            V(lambda: nc.vector.tensor_copy(out=glrT[s].t[:], in_=pgT.t[:]), [pgT], [glrT[s]])
            PE(lambda: nc.tensor.matmul(out=pz.t[:], lhsT=glrT[s].t[:], rhs=w2.t[:], start=True, stop=True), [glrT[s], w2], [pz])
            V(lambda: nc.vector.tensor_tensor(out=zv[s].t[:], in0=pz.t[:], in1=bb.t[:], op=ALU.add), [pz, bb], [zv[s]])
            V(lambda: nc.vector.scalar_tensor_tensor(out=az[s].t[:], in0=zv[s].t[:], scalar=-1.0, in1=zv[s].t[:], op0=ALU.mult, op1=ALU.max),
              [zv[s]], [az[s]])
            AC(lambda: nc.scalar.activation(out=az[s].t[:], in_=az[s].t[:], func=AF.Exp, scale=-1.0), [az[s]], [az[s]])
            AC(lambda: nc.scalar.activation(out=az[s].t[:], in_=az[s].t[:], func=AF.Ln, bias=1.0), [az[s]], [az[s]])
            V(lambda: nc.vector.scalar_tensor_tensor(out=la[s].t[:], in0=zv[s].t[:], scalar=0.0, in1=az[s].t[:], op0=ALU.min, op1=ALU.subtract),
              [zv[s], az[s]], [la[s]])
            PE(lambda: nc.tensor.matmul(out=pbc.t[:], lhsT=tri16.t[:], rhs=la[s].t[:], start=True, stop=True), [tri16, la[s]], [pbc])
            PE(lambda: nc.tensor.matmul(out=pgt.t[:], lhsT=la[s].t[:], rhs=o16.t[:], start=True, stop=True), [la[s], o16], [pgt])
            AC(lambda: nc.scalar.activation(out=eb[s].t[:], in_=pbc.t[:], func=AF.Exp), [pbc], [eb[s]])
            AC(lambda: nc.scalar.activation(out=enb[s].t[:], in_=pbc.t[:], func=AF.Exp, scale=-1.0), [pbc], [enb[s]])
            AC(lambda: nc.scalar.activation(out=egt[s].t[:], in_=pgt.t[:], func=AF.Exp), [pgt], [egt[s]])
            V(lambda: nc.vector.scalar_tensor_tensor(out=qt[s].t[:], in0=q_, scalar=32.0 ** -0.5, in1=eb[s].t[:], op0=ALU.mult, op1=ALU.mult),
              [gin[s], eb[s]], [qt[s]])
            V(lambda: nc.vector.tensor_tensor(out=kt[s].t[:], in0=k_, in1=enb[s].t[:], op=ALU.mult), [gin[s], enb[s]], [kt[s]])
            GP(lambda: nc.gpsimd.tensor_copy(out=vb[s].t[:], in_=v_), [gin[s]], [vb[s]])
            PE(lambda: nc.tensor.transpose(out=pqk.t[:, 0, :], in_=qt[s].t[:], identity=idb.t[:]), [qt[s], idb], [pqk])
            PE(lambda: nc.tensor.transpose(out=pqk.t[:, 1, :], in_=kt[s].t[:], identity=idb.t[:]), [kt[s], idb], [pqk])
            AC(lambda: nc.scalar.copy(out=qT[s].t[:], in_=pqk.t[:, 0, :]), [pqk], [qT[s]])
            for h in range(4):
                AC(lambda: nc.scalar.activation(out=kTm[s].t[:, h, :], in_=pqk.t[:, 1, :], func=AF.Copy, scale=hm.t[:, h:h + 1]),
                   [pqk, hm], [kTm[s]])
            for h in range(4):
                PE(lambda: nc.tensor.matmul(out=pA.t[:, h, :], lhsT=kTm[s].t[:, h, :], rhs=qT[s].t[:], start=True, stop=True),
                   [kTm[s], qT[s]], [pA])
            V(lambda: nc.vector.tensor_tensor(out=ATm[s].t[:], in0=pA.t[:], in1=tri.t[:].unsqueeze(1).to_broadcast([128, 4, 128]), op=ALU.mult),
              [pA, tri], [ATm[s]])
        def tail(i):
            s = i % 2
            t0 = i * 128
            r_ = gin[s].t[:, C_GR:C_GR + 256]
            PE(lambda: nc.tensor.matmul(out=po.t[:], lhsT=qT[s].t[:], rhs=Sbb.t[:], start=True, stop=False), [qT[s], Sbb], [po])
            for h in range(4):
                PE(lambda: nc.tensor.matmul(out=po.t[:, h * 64:(h + 1) * 64], lhsT=ATm[s].t[:, h, :], rhs=vb[s].t[:, h * 64:(h + 1) * 64],
                                            start=False, stop=(h == 3)), [ATm[s], vb[s]], [po])
            PE(lambda: nc.tensor.matmul(out=pU.t[:], lhsT=kt[s].t[:], rhs=vb[s].t[:], start=True, stop=True), [kt[s], vb[s]], [pU])
            V(lambda: nc.vector.tensor_tensor(out=um[s].t[:], in0=pU.t[:], in1=bd.t[:], op=ALU.mult), [pU, bd], [um[s]])
            V(lambda: nc.vector.tensor_tensor(out=Sbd.t[:], in0=Sbd.t[:], in1=um[s].t[:], op=ALU.add), [Sbd, um[s]], [Sbd])
            V(lambda: nc.vector.tensor_scalar(out=Sbd.t[:], in0=Sbd.t[:], scalar1=egt[s].t[:, 0:1], scalar2=None, op0=ALU.mult), [Sbd, egt[s]], [Sbd])
            AC(lambda: nc.scalar.copy(out=Sbb.t[:], in_=Sbd.t[:]), [Sbd], [Sbb])
            AC(lambda: nc.scalar.activation(out=sq[s].t[:], in_=po.t[:], func=AF.Square), [po], [sq[s]])
            V(lambda: nc.vector.tensor_reduce(out=ss[s].t[:], in_=sq[s].t[:].rearrange("p (h d) -> p h d", h=4), axis=AX.X, op=ALU.add),
              [sq[s]], [ss[s]])
            V(lambda: nc.vector.tensor_scalar(out=ss[s].t[:], in0=ss[s].t[:], scalar1=1.0 / 64, scalar2=EPS, op0=ALU.mult, op1=ALU.add), [ss[s]], [ss[s]])
            AC(lambda: nc.scalar.activation(out=ss[s].t[:], in_=ss[s].t[:], func=AF.Sqrt), [ss[s]], [ss[s]])
            V(lambda: nc.vector.reciprocal(out=ss[s].t[:], in_=ss[s].t[:]), [ss[s]], [ss[s]])
            V(lambda: nc.vector.tensor_tensor(out=on[s].t[:].rearrange("p (h d) -> p h d", h=4), in0=po.t[:].rearrange("p (h d) -> p h d", h=4),
                                              in1=ss[s].t[:].unsqueeze(2).to_broadcast([128, 4, 64]), op=ALU.mult), [po, ss[s]], [on[s]])
            V(lambda: nc.vector.tensor_tensor(out=mo[s].t[:], in0=on[s].t[:], in1=sr[s].t[:], op=ALU.mult), [on[s], sr[s]], [mo[s]])
            kb.dma("sp", mo[s].k, self.mix[t0:t0 + 128, 0:256], mo[s].t[:], reads=[mo[s].k], writes=[("mix_gla", i)])

        loads(0)
        if NT > 1:
            loads(1)
        front(0)
        for i in range(NT):
            if i + 2 < NT:
                loads(i + 2)
            if i + 1 < NT:
                interleave(kb, (lambda i=i: tail(i)), (lambda i=i: front(i + 1)))
            else:
                tail(i)


Prog.stage_gla = _stage_gla


def build_test_mix(S, which, limit=None):
    p = Prog(S, L=1, dbg=[("o_mix", [S, D])])
    with p.kb.es as es:
        p.stage_d1(0, p.x)
        p.kb.barrier()
        if limit is not None:
            p.kb.limit = p.kb.ninst + limit
        if "ssd" in which:
            p.stage_ssd(0)
        if "gla" in which:
            p.stage_gla(0)
        if "nsa" in which:
            p.stage_nsa(0)
        kb, nc = p.kb, p.nc
        kb.limit = None
        kb.barrier()
        mb = p.sb(es, "dump_mb", [128, D], BF16)
        mf = p.sb(es, "dump_mf", [128, D])
        for i in range(p.NT):
            kb.dma("sp", "dump_mb", mb[:], p.mix[i * 128:(i + 1) * 128, :], writes=["dump_mb"])
            kb.op("dve", lambda: nc.vector.tensor_copy(out=mf[:], in_=mb[:]), reads=["dump_mb"], writes=["dump_mf"])
            kb.dma("sp", "dump_mf", p.outs["o_mix"][i * 128:(i + 1) * 128, :], mf[:], reads=["dump_mf"], writes=[("o_mix", i)])
        kb.drain("sp")
    return p


def _stage_d2(self, l, hsrc, hdst, final):
    nc, kb, NT = self.nc, self.kb, self.NT
    with ExitStack() as es:
        def T(name, shape, dt=F32):
            return Tile(self.sb(es, "d2_" + name, shape, dt), "d2_" + name)

        def T2(name, shape, dt=F32):
            return [T(f"{name}{j}", shape, dt) for j in range(2)]

        def PT(name, shape, dt=F32):
            return Tile(self.ps(es, "d2_" + name, shape, dt), "d2_" + name)

        def V(fn, r, w):
            return kb.op("dve", fn, [t.k for t in r], [t.k for t in w])

        def AC(fn, r, w):
            return kb.op("act", fn, [t.k for t in r], [t.k for t in w])

        def PE(fn, r, w):
            return kb.op("pe", fn, [t.k for t in r], [t.k for t in w])

        def GP(fn, r, w):
            return kb.op("pool", fn, [t.k for t in r], [t.k for t in w])

        Wo = T("Wo", [128, 8, 1024], BF16); Wu = T("Wu", [128, 8, 4096], BF16); Wd = T("Wd", [128, 32, 1024], BF16)
        stg = T2("stg", [128, 1024]); nw2 = T("nw2", [128, 8]); idb = T("idb", [128, 128], BF16)
        ht = T2("ht", [128, 1024]); mt = T2("mt", [128, 1024], BF16)
        mT = T("mT", [128, 8, 128], BF16); h1 = T2("h1", [128, 1024]); junk = T("junk", [128, 1024], BF16)
        ss = T("ss", [128, 1]); u2 = T("u2", [128, 1024], BF16); u2T = T2("u2T", [128, 8, 128], BF16)
        tmp = T2("tmp", [128, 512]); hidT = T("hidT", [128, 32, 128], BF16); ho = T2("ho", [128, 1024])
        pT = PT("pT", [128, 8, 128], BF16); po = [PT(f"po{j}", [128, 512]) for j in range(2)]
        pu = [PT(f"pu{j}", [128, 4, 128]) for j in range(2)]
        pd = [PT(f"pd{j}", [128, 512]) for j in range(2)]
        if final:
            fnw = T("fnw", [128, 1024]); ss2 = T("ss2", [128, 1])
            kb.dma("sp", fnw.k, fnw.t[:], self.final_norm_w.partition_broadcast(128), writes=[fnw.k])

        kb.dma("sp", idb.k, idb.t[:], self.ident_b[:, :], writes=[idb.k])
        for c in range(8):
            kb.dma("sp", nw2.k, nw2.t[:, c:c + 1], self.norm2_w[l, c * 128:(c + 1) * 128].rearrange("(p o) -> p o", o=1), writes=[nw2.k])
        n = 0
        jobs = [(Wo, c, 0, self.w_out[l, c * 128:(c + 1) * 128, :], False) for c in range(8)]
        jobs += [(Wu, c, q * 1024, self.w_up[l, c * 128:(c + 1) * 128, q * 1024:(q + 1) * 1024], True) for c in range(8) for q in range(4)]
        jobs += [(Wd, f, 0, self.w_down[l, f * 128:(f + 1) * 128, :], False) for f in range(32)]
        for (W, c, off, src, scaled) in jobs:
            sg = stg[n % 2]
            kb.dma("sp", sg.k, sg.t[:], src, writes=[sg.k])
            dst = W.t[:, c, off:off + 1024]
            if scaled:
                if n % 2 == 0:
                    V(lambda: nc.vector.tensor_scalar(out=dst, in0=sg.t[:], scalar1=nw2.t[:, c:c + 1], scalar2=None, op0=ALU.mult), [sg, nw2], [W])
                else:
                    AC(lambda: nc.scalar.activation(out=dst, in_=sg.t[:], func=AF.Copy, scale=nw2.t[:, c:c + 1]), [sg, nw2], [W])
            else:
                if n % 2 == 0:
                    V(lambda: nc.vector.tensor_copy(out=dst, in_=sg.t[:]), [sg], [W])
                else:
                    AC(lambda: nc.scalar.copy(out=dst, in_=sg.t[:]), [sg], [W])
            n += 1

        def loads(i):
            s = i % 2
            t0 = i * 128
            kb.dma("sp", ht[s].k, ht[s].t[:], hsrc[t0:t0 + 128, :], reads=[("h", i)], writes=[ht[s].k])
            kb.dma("sp", mt[s].k, mt[s].t[:], self.mix[t0:t0 + 128, :], reads=[("mix_gla", i), ("mix_nsa", i), ("mix_ssd", i)], writes=[mt[s].k])

        def front(i):
            s = i % 2
            t0 = i * 128
            for c in range(8):
                PE(lambda: nc.tensor.transpose(out=pT.t[:, c, :], in_=mt[s].t[:, c * 128:(c + 1) * 128], identity=idb.t[:]), [mt[s], idb], [pT])
            AC(lambda: nc.scalar.copy(out=mT.t[:], in_=pT.t[:]), [pT], [mT])
            for hf in range(2):
                for c in range(8):
                    PE(lambda: nc.tensor.matmul(out=po[hf].t[:], lhsT=mT.t[:, c, :], rhs=Wo.t[:, c, hf * 512:(hf + 1) * 512],
                                                start=(c == 0), stop=(c == 7)), [mT, Wo], [po[hf]])
                V(lambda: nc.vector.tensor_tensor(out=h1[s].t[:, hf * 512:(hf + 1) * 512], in0=po[hf].t[:], in1=ht[s].t[:, hf * 512:(hf + 1) * 512], op=ALU.add),
                  [po[hf], ht[s]], [h1[s]])
            AC(lambda: nc.scalar.activation(out=junk.t[:], in_=h1[s].t[:], func=AF.Square, accum_out=ss.t[:]), [h1[s]], [junk, ss])
            V(lambda: nc.vector.tensor_scalar(out=ss.t[:], in0=ss.t[:], scalar1=1.0 / D, scalar2=EPS, op0=ALU.mult, op1=ALU.add), [ss], [ss])
            AC(lambda: nc.scalar.activation(out=ss.t[:], in_=ss.t[:], func=AF.Sqrt), [ss], [ss])
            V(lambda: nc.vector.reciprocal(out=ss.t[:], in_=ss.t[:]), [ss], [ss])
            V(lambda: nc.vector.tensor_scalar(out=u2.t[:], in0=h1[s].t[:], scalar1=ss.t[:], scalar2=None, op0=ALU.mult), [h1[s], ss], [u2])
            for c in range(8):
                PE(lambda: nc.tensor.transpose(out=pT.t[:, c, :], in_=u2.t[:, c * 128:(c + 1) * 128], identity=idb.t[:]), [u2, idb], [pT])
            AC(lambda: nc.scalar.copy(out=u2T[s].t[:], in_=pT.t[:]), [pT], [u2T[s]])
        def tail(i):
            s = i % 2
            t0 = i * 128
            for fg in range(8):
                p = pu[fg % 2]
                for j in range(4):
                    f = fg * 4 + j
                    for c in range(8):
                        PE(lambda: nc.tensor.matmul(out=p.t[:, j, :], lhsT=Wu.t[:, c, f * 128:(f + 1) * 128], rhs=u2T[s].t[:, c, :],
                                                    start=(c == 0), stop=(c == 7)), [Wu, u2T[s]], [p])
                tm = tmp[fg % 2]
                AC(lambda: nc.scalar.activation(out=tm.t[:], in_=p.t[:].rearrange("p a b -> p (a b)"), func=AF.Relu), [p], [tm])
                GP(lambda: nc.gpsimd.tensor_tensor(out=hidT.t[:, fg * 4:(fg + 1) * 4, :].rearrange("p a b -> p (a b)"), in0=tm.t[:], in1=tm.t[:], op=ALU.mult),
                   [tm], [Tile(None, f"d2_hid{fg}")])
            hk = [Tile(None, f"d2_hid{fg}") for fg in range(8)]
            for f in range(32):
                for hf in range(2):
                    PE(lambda: nc.tensor.matmul(out=pd[hf].t[:], lhsT=hidT.t[:, f, :], rhs=Wd.t[:, f, hf * 512:(hf + 1) * 512],
                                                start=(f == 0), stop=(f == 31)), [hk[f // 4], Wd], [pd[hf]])
            for hf in range(2):
                V(lambda: nc.vector.tensor_tensor(out=ho[s].t[:, hf * 512:(hf + 1) * 512], in0=pd[hf].t[:], in1=h1[s].t[:, hf * 512:(hf + 1) * 512], op=ALU.add),
                  [pd[hf], h1[s]], [ho[s]])
            if final:
                AC(lambda: nc.scalar.activation(out=junk.t[:], in_=ho[s].t[:], func=AF.Square, accum_out=ss2.t[:]), [ho[s]], [junk, ss2])
                V(lambda: nc.vector.tensor_scalar(out=ss2.t[:], in0=ss2.t[:], scalar1=1.0 / D, scalar2=EPS, op0=ALU.mult, op1=ALU.add), [ss2], [ss2])
                AC(lambda: nc.scalar.activation(out=ss2.t[:], in_=ss2.t[:], func=AF.Sqrt), [ss2], [ss2])
                V(lambda: nc.vector.reciprocal(out=ss2.t[:], in_=ss2.t[:]), [ss2], [ss2])
                V(lambda: nc.vector.scalar_tensor_tensor(out=ho[s].t[:], in0=ho[s].t[:], scalar=ss2.t[:], in1=fnw.t[:], op0=ALU.mult, op1=ALU.mult),
                  [ho[s], ss2, fnw], [ho[s]])
            kb.dma("sp", ho[s].k, hdst[t0:t0 + 128, :], ho[s].t[:], reads=[ho[s].k], writes=[("hout", l, i)] if final else [("h", i)])

        loads(0)
        if NT > 1:
            loads(1)
        front(0)
        for i in range(NT):
            if i + 2 < NT:
                loads(i + 2)
            if i + 1 < NT:
                interleave(kb, (lambda i=i: tail(i)), (lambda i=i: front(i + 1)), ra=7, rb=1)
            else:
                tail(i)


Prog.stage_d2 = _stage_d2


def _stage_nsa(self, l):
    nc, kb, NT, S, NS, NCC = self.nc, self.kb, self.NT, self.S, self.NS, self.NCC
    NCB = S // 16 - 1
    with ExitStack() as es:
        def Tn(es_, name, shape, dt=F32):
            return Tile(self.sb(es_, "nsa_" + name, shape, dt), "nsa_" + name)

        def T(name, shape, dt=F32):
            return Tn(es, name, shape, dt)

        def V(fn, r, w):
            return kb.op("dve", fn, [t.k for t in r], [t.k for t in w])

        def AC(fn, r, w):
            return kb.op("act", fn, [t.k for t in r], [t.k for t in w])

        def PE(fn, r, w):
            return kb.op("pe", fn, [t.k for t in r], [t.k for t in w])

        def GP(fn, r, w):
            return kb.op("pool", fn, [t.k for t in r], [t.k for t in w])

        def LD(t, dst, src, reads=(), **kw):
            return kb.dma("sp", t.k, dst, src, reads=list(reads), writes=[t.k], **kw)

        QT = T("QT", [128, NT, 2, 128], BF16); KsT = T("KsT", [128, S], BF16); KwT = T("KwT", [128, S], BF16)
        KCT = T("KCT", [128, NCC * 128], BF16)
        VsA = T("VsA", [128, NT, 2, 65], BF16); VwA = T("VwA", [128, NT, 2, 65], BF16)
        RC = T("RC", [128, NCC, 2, 65 + NS], BF16); GA = T("GA", [128, NT, 12])
        idb = T("idb", [128, 128], BF16); nwb = T("nwb", [128, 256])
        LD(idb, idb.t[:], self.ident_b[:, :])
        LD(nwb, nwb.t[:], self.nsa_norm_w[l, :].partition_broadcast(128))
        V(lambda: nc.vector.memset(KCT.t[:], 0.0), [], [KCT])
        V(lambda: nc.vector.memset(RC.t[:], 0.0), [], [RC])
        V(lambda: nc.vector.memset(VsA.t[:], 1.0), [], [VsA])
        V(lambda: nc.vector.memset(VwA.t[:], 1.0), [], [VwA])

        with ExitStack() as e1:
            nin = [Tn(e1, f"nin{j}", [128, 1036]) for j in range(2)]
            cs = [Tn(e1, f"cs{j}", [128, 2, 32]) for j in range(2)]
            ta2 = [Tn(e1, f"ta{j}", [128, 10, 32]) for j in range(2)]; tb2 = [Tn(e1, f"tb{j}", [128, 10, 32]) for j in range(2)]
            rq = [Tn(e1, f"rq{j}", [128, 768], BF16) for j in range(2)]
            kv = [Tn(e1, f"kv{j}", [128, 2, 128], BF16) for j in range(2)]
            pT2 = [Tile(self.ps(e1, f"nsa_pT1{j}", [128, 6, 128], BF16), f"nsa_pT1{j}") for j in range(2)]

            def n1(i):
                s = i % 2
                t0 = i * 128
                ta, tb, pT = ta2[s], tb2[s], pT2[s]
                kb.dma("sp", nin[s].k, nin[s].t[:], self.proj[t0:t0 + 128, C_NQ:C_NQ + 1036], reads=[("proj", i)], writes=[nin[s].k])
                kb.dma("sp", cs[s].k + "c", cs[s].t[:, 0, :], self.rope_cos[t0:t0 + 128, :], writes=[cs[s].k + "c"])
                kb.dma("sp", cs[s].k + "s", cs[s].t[:, 1, :], self.rope_sin[t0:t0 + 128, :], writes=[cs[s].k + "s"])
                csk = [Tile(None, cs[s].k + "c"), Tile(None, cs[s].k + "s")]
                x3 = nin[s].t[:, 0:640].rearrange("p (h d) -> p h d", h=10)
                o3 = rq[s].t[:, 0:640].rearrange("p (h d) -> p h d", h=10)
                cb_ = cs[s].t[:, 0, :].unsqueeze(1).to_broadcast([128, 10, 32])
                sb_ = cs[s].t[:, 1, :].unsqueeze(1).to_broadcast([128, 10, 32])
                V(lambda: nc.vector.tensor_tensor(out=ta.t[:], in0=x3[:, :, 0:32], in1=cb_, op=ALU.mult), [nin[s]] + csk, [ta])
                V(lambda: nc.vector.tensor_tensor(out=tb.t[:], in0=x3[:, :, 32:64], in1=sb_, op=ALU.mult), [nin[s]] + csk, [tb])
                V(lambda: nc.vector.tensor_tensor(out=o3[:, :, 0:32], in0=ta.t[:], in1=tb.t[:], op=ALU.subtract), [ta, tb], [Tile(None, rq[s].k + "a")])
                V(lambda: nc.vector.tensor_tensor(out=ta.t[:], in0=x3[:, :, 32:64], in1=cb_, op=ALU.mult), [nin[s]] + csk, [ta])
                V(lambda: nc.vector.tensor_tensor(out=tb.t[:], in0=x3[:, :, 0:32], in1=sb_, op=ALU.mult), [nin[s]] + csk, [tb])
                V(lambda: nc.vector.tensor_tensor(out=o3[:, :, 32:64], in0=ta.t[:], in1=tb.t[:], op=ALU.add), [ta, tb], [Tile(None, rq[s].k + "b")])
                AC(lambda: nc.scalar.copy(out=rq[s].t[:, 640:768], in_=nin[s].t[:, 640:768]), [nin[s]], [Tile(None, rq[s].k + "c")])
                rqk = [Tile(None, rq[s].k + x) for x in "abc"]
                for b in range(6):
                    PE(lambda: nc.tensor.transpose(out=pT.t[:, b, :], in_=rq[s].t[:, b * 128:(b + 1) * 128], identity=idb.t[:]), rqk + [idb], [pT])
                AC(lambda: nc.scalar.copy(out=QT.t[:, i, :, :], in_=pT.t[:, 0:2, :]), [pT], [Tile(None, f"nsa_QT{i}")])
                AC(lambda: nc.scalar.copy(out=KsT.t[:, t0:t0 + 128], in_=pT.t[:, 3, :]), [pT], [Tile(None, f"nsa_KsT{i}")])
                AC(lambda: nc.scalar.copy(out=KwT.t[:, t0:t0 + 128], in_=pT.t[:, 4, :]), [pT], [Tile(None, f"nsa_KwT{i}")])
                AC(lambda: nc.scalar.copy(out=kv[s].t[:, 0, :], in_=pT.t[:, 2, :]), [pT], [kv[s]])
                AC(lambda: nc.scalar.copy(out=kv[s].t[:, 1, :], in_=pT.t[:, 5, :]), [pT], [kv[s]])
                kb.dma("sp", kv[s].k + "k", self.kcT_d[:, t0:t0 + 128], kv[s].t[:, 0, :], reads=[kv[s].k], writes=[("kcT_d", i)])
                kb.dma("sp", kv[s].k + "v", self.vcT_d[:, t0:t0 + 128], kv[s].t[:, 1, :], reads=[kv[s].k], writes=[("vcT_d", i)])
                GP(lambda: nc.gpsimd.tensor_copy(out=VsA.t[:, i, :, 0:64], in_=nin[s].t[:, 768:896].rearrange("p (g d) -> p g d", g=2)),
                   [nin[s], VsA], [Tile(None, f"nsa_VsA{i}")])
                GP(lambda: nc.gpsimd.tensor_copy(out=VwA.t[:, i, :, 0:64], in_=nin[s].t[:, 896:1024].rearrange("p (g d) -> p g d", g=2)),
                   [nin[s], VwA], [Tile(None, f"nsa_VwA{i}")])
                AC(lambda: nc.scalar.activation(out=GA.t[:, i, :], in_=nin[s].t[:, 1024:1036], func=AF.Sigmoid), [nin[s]], [Tile(None, f"nsa_GA{i}")])

            for i in range(0, NT, 2):
                if i + 1 < NT:
                    interleave(kb, (lambda i=i: n1(i)), (lambda i=i: n1(i + 1)))
                else:
                    n1(i)
            kb.barrier()

        with ExitStack() as e2:
            wst = Tn(e2, "wst", [128, 32, 128]); W1 = [Tn(e2, f"W1{j}", [128, 32, 128], BF16) for j in range(2)]
            w2s = Tn(e2, "w2s", [128, 64]); W2p = Tn(e2, "W2p", [128, 2, 128], BF16); W2v = Tn(e2, "W2v", [128, 64], BF16)
            psT = Tn(e2, "psT", [128, 32]); posb = [Tn(e2, f"posb{j}", [128, 32, 2], BF16) for j in range(2)]
            cbv = [Tn(e2, f"cbv{j}", [128, 2]) for j in range(2)]
            XT = Tn(e2, "XT", [128, 128 * 16 + 16], BF16)
            hb = Tn(e2, "hb", [128, 128]); tt = Tn(e2, "tt", [128, 128]); GT = [Tn(e2, f"GT{g}", [128, 128], BF16) for g in range(2)]
            ph = Tile(self.ps(e2, "nsa_ph", [128, 128]), "nsa_ph"); pc = Tile(self.ps(e2, "nsa_pc", [128, 128]), "nsa_pc")
            pb = Tile(self.ps(e2, "nsa_pb", [128, 2]), "nsa_pb")
            V(lambda: nc.vector.memset(W2p.t[:], 0.0), [], [W2p])
            for wi, (w1d, w2d, posd) in enumerate([(self.nsa_cmp_w1_k, self.nsa_cmp_w2_k, self.nsa_cmp_pos_k),
                                                   (self.nsa_cmp_w1_v, self.nsa_cmp_w2_v, self.nsa_cmp_pos_v)]):
                for half in range(2):
                    LD(wst, wst.t[half * 64:(half + 1) * 64, :, :], w1d[l].rearrange("(i d) j -> d i j", d=64))
                V(lambda: nc.vector.tensor_copy(out=W1[wi].t[:], in_=wst.t[:]), [wst], [W1[wi]])
                LD(w2s, w2s.t[:], w2d[l, :, :])
                if wi == 0:
                    for g in range(2):
                        V(lambda: nc.vector.tensor_copy(out=W2p.t[:, g, g * 64:(g + 1) * 64], in_=w2s.t[:]), [w2s], [W2p])
                else:
                    V(lambda: nc.vector.tensor_copy(out=W2v.t[:], in_=w2s.t[:]), [w2s], [W2v])
                for half in range(2):
                    LD(psT, psT.t[half * 64:(half + 1) * 64, :], posd[l].rearrange("i d -> d i"), allow_slow_non_contiguous=True)
                for j in range(2):
                    V(lambda: nc.vector.tensor_copy(out=posb[wi].t[:, :, j], in_=psT.t[:]), [psT], [posb[wi]])
                for i_ in range(32):
                    PE(lambda: nc.tensor.matmul(out=pb.t[:], lhsT=W1[wi].t[0:64, i_, :], rhs=posb[wi].t[0:64, i_, :], start=(i_ == 0), stop=(i_ == 31)),
                       [W1[wi], posb[wi]], [pb])
                V(lambda: nc.vector.tensor_copy(out=cbv[wi].t[:], in_=pb.t[:]), [pb], [cbv[wi]])
            for c in range(NCC):
                n0 = c * 128
                nn = min(128, NCB - n0)
                ntok = 16 * nn + 16
                for wi, xd, xkey in [(0, self.kcT_d, "kcT_d"), (1, self.vcT_d, "vcT_d")]:
                    LD(XT, XT.t[:, 0:ntok], xd[:, 16 * n0:16 * n0 + ntok],
                       reads=[(xkey, j) for j in range((16 * n0) // 128, min(NT, (16 * n0 + ntok + 127) // 128))])
                    xv = XT.t[:, 0:ntok].rearrange("p (n s) -> p n s", s=16)
                    for g in range(2):
                        for i_ in range(32):
                            a, b = i_ // 16, i_ % 16
                            PE(lambda: nc.tensor.matmul(out=ph.t[:, 0:nn], lhsT=W1[wi].t[g * 64:(g + 1) * 64, i_, :],
                                                        rhs=xv[g * 64:(g + 1) * 64, a:a + nn, b], start=(i_ == 0), stop=(i_ == 31)),
                               [W1[wi], XT], [ph])
                        AC(lambda: nc.scalar.activation(out=hb.t[:, 0:nn], in_=ph.t[:, 0:nn], func=AF.Identity, bias=cbv[wi].t[:, 0:1]), [ph, cbv[wi]], [hb])
                        V(lambda: nc.vector.tensor_tensor(out=tt.t[:, 0:nn], in0=hb.t[:, 0:nn], in1=hb.t[:, 0:nn], op=ALU.mult), [hb], [tt])
                        V(lambda: nc.vector.tensor_scalar(out=tt.t[:, 0:nn], in0=tt.t[:, 0:nn], scalar1=0.044715, scalar2=1.0, op0=ALU.mult, op1=ALU.add), [tt], [tt])
                        V(lambda: nc.vector.tensor_tensor(out=tt.t[:, 0:nn], in0=tt.t[:, 0:nn], in1=hb.t[:, 0:nn], op=ALU.mult), [tt, hb], [tt])
                        AC(lambda: nc.scalar.activation(out=tt.t[:, 0:nn], in_=tt.t[:, 0:nn], func=AF.Tanh, scale=0.7978845608028654), [tt], [tt])
                        V(lambda: nc.vector.tensor_scalar(out=tt.t[:, 0:nn], in0=tt.t[:, 0:nn], scalar1=0.5, scalar2=0.5, op0=ALU.mult, op1=ALU.add), [tt], [tt])
                        if nn < 128:
                            V(lambda: nc.vector.memset(GT[g].t[:], 0.0), [], [GT[g]])
                        V(lambda: nc.vector.tensor_tensor(out=GT[g].t[:, 0:nn], in0=tt.t[:, 0:nn], in1=hb.t[:, 0:nn], op=ALU.mult), [tt, hb], [GT[g]])
                    if wi == 0:
                        for g in range(2):
                            PE(lambda: nc.tensor.matmul(out=pc.t[:, 0:nn], lhsT=W2p.t[:, g, :], rhs=GT[g].t[:, 0:nn], start=(g == 0), stop=(g == 1)),
                               [W2p, GT[g]], [pc])
                        AC(lambda: nc.scalar.copy(out=KCT.t[:, n0:n0 + nn], in_=pc.t[:, 0:nn]), [pc], [KCT])
                    else:
                        for g in range(2):
                            PE(lambda: nc.tensor.matmul(out=pc.t[:, g * 64:(g + 1) * 64], lhsT=GT[g].t[:], rhs=W2v.t[:], start=True, stop=True),
                               [GT[g], W2v], [pc])
                        AC(lambda: nc.scalar.copy(out=RC.t[:, c, :, 0:64], in_=pc.t[:].rearrange("p (g d) -> p g d", g=2)), [pc], [RC])
            ovs = Tn(e2, "ovs", [128, NCC, NS], BF16)
            LD(ovs, ovs.t[:], self.c_ovl[:, :, :])
            for g in range(2):
                V(lambda: nc.vector.tensor_copy(out=RC.t[:, :, g, 65:65 + NS], in_=ovs.t[:]), [ovs, RC], [RC])
                V(lambda: nc.vector.memset(RC.t[:, :, g, 64:65], 1.0), [RC], [RC])
            kb.barrier()

        with ExitStack() as e3:
            def T3(name, shape, dt=F32):
                return Tn(e3, name, shape, dt)
            LA = 4
            Ew = T3("Ew", [128, S], BF16); cmk = T3("cmk", [128, 17, 128], BF16)
            Fw = T3("Fw", [128, 2 * NS - 2]); Kw = T3("Kw", [128, 2 * NS - 2])
            trib = T3("trib", [128, 128], BF16); sltb = T3("sltb", [128, 128], BF16)
            LD(Ew, Ew.t[:], self.c_Ew[:, :]); LD(cmk, cmk.t[:], self.c_cmask[:, :, :])
            LD(Fw, Fw.t[:], self.c_Fw[:, :]); LD(Kw, Kw.t[:], self.c_Kw[:, :])
            LD(trib, trib.t[:], self.c_trib[:, :]); LD(sltb, sltb.t[:], self.c_sltb[:, :])
            qz = [[T3(f"qz{p_}{g}", [128, 2, 128], BF16) for g in range(2)] for p_ in range(2)]
            pe_ = [T3(f"pe{j}", [128, 2, 128], BF16) for j in range(LA + 1)]
            rl = T3("rl", [128, 1]); wgt = T3("wgt", [128, 1])
            IMP = [T3(f"IMP{g}", [128, NS]) for g in range(2)]; impm = [T3(f"impm{g}", [128, NS]) for g in range(2)]
            wk = [T3(f"wk{g}", [128, NS]) for g in range(2)]
            m8a = [T3(f"m8a{g}", [128, 8]) for g in range(2)]; m8b = [T3(f"m8b{g}", [128, 8]) for g in range(2)]
            sneg = [T3(f"sneg{g}", [128, NS], BF16) for g in range(2)]
            SNT = [T3(f"SNT{g}", [128, 2, 128], BF16) for g in range(2)]
            oacc = [T3(f"oacc{j}", [128, 4, 64]) for j in range(2)]
            sq = T3("sq", [128, 256]); ss = T3("ss", [128, 4]); mo = [T3(f"mo{j}", [128, 256], BF16) for j in range(2)]
            psc = [Tile(self.ps(e3, f"nsa_psc{j}", [128, 2, 128]), f"nsa_psc{j}") for j in range(LA + 1)]
            pcvb = Tile(self.ps(e3, "nsa_pcv", [128, 2, 65 + NS]), "nsa_pcv")
            posb = Tile(self.ps(e3, "nsa_pos", [128, 2, 65]), "nsa_pos")
            pcv = [Tile(pcvb.t[:, r, :], pcvb.k) for r in range(2)]
            pos_ = [Tile(posb.t[:, r, :], posb.k) for r in range(2)]
            pst = Tile(self.ps(e3, "nsa_pst", [128, 128], BF16), "nsa_pst")
            for g in range(2):
                V(lambda: nc.vector.memset(SNT[g].t[:], 0.0), [], [SNT[g]])
            nsc = [0]
            pending = []

            def emit_score(job):
                j = nsc[0] % (LA + 1)
                nsc[0] += 1
                q_ = job["q"]
                extra = job.get("extra")
                PE(lambda: nc.tensor.matmul(out=psc[j].t[:], lhsT=job["lhsT"], rhs=q_.t[:], start=True, stop=(extra is None)),
                   job["lt"] + [q_], [psc[j]])
                if extra is not None:
                    sn = job["snt"]
                    PE(lambda: nc.tensor.matmul(out=psc[j].t[:], lhsT=extra, rhs=sn.t[:], start=False, stop=True), [Ew, sn], [psc[j]])
                AC(lambda: nc.scalar.activation(out=pe_[j].t[:], in_=psc[j].t[:], func=AF.Exp, scale=0.125), [psc[j]], [pe_[j]])
                if job.get("mask") is not None:
                    mt_, mk = job["mask"]
                    GP(lambda: nc.gpsimd.tensor_tensor(out=pe_[j].t[:], in0=pe_[j].t[:], in1=mk.unsqueeze(1).to_broadcast([128, 2, 128]), op=ALU.mult),
                       [pe_[j], mt_], [pe_[j]])
                job["pe"] = pe_[j]

            def emit_pv(job):
                pt = job["pe"]
                for r in range(2):
                    tgt = job["tgt"][r]
                    PE(lambda: nc.tensor.matmul(out=tgt.t[:], lhsT=pt.t[:, r, :], rhs=job["rhs"], start=(job["start"] and r == 0),
                                                stop=(job["stop"] and r == 1)),
                       [pt] + job["vt"], [tgt])
                if job.get("after") is not None:
                    job["after"]()

            def push(job):
                emit_score(job)
                pending.append(job)
                while len(pending) > LA:
                    emit_pv(pending.pop(0))

            def finish(pt, oa, h, b, i, first):
                V(lambda: nc.vector.tensor_scalar(out=rl.t[:], in0=pt.t[:, 64:65], scalar1=1e-30, scalar2=None, op0=ALU.max), [pt], [rl])
                V(lambda: nc.vector.reciprocal(out=rl.t[:], in_=rl.t[:]), [rl], [rl])
                V(lambda: nc.vector.tensor_tensor(out=wgt.t[:], in0=rl.t[:], in1=GA.t[:, i, 3 * h + b:3 * h + b + 1], op=ALU.mult),
                  [rl, Tile(None, f"nsa_GA{i}")], [wgt])
                if first:
                    V(lambda: nc.vector.tensor_scalar(out=oa.t[:, h, :], in0=pt.t[:, 0:64], scalar1=wgt.t[:], scalar2=None, op0=ALU.mult), [pt, wgt], [oa])
                else:
                    V(lambda: nc.vector.scalar_tensor_tensor(out=oa.t[:, h, :], in0=pt.t[:, 0:64], scalar=wgt.t[:], in1=oa.t[:, h, :],
                                                             op0=ALU.mult, op1=ALU.add), [pt, wgt, oa], [oa])

            def after_cmp(i, g, oa):
                def f():
                    for r in range(2):
                        finish(pcv[r], oa, 2 * g + r, 0, i, True)
                        if r == 0:
                            V(lambda: nc.vector.tensor_scalar(out=IMP[g].t[:], in0=pcv[r].t[:, 65:65 + NS], scalar1=rl.t[:], scalar2=None, op0=ALU.mult),
                              [pcv[r], rl], [IMP[g]])
                        else:
                            V(lambda: nc.vector.scalar_tensor_tensor(out=IMP[g].t[:], in0=pcv[r].t[:, 65:65 + NS], scalar=rl.t[:], in1=IMP[g].t[:],
                                                                     op0=ALU.mult, op1=ALU.add), [pcv[r], rl, IMP[g]], [IMP[g]])
                    fo = NS - 2 - 2 * i
                    V(lambda: nc.vector.tensor_tensor(out=impm[g].t[:], in0=IMP[g].t[:], in1=Kw.t[:, fo:fo + NS], op=ALU.mult), [IMP[g], Kw], [impm[g]])
                    V(lambda: nc.vector.tensor_tensor(out=impm[g].t[:], in0=impm[g].t[:], in1=Fw.t[:, fo:fo + NS], op=ALU.add), [impm[g], Fw], [impm[g]])
                    V(lambda: nc.vector.memset(impm[g].t[:, 0:1], 1e30), [impm[g]], [impm[g]])
                    V(lambda: nc.vector.max(out=m8a[g].t[:], in_=impm[g].t[:]), [impm[g]], [m8a[g]])
                    V(lambda: nc.vector.match_replace(out=wk[g].t[:], in_to_replace=m8a[g].t[:], in_values=impm[g].t[:], imm_value=-3e38),
                      [m8a[g], impm[g]], [wk[g]])
                    V(lambda: nc.vector.max(out=m8b[g].t[:], in_=wk[g].t[:]), [wk[g]], [m8b[g]])
                    V(lambda: nc.vector.tensor_scalar(out=sneg[g].t[:], in0=impm[g].t[:], scalar1=m8b[g].t[:, 7:8], scalar2=1.0, op0=ALU.is_ge, op1=ALU.subtract),
                      [impm[g], m8b[g]], [sneg[g]])
                return f

            def after_fin(tgt, i, g, b, oa):
                def f():
                    for r in range(2):
                        finish(tgt[r], oa, 2 * g + r, b, i, False)
                return f

            def tail(i, oa):
                def f():
                    s = i % 2
                    t0 = i * 128
                    o2 = oa.t[:].rearrange("p h d -> p (h d)")
                    AC(lambda: nc.scalar.activation(out=sq.t[:], in_=o2, func=AF.Square), [oa], [sq])
                    V(lambda: nc.vector.tensor_reduce(out=ss.t[:], in_=sq.t[:].rearrange("p (h d) -> p h d", h=4), axis=AX.X, op=ALU.add), [sq], [ss])
                    V(lambda: nc.vector.tensor_scalar(out=ss.t[:], in0=ss.t[:], scalar1=1.0 / 64, scalar2=EPS, op0=ALU.mult, op1=ALU.add), [ss], [ss])
                    AC(lambda: nc.scalar.activation(out=ss.t[:], in_=ss.t[:], func=AF.Sqrt), [ss], [ss])
                    V(lambda: nc.vector.reciprocal(out=ss.t[:], in_=ss.t[:]), [ss], [ss])
                    V(lambda: nc.vector.tensor_tensor(out=sq.t[:].rearrange("p (h d) -> p h d", h=4), in0=oa.t[:],
                                                      in1=ss.t[:].unsqueeze(2).to_broadcast([128, 4, 64]), op=ALU.mult), [oa, ss, sq], [sq])
                    V(lambda: nc.vector.tensor_tensor(out=mo[s].t[:], in0=sq.t[:], in1=nwb.t[:], op=ALU.mult), [sq, nwb], [mo[s]])
                    kb.dma("sp", mo[s].k, self.mix[t0:t0 + 128, 256:512], mo[s].t[:], reads=[mo[s].k], writes=[("mix_nsa", i)])
                return f

            for i in range(NT):
                qq = qz[i % 2]
                oa = oacc[i % 2]
                for g in range(2):
                    AC(lambda: nc.scalar.copy(out=qq[g].t[:], in_=QT.t[:, i, :, :]), [Tile(None, f"nsa_QT{i}")], [qq[g]])
                    o0 = (1 - g) * 64
                    GP(lambda: nc.gpsimd.memset(qq[g].t[o0:o0 + 64, :, :], 0.0), [qq[g]], [qq[g]])
                ncs = min(NCC, i // 16 + 1)
                k0 = max(0, i - 4)
                for g in range(2):
                    for c in range(ncs):
                        dl = i - 16 * c
                        push(dict(lhsT=KCT.t[:, c * 128:(c + 1) * 128], lt=[KCT], q=qq[g],
                                  mask=(cmk, cmk.t[:, dl, :]) if dl <= 16 else None,
                                  tgt=pcv, rhs=RC.t[:, c, g, :], vt=[RC], start=(c == 0), stop=(c == ncs - 1),
                                  after=after_cmp(i, g, oa) if c == ncs - 1 else None, cmpg=g))
                    for kt in range(k0, i + 1):
                        mf = (trib, trib.t[:]) if kt == i else ((sltb, sltb.t[:]) if kt == i - 4 else None)
                        push(dict(lhsT=KwT.t[:, kt * 128:(kt + 1) * 128], lt=[Tile(None, f"nsa_KwT{kt}")], q=qq[g], mask=mf,
                                  tgt=[Tile(pcv[0].t[:, 0:65], pcv[0].k), Tile(pcv[1].t[:, 0:65], pcv[1].k)],
                                  rhs=VwA.t[:, kt, g, :], vt=[Tile(None, f"nsa_VwA{kt}")], start=(kt == k0), stop=(kt == i),
                                  after=after_fin(pcv, i, g, 2, oa) if kt == i else None))
                while any(j_.get("cmpg") is not None for j_ in pending):
                    emit_pv(pending.pop(0))
                for g in range(2):
                    PE(lambda: nc.tensor.transpose(out=pst.t[0:NS, :], in_=sneg[g].t[:], identity=idb.t[:]), [sneg[g], idb], [pst])
                    for r in range(2):
                        AC(lambda: nc.scalar.copy(out=SNT[g].t[0:NS, r, :], in_=pst.t[0:NS, :]), [pst], [SNT[g]])
                    for kt in range(i + 1):
                        last = (kt == i)
                        aft = None
                        if last:
                            fin = after_fin(pos_, i, g, 1, oa)
                            if g == 1:
                                tl_ = tail(i, oa)
                                aft = (lambda fin=fin, tl_=tl_: (fin(), tl_()))
                            else:
                                aft = fin
                        push(dict(lhsT=KsT.t[:, kt * 128:(kt + 1) * 128], lt=[Tile(None, f"nsa_KsT{kt}")], q=qq[g],
                                  mask=(trib, trib.t[:]) if last else None, extra=Ew.t[:, kt * 128:(kt + 1) * 128], snt=SNT[g],
                                  tgt=pos_, rhs=VsA.t[:, kt, g, :], vt=[Tile(None, f"nsa_VsA{kt}")], start=(kt == 0), stop=last, after=aft))
            while pending:
                emit_pv(pending.pop(0))


Prog.stage_nsa = _stage_nsa


def build_full(S, L=2):
    p = Prog(S, L=L, dbg=[("out", [S, D])])
    kb = p.kb
    with kb.es:
        h = p.x
        for l in range(L):
            p.stage_d1(l, h)
            kb.barrier()
            p.stage_ssd(l)
            kb.barrier()
            p.stage_gla(l)
            kb.barrier()
            p.stage_nsa(l)
            kb.barrier()
            final = (l == L - 1)
            p.stage_d2(l, h, p.outs["out"] if final else p.hbuf, final)
            kb.barrier()
            h = p.hbuf
        kb.drain("sp")
    return p


_WNAMES = ["norm1_w", "w_in", "gla_gate_w2", "gla_gate_b", "gla_norm_w", "nsa_cmp_pos_k", "nsa_cmp_w1_k", "nsa_cmp_w2_k",
           "nsa_cmp_pos_v", "nsa_cmp_w1_v", "nsa_cmp_w2_v", "nsa_norm_w", "ssd_conv_w", "ssd_conv_b", "ssd_dt_bias",
           "ssd_a_log", "ssd_d", "ssd_norm_w", "w_out", "norm2_w", "w_up", "w_down", "final_norm_w"]


def kernel(**inputs):
    x = np.asarray(inputs["x"], dtype=np.float32)
    B, S, _ = x.shape
    L = int(np.asarray(inputs["w_in"]).shape[0])
    p = build_full(S, L)
    base = {k: np.ascontiguousarray(np.asarray(inputs[k], dtype=np.float32)) for k in _WNAMES}
    base.update(consts(S))
    in_maps = []
    for b in range(B):
        m = dict(base)
        m["x"] = np.ascontiguousarray(x[b])
        in_maps.append(m)
    res = run_bass_kernel_spmd(p.nc, in_maps, core_ids=list(range(B)))
    return np.stack([np.asarray(res.results[b]["out"], dtype=np.float32) for b in range(B)], axis=0)
```
